# Optimizing a Trainium2 kernel written in Bass

```python
import math
import jax, jax.numpy as jnp
from jax import lax
import numpy as np

D_MODEL = 2048
BATCH = 8
SEQ = 2048
DEPTH = 4

S5_WIDTH = D_MODEL
S5_GROUP = 16
S5_GROUPS = S5_WIDTH // S5_GROUP
S5_STATE = 64
S5_DT_MIN = 1e-3
S5_DT_MAX = 1e-1
ATT_HEADS = 16
ATT_HEAD_DIM = D_MODEL // ATT_HEADS
Q_LORA = 512
KV_LORA = 512
IDX_HEADS = 16
IDX_DIM = 128
IDX_TOPK = 256
Q_BLOCK = 128
DSA_IN_WIDTH = Q_LORA + KV_LORA + IDX_DIM + IDX_HEADS
REL_BUCKETS = 32
REL_MAX_DIST = 128
MOE_GROUPS = 4
MOE_PER_GROUP = 8
MOE_EXPERTS = MOE_GROUPS * MOE_PER_GROUP
MOE_TOPK = 2
MOE_FF = 256
DN_ALPHA = (2 * DEPTH) ** 0.25
DN_BETA = (8 * DEPTH) ** -0.25
N_S5_LAYERS = (DEPTH + 1) // 2
N_DSA_LAYERS = DEPTH // 2
LN_EPS = 1e-5
RMS_EPS = 1e-6

kernel_name = "hybrid_s5_dsa_hmoe_deepnorm"


def _layer_norm(x, g, b):
    xf = x.astype(jnp.float32)
    xc = xf - jnp.mean(xf, -1, keepdims=True)
    var = jnp.mean(xc * xc, -1, keepdims=True)
    return (xc * lax.rsqrt(var + LN_EPS) * g.astype(jnp.float32) + b.astype(jnp.float32)).astype(x.dtype)


def _rms_norm(x, g):
    xf = x.astype(jnp.float32)
    return (xf * lax.rsqrt(jnp.mean(xf * xf, -1, keepdims=True) + RMS_EPS) * g.astype(jnp.float32)).astype(x.dtype)


def _rel_bucket(rel):
    n = jnp.maximum(rel, 0)
    max_exact = REL_BUCKETS // 2
    nf = jnp.maximum(n, 1).astype(jnp.float32)
    large = max_exact + (jnp.log(nf / max_exact) / math.log(REL_MAX_DIST / max_exact)
                         * (REL_BUCKETS - max_exact)).astype(jnp.int32)
    large = jnp.minimum(large, REL_BUCKETS - 1)
    return jnp.where(n < max_exact, n, large)


def _s5_mixer(x, w_in, a_re, a_im, log_dt, b_re, b_im, c_re, c_im, d, w_glu, w_out):
    bsz, L, _ = x.shape
    f32 = jnp.float32
    u = (x @ w_in).astype(f32).reshape(bsz, L, S5_GROUPS, S5_GROUP)
    lam = lax.complex(jnp.minimum(a_re.astype(f32), -1e-4), a_im.astype(f32))
    dt = jnp.exp(log_dt.astype(f32))[:, None]
    a_bar = jnp.exp(lam * dt)
    b_bar = ((a_bar - 1.0) / lam)[..., None] * lax.complex(b_re.astype(f32), b_im.astype(f32))
    bu = jnp.einsum('blgp,gnp->blgn', u.astype(jnp.complex64), b_bar)
    a_seq = jnp.broadcast_to(a_bar, (1, L) + a_bar.shape)

    def combine(e1, e2):
        a1, b1 = e1
        a2, b2 = e2
        return a2 * a1, a2 * b1 + b2

    _, h = lax.associative_scan(combine, (a_seq, bu), axis=1)
    c = lax.complex(c_re.astype(f32), c_im.astype(f32))
    y = jnp.einsum('blgn,gpn->blgp', h, c).real + d.astype(f32) * u
    z = jax.nn.gelu(y.reshape(bsz, L, S5_WIDTH)).astype(x.dtype)
    return (z * jax.nn.sigmoid(z @ w_glu)) @ w_out


def _dsa_mixer(x, rel_bias, w_in, q_norm, kv_norm, w_uq, w_qidx, w_uk, w_uv, w_out):
    bsz, L, _ = x.shape
    proj = x @ w_in
    c_q, c_kv, k_idx, w_idx = jnp.split(proj, [Q_LORA, Q_LORA + KV_LORA, Q_LORA + KV_LORA + IDX_DIM], axis=-1)
    c_q = _rms_norm(c_q, q_norm)
    c_kv = _rms_norm(c_kv, kv_norm)
    q = (c_q @ w_uq).reshape(bsz, L, ATT_HEADS, ATT_HEAD_DIM)
    q_idx = (c_q @ w_qidx).reshape(bsz, L, IDX_HEADS, IDX_DIM)
    w_idx = w_idx * (IDX_HEADS ** -0.5)
    n_sel = min(IDX_TOPK, L // 4)
    n_blk = L // Q_BLOCK
    att_scale = ATT_HEAD_DIM ** -0.5
    idx_scale = IDX_DIM ** -0.5
    key_pos = jnp.arange(L, dtype=jnp.int32)
    gather_rows = jax.vmap(lambda rows, ids: rows[ids])

    def block(jb):
        t0 = jb * Q_BLOCK
        q_pos = t0 + jnp.arange(Q_BLOCK, dtype=jnp.int32)
        qb = lax.dynamic_slice_in_dim(q, t0, Q_BLOCK, axis=1)
        qib = lax.dynamic_slice_in_dim(q_idx, t0, Q_BLOCK, axis=1)
        wib = lax.dynamic_slice_in_dim(w_idx, t0, Q_BLOCK, axis=1)
        rel_scores = jax.nn.relu(jnp.einsum('bqhd,bsd->bqhs', qib, k_idx) * idx_scale)
        index = jnp.einsum('bqhs,bqh->bqs', rel_scores, wib).astype(jnp.float32)
        causal = key_pos[None, :] <= q_pos[:, None]
        index = jnp.where(causal[None], index, -jnp.inf)
        _, sel = lax.top_k(index, n_sel)
        valid = sel <= q_pos[None, :, None]
        kv_sel = gather_rows(c_kv, sel)
        q_lat = jnp.einsum('bqhd,hdc->bqhc', qb, w_uk)
        logits = jnp.einsum('bqhc,bqkc->bhqk', q_lat, kv_sel).astype(jnp.float32) * att_scale
        bias = rel_bias[_rel_bucket(q_pos[None, :, None] - sel)]
        logits = logits + jnp.moveaxis(bias, -1, 1).astype(jnp.float32)
        logits = jnp.where(valid[:, None], logits, -jnp.inf)
        p = jax.nn.softmax(logits, axis=-1).astype(x.dtype)
        o_lat = jnp.einsum('bhqk,bqkc->bqhc', p, kv_sel)
        o = jnp.einsum('bqhc,hcd->bqhd', o_lat, w_uv)
        return o.reshape(bsz, Q_BLOCK, ATT_HEADS * ATT_HEAD_DIM)

    out = lax.map(block, jnp.arange(n_blk, dtype=jnp.int32))
    out = jnp.moveaxis(out, 0, 1).reshape(bsz, L, ATT_HEADS * ATT_HEAD_DIM)
    return out @ w_out


def _hier_moe(x, w_group, b_group, w_expert, b_expert, w_gate, w_up, w_down):
    bsz, L, dm = x.shape
    t = x.reshape(bsz * L, dm)
    g_prob = jax.nn.softmax((t @ w_group).astype(jnp.float32) + b_group.astype(jnp.float32), axis=-1)
    g_p, g_idx = lax.top_k(g_prob, 1)
    g_onehot = jax.nn.one_hot(g_idx[:, 0], MOE_GROUPS, dtype=jnp.float32)
    e_logits = ((t @ w_expert).astype(jnp.float32) + b_expert.astype(jnp.float32)).reshape(-1, MOE_GROUPS, MOE_PER_GROUP)
    e_logits = jnp.einsum('tge,tg->te', e_logits, g_onehot)
    e_p, e_idx = lax.top_k(jax.nn.softmax(e_logits, axis=-1), MOE_TOPK)
    e_p = e_p / jnp.sum(e_p, -1, keepdims=True)
    within = jnp.einsum('tk,tke->te', e_p, jax.nn.one_hot(e_idx, MOE_PER_GROUP, dtype=jnp.float32))
    gate = ((g_onehot * g_p)[:, :, None] * within[:, None, :]).reshape(-1, MOE_EXPERTS).astype(x.dtype)
    h = jax.nn.silu(jnp.einsum('td,edf->tef', t, w_gate)) * jnp.einsum('td,edf->tef', t, w_up)
    y = jnp.einsum('tef,efd->td', h * gate[:, :, None], w_down)
    return y.reshape(bsz, L, dm)


def setup_inputs(seed: int = 0) -> dict:
    key = jax.random.key(seed)
    ks = iter(jax.random.split(key, 40))
    f32 = jnp.float32

    def nrm(shape, scale):
        return jax.random.normal(next(ks), shape, f32) * scale

    NS, ND = N_S5_LAYERS, N_DSA_LAYERS
    HD = ATT_HEADS * ATT_HEAD_DIM
    n_idx = jnp.arange(S5_STATE, dtype=f32)
    x = nrm((BATCH, SEQ, D_MODEL), 1.0)
    rel_bias = nrm((REL_BUCKETS, ATT_HEADS), 0.5)
    s5_w_in = nrm((NS, D_MODEL, S5_WIDTH), D_MODEL ** -0.5)
    s5_a_re = -0.5 + nrm((NS, S5_GROUPS, S5_STATE), 0.01)
    s5_a_im = math.pi * n_idx + nrm((NS, S5_GROUPS, S5_STATE), 0.01)
    s5_log_dt = jax.random.uniform(next(ks), (NS, S5_GROUPS), f32, math.log(S5_DT_MIN), math.log(S5_DT_MAX))
    s5_b_re = nrm((NS, S5_GROUPS, S5_STATE, S5_GROUP), (2 * S5_GROUP) ** -0.5)
    s5_b_im = nrm((NS, S5_GROUPS, S5_STATE, S5_GROUP), (2 * S5_GROUP) ** -0.5)
    s5_c_re = nrm((NS, S5_GROUPS, S5_GROUP, S5_STATE), S5_STATE ** -0.5)
    s5_c_im = nrm((NS, S5_GROUPS, S5_GROUP, S5_STATE), S5_STATE ** -0.5)
    s5_d = nrm((NS, S5_GROUPS, S5_GROUP), 1.0)
    s5_w_glu = nrm((NS, S5_WIDTH, S5_WIDTH), S5_WIDTH ** -0.5)
    s5_w_out = nrm((NS, S5_WIDTH, D_MODEL), S5_WIDTH ** -0.5 * DN_BETA)
    dsa_w_in = nrm((ND, D_MODEL, DSA_IN_WIDTH), D_MODEL ** -0.5)
    dsa_q_norm = 1.0 + nrm((ND, Q_LORA), 0.02)
    dsa_kv_norm = 1.0 + nrm((ND, KV_LORA), 0.02)
    dsa_w_uq = nrm((ND, Q_LORA, HD), Q_LORA ** -0.5)
    dsa_w_qidx = nrm((ND, Q_LORA, IDX_HEADS * IDX_DIM), Q_LORA ** -0.5)
    dsa_w_uk = nrm((ND, ATT_HEADS, ATT_HEAD_DIM, KV_LORA), KV_LORA ** -0.5)
    dsa_w_uv = nrm((ND, ATT_HEADS, KV_LORA, ATT_HEAD_DIM), KV_LORA ** -0.5)
    dsa_w_out = nrm((ND, HD, D_MODEL), HD ** -0.5 * DN_BETA)
    moe_w_group = nrm((DEPTH, D_MODEL, MOE_GROUPS), D_MODEL ** -0.5)
    moe_b_group = nrm((DEPTH, MOE_GROUPS), 0.01)
    moe_w_expert = nrm((DEPTH, D_MODEL, MOE_EXPERTS), D_MODEL ** -0.5)
    moe_b_expert = nrm((DEPTH, MOE_EXPERTS), 0.01)
    moe_w_gate = nrm((DEPTH, MOE_EXPERTS, D_MODEL, MOE_FF), D_MODEL ** -0.5)
    moe_w_up = nrm((DEPTH, MOE_EXPERTS, D_MODEL, MOE_FF), D_MODEL ** -0.5)
    moe_w_down = nrm((DEPTH, MOE_EXPERTS, MOE_FF, D_MODEL), MOE_FF ** -0.5 * DN_BETA)
    ln_mix_g = 1.0 + nrm((DEPTH, D_MODEL), 0.02)
    ln_mix_b = nrm((DEPTH, D_MODEL), 0.02)
    ln_ffn_g = 1.0 + nrm((DEPTH, D_MODEL), 0.02)
    ln_ffn_b = nrm((DEPTH, D_MODEL), 0.02)
    return {
        "x": x, "rel_bias": rel_bias,
        "s5_w_in": s5_w_in, "s5_a_re": s5_a_re, "s5_a_im": s5_a_im, "s5_log_dt": s5_log_dt,
        "s5_b_re": s5_b_re, "s5_b_im": s5_b_im, "s5_c_re": s5_c_re, "s5_c_im": s5_c_im,
        "s5_d": s5_d, "s5_w_glu": s5_w_glu, "s5_w_out": s5_w_out,
        "dsa_w_in": dsa_w_in, "dsa_q_norm": dsa_q_norm, "dsa_kv_norm": dsa_kv_norm,
        "dsa_w_uq": dsa_w_uq, "dsa_w_qidx": dsa_w_qidx, "dsa_w_uk": dsa_w_uk,
        "dsa_w_uv": dsa_w_uv, "dsa_w_out": dsa_w_out,
        "moe_w_group": moe_w_group, "moe_b_group": moe_b_group,
        "moe_w_expert": moe_w_expert, "moe_b_expert": moe_b_expert,
        "moe_w_gate": moe_w_gate, "moe_w_up": moe_w_up, "moe_w_down": moe_w_down,
        "ln_mix_g": ln_mix_g, "ln_mix_b": ln_mix_b, "ln_ffn_g": ln_ffn_g, "ln_ffn_b": ln_ffn_b,
    }


def reference(x, rel_bias, s5_w_in, s5_a_re, s5_a_im, s5_log_dt, s5_b_re, s5_b_im, s5_c_re, s5_c_im,
              s5_d, s5_w_glu, s5_w_out, dsa_w_in, dsa_q_norm, dsa_kv_norm, dsa_w_uq, dsa_w_qidx,
              dsa_w_uk, dsa_w_uv, dsa_w_out, moe_w_group, moe_b_group, moe_w_expert, moe_b_expert,
              moe_w_gate, moe_w_up, moe_w_down, ln_mix_g, ln_mix_b, ln_ffn_g, ln_ffn_b):
    h = x
    for i in range(DEPTH):
        j = i // 2
        if i % 2 == 0:
            mix = _s5_mixer(h, s5_w_in[j], s5_a_re[j], s5_a_im[j], s5_log_dt[j], s5_b_re[j], s5_b_im[j],
                            s5_c_re[j], s5_c_im[j], s5_d[j], s5_w_glu[j], s5_w_out[j])
        else:
            mix = _dsa_mixer(h, rel_bias, dsa_w_in[j], dsa_q_norm[j], dsa_kv_norm[j], dsa_w_uq[j],
                             dsa_w_qidx[j], dsa_w_uk[j], dsa_w_uv[j], dsa_w_out[j])
        h = _layer_norm(DN_ALPHA * h + mix, ln_mix_g[i], ln_mix_b[i])
        ffn = _hier_moe(h, moe_w_group[i], moe_b_group[i], moe_w_expert[i], moe_b_expert[i],
                        moe_w_gate[i], moe_w_up[i], moe_w_down[i])
        h = _layer_norm(DN_ALPHA * h + ffn, ln_ffn_g[i], ln_ffn_b[i])
    return h
```

```python
from concourse.bass_utils import run_bass_kernel_spmd
import numpy as np
import concourse.bass as bass
import concourse.mybir as mybir
from contextlib import ExitStack

F32 = mybir.dt.float32
BF16 = mybir.dt.bfloat16
I32 = mybir.dt.int32
ALU = mybir.AluOpType
AF = mybir.ActivationFunctionType
AX = mybir.AxisListType

ENGINES = ("tensor", "vector", "scalar", "gpsimd", "sync")
DMA_RING = 8


class Dep:
    __slots__ = ("name", "w", "r")

    def __init__(self, name=""):
        self.name = name
        self.w = None
        self.r = {}


class Sched:
    def __init__(self, nc, stack, same_engine_sync=True):
        self.nc = nc
        self.stack = stack
        self.streams = {e: [] for e in ENGINES}
        self.count = {e: 0 for e in ENGINES}
        self.seen = {e: {} for e in ENGINES}
        self.sems = {}
        for e in ENGINES:
            self.sems[e] = stack.enter_context(nc.semaphore("s_" + e))
        self.ring = {}
        self.ring_cnt = {}
        self.ring_pos = {}
        for q in ("sync", "gpsimd", "scalar"):
            self.ring[q] = []
            for i in range(DMA_RING):
                key = "d_%s_%d" % (q, i)
                self.sems[key] = stack.enter_context(nc.semaphore(key))
                self.ring[q].append(key)
            self.ring_cnt[q] = [0] * DMA_RING
            self.ring_pos[q] = 0
        self.same_engine_sync = same_engine_sync
        self.out_deps = []

    def _collect(self, reads, writes):
        need = {}

        def add(kv):
            if kv is None:
                return
            k, v = kv
            if need.get(k, 0) < v:
                need[k] = v
        for d in reads:
            add(d.w)
        for d in writes:
            add(d.w)
            for k, v in d.r.items():
                add((k, v))
        return need

    def _waits(self, eng, need, skip_self):
        ws = []
        seen = self.seen[eng]
        for k, v in need.items():
            if k == eng and skip_self:
                continue
            if seen.get(k, 0) >= v:
                continue
            seen[k] = v
            ws.append((k, v))
        return ws

    def _update(self, reads, writes, ticket):
        k, v = ticket
        for d in writes:
            d.w = ticket
            d.r = {}
        for d in reads:
            if d.r.get(k, 0) < v:
                d.r[k] = v

    def op(self, eng, fn, reads=(), writes=(), inc=True):
        need = self._collect(reads, writes)
        skip_self = (eng == "tensor") or (not self.same_engine_sync)
        ws = self._waits(eng, need, skip_self)
        if inc:
            self.count[eng] += 1
            ticket = (eng, self.count[eng])
        else:
            ticket = (eng, self.count[eng] + 1)
        self.streams[eng].append((ws, fn, (eng, 1) if inc else None))
        self._update(reads, writes, ticket)
        return ticket

    def dma(self, q, fn, reads=(), writes=()):
        need = self._collect(reads, writes)
        pos = self.ring_pos[q]
        self.ring_pos[q] = (pos + 1) % DMA_RING
        key = self.ring[q][pos]
        prev = self.ring_cnt[q][pos]
        if prev > 0:
            if need.get(key, 0) < prev * 16:
                need[key] = prev * 16
        ws = self._waits(q, need, False)
        self.ring_cnt[q][pos] = prev + 1
        ticket = (key, (prev + 1) * 16)
        self.streams[q].append((ws, fn, (key, 16)))
        self._update(reads, writes, ticket)
        return ticket

    def finish(self, deps):
        need = self._collect(deps, deps)
        ws = self._waits("sync", need, False)
        self.streams["sync"].append((ws, None, None))

    def emit(self):
        nc = self.nc
        sems = self.sems
        streams = self.streams

        def run(engh, name):
            for ws, fn, inc in streams[name]:
                for k, v in ws:
                    engh.wait_ge(sems[k], v)
                if fn is not None:
                    ins = fn(engh)
                    if inc is not None:
                        ins.then_inc(sems[inc[0]], inc[1])

        with nc.Block() as block:
            @block.tensor
            def _(e):
                run(e, "tensor")

            @block.vector
            def _(e):
                run(e, "vector")

            @block.scalar
            def _(e):
                run(e, "scalar")

            @block.gpsimd
            def _(e):
                run(e, "gpsimd")

            @block.sync
            def _(e):
                run(e, "sync")
D = 2048
L = 2048
NT = 16
DEPTH = 4
DN_ALPHA = (2 * DEPTH) ** 0.25
LN_EPS = 1e-5
RMS_EPS = 1e-6
NE = 32
FF = 256
NEG = -1.0e30


class Arena:
    def __init__(self, nc, stack, nelem):
        self.t = stack.enter_context(nc.sbuf_tensor("arena", [128, nelem], F32))
        self.n = nelem
        self.off = 0

    def mark(self):
        return self.off

    def reset(self, m):
        self.off = m

    def f32(self, n, shape=None):
        assert self.off + n <= self.n, ("arena overflow", self.off, n, self.n)
        v = self.t[:, self.off:self.off + n]
        self.off += n
        return v

    def bf16(self, n):
        m = (n + 1) // 2
        return self.f32(m).bitcast(BF16)[:, 0:n]


class K:
    pass


def r3(ap, **kw):
    return ap.rearrange("p (a b) -> p a b", **kw)


def barrier(k):
    S = k.S
    cur = {}
    for e in ENGINES:
        if S.count[e] > 0:
            cur[e] = S.count[e]
    for q in S.ring:
        for i, key in enumerate(S.ring[q]):
            if S.ring_cnt[q][i] > 0:
                cur[key] = S.ring_cnt[q][i] * 16
    for e in ENGINES:
        ws = S._waits(e, dict(cur), False)
        if ws:
            S.streams[e].append((ws, None, None))


def ln_load_params(k, g_ap, b_ap, gt, bt, dgb):
    S = k.S
    S.dma("sync", lambda e: e.dma_start(out=gt, in_=g_ap.rearrange("(o d) -> o d", o=1).partition_broadcast(128)), writes=[dgb])
    S.dma("sync", lambda e: e.dma_start(out=bt, in_=b_ap.rearrange("(o d) -> o d", o=1).partition_broadcast(128)), writes=[dgb])


def ln_tile(k, a, da, gt, bt, dgb, tt, h_out, hT_out, st, dst, write_hT=True):
    S = k.S
    s1, s2, mean, var, rstd, junk = st
    S.op("scalar", lambda e: e.activation(out=junk, in_=a, func=AF.Identity, accum_out=s1), reads=[da], writes=[dst])
    S.op("scalar", lambda e: e.activation(out=junk, in_=a, func=AF.Square, accum_out=s2), reads=[da], writes=[dst])
    S.op("vector", lambda e: e.tensor_scalar(out=mean, in0=s1, scalar1=1.0 / D, scalar2=None, op0=ALU.mult), reads=[dst], writes=[dst])
    S.op("vector", lambda e: e.tensor_tensor(out=var, in0=mean, in1=mean, op=ALU.mult), reads=[dst], writes=[dst])
    S.op("vector", lambda e: e.scalar_tensor_tensor(out=var, in0=s2, scalar=1.0 / D, in1=var, op0=ALU.mult, op1=ALU.subtract), reads=[dst], writes=[dst])
    S.op("vector", lambda e: e.tensor_scalar(out=var, in0=var, scalar1=LN_EPS, scalar2=None, op0=ALU.add), reads=[dst], writes=[dst])
    S.op("scalar", lambda e: e.activation(out=var, in_=var, func=AF.Sqrt), reads=[dst], writes=[dst])
    S.op("vector", lambda e: e.reciprocal(out=rstd, in_=var), reads=[dst], writes=[dst])
    S.op("vector", lambda e: e.tensor_scalar(out=a, in0=a, scalar1=mean, scalar2=rstd, op0=ALU.subtract, op1=ALU.mult), reads=[dst, da], writes=[da])
    S.op("gpsimd", lambda e: e.tensor_tensor(out=a, in0=a, in1=gt, op=ALU.mult), reads=[da, dgb], writes=[da])
    S.op("gpsimd", lambda e: e.tensor_tensor(out=a, in0=a, in1=bt, op=ALU.add), reads=[da, dgb], writes=[da])
    S.dma("gpsimd", lambda e: e.dma_start(out=h_out[tt * 128:(tt + 1) * 128, :], in_=a), reads=[da], writes=[k.d_h[tt]])
    if write_hT:
        emit_hT(k, a, da, tt, hT_out)


def emit_hT(k, a, da, tt, hT_out):
    S = k.S
    slot = k.hTt_pos
    k.hTt_pos = (slot + 1) % 2
    hTt, dhTt = k.hTt[slot], k.d_hTt[slot]
    for q in range(4):
        bank = 6 + (q % 2)
        ps, dps = k.ps[bank], k.dps[bank]
        for j in range(4):
            kc = q * 4 + j
            S.op("tensor", lambda e, kc=kc, j=j, ps=ps: e.transpose(ps[:, j * 128:(j + 1) * 128], a[:, kc * 128:(kc + 1) * 128], k.ident),
                 reads=[da], writes=[dps], inc=(j == 3))
        S.op("scalar", lambda e, q=q, ps=ps: e.activation(out=hTt[:, q * 512:(q + 1) * 512], in_=ps, func=AF.Copy),
             reads=[dps], writes=[dhTt])
    S.dma("gpsimd", lambda e: e.dma_start(out=hT_out[tt], in_=hTt), reads=[dhTt], writes=[k.d_hT[tt]])


def phase_prep(k):
    S = k.S
    A = k.A
    m = A.mark()
    k.hTt = [A.bf16(2048), A.bf16(2048)]
    xt = [A.f32(D), A.f32(D)]
    dxt = [Dep(), Dep()]
    for tt in range(NT):
        b = tt % 2
        S.dma("sync", lambda e, tt=tt, b=b: e.dma_start(out=xt[b], in_=k.x[tt * 128:(tt + 1) * 128, :]), writes=[dxt[b]])
        emit_hT(k, xt[b], dxt[b], tt, k.hT)
    barrier(k)
    A.reset(m)


def phase_moe(k, li, h_in, h_out, write_hT=True):
    S = k.S
    A = k.A
    m0 = A.mark()
    k.hTt = [A.bf16(2048), A.bf16(2048)]
    NH = 2
    TH = NT // NH
    HT = A.bf16(TH * 16 * 128)
    HTv = HT.rearrange("p (t c x) -> p t c x", t=TH, c=16)
    dHT = Dep()
    yacc = [A.f32(D) for _ in range(TH)]
    dy = [Dep() for _ in range(TH)]
    wg = A.bf16(16 * FF); wu = A.bf16(16 * FF); wd = A.bf16(2 * D)
    wgv = r3(wg, a=16); wuv = r3(wu, a=16); wdv = r3(wd, a=2)
    dwg = [Dep(), Dep()]; dwu = [Dep(), Dep()]; dwd = [Dep(), Dep()]
    NSTG = 4
    stg = [A.f32(2048) for _ in range(NSTG)]
    dstg = [Dep() for _ in range(NSTG)]
    gt = A.f32(D); bt = A.f32(D); dgb = Dep()
    hh = A.bf16(2 * 2 * 512)
    hhv = hh.rearrange("p (t f x) -> p t f x", t=2, f=2)
    dhh = [[Dep(), Dep()], [Dep(), Dep()]]
    sl = [A.f32(512), A.f32(512)]
    dsl = [Dep(), Dep()]
    wr_s = A.f32(16 * 36); wr = A.bf16(16 * 36); dwr = Dep()
    wr_sv = r3(wr_s, a=16); wrv = r3(wr, a=16)
    rb = A.f32(36); drb = Dep()
    gates = A.f32(TH * NE); dgates = [Dep() for _ in range(TH)]
    gv = r3(gates, a=TH)
    rt = A.f32(256); drt = Dep()
    lnst_raw = A.f32(8 + D)
    lnst = (lnst_raw[:, 0:1], lnst_raw[:, 1:2], lnst_raw[:, 2:3], lnst_raw[:, 3:4], lnst_raw[:, 4:5], lnst_raw[:, 8:8 + D])
    dlnst = Dep()

    nc = k.nc
    S.dma("sync", lambda e: e.dma_start(out=wr_sv[:, :, 0:4], in_=k.w["moe_w_group"][li].rearrange("(c p) g -> p c g", p=128)), writes=[dwr])
    S.dma("sync", lambda e: e.dma_start(out=wr_sv[:, :, 4:36], in_=k.w["moe_w_expert"][li].rearrange("(c p) g -> p c g", p=128)), writes=[dwr])
    S.op("vector", lambda e: e.tensor_copy(out=wr, in_=wr_s), reads=[dwr], writes=[dwr])
    S.dma("sync", lambda e: e.dma_start(out=rb[:, 0:4], in_=k.w["moe_b_group"][li].rearrange("(o g) -> o g", o=1).partition_broadcast(128)), writes=[drb])
    S.dma("sync", lambda e: e.dma_start(out=rb[:, 4:36], in_=k.w["moe_b_expert"][li].rearrange("(o g) -> o g", o=1).partition_broadcast(128)), writes=[drb])
    ln_load_params(k, k.w["ln_ffn_g"][li], k.w["ln_ffn_b"][li], gt, bt, dgb)

    stg_pos = [0]

    def load_cast(src_ap, dst_ap, ddst, eng):
        i = stg_pos[0]
        stg_pos[0] = (i + 1) % NSTG
        s = stg[i]
        sv = s if len(src_ap.shape) == 2 else r3(s, a=src_ap.shape[1])
        S.dma("sync", lambda e: e.dma_start(out=sv, in_=src_ap), writes=[dstg[i]])
        S.op(eng, lambda e: e.tensor_copy(out=dst_ap, in_=sv), reads=[dstg[i]], writes=[ddst])

    wgate = k.w["moe_w_gate"][li]
    wup = k.w["moe_w_up"][li]
    wdown = k.w["moe_w_down"][li]

    for half in range(NH):
        t0 = half * TH
        for t in range(TH):
            S.dma("sync", lambda e, t=t, t0=t0: e.dma_start(out=HTv[:, t].rearrange("p c x -> p (c x)"), in_=k.hT[t0 + t]),
                  reads=[k.d_hT[t0 + t]], writes=[dHT])
        for t in range(TH):
            S.dma("sync", lambda e, t=t, t0=t0: e.dma_start(out=yacc[t], in_=h_in[(t0 + t) * 128:(t0 + t + 1) * 128, :]),
                  reads=[k.d_h[t0 + t]], writes=[dy[t]])
            S.op("gpsimd", lambda e, t=t: e.tensor_scalar(out=yacc[t], in0=yacc[t], scalar1=DN_ALPHA, scalar2=None, op0=ALU.mult),
                 reads=[dy[t]], writes=[dy[t]])
        for t in range(TH):
            bank = 6 + (t % 2)
            ps, dps = k.ps[bank], k.dps[bank]
            for kc in range(16):
                S.op("tensor", lambda e, t=t, kc=kc, ps=ps: e.matmul(ps[:, 0:36], lhsT=HTv[:, t, kc, :], rhs=wrv[:, kc, :], start=(kc == 0), stop=(kc == 15)),
                     reads=[dHT, dwr], writes=[dps], inc=(kc == 15))
            lg = rt[:, 0:36]; gmax = rt[:, 40:41]; gexp = rt[:, 44:48]; gsum = rt[:, 48:49]; gone = rt[:, 52:56]
            elc = rt[:, 56:64]; mx8 = rt[:, 64:72]; nm1 = rt[:, 72:73]; ew = rt[:, 80:88]; selm = rt[:, 88:96]
            den = rt[:, 96:97]; coef = rt[:, 100:104]; ngmax = rt[:, 104:105]; gp = rt[:, 105:106]; within = rt[:, 112:120]
            V = lambda fn, rd=(), wr_=(): S.op("vector", fn, reads=[drt] + list(rd), writes=[drt] + list(wr_))
            V(lambda e, ps=ps: e.tensor_tensor(out=lg, in0=ps[:, 0:36], in1=rb, op=ALU.add), rd=[dps, drb])
            V(lambda e: e.tensor_reduce(out=gmax, in_=lg[:, 0:4], axis=AX.X, op=ALU.max))
            V(lambda e: e.tensor_scalar(out=ngmax, in0=gmax, scalar1=-1.0, scalar2=None, op0=ALU.mult))
            S.op("scalar", lambda e: e.activation(out=gexp, in_=lg[:, 0:4], func=AF.Exp, bias=ngmax, scale=1.0, accum_out=gsum), reads=[drt], writes=[drt])
            V(lambda e: e.reciprocal(out=gp, in_=gsum))
            V(lambda e: e.tensor_scalar(out=gone, in0=lg[:, 0:4], scalar1=gmax, scalar2=None, op0=ALU.is_equal))
            V(lambda e: e.tensor_scalar(out=elc, in0=lg[:, 4:12], scalar1=gone[:, 0:1], scalar2=None, op0=ALU.mult))
            for g in range(1, 4):
                V(lambda e, g=g: e.scalar_tensor_tensor(out=elc, in0=lg[:, 4 + 8 * g:12 + 8 * g], scalar=gone[:, g:g + 1], in1=elc, op0=ALU.mult, op1=ALU.add))
            V(lambda e: e.max(out=mx8, in_=elc))
            V(lambda e: e.tensor_scalar(out=nm1, in0=mx8[:, 0:1], scalar1=-1.0, scalar2=None, op0=ALU.mult))
            S.op("scalar", lambda e: e.activation(out=ew, in_=elc, func=AF.Exp, bias=nm1, scale=1.0), reads=[drt], writes=[drt])
            V(lambda e: e.tensor_scalar(out=selm, in0=elc, scalar1=mx8[:, 1:2], scalar2=None, op0=ALU.is_ge))
            V(lambda e: e.tensor_tensor(out=ew, in0=ew, in1=selm, op=ALU.mult))
            V(lambda e: e.tensor_reduce(out=den, in_=ew, axis=AX.X, op=ALU.add))
            V(lambda e: e.reciprocal(out=den, in_=den))
            V(lambda e: e.tensor_scalar(out=within, in0=ew, scalar1=den, scalar2=None, op0=ALU.mult))
            V(lambda e: e.tensor_scalar(out=coef, in0=gone, scalar1=gp, scalar2=None, op0=ALU.mult))
            for g in range(4):
                V(lambda e, g=g, t=t: e.tensor_scalar(out=gv[:, t, g * 8:(g + 1) * 8], in0=within, scalar1=coef[:, g:g + 1], scalar2=None, op0=ALU.mult),
                  wr_=[dgates[t]])
        for ex in range(getattr(k, 'ne_limit', NE)):
            for hf in range(2):
                load_cast(wgate[ex, hf * 1024:(hf + 1) * 1024, :].rearrange("(c p) f -> p c f", p=128), wgv[:, hf * 8:(hf + 1) * 8, :], dwg[hf], "gpsimd")
                load_cast(wup[ex, hf * 1024:(hf + 1) * 1024, :].rearrange("(c p) f -> p c f", p=128), wuv[:, hf * 8:(hf + 1) * 8, :], dwu[hf], "gpsimd")
            for tt in range(2):
                for fc in range(2):
                    pg, dpg = k.ps[fc], k.dps[fc]
                    pu, dpu = k.ps[2 + fc], k.dps[2 + fc]
                    for kc in range(16):
                        S.op("tensor", lambda e, kc=kc, fc=fc, tt=tt, pg=pg: e.matmul(pg, lhsT=wgv[:, kc, fc * 128:(fc + 1) * 128], rhs=HTv[:, tt * 4:(tt + 1) * 4, kc, :], start=(kc == 0), stop=(kc == 15)),
                             reads=[dHT, dwg[kc // 8]], writes=[dpg], inc=(kc == 15))
                    for kc in range(16):
                        S.op("tensor", lambda e, kc=kc, fc=fc, tt=tt, pu=pu: e.matmul(pu, lhsT=wuv[:, kc, fc * 128:(fc + 1) * 128], rhs=HTv[:, tt * 4:(tt + 1) * 4, kc, :], start=(kc == 0), stop=(kc == 15)),
                             reads=[dHT, dwu[kc // 8]], writes=[dpu], inc=(kc == 15))
                    S.op("scalar", lambda e, fc=fc, pg=pg: e.activation(out=sl[fc], in_=pg, func=AF.Silu), reads=[dpg], writes=[dsl[fc]])
                    S.op("vector", lambda e, fc=fc, tt=tt, pu=pu: e.tensor_tensor(out=hhv[:, tt, fc, :], in0=sl[fc], in1=pu, op=ALU.mult),
                         reads=[dsl[fc], dpu], writes=[dhh[tt][fc]])
            for hf in range(2):
                load_cast(wdown[ex, hf * 128:(hf + 1) * 128, :], wdv[:, hf, :], dwd[hf], "gpsimd")
            cnt = 0
            for tt in range(2):
                for sub in range(4):
                    t = tt * 4 + sub
                    for ds in range(4):
                        bank = 4 + (cnt % 2)
                        cnt += 1
                        po, dpo = k.ps[bank], k.dps[bank]
                        for fc in range(2):
                            S.op("tensor", lambda e, fc=fc, tt=tt, sub=sub, ds=ds, po=po: e.matmul(po, lhsT=hhv[:, tt, fc, sub * 128:(sub + 1) * 128], rhs=wdv[:, fc, ds * 512:(ds + 1) * 512], start=(fc == 0), stop=(fc == 1)),
                                 reads=[dhh[tt][fc], dwd[fc]], writes=[dpo], inc=(fc == 1))
                        S.op("vector", lambda e, t=t, ds=ds, po=po, ex=ex: e.scalar_tensor_tensor(out=yacc[t][:, ds * 512:(ds + 1) * 512], in0=po, scalar=gv[:, t, ex:ex + 1], in1=yacc[t][:, ds * 512:(ds + 1) * 512], op0=ALU.mult, op1=ALU.add),
                             reads=[dpo, dgates[t], dy[t]], writes=[dy[t]])
        for t in range(TH):
            ln_tile(k, yacc[t], dy[t], gt, bt, dgb, t0 + t, h_out, k.hT, lnst, dlnst, write_hT=write_hT)
    barrier(k)
    A.reset(m0)
def MM(k, out, lhsT, rhs, start, stop, reads, writes, inc):
    k.S.op("tensor", lambda e: e.matmul(out, lhsT=lhsT, rhs=rhs, start=start, stop=stop), reads=reads, writes=writes, inc=inc)


def TR(k, out, in_, ident, reads, writes, inc):
    k.S.op("tensor", lambda e: e.transpose(out, in_, ident), reads=reads, writes=writes, inc=inc)


def ACT(k, out, in_, func, reads, writes, bias=None, scale=None, accum_out=None):
    kw = {}
    if bias is not None:
        kw["bias"] = bias
    if scale is not None:
        kw["scale"] = scale
    if accum_out is not None:
        kw["accum_out"] = accum_out
    k.S.op("scalar", lambda e: e.activation(out=out, in_=in_, func=func, **kw), reads=reads, writes=writes)


def TS(k, eng, out, in0, s1, s2, op0, op1, reads, writes):
    if op1 is None:
        k.S.op(eng, lambda e: e.tensor_scalar(out=out, in0=in0, scalar1=s1, scalar2=None, op0=op0), reads=reads, writes=writes)
    else:
        k.S.op(eng, lambda e: e.tensor_scalar(out=out, in0=in0, scalar1=s1, scalar2=s2, op0=op0, op1=op1), reads=reads, writes=writes)


def TT(k, eng, out, in0, in1, op, reads, writes):
    k.S.op(eng, lambda e: e.tensor_tensor(out=out, in0=in0, in1=in1, op=op), reads=reads, writes=writes)


def STT(k, eng, out, in0, scalar, in1, op0, op1, reads, writes):
    k.S.op(eng, lambda e: e.scalar_tensor_tensor(out=out, in0=in0, scalar=scalar, in1=in1, op0=op0, op1=op1), reads=reads, writes=writes)


def CP(k, eng, out, in_, reads, writes):
    k.S.op(eng, lambda e: e.tensor_copy(out=out, in_=in_), reads=reads, writes=writes)


def DMA(k, q, out, in_, reads, writes, slow=False):
    if slow:
        k.S.dma(q, lambda e: e.dma_start(out=out, in_=in_, allow_slow_non_contiguous=True), reads=reads, writes=writes)
    else:
        k.S.dma(q, lambda e: e.dma_start(out=out, in_=in_), reads=reads, writes=writes)


class Stager:
    def __init__(self, k, nbuf, nelem):
        self.k = k
        self.bufs = [k.A.f32(nelem) for _ in range(nbuf)]
        self.deps = [Dep() for _ in range(nbuf)]
        self.pos = 0
        self.nelem = nelem
        self.engs = ["gpsimd", "vector"]
        self.epos = 0

    def load(self, src_ap, dst_ap, ddst, eng=None, slow=False):
        i = self.pos
        self.pos = (i + 1) % len(self.bufs)
        n = 1
        for d_ in src_ap.shape[1:]:
            n *= d_
        assert n <= self.nelem, (n, self.nelem)
        s = self.bufs[i][:, 0:n]
        if len(src_ap.shape) == 3:
            s = s.rearrange("p (a b) -> p a b", a=src_ap.shape[1])
        elif len(src_ap.shape) == 4:
            s = s.rearrange("p (a b c) -> p a b c", a=src_ap.shape[1], b=src_ap.shape[2])
        s = s[0:src_ap.shape[0]]
        DMA(self.k, "sync", s, src_ap, [], [self.deps[i]], slow=slow)
        if eng is None:
            eng = self.engs[self.epos]
            self.epos = (self.epos + 1) % len(self.engs)
        CP(self.k, eng, dst_ap, s, [self.deps[i]], [ddst])


def phase_outproj_ln(k, srcT, d_src, w_ap, g_ap, b_ap, h_in, h_out):
    S = k.S
    A = k.A
    m0 = A.mark()
    k.hTt = [A.bf16(2048), A.bf16(2048)]
    wo = A.bf16(16 * D)
    wov = r3(wo, a=16)
    dwo = [Dep() for _ in range(16)]
    stg = Stager(k, 3, 2048)
    gt = A.f32(D); bt = A.f32(D); dgb = Dep()
    src = [A.bf16(2048), A.bf16(2048)]
    dsrc = [Dep(), Dep()]
    at = [A.f32(D), A.f32(D)]
    dat = [Dep(), Dep()]
    lnst_raw = A.f32(8 + D)
    lnst = (lnst_raw[:, 0:1], lnst_raw[:, 1:2], lnst_raw[:, 2:3], lnst_raw[:, 3:4], lnst_raw[:, 4:5], lnst_raw[:, 8:8 + D])
    dlnst = Dep()
    ln_load_params(k, g_ap, b_ap, gt, bt, dgb)
    for kc in range(16):
        stg.load(w_ap[kc * 128:(kc + 1) * 128, :], wov[:, kc, :], dwo[kc])
    for tt in range(NT):
        b = tt % 2
        DMA(k, "sync", src[b], srcT[tt], [d_src[tt]], [dsrc[b]])
        DMA(k, "sync", at[b], h_in[tt * 128:(tt + 1) * 128, :], [k.d_h[tt]], [dat[b]])
        sv = r3(src[b], a=16)
        for ns in range(4):
            ps, dps = k.ps[ns], k.dps[ns]
            for kc in range(16):
                MM(k, ps, sv[:, kc, :], wov[:, kc, ns * 512:(ns + 1) * 512], kc == 0, kc == 15, [dsrc[b], dwo[kc]], [dps], kc == 15)
            STT(k, "vector", at[b][:, ns * 512:(ns + 1) * 512], at[b][:, ns * 512:(ns + 1) * 512], DN_ALPHA, ps, ALU.mult, ALU.add, [dps, dat[b]], [dat[b]])
        ln_tile(k, at[b], dat[b], gt, bt, dgb, tt, h_out, k.hT, lnst, dlnst, write_hT=True)
    barrier(k)
    A.reset(m0)


def rel_bucket_np(n):
    n = np.maximum(n, 0)
    nf = np.maximum(n, 1).astype(np.float32)
    large = 16 + (np.log(nf / np.float32(16)) / np.float32(np.log(128 / 16)) * np.float32(16)).astype(np.int32)
    large = np.minimum(large, 31)
    return np.where(n < 16, n, large)


def dsa_consts():
    ql = np.arange(128)[:, None]
    x = np.arange(256)[None, :]
    dist = np.where(x < 128, 128 + ql - x, ql - (x - 128))
    bk = rel_bucket_np(dist)
    oh = np.zeros((128, 32, 256), np.float32)
    for b in range(32):
        oh[:, b, :] = (bk == b)
    caus = np.where(np.arange(128)[None, :] <= np.arange(128)[:, None], 0.0, NEG).astype(np.float32)
    return {"c_ohb": oh.reshape(128, 32 * 256), "c_caus": caus}


def phase_dsa(k, j, li, h_in, h_out):
    S = k.S
    A = k.A
    W = k.w
    NH_ = 16
    att_scale = 128 ** -0.5
    widx_scale = (16 ** -0.5) * (128 ** -0.5)
    QT_d = k.dsa_QT; QIT_d = k.dsa_QIT; OT_d = k.dsa_OT
    d_OT = [Dep() for _ in range(NT)]
    d_QT = Dep(); d_QIT = Dep()
    m_phase = A.mark()
    CQT = A.bf16(4 * L); CQTv = r3(CQT, a=4); dCQT = Dep()
    CKVT = A.bf16(4 * L); CKVTv = r3(CKVT, a=4); dCKVT = Dep()
    KIT = A.bf16(L); dKIT = Dep()
    WI = A.f32(NT * 16); WIv = r3(WI, a=NT); dWI = Dep()
    identb = A.bf16(128); didb = Dep()
    CP(k, "vector", identb, k.ident, [], [didb])
    mA = A.mark()
    HTb = [A.bf16(2048), A.bf16(2048)]; dHTb = [Dep(), Dep()]
    win = A.bf16(16 * 1168); winv = r3(win, a=16); dwin = [Dep() for _ in range(16)]
    stg = Stager(k, 3, 2048)
    qg = A.f32(512); kg = A.f32(512); dqg = Dep()
    DMA(k, "sync", qg, W["dsa_q_norm"][j].rearrange("(o d) -> o d", o=1).partition_broadcast(128), [], [dqg])
    DMA(k, "sync", kg, W["dsa_kv_norm"][j].rearrange("(o d) -> o d", o=1).partition_broadcast(128), [], [dqg])
    for kc in range(16):
        stg.load(W["dsa_w_in"][j, kc * 128:(kc + 1) * 128, :], winv[:, kc, :], dwin[kc])
    pj = [A.f32(1168), A.f32(1168)]; dpj = [Dep(), Dep()]
    sm = A.f32(16); dsm = Dep()
    junk = A.f32(512)
    for t in range(NT):
        b = t % 2
        DMA(k, "sync", HTb[b], k.hT[t], [k.d_hT[t]], [dHTb[b]])
        HTt = r3(HTb[b], a=16)
        for ns, (c0, c1) in enumerate([(0, 512), (512, 1024), (1024, 1168)]):
            ps, dps = k.ps[ns], k.dps[ns]
            for kc in range(16):
                MM(k, ps[:, 0:c1 - c0], HTt[:, kc, :], winv[:, kc, c0:c1], kc == 0, kc == 15, [dHTb[b], dwin[kc]], [dps], kc == 15)
            CP(k, "vector", pj[b][:, c0:c1], ps[:, 0:c1 - c0], [dps], [dpj[b]])
        for qi, (c0, gain) in enumerate([(0, qg), (512, kg)]):
            ss = sm[:, qi * 4:qi * 4 + 1]; rs = sm[:, qi * 4 + 1:qi * 4 + 2]
            ACT(k, junk, pj[b][:, c0:c0 + 512], AF.Square, [dpj[b]], [dsm], accum_out=ss)
            TS(k, "vector", rs, ss, 1.0 / 512, RMS_EPS, ALU.mult, ALU.add, [dsm], [dsm])
            ACT(k, rs, rs, AF.Sqrt, [dsm], [dsm])
            k.S.op("vector", lambda e, rs=rs: e.reciprocal(out=rs, in_=rs), reads=[dsm], writes=[dsm])
            STT(k, "vector", pj[b][:, c0:c0 + 512], pj[b][:, c0:c0 + 512], rs, gain, ALU.mult, ALU.mult, [dsm, dpj[b], dqg], [dpj[b]])
        TS(k, "vector", WIv[:, t, :], pj[b][:, 1152:1168], widx_scale, None, ALU.mult, None, [dpj[b]], [dWI])
        for grp, (c0, dstv, ddst) in enumerate([(0, CQTv, dCQT), (512, CKVTv, dCKVT)]):
            ps, dps = k.ps[4 + grp], k.dps[4 + grp]
            for kc in range(4):
                TR(k, ps[:, kc * 128:(kc + 1) * 128], pj[b][:, c0 + kc * 128:c0 + (kc + 1) * 128], k.ident, [dpj[b]], [dps], kc == 3)
            ACT(k, dstv[:, :, t * 128:(t + 1) * 128], ps.rearrange("p (a b) -> p a b", a=4), AF.Copy, [dps], [ddst])
        ps, dps = k.ps[6], k.dps[6]
        TR(k, ps[:, 0:128], pj[b][:, 1024:1152], k.ident, [dpj[b]], [dps], True)
        ACT(k, KIT[:, t * 128:(t + 1) * 128], ps[:, 0:128], AF.Copy, [dps], [dKIT])
    barrier(k)
    A.reset(mA)
    mB = A.mark()
    wq = A.bf16(4 * 2048); wqv = r3(wq, a=4); dwq = [Dep() for _ in range(4)]
    stg = Stager(k, 3, 2048)
    ev = [A.bf16(512), A.bf16(512)]; dev_ = [Dep(), Dep()]
    cnt = 0
    for wname, dst_d, ddst in [("dsa_w_uq", QT_d, d_QT), ("dsa_w_qidx", QIT_d, d_QIT)]:
        for kc in range(4):
            stg.load(W[wname][j, kc * 128:(kc + 1) * 128, :], wqv[:, kc, :], dwq[kc])
        for h in range(NH_):
            for sl_ in range(4):
                ps, dps = k.ps[cnt % 4], k.dps[cnt % 4]
                b = cnt % 2
                cnt += 1
                for kc in range(4):
                    MM(k, ps, wqv[:, kc, h * 128:(h + 1) * 128], CQTv[:, kc, sl_ * 512:(sl_ + 1) * 512], kc == 0, kc == 3, [dwq[kc], dCQT], [dps], kc == 3)
                if b == 0:
                    ACT(k, ev[b], ps, AF.Copy, [dps], [dev_[b]])
                else:
                    CP(k, "vector", ev[b], ps, [dps], [dev_[b]])
                DMA(k, "gpsimd", dst_d[:, h, sl_ * 512:(sl_ + 1) * 512], ev[b], [dev_[b]], [ddst])
    barrier(k)
    A.reset(mB)
    KT_d = k.dsa_KT; V_d = k.dsa_V
    dKT = Dep(); dV = Dep()
    mC = A.mark()
    evc = [A.bf16(512), A.bf16(512)]; devc = [Dep(), Dep()]
    stg = Stager(k, 3, 2048)
    wukT = A.bf16(4 * 2048); wukTv = wukT.rearrange("p (c h d) -> p c h d", c=4, h=NH_); dwukT = Dep()
    wuv = A.bf16(4 * 2048); wuvv = wuv.rearrange("p (c h d) -> p c h d", c=4, h=NH_); dwuv = Dep()
    uk = [A.f32(512), A.f32(512)]; duk = [Dep(), Dep()]
    for h in range(NH_):
        b = h % 2
        DMA(k, "sync", uk[b], W["dsa_w_uk"][j, h], [], [duk[b]])
        ps, dps = k.ps[4 + b], k.dps[4 + b]
        for kc in range(4):
            TR(k, ps[:, kc * 128:(kc + 1) * 128], uk[b][:, kc * 128:(kc + 1) * 128], k.ident, [duk[b]], [dps], kc == 3)
        ACT(k, wukTv[:, :, h, :], ps.rearrange("p (a b) -> p a b", a=4), AF.Copy, [dps], [dwukT])
    for h in range(NH_):
        stg.load(W["dsa_w_uv"][j, h].rearrange("(c p) d -> p c d", p=128), wuvv[:, :, h, :], dwuv)
    cnt = 0
    for h in range(NH_):
        for sl_ in range(4):
            ps, dps = k.ps[cnt % 4], k.dps[cnt % 4]
            cnt += 1
            for kc in range(4):
                MM(k, ps, wukTv[:, kc, h, :], CKVTv[:, kc, sl_ * 512:(sl_ + 1) * 512], kc == 0, kc == 3, [dwukT, dCKVT], [dps], kc == 3)
            b = cnt % 2
            if b == 0:
                ACT(k, evc[b], ps, AF.Copy, [dps], [devc[b]])
            else:
                CP(k, "vector", evc[b], ps, [dps], [devc[b]])
            DMA(k, "gpsimd", KT_d[h, :, sl_ * 512:(sl_ + 1) * 512], evc[b], [devc[b]], [dKT])
    for st_ in range(NT):
        for hg in range(4):
            ps, dps = k.ps[cnt % 4], k.dps[cnt % 4]
            cnt += 1
            for kc in range(4):
                MM(k, ps, CKVTv[:, kc, st_ * 128:(st_ + 1) * 128], wuvv[:, kc, hg * 4:(hg + 1) * 4, :], kc == 0, kc == 3, [dwuv, dCKVT], [dps], kc == 3)
            b = cnt % 2
            if b == 0:
                ACT(k, evc[b], ps, AF.Copy, [dps], [devc[b]])
            else:
                CP(k, "vector", evc[b], ps, [dps], [devc[b]])
            for hh in range(4):
                DMA(k, "gpsimd", V_d[hg * 4 + hh, :, st_ * 128:(st_ + 1) * 128], evc[b][:, hh * 128:(hh + 1) * 128], [devc[b]], [dV])
    barrier(k)
    A.reset(mC)
    Tn = A.f32(NH_ * 256); Tnv = r3(Tn, a=NH_); dTn = Dep()
    caus = A.f32(128); dcaus = Dep()
    DMA(k, "sync", caus, k.c["c_caus"], [], [dcaus])
    mT = A.mark()
    ohb = A.f32(32 * 256); ohbv = r3(ohb, a=32); dohb = Dep()
    rbB = A.f32(512); drbB = Dep()
    DMA(k, "sync", ohb, k.c["c_ohb"], [], [dohb])
    DMA(k, "sync", rbB, W["rel_bias"].rearrange("(o b) h -> o (b h)", o=1).partition_broadcast(128), [], [drbB])
    for h in range(NH_):
        eng = "vector"
        TS(k, eng, Tnv[:, h, :], ohbv[:, 0, :], rbB[:, h:h + 1], None, ALU.mult, None, [dohb, drbB], [dTn])
        for b_ in range(1, 32):
            STT(k, eng, Tnv[:, h, :], ohbv[:, b_, :], rbB[:, b_ * 16 + h:b_ * 16 + h + 1], Tnv[:, h, :], ALU.mult, ALU.add, [dohb, drbB, dTn], [dTn])
        TS(k, eng, Tnv[:, h, :], Tnv[:, h, :], rbB[:, 31 * 16 + h:31 * 16 + h + 1], None, ALU.subtract, None, [drbB, dTn], [dTn])
    barrier(k)
    A.reset(mT)
    acc = A.f32(L); dacc = Dep()
    tmp = [A.f32(512), A.f32(512)]; dtmp = [Dep(), Dep()]
    madd = A.bf16(L); dmadd = Dep()
    X = A.f32(L); dX = Dep()
    P = A.bf16(L); dP = Dep()
    PT = A.bf16(NT * 128); PTv = r3(PT, a=NT); dPT = Dep()
    QIb = A.bf16(NH_ * 128); QIbv = r3(QIb, a=NH_); dQIb = Dep()
    QTb = A.bf16(NH_ * 128); QTbv = r3(QTb, a=NH_); dQTb = Dep()
    OT = [A.bf16(NH_ * 128), A.bf16(NH_ * 128)]; dOTs = [Dep(), Dep()]
    mx8 = A.f32(8); dmx = Dep()
    sm2 = A.f32(8); dsm2 = Dep()
    dgr = A.bf16(128); ddgr = Dep()
    Kh = [A.bf16(L), A.bf16(L)]; dKh = [Dep(), Dep()]
    Vh = [A.bf16(L), A.bf16(L)]; dVh = [Dep(), Dep()]
    z1 = A.f32(1); dz1 = Dep()
    k.S.op("vector", lambda e: e.memset(z1, 0.0), reads=[], writes=[dz1])
    for jb in range(NT):
        SL = (jb + 1) * 128
        nbk = (SL + 511) // 512
        DMA(k, "sync", QIbv, QIT_d[:, :, jb * 128:(jb + 1) * 128], [d_QIT], [dQIb])
        DMA(k, "sync", QTbv, QT_d[:, :, jb * 128:(jb + 1) * 128], [d_QT], [dQTb])
        cnt = 0
        for h in range(NH_):
            for bk in range(nbk):
                w_ = min(512, SL - bk * 512)
                ps, dps = k.ps[cnt % 4], k.dps[cnt % 4]
                tb = cnt % 2
                cnt += 1
                MM(k, ps[:, 0:w_], QIbv[:, h, :], KIT[:, bk * 512:bk * 512 + w_], True, True, [dQIb, dKIT], [dps], True)
                if h == 0:
                    TS(k, "vector", acc[:, bk * 512:bk * 512 + w_], ps[:, 0:w_], z1, WIv[:, jb, h:h + 1], ALU.max, ALU.mult, [dps, dWI, dz1], [dacc])
                else:
                    TS(k, "vector", tmp[tb][:, 0:w_], ps[:, 0:w_], z1, WIv[:, jb, h:h + 1], ALU.max, ALU.mult, [dps, dWI, dz1], [dtmp[tb]])
                    TT(k, "gpsimd", acc[:, bk * 512:bk * 512 + w_], acc[:, bk * 512:bk * 512 + w_], tmp[tb][:, 0:w_], ALU.add, [dtmp[tb], dacc], [dacc])
        TT(k, "gpsimd", acc[:, jb * 128:SL], acc[:, jb * 128:SL], caus, ALU.add, [dacc, dcaus], [dacc])
        if SL > 256:
            for r in range(32):
                k.S.op("vector", lambda e, SL=SL: e.max(out=mx8, in_=acc[:, 0:SL]), reads=[dacc], writes=[dmx])
                k.S.op("vector", lambda e, SL=SL: e.match_replace(out=acc[:, 0:SL], in_to_replace=mx8, in_values=acc[:, 0:SL], imm_value=-2.0e30), reads=[dacc, dmx], writes=[dacc])
            TS(k, "vector", madd[:, 0:SL], acc[:, 0:SL], -1.5e30, NEG, ALU.is_gt, ALU.mult, [dacc], [dmadd])
        else:
            TS(k, "vector", madd[:, 0:SL], acc[:, 0:SL], -1.0e29, NEG, ALU.is_lt, ALU.mult, [dacc], [dmadd])
        ot, dot = OT[jb % 2], dOTs[jb % 2]
        otv = r3(ot, a=NH_)
        for h in range(NH_):
            hb = h % 2
            DMA(k, "sync", Kh[hb][:, 0:SL], KT_d[h, :, 0:SL], [dKT], [dKh[hb]])
            DMA(k, "sync", Vh[hb][:, 0:SL], V_d[h, :, 0:SL], [dV], [dVh[hb]])
            Vhv = r3(Vh[hb], a=NT)
            for bk in range(nbk):
                w_ = min(512, SL - bk * 512)
                ps, dps = k.ps[bk], k.dps[bk]
                MM(k, ps[:, 0:w_], QTbv[:, h, :], Kh[hb][:, bk * 512:bk * 512 + w_], True, True, [dQTb, dKh[hb]], [dps], True)
                STT(k, "vector", X[:, bk * 512:bk * 512 + w_], ps[:, 0:w_], att_scale, madd[:, bk * 512:bk * 512 + w_], ALU.mult, ALU.add, [dps, dmadd], [dX])
            lo = max(0, jb - 1) * 128
            tlo = 0 if jb >= 1 else 128
            TT(k, "gpsimd", X[:, lo:SL], X[:, lo:SL], Tnv[:, h, tlo:256], ALU.add, [dX, dTn], [dX])
            rmax = sm2[:, 0:1]; nmax = sm2[:, 1:2]; rsum = sm2[:, 2:3]; rinv = sm2[:, 3:4]
            k.S.op("vector", lambda e, SL=SL: e.tensor_reduce(out=rmax, in_=X[:, 0:SL], axis=AX.X, op=ALU.max), reads=[dX], writes=[dsm2])
            TS(k, "vector", nmax, rmax, -1.0, None, ALU.mult, None, [dsm2], [dsm2])
            ACT(k, P[:, 0:SL], X[:, 0:SL], AF.Exp, [dX, dsm2], [dP, dsm2], bias=nmax, scale=1.0, accum_out=rsum)
            k.S.op("vector", lambda e: e.reciprocal(out=rinv, in_=rsum), reads=[dsm2], writes=[dsm2])
            TS(k, "vector", dgr, identb, rinv, None, ALU.mult, None, [dsm2, didb], [ddgr])
            for st_ in range(jb + 1):
                bank = 4 + (st_ // 4) % 2
                ps, dps = k.ps[bank], k.dps[bank]
                MM(k, ps[:, (st_ % 4) * 128:(st_ % 4 + 1) * 128], P[:, st_ * 128:(st_ + 1) * 128], dgr, True, True, [dP, ddgr], [dps], (st_ % 4 == 3) or (st_ == jb))
                if (st_ % 4 == 3) or (st_ == jb):
                    s0 = (st_ // 4) * 4
                    n_ = st_ - s0 + 1
                    ACT(k, PTv[:, s0:s0 + n_, :], ps[:, 0:n_ * 128].rearrange("p (a b) -> p a b", a=n_), AF.Copy, [dps], [dPT])
            ps, dps = k.ps[6 + h % 2], k.dps[6 + h % 2]
            for st_ in range(jb + 1):
                MM(k, ps[:, 0:128], Vhv[:, st_, :], PTv[:, st_, :], st_ == 0, st_ == jb, [dVh[hb], dPT], [dps], st_ == jb)
            CP(k, "vector", otv[:, h, :], ps[:, 0:128], [dps], [dot])
        DMA(k, "gpsimd", OT_d[jb], ot, [dot], [d_OT[jb]])
    barrier(k)
    A.reset(m_phase)
    phase_outproj_ln(k, OT_d, d_OT, W["dsa_w_out"][j], W["ln_mix_g"][li], W["ln_mix_b"][li], h_in, h_out)
import math as _math

S5_KVEC = [0, -1, -2, -3, -4, -5, -6, -7, 7, 6, 5, 4, 3, 2, 1, 0, 0, 1, 2, 3, 4, 5, 6, 7, 1, 2, 3, 4, 5, 6, 7, 8, 8, 16, 32, 64, 128, 256, 512, 1024]
NK = 40


def s5_consts():
    kv = np.tile(np.array(S5_KVEC, np.float32)[None, :], (128, 1))
    s_ = (np.arange(128) // 16)[:, None]
    t_ = (np.arange(128) // 16)[None, :]
    msk = (t_ >= s_).astype(np.float32)
    return {"c_kv40": kv, "c_s5mask": msk}


def bc(ap, axis, shape):
    return ap.unsqueeze(axis).to_broadcast(list(shape))


def phase_s5(k, sj, li, h_in, h_out):
    S = k.S
    A = k.A
    W = k.w
    M_d, W1_d, W2_d, ZT_d = k.s5_M, k.s5_W1, k.s5_W2, k.s5_ZT
    dM = Dep(); dW1 = Dep(); dW2 = Dep()
    d_ZT = [Dep() for _ in range(NT)]
    TWO_PI = 2.0 * _math.pi
    m_phase = A.mark()
    DCOL = A.f32(128); dDCOL = Dep()
    ASr = A.f32(512); ASi = A.f32(512); ASn = A.f32(512); dAS = Dep()
    ASrv = r3(ASr, a=64); ASiv = r3(ASi, a=64); ASnv = r3(ASn, a=64)
    mP = A.mark()
    KV = A.f32(NK); dKV = Dep()
    DMA(k, "sync", KV, k.c["c_kv40"], [], [dKV])
    MASK = A.f32(128); dMASK = Dep()
    DMA(k, "sync", MASK, k.c["c_s5mask"], [], [dMASK])
    PAre = A.f32(64); PAim = A.f32(64); PDT = A.f32(64); dPA = Dep()
    PBre = A.f32(1024); PBim = A.f32(1024); PCre = A.f32(1024); PCim = A.f32(1024); dPB = Dep(); dPC = Dep()
    PBrev = r3(PBre, a=64); PBimv = r3(PBim, a=64); PCrev = r3(PCre, a=64); PCimv = r3(PCim, a=64)
    ld = [A.f32(2048), A.f32(2048)]; dld = [Dep(), Dep()]
    ld2 = A.f32(2048); dld2 = Dep()
    id64 = k.ident[0:64, 0:64]
    DMA(k, "sync", ld[0][:, 0:16], W["s5_d"][sj], [], [dld[0]])
    CP(k, "vector", ld[0][:, 16:144].rearrange("p (t q) -> p t q", t=8), bc(ld[0][:, 0:16], 1, [128, 8, 16]), [dld[0]], [dld[0]])
    TR(k, k.ps[0][:, 0:128], ld[0][:, 16:144], k.ident, [dld[0]], [k.dps[0]], True)
    CP(k, "vector", DCOL, k.ps[0][:, 0:128], [k.dps[0]], [dDCOL])
    DMA(k, "sync", ld[1][0:64, 0:128], W["s5_a_re"][sj].rearrange("(j g2) n -> j (g2 n)", g2=2), [], [dld[1]])
    DMA(k, "sync", ld[1][0:64, 128:256], W["s5_a_im"][sj].rearrange("(j g2) n -> j (g2 n)", g2=2), [], [dld[1]])
    DMA(k, "sync", ld[1][0:64, 256:258], W["s5_log_dt"][sj].rearrange("(j g2) -> j g2", g2=2), [], [dld[1]])
    CP(k, "vector", ld[1][0:64, 384:512].rearrange("p (g n) -> p g n", g=2), bc(ld[1][0:64, 256:258], 2, [64, 2, 64]), [dld[1]], [dld[1]])
    for i_, (c0, dst) in enumerate([(0, PAre), (128, PAim), (384, PDT)]):
        TR(k, k.ps[1][:, i_ * 64:(i_ + 1) * 64], ld[1][0:64, c0:c0 + 128], id64, [dld[1]], [k.dps[1]], True)
        CP(k, "vector", dst, k.ps[1][:, i_ * 64:(i_ + 1) * 64], [k.dps[1]], [dPA])
    cnt_ = 0
    for name, dstv, ddst, is_c in [("s5_b_re", PBrev, dPB, False), ("s5_b_im", PBimv, dPB, False), ("s5_c_re", PCrev, dPC, True), ("s5_c_im", PCimv, dPC, True)]:
        lb = ld[cnt_ % 2]; dlb = dld[cnt_ % 2]
        cnt_ += 1
        if is_c:
            DMA(k, "sync", lb[0:64, :], W[name][sj].rearrange("(j g2) p n -> j (g2 p n)", g2=2), [], [dlb])
            lb2 = ld2[0:64, :]
            CP(k, "vector", lb2.rearrange("j (p g n) -> j p g n", p=16, g=2), lb[0:64, :].rearrange("j (g p n) -> j p g n", g=2, p=16), [dlb, dld2], [dld2])
            lv = lb2.rearrange("j (p gn) -> j p gn", p=16)
        else:
            DMA(k, "sync", lb[0:64, :], W[name][sj].rearrange("(j g2) n p -> j (g2 n p)", g2=2), [], [dlb])
            lv = lb[0:64, :].rearrange("j (gn p) -> j gn p", p=16)
        for q4 in range(2):
            bank = 2 + q4
            ps, dps = k.ps[bank], k.dps[bank]
            for p8 in range(8):
                p_ = q4 * 8 + p8
                src = lv[:, p_, :] if is_c else lv[:, :, p_]
                TR(k, ps[:, p8 * 64:(p8 + 1) * 64], src, id64, [dlb, dld2], [dps], p8 == 7)
            CP(k, "vector", dstv[:, :, q4 * 8:(q4 + 1) * 8].rearrange("p j q -> p q j"), ps.rearrange("p (q j) -> p q j", q=8), [dps], [ddst])
    if getattr(k, 's5_stop', '') == 'P1':
        barrier(k)
        return
    dE = Dep()
    lr = A.f32(64); ldr = A.f32(64); th = A.f32(64); dtt = A.f32(64)
    TS(k, "vector", lr, PAre, -1.0e-4, None, ALU.min, None, [dPA], [dE])
    ACT(k, dtt, PDT, AF.Exp, [dPA], [dE])
    TT(k, "vector", ldr, lr, dtt, ALU.mult, [dE], [dE])
    TT(k, "vector", th, PAim, dtt, ALU.mult, [dE, dPA], [dE])
    NKK = 64 * NK
    shp = [128, 64, NK]
    ARG = A.f32(NKK); PHI = A.f32(NKK); RHO = A.f32(NKK); QF = A.f32(NKK); MSK2 = A.f32(NKK)
    ARE = A.f32(NKK); AIM = A.f32(NKK)
    QI = A.f32(NKK).bitcast(I32)
    v3 = lambda t_: r3(t_, a=64)
    TT(k, "vector", v3(ARG), bc(ldr, 2, shp), bc(KV, 1, shp), ALU.mult, [dE, dKV], [dE])
    ACT(k, RHO, ARG, AF.Exp, [dE], [dE])
    TT(k, "vector", v3(PHI), bc(th, 2, shp), bc(KV, 1, shp), ALU.mult, [dE, dKV], [dE])

    def sin_of(dst, off):
        TS(k, "vector", ARG, PHI, off, None, ALU.add, None, [dE], [dE])
        TS(k, "vector", QF, ARG, 1.0 / TWO_PI, None, ALU.mult, None, [dE], [dE])
        CP(k, "vector", QI, QF, [dE], [dE])
        CP(k, "vector", QF, QI, [dE], [dE])
        STT(k, "vector", ARG, QF, -TWO_PI, ARG, ALU.mult, ALU.add, [dE], [dE])
        TS(k, "vector", MSK2, ARG, _math.pi, -TWO_PI, ALU.is_gt, ALU.mult, [dE], [dE])
        TT(k, "vector", ARG, ARG, MSK2, ALU.add, [dE], [dE])
        TS(k, "vector", MSK2, ARG, -_math.pi, TWO_PI, ALU.is_lt, ALU.mult, [dE], [dE])
        TT(k, "vector", ARG, ARG, MSK2, ALU.add, [dE], [dE])
        ACT(k, dst, ARG, AF.Sin, [dE], [dE])

    sin_of(AIM, 64.0 * _math.pi)
    sin_of(ARE, 64.5 * _math.pi)
    TT(k, "vector", AIM, AIM, RHO, ALU.mult, [dE], [dE])
    TT(k, "vector", ARE, ARE, RHO, ALU.mult, [dE], [dE])
    AREv = v3(ARE); AIMv = v3(AIM)
    CP(k, "vector", ASrv, AREv[:, :, 32:40], [dE], [dAS])
    CP(k, "vector", ASiv, AIMv[:, :, 32:40], [dE], [dAS])
    TS(k, "vector", ASnv, AIMv[:, :, 32:40], -1.0, None, ALU.mult, None, [dE], [dAS])
    er = A.f32(64); ei = A.f32(64); qr = A.f32(64); qi_ = A.f32(64); den = A.f32(64); t1 = A.f32(64); fr = A.f32(64); fi = A.f32(64)
    TS(k, "vector", er, AREv[:, :, 24], -1.0, None, ALU.add, None, [dE], [dE])
    CP(k, "vector", ei, AIMv[:, :, 24], [dE], [dE])
    TT(k, "vector", qr, er, lr, ALU.mult, [dE], [dE])
    TT(k, "vector", t1, ei, PAim, ALU.mult, [dE], [dE])
    TT(k, "vector", qr, qr, t1, ALU.add, [dE], [dE])
    TT(k, "vector", qi_, ei, lr, ALU.mult, [dE], [dE])
    TT(k, "vector", t1, er, PAim, ALU.mult, [dE], [dE])
    TT(k, "vector", qi_, qi_, t1, ALU.subtract, [dE], [dE])
    TT(k, "vector", den, lr, lr, ALU.mult, [dE], [dE])
    TT(k, "vector", t1, PAim, PAim, ALU.mult, [dE], [dE])
    TT(k, "vector", den, den, t1, ALU.add, [dE], [dE])
    k.S.op("vector", lambda e: e.reciprocal(out=den, in_=den), reads=[dE], writes=[dE])
    TT(k, "vector", fr, qr, den, ALU.mult, [dE], [dE])
    TT(k, "vector", fi, qi_, den, ALU.mult, [dE], [dE])
    BBre = A.f32(1024); BBim = A.f32(1024); tb_ = A.f32(1024)
    BBrev = r3(BBre, a=64); BBimv = r3(BBim, a=64); tbv = r3(tb_, a=64)
    s16 = [128, 64, 16]
    TT(k, "vector", BBrev, bc(fr, 2, s16), PBrev, ALU.mult, [dE, dPB], [dE])
    TT(k, "vector", tbv, bc(fi, 2, s16), PBimv, ALU.mult, [dE, dPB], [dE])
    TT(k, "vector", BBre, BBre, tb_, ALU.subtract, [dE], [dE])
    TT(k, "vector", BBimv, bc(fr, 2, s16), PBimv, ALU.mult, [dE, dPB], [dE])
    TT(k, "vector", tbv, bc(fi, 2, s16), PBrev, ALU.mult, [dE, dPB], [dE])
    TT(k, "vector", BBim, BBim, tb_, ALU.add, [dE], [dE])
    if getattr(k, 's5_stop', '') == 'P2':
        barrier(k)
        return
    PB_ = 8
    PM = A.f32(2); dPM = Dep()
    k.S.op("vector", lambda e: e.memset(PM, 0.0), reads=[], writes=[dPM])
    k.S.op("vector", lambda e: e.memset(PM[0:64, 0:1], 1.0), reads=[dPM], writes=[dPM])
    k.S.op("vector", lambda e: e.memset(PM[64:128, 1:2], 1.0), reads=[dPM], writes=[dPM])
    LM = [[A.bf16(1024), A.bf16(1024)], [A.bf16(1024), A.bf16(1024)]]
    T = [A.f32(1024) for _ in range(4)]
    Tv = [t_.rearrange("p (j s q) -> p j s q", j=PB_, s=8) for t_ in T]
    RREb = A.bf16(1024); RIMb = A.bf16(1024)
    L2RE = A.f32(1024); L2IM = A.f32(1024)
    W2o = A.bf16(4096)
    W2ov = W2o.rearrange("p (j r m) -> p j r m", j=PB_, r=4)
    Mout = [A.bf16(512), A.bf16(512)]; dMout = [Dep(), Dep()]
    TAB = [A.bf16(512), A.bf16(512)]; dTAB = [Dep(), Dep()]
    for tb2 in TAB:
        k.S.op("vector", lambda e, tb2=tb2: e.memset(tb2, 0.0), reads=[], writes=[dE])
    dCh = Dep()
    s4 = [128, PB_, 8, 16]
    j8 = lambda t_: r3(t_, a=PB_)

    def products(blk, Bre, Bim, j0):
        a0 = blk * 8
        Ar = AREv[:, j0:j0 + PB_, a0:a0 + 8]; Ai = AIMv[:, j0:j0 + PB_, a0:a0 + 8]
        br = Bre[:, j0:j0 + PB_, :]; bi = Bim[:, j0:j0 + PB_, :]
        TT(k, "vector", Tv[0], bc(Ar, 3, s4), bc(br, 2, s4), ALU.mult, [dE, dPC, dCh], [dCh])
        TT(k, "vector", Tv[1], bc(Ai, 3, s4), bc(bi, 2, s4), ALU.mult, [dE, dPC, dCh], [dCh])
        TT(k, "vector", Tv[2], bc(Ai, 3, s4), bc(br, 2, s4), ALU.mult, [dE, dPC, dCh], [dCh])
        TT(k, "vector", Tv[3], bc(Ar, 3, s4), bc(bi, 2, s4), ALU.mult, [dE, dPC, dCh], [dCh])

    mcnt = 0
    for ch in range(64 // PB_):
        j0 = ch * PB_
        products(0, BBrev, BBimv, j0)
        TT(k, "vector", T[0], T[0], T[1], ALU.subtract, [dCh], [dCh])
        TT(k, "vector", T[2], T[2], T[3], ALU.add, [dCh], [dCh])
        for g2 in range(2):
            TS(k, "vector", LM[g2][0], T[0], PM[:, g2:g2 + 1], None, ALU.mult, None, [dCh, dPM], [dCh])
            TS(k, "vector", LM[g2][1], T[2], PM[:, g2:g2 + 1], None, ALU.mult, None, [dCh, dPM], [dCh])
        products(2, PCrev, PCimv, j0)
        TT(k, "vector", RREb, T[0], T[1], ALU.subtract, [dCh], [dCh])
        STT(k, "vector", RIMb, T[2], -1.0, T[3], ALU.mult, ALU.subtract, [dCh], [dCh])
        for half in range(PB_ // 2):
            bank = mcnt % 2
            mo, dmo = Mout[mcnt % 2], dMout[mcnt % 2]
            mcnt += 1
            ps, dps = k.ps[bank], k.dps[bank]
            for q in range(4):
                jj = half * 2 + q // 2
                g2 = q % 2
                MM(k, ps[:, q * 128:(q + 1) * 128], j8(LM[g2][0])[:, jj, :], j8(RREb)[:, jj, :], True, False, [dCh], [dps], False)
                MM(k, ps[:, q * 128:(q + 1) * 128], j8(LM[g2][1])[:, jj, :], j8(RIMb)[:, jj, :], False, True, [dCh], [dps], q == 3)
            TT(k, "vector", r3(mo, a=4), r3(ps, a=4), bc(MASK, 1, [128, 4, 128]), ALU.mult, [dps, dMASK], [dmo])
            g0 = (j0 + half * 2) * 2
            for q in range(4):
                DMA(k, "gpsimd", M_d[g0 + q], mo[:, q * 128:(q + 1) * 128], [dmo], [dM])
        if getattr(k, 's5_stop', '') == 'P3a':
            continue
        products(1, BBrev, BBimv, j0)
        TT(k, "vector", L2RE, T[0], T[1], ALU.subtract, [dCh], [dCh])
        TT(k, "vector", L2IM, T[2], T[3], ALU.add, [dCh], [dCh])
        for jj in range(PB_):
            bank = 2 + jj % 2
            ps, dps = k.ps[bank], k.dps[bank]
            tab, dtab = TAB[jj % 2], dTAB[jj % 2]
            TR(k, ps[:, 0:128], j8(L2RE)[:, jj, :], k.ident, [dCh], [dps], False)
            TR(k, ps[:, 128:256], j8(L2IM)[:, jj, :], k.ident, [dCh], [dps], True)
            tabv = tab.rearrange("p (r a m) -> p r a m", r=2, a=2)
            psv = ps[:, 0:256].rearrange("p (r m) -> p r m", r=2)
            CP(k, "vector", tabv[:, :, 0, 0:64], psv[:, :, 0:64], [dps], [dtab])
            CP(k, "vector", tabv[:, :, 1, 64:128], psv[:, :, 64:128], [dps], [dtab])
            DMA(k, "gpsimd", W1_d[j0 + jj], tab, [dtab], [dW1])
        if getattr(k, 's5_stop', '') == 'P3b':
            continue
        products(3, PCrev, PCimv, j0)
        TT(k, "vector", T[0], T[0], T[1], ALU.subtract, [dCh], [dCh])
        STT(k, "vector", T[2], T[2], -1.0, T[3], ALU.mult, ALU.subtract, [dCh], [dCh])
        for g2 in range(2):
            TS(k, "vector", W2ov[:, :, 2 * g2, :], j8(T[0]), PM[:, g2:g2 + 1], None, ALU.mult, None, [dCh, dPM], [dCh])
            TS(k, "vector", W2ov[:, :, 2 * g2 + 1, :], j8(T[2]), PM[:, g2:g2 + 1], None, ALU.mult, None, [dCh, dPM], [dCh])
        for jj in range(PB_):
            DMA(k, "gpsimd", W2_d[j0 + jj], W2o[:, jj * 512:(jj + 1) * 512], [dCh], [dW2, dCh])
    barrier(k)
    A.reset(mP)
    if getattr(k, 's5_stop', '') in ('P', 'P3a', 'P3b'):
        return
    R1 = A.bf16(16 * 2048)
    R1v = R1.rearrange("p (b s c) -> p b s c", b=16, s=8)
    dR1 = Dep()
    mR2 = A.mark()
    HT = A.bf16(NT * 2048); HTv = HT.rearrange("p (t c x) -> p t c x", t=NT, c=16); dHT = Dep()
    for t in range(NT):
        DMA(k, "sync", HTv[:, t].rearrange("p c x -> p (c x)"), k.hT[t], [k.d_hT[t]], [dHT])
    stg = Stager(k, 3, 2048)
    wcb = [A.bf16(2048), A.bf16(2048)]; dwcb = [Dep(), Dep()]
    cnt = 0
    for chb in range(16):
        b = chb % 2
        stg.load(W["s5_w_in"][sj].rearrange("(kc p) n -> p kc n", p=128)[:, :, chb * 128:(chb + 1) * 128], r3(wcb[b], a=16), dwcb[b])
        for sl_ in range(4):
            ps, dps = k.ps[cnt % 4], k.dps[cnt % 4]
            cnt += 1
            for kc in range(16):
                MM(k, ps, r3(wcb[b], a=16)[:, kc, :], HTv[:, sl_ * 4:(sl_ + 1) * 4, kc, :], kc == 0, kc == 15, [dwcb[b], dHT], [dps], kc == 15)
            src = ps.rearrange("p (c s) -> p s c", s=8)
            dst = R1v[:, chb, :, sl_ * 64:(sl_ + 1) * 64]
            if cnt % 2 == 0:
                ACT(k, dst, src, AF.Copy, [dps], [dR1])
            else:
                CP(k, "vector", dst, src, [dps], [dR1])
    barrier(k)
    A.reset(mR2)
    if getattr(k, 's5_stop', '') == 'U':
        return
    R2 = A.bf16(128 * 256)
    Xv = r3(R2, a=128)
    dX = Dep()
    for g in range(128):
        for s_ in range(8):
            q = "sync" if (g * 8 + s_) % 2 == 0 else "gpsimd"
            DMA(k, q, Xv[s_ * 16:(s_ + 1) * 16, g, :], R1v[(g % 8) * 16:(g % 8 + 1) * 16, g // 8, s_, :], [dR1], [dX])
    barrier(k)
    if getattr(k, 's5_stop', '') == 'X':
        return
    mL = A.mark()
    dZF = Dep()
    Mg = [A.bf16(256), A.bf16(256)]; W1t = [A.bf16(512), A.bf16(512)]; W2t = [A.bf16(512), A.bf16(512)]
    dMg = [Dep(), Dep()]; dW1t = [Dep(), Dep()]; dW2t = [Dep(), Dep()]
    REb = [A.f32(384), A.f32(384)]; IMb = [A.f32(384), A.f32(384)]
    dSC = Dep()
    for t_ in REb + IMb:
        k.S.op("vector", lambda e, t_=t_: e.memset(t_, 0.0), reads=[], writes=[dSC])
    HRE = A.bf16(256); HIM = A.bf16(256); dH = Dep()
    yb = [A.f32(256), A.f32(256)]; y2b = [A.f32(256), A.f32(256)]; dyb = [Dep(), Dep()]
    Zg = [A.bf16(256), A.bf16(256)]; dZg = [Dep(), Dep()]
    for j in range(64):
        b = j % 2
        for g2 in range(2):
            DMA(k, "sync", Mg[b][:, g2 * 128:(g2 + 1) * 128], M_d[2 * j + g2], [dM], [dMg[b]])
        DMA(k, "sync", W1t[b], W1_d[j], [dW1], [dW1t[b]])
        DMA(k, "sync", W2t[b], W2_d[j], [dW2], [dW2t[b]])
        ps, dps = k.ps[b], k.dps[b]
        for ri in range(2):
            MM(k, ps[:, ri * 256:(ri + 1) * 256], W1t[b][:, (2 * ri) * 128:(2 * ri + 1) * 128], Xv[:, 2 * j, :], True, False, [dW1t[b], dX], [dps], False)
            MM(k, ps[:, ri * 256:(ri + 1) * 256], W1t[b][:, (2 * ri + 1) * 128:(2 * ri + 2) * 128], Xv[:, 2 * j + 1, :], False, True, [dW1t[b], dX], [dps], ri == 1)
        ACT(k, REb[0][:, 128:384], ps[:, 0:256], AF.Copy, [dps], [dSC])
        ACT(k, IMb[0][:, 128:384], ps[:, 256:512], AF.Copy, [dps], [dSC])
        cur = 0
        for i in range(8):
            sft = 1 << i
            ra, ia = REb[cur], IMb[cur]
            rb_, ib_ = REb[1 - cur], IMb[1 - cur]
            ar = ASrv[:, j, i:i + 1]; ai = ASiv[:, j, i:i + 1]; an = ASnv[:, j, i:i + 1]
            STT(k, "vector", rb_[:, 128:384], ra[:, 128 - sft:384 - sft], ar, ra[:, 128:384], ALU.mult, ALU.add, [dSC, dAS], [dSC])
            STT(k, "vector", rb_[:, 128:384], ia[:, 128 - sft:384 - sft], an, rb_[:, 128:384], ALU.mult, ALU.add, [dSC, dAS], [dSC])
            STT(k, "vector", ib_[:, 128:384], ia[:, 128 - sft:384 - sft], ar, ia[:, 128:384], ALU.mult, ALU.add, [dSC, dAS], [dSC])
            STT(k, "vector", ib_[:, 128:384], ra[:, 128 - sft:384 - sft], ai, ib_[:, 128:384], ALU.mult, ALU.add, [dSC, dAS], [dSC])
            cur = 1 - cur
        ACT(k, HRE, REb[cur][:, 127:383], AF.Copy, [dSC], [dH])
        ACT(k, HIM, IMb[cur][:, 127:383], AF.Copy, [dSC], [dH])
        for g2 in range(2):
            g = 2 * j + g2
            prt = slice(g2 * 64, (g2 + 1) * 64)
            py, dpy = k.ps[2 + g2], k.dps[2 + g2]
            MM(k, py[:, 0:256], r3(Mg[b], a=2)[:, g2, :], Xv[:, g, :], True, False, [dMg[b], dX], [dpy], False)
            MM(k, py[:, 0:256], W2t[b][:, (2 * g2) * 128:(2 * g2 + 1) * 128], HRE, False, False, [dW2t[b], dH], [dpy], False)
            MM(k, py[:, 0:256], W2t[b][:, (2 * g2 + 1) * 128:(2 * g2 + 2) * 128], HIM, False, True, [dW2t[b], dH], [dpy], True)
            y = yb[g2]; y2 = y2b[g2]; dy_ = dyb[g2]
            STT(k, "vector", y, Xv[:, g, :], DCOL[:, g:g + 1], py[:, 0:256], ALU.mult, ALU.add, [dpy, dX, dDCOL], [dy_])
            TT(k, "gpsimd", y2, y, y, ALU.mult, [dy_], [dy_])
            TS(k, "gpsimd", y2, y2, 0.044715, 1.0, ALU.mult, ALU.add, [dy_], [dy_])
            TT(k, "gpsimd", y2, y2, y, ALU.mult, [dy_], [dy_])
            ACT(k, y2, y2, AF.Sigmoid, [dy_], [dy_], scale=1.5957691216057308)
            TT(k, "gpsimd", Zg[g2], y, y2, ALU.mult, [dy_, dZg[g2]], [dZg[g2]])
            for s_ in range(8):
                q = "sync" if s_ % 2 == 0 else "gpsimd"
                DMA(k, q, R1v[(g % 8) * 16:(g % 8 + 1) * 16, g // 8, s_, :], Zg[g2][s_ * 16:(s_ + 1) * 16, :], [dZg[g2]], [dZF])
    barrier(k)
    A.reset(mR2)
    if getattr(k, 's5_stop', '') == 'L':
        return
    Z2N = A.bf16(NT * 2048)
    Z2Nv = Z2N.rearrange("p (t c l s) -> p t c l s", t=NT, c=16, l=16)
    dZ2 = Dep()
    stg = Stager(k, 3, 2048)
    wgb = [A.bf16(2048), A.bf16(2048)]; dwgb = [Dep(), Dep()]
    sg = [A.f32(512), A.f32(512)]; dsg = [Dep(), Dep()]
    cnt = 0
    for nb in range(16):
        b = nb % 2
        stg.load(W["s5_w_glu"][sj].rearrange("(kc p) n -> p kc n", p=128)[:, :, nb * 128:(nb + 1) * 128], r3(wgb[b], a=16), dwgb[b])
        for q in range(4):
            ps, dps = k.ps[cnt % 4], k.dps[cnt % 4]
            sb_ = cnt % 2
            cnt += 1
            zsl = lambda kc: R1v[:, kc, 2 * q:2 * q + 2, :]
            for kc in range(16):
                MM(k, ps, r3(wgb[b], a=16)[:, kc, :], zsl(kc), kc == 0, kc == 15, [dwgb[b], dZF], [dps], kc == 15)
            ACT(k, sg[sb_], ps, AF.Sigmoid, [dps], [dsg[sb_]])
            dst = Z2Nv[:, :, nb, :, 2 * q:2 * q + 2].rearrange("p t l s -> p s t l")
            in0 = sg[sb_].rearrange("p (s t l) -> p s t l", s=2, t=16)
            in1 = R1v[:, nb, 2 * q:2 * q + 2, :].rearrange("p s (t l) -> p s t l", t=16)
            TT(k, "vector", dst, in0, in1, ALU.mult, [dsg[sb_], dZF], [dZ2])
    for t in range(NT):
        DMA(k, "gpsimd", ZT_d[t], Z2N[:, t * 2048:(t + 1) * 2048], [dZ2], [d_ZT[t]])
    barrier(k)
    A.reset(m_phase)
    phase_outproj_ln(k, ZT_d, d_ZT, W["s5_w_out"][sj], W["ln_mix_g"][li], W["ln_mix_b"][li], h_in, h_out)
W_SPECS = [
    ("rel_bias", [32, 16]),
    ("s5_w_in", [2, 2048, 2048]), ("s5_a_re", [2, 128, 64]), ("s5_a_im", [2, 128, 64]), ("s5_log_dt", [2, 128]),
    ("s5_b_re", [2, 128, 64, 16]), ("s5_b_im", [2, 128, 64, 16]), ("s5_c_re", [2, 128, 16, 64]), ("s5_c_im", [2, 128, 16, 64]),
    ("s5_d", [2, 128, 16]), ("s5_w_glu", [2, 2048, 2048]), ("s5_w_out", [2, 2048, 2048]),
    ("dsa_w_in", [2, 2048, 1168]), ("dsa_q_norm", [2, 512]), ("dsa_kv_norm", [2, 512]),
    ("dsa_w_uq", [2, 512, 2048]), ("dsa_w_qidx", [2, 512, 2048]), ("dsa_w_uk", [2, 16, 128, 512]),
    ("dsa_w_uv", [2, 16, 512, 128]), ("dsa_w_out", [2, 2048, 2048]),
    ("moe_w_group", [4, 2048, 4]), ("moe_b_group", [4, 4]), ("moe_w_expert", [4, 2048, 32]), ("moe_b_expert", [4, 32]),
    ("moe_w_gate", [4, 32, 2048, 256]), ("moe_w_up", [4, 32, 2048, 256]), ("moe_w_down", [4, 32, 256, 2048]),
    ("ln_mix_g", [4, 2048]), ("ln_mix_b", [4, 2048]), ("ln_ffn_g", [4, 2048]), ("ln_ffn_b", [4, 2048]),
]


def host_consts():
    c = {}
    c["c_ident"] = np.eye(128, dtype=np.float32)
    c.update(dsa_consts())
    c.update(s5_consts())
    return c


def build_nc(mode="full", used=None, **kw):
    nc = bass.Bass("TRN2", target_bir_lowering=False)
    k = K()
    for a_, b_ in kw.items():
        setattr(k, a_, b_)
    k.nc = nc
    k.x = nc.dram_tensor("x", [L, D], F32, kind="ExternalInput").ap()
    k.w = {}
    for name, shp in W_SPECS:
        if used is not None and name not in used:
            continue
        k.w[name] = nc.dram_tensor(name, getattr(k, 'wshape', {}).get(name, shp), F32, kind="ExternalInput").ap()
    k.c = {}
    for name, arr in host_consts().items():
        k.c[name] = nc.dram_tensor(name, list(arr.shape), F32, kind="ExternalInput").ap()
    k.out = nc.dram_tensor("out", [L, D], F32, kind="ExternalOutput").ap()
    k.hA = nc.dram_tensor("hA", [L, D], F32).ap()
    k.hB = nc.dram_tensor("hB", [L, D], F32).ap()
    k.hT = nc.dram_tensor("hT", [NT, 128, 16 * 128], BF16).ap()
    k.s5_M = nc.dram_tensor("s5_M", [128, 128, 128], BF16).ap()
    k.s5_W1 = nc.dram_tensor("s5_W1", [64, 128, 512], BF16).ap()
    k.s5_W2 = nc.dram_tensor("s5_W2", [64, 128, 512], BF16).ap()
    k.s5_ZT = nc.dram_tensor("s5_ZT", [NT, 128, 16 * 128], BF16).ap()
    k.dsa_QT = nc.dram_tensor("dsa_QT", [128, 16, L], BF16).ap()
    k.dsa_QIT = nc.dram_tensor("dsa_QIT", [128, 16, L], BF16).ap()
    k.dsa_OT = nc.dram_tensor("dsa_OT", [NT, 128, 16 * 128], BF16).ap()
    k.dsa_KT = nc.dram_tensor("dsa_KT", [16, 128, L], BF16).ap()
    k.dsa_V = nc.dram_tensor("dsa_V", [16, 128, L], BF16).ap()
    k.d_h = [Dep() for _ in range(NT)]
    k.d_hT = [Dep() for _ in range(NT)]
    with ExitStack() as st:
        k.S = Sched(nc, st)
        k.A = Arena(nc, st, 52000)
        k.ps = []
        k.dps = []
        for i in range(8):
            k.ps.append(st.enter_context(nc.psum_tensor("ps%d" % i, [128, 512], F32))[:])
            k.dps.append(Dep())
        A = k.A
        k.ident = A.f32(128)
        d_id = Dep()
        k.S.dma("sync", lambda e: e.dma_start(out=k.ident, in_=k.c["c_ident"]), writes=[d_id])
        k.d_hTt = [Dep(), Dep()]
        k.hTt_pos = 0
        barrier(k)
        if mode == "dsa_only":
            phase_prep(k)
            phase_dsa(k, 0, 1, k.x, k.out)
        elif mode == "s5_only":
            phase_prep(k)
            phase_s5(k, 0, 0, k.x, k.out)
        elif mode == "moe_only":
            phase_prep(k)
            phase_moe(k, 0, k.x, k.out, write_hT=False)
        elif mode == "full":
            phase_prep(k)
            h_in = k.x
            bufs = [k.hA, k.hB]
            bi = 0
            for li in range(DEPTH):
                hm = bufs[bi]; bi ^= 1
                if li % 2 == 0:
                    phase_s5(k, li // 2, li, h_in, hm)
                else:
                    phase_dsa(k, li // 2, li, h_in, hm)
                last = (li == DEPTH - 1)
                hf = k.out if last else bufs[bi]
                bi ^= 1
                phase_moe(k, li, hm, hf, write_hT=not last)
                h_in = hf
        k.S.finish(k.d_h)
        k.S.emit()
    return nc


def kernel(**inputs):
    nc = build_nc("full")
    consts = host_consts()
    x = np.ascontiguousarray(inputs["x"], dtype=np.float32)
    shared = {name: np.ascontiguousarray(inputs[name], dtype=np.float32) for name, _ in W_SPECS}
    shared.update(consts)
    in_maps = []
    for c in range(8):
        m = dict(shared)
        m["x"] = x[c]
        in_maps.append(m)
    res = run_bass_kernel_spmd(nc, in_maps, core_ids=list(range(8)))
    return np.stack([np.asarray(r["out"], dtype=np.float32) for r in res.results], axis=0)
```

```python
from concourse.bass_utils import run_bass_kernel_spmd
import numpy as np
import concourse.bass as bass
import concourse.mybir as mybir
from contextlib import ExitStack

F32 = mybir.dt.float32
BF16 = mybir.dt.bfloat16
I32 = mybir.dt.int32
ALU = mybir.AluOpType
AF = mybir.ActivationFunctionType
AX = mybir.AxisListType

ENGINES = ("tensor", "vector", "scalar", "gpsimd", "sync")
DMA_RING = 8


class Dep:
    __slots__ = ("name", "w", "r")

    def __init__(self, name=""):
        self.name = name
        self.w = None
        self.r = {}


class Sched:
    def __init__(self, nc, stack, same_engine_sync=True):
        self.nc = nc
        self.stack = stack
        self.streams = {e: [] for e in ENGINES}
        self.count = {e: 0 for e in ENGINES}
        self.seen = {e: {} for e in ENGINES}
        self.sems = {}
        for e in ENGINES:
            self.sems[e] = stack.enter_context(nc.semaphore("s_" + e))
        self.ring = {}
        self.ring_cnt = {}
        self.ring_pos = {}
        for q in ("sync", "gpsimd", "scalar"):
            self.ring[q] = []
            for i in range(DMA_RING):
                key = "d_%s_%d" % (q, i)
                self.sems[key] = stack.enter_context(nc.semaphore(key))
                self.ring[q].append(key)
            self.ring_cnt[q] = [0] * DMA_RING
            self.ring_pos[q] = 0
        self.same_engine_sync = same_engine_sync
        self.out_deps = []

    def _collect(self, reads, writes):
        need = {}

        def add(kv):
            if kv is None:
                return
            k, v = kv
            if need.get(k, 0) < v:
                need[k] = v
        for d in reads:
            add(d.w)
        for d in writes:
            add(d.w)
            for k, v in d.r.items():
                add((k, v))
        return need

    def _waits(self, eng, need, skip_self):
        ws = []
        seen = self.seen[eng]
        for k, v in need.items():
            if k == eng and skip_self:
                continue
            if seen.get(k, 0) >= v:
                continue
            seen[k] = v
            ws.append((k, v))
        return ws

    def _update(self, reads, writes, ticket):
        k, v = ticket
        for d in writes:
            d.w = ticket
            d.r = {}
        for d in reads:
            if d.r.get(k, 0) < v:
                d.r[k] = v

    def op(self, eng, fn, reads=(), writes=(), inc=True):
        need = self._collect(reads, writes)
        skip_self = (eng == "tensor") or (not self.same_engine_sync)
        ws = self._waits(eng, need, skip_self)
        if inc:
            self.count[eng] += 1
            ticket = (eng, self.count[eng])
        else:
            ticket = (eng, self.count[eng] + 1)
        self.streams[eng].append((ws, fn, (eng, 1) if inc else None))
        self._update(reads, writes, ticket)
        return ticket

    def dma(self, q, fn, reads=(), writes=()):
        need = self._collect(reads, writes)
        pos = self.ring_pos[q]
        self.ring_pos[q] = (pos + 1) % DMA_RING
        key = self.ring[q][pos]
        prev = self.ring_cnt[q][pos]
        if prev > 0:
            if need.get(key, 0) < prev * 16:
                need[key] = prev * 16
        ws = self._waits(q, need, False)
        self.ring_cnt[q][pos] = prev + 1
        ticket = (key, (prev + 1) * 16)
        self.streams[q].append((ws, fn, (key, 16)))
        self._update(reads, writes, ticket)
        return ticket

    def finish(self, deps):
        need = self._collect(deps, deps)
        ws = self._waits("sync", need, False)
        self.streams["sync"].append((ws, None, None))

    def emit(self):
        nc = self.nc
        sems = self.sems
        streams = self.streams

        def run(engh, name):
            for ws, fn, inc in streams[name]:
                for k, v in ws:
                    engh.wait_ge(sems[k], v)
                if fn is not None:
                    ins = fn(engh)
                    if inc is not None:
                        ins.then_inc(sems[inc[0]], inc[1])

        with nc.Block() as block:
            @block.tensor
            def _(e):
                run(e, "tensor")

            @block.vector
            def _(e):
                run(e, "vector")

            @block.scalar
            def _(e):
                run(e, "scalar")

            @block.gpsimd
            def _(e):
                run(e, "gpsimd")

            @block.sync
            def _(e):
                run(e, "sync")
D = 2048
L = 2048
NT = 16
DEPTH = 4
DN_ALPHA = (2 * DEPTH) ** 0.25
LN_EPS = 1e-5
RMS_EPS = 1e-6
NE = 32
FF = 256
NEG = -1.0e30


class Arena:
    def __init__(self, nc, stack, nelem):
        self.t = stack.enter_context(nc.sbuf_tensor("arena", [128, nelem], F32))
        self.n = nelem
        self.off = 0

    def mark(self):
        return self.off

    def reset(self, m):
        self.off = m

    def f32(self, n, shape=None):
        assert self.off + n <= self.n, ("arena overflow", self.off, n, self.n)
        v = self.t[:, self.off:self.off + n]
        self.off += n
        return v

    def bf16(self, n):
        m = (n + 1) // 2
        return self.f32(m).bitcast(BF16)[:, 0:n]


class K:
    pass


def r3(ap, **kw):
    return ap.rearrange("p (a b) -> p a b", **kw)


def barrier(k):
    S = k.S
    cur = {}
    for e in ENGINES:
        if S.count[e] > 0:
            cur[e] = S.count[e]
    for q in S.ring:
        for i, key in enumerate(S.ring[q]):
            if S.ring_cnt[q][i] > 0:
                cur[key] = S.ring_cnt[q][i] * 16
    for e in ENGINES:
        ws = S._waits(e, dict(cur), False)
        if ws:
            S.streams[e].append((ws, None, None))


def ln_load_params(k, g_ap, b_ap, gt, bt, dgb):
    S = k.S
    S.dma("sync", lambda e: e.dma_start(out=gt, in_=g_ap.rearrange("(o d) -> o d", o=1).partition_broadcast(128)), writes=[dgb])
    S.dma("sync", lambda e: e.dma_start(out=bt, in_=b_ap.rearrange("(o d) -> o d", o=1).partition_broadcast(128)), writes=[dgb])


def ln_tile(k, a, da, gt, bt, dgb, tt, h_out, hT_out, st, dst, write_hT=True):
    S = k.S
    s1, s2, mean, var, rstd, junk = st
    S.op("scalar", lambda e: e.activation(out=junk, in_=a, func=AF.Identity, accum_out=s1), reads=[da], writes=[dst])
    S.op("scalar", lambda e: e.activation(out=junk, in_=a, func=AF.Square, accum_out=s2), reads=[da], writes=[dst])
    S.op("vector", lambda e: e.tensor_scalar(out=mean, in0=s1, scalar1=1.0 / D, scalar2=None, op0=ALU.mult), reads=[dst], writes=[dst])
    S.op("vector", lambda e: e.tensor_tensor(out=var, in0=mean, in1=mean, op=ALU.mult), reads=[dst], writes=[dst])
    S.op("vector", lambda e: e.scalar_tensor_tensor(out=var, in0=s2, scalar=1.0 / D, in1=var, op0=ALU.mult, op1=ALU.subtract), reads=[dst], writes=[dst])
    S.op("vector", lambda e: e.tensor_scalar(out=var, in0=var, scalar1=LN_EPS, scalar2=None, op0=ALU.add), reads=[dst], writes=[dst])
    S.op("scalar", lambda e: e.activation(out=var, in_=var, func=AF.Sqrt), reads=[dst], writes=[dst])
    S.op("vector", lambda e: e.reciprocal(out=rstd, in_=var), reads=[dst], writes=[dst])
    S.op("vector", lambda e: e.tensor_scalar(out=a, in0=a, scalar1=mean, scalar2=rstd, op0=ALU.subtract, op1=ALU.mult), reads=[dst, da], writes=[da])
    S.op("gpsimd", lambda e: e.tensor_tensor(out=a, in0=a, in1=gt, op=ALU.mult), reads=[da, dgb], writes=[da])
    S.op("gpsimd", lambda e: e.tensor_tensor(out=a, in0=a, in1=bt, op=ALU.add), reads=[da, dgb], writes=[da])
    S.dma("gpsimd", lambda e: e.dma_start(out=h_out[tt * 128:(tt + 1) * 128, :], in_=a), reads=[da], writes=[k.d_h[tt]])
    if write_hT:
        emit_hT(k, a, da, tt, hT_out)


def emit_hT(k, a, da, tt, hT_out):
    S = k.S
    slot = k.hTt_pos
    k.hTt_pos = (slot + 1) % 2
    hTt, dhTt = k.hTt[slot], k.d_hTt[slot]
    for q in range(4):
        bank = 6 + (q % 2)
        ps, dps = k.ps[bank], k.dps[bank]
        for j in range(4):
            kc = q * 4 + j
            S.op("tensor", lambda e, kc=kc, j=j, ps=ps: e.transpose(ps[:, j * 128:(j + 1) * 128], a[:, kc * 128:(kc + 1) * 128], k.ident),
                 reads=[da], writes=[dps], inc=(j == 3))
        S.op("scalar", lambda e, q=q, ps=ps: e.activation(out=hTt[:, q * 512:(q + 1) * 512], in_=ps, func=AF.Copy),
             reads=[dps], writes=[dhTt])
    S.dma("gpsimd", lambda e: e.dma_start(out=hT_out[tt], in_=hTt), reads=[dhTt], writes=[k.d_hT[tt]])


def phase_prep(k):
    S = k.S
    A = k.A
    m = A.mark()
    k.hTt = [A.bf16(2048), A.bf16(2048)]
    xt = [A.f32(D), A.f32(D)]
    dxt = [Dep(), Dep()]
    for tt in range(NT):
        b = tt % 2
        S.dma("sync", lambda e, tt=tt, b=b: e.dma_start(out=xt[b], in_=k.x[tt * 128:(tt + 1) * 128, :]), writes=[dxt[b]])
        emit_hT(k, xt[b], dxt[b], tt, k.hT)
    barrier(k)
    A.reset(m)


def phase_moe(k, li, h_in, h_out, write_hT=True):
    S = k.S
    A = k.A
    m0 = A.mark()
    k.hTt = [A.bf16(2048), A.bf16(2048)]
    NH = 2
    TH = NT // NH
    HT = A.bf16(TH * 16 * 128)
    HTv = HT.rearrange("p (t c x) -> p t c x", t=TH, c=16)
    dHT = Dep()
    yacc = [A.f32(D) for _ in range(TH)]
    dy = [Dep() for _ in range(TH)]
    wg = A.bf16(16 * FF); wu = A.bf16(16 * FF); wd = A.bf16(2 * D)
    wgv = r3(wg, a=16); wuv = r3(wu, a=16); wdv = r3(wd, a=2)
    dwg = [Dep(), Dep()]; dwu = [Dep(), Dep()]; dwd = [Dep(), Dep()]
    NSTG = 4
    stg = [A.f32(2048) for _ in range(NSTG)]
    dstg = [Dep() for _ in range(NSTG)]
    gt = A.f32(D); bt = A.f32(D); dgb = Dep()
    hh = A.bf16(2 * 2 * 512)
    hhv = hh.rearrange("p (t f x) -> p t f x", t=2, f=2)
    dhh = [[Dep(), Dep()], [Dep(), Dep()]]
    sl = [A.f32(512), A.f32(512)]
    dsl = [Dep(), Dep()]
    wr_s = A.f32(16 * 36); wr = A.bf16(16 * 36); dwr = Dep()
    wr_sv = r3(wr_s, a=16); wrv = r3(wr, a=16)
    rb = A.f32(36); drb = Dep()
    gates = A.f32(TH * NE); dgates = [Dep() for _ in range(TH)]
    gv = r3(gates, a=TH)
    rt = A.f32(256); drt = Dep()
    lnst_raw = A.f32(8 + D)
    lnst = (lnst_raw[:, 0:1], lnst_raw[:, 1:2], lnst_raw[:, 2:3], lnst_raw[:, 3:4], lnst_raw[:, 4:5], lnst_raw[:, 8:8 + D])
    dlnst = Dep()

    nc = k.nc
    S.dma("sync", lambda e: e.dma_start(out=wr_sv[:, :, 0:4], in_=k.w["moe_w_group"][li].rearrange("(c p) g -> p c g", p=128)), writes=[dwr])
    S.dma("sync", lambda e: e.dma_start(out=wr_sv[:, :, 4:36], in_=k.w["moe_w_expert"][li].rearrange("(c p) g -> p c g", p=128)), writes=[dwr])
    S.op("vector", lambda e: e.tensor_copy(out=wr, in_=wr_s), reads=[dwr], writes=[dwr])
    S.dma("sync", lambda e: e.dma_start(out=rb[:, 0:4], in_=k.w["moe_b_group"][li].rearrange("(o g) -> o g", o=1).partition_broadcast(128)), writes=[drb])
    S.dma("sync", lambda e: e.dma_start(out=rb[:, 4:36], in_=k.w["moe_b_expert"][li].rearrange("(o g) -> o g", o=1).partition_broadcast(128)), writes=[drb])
    ln_load_params(k, k.w["ln_ffn_g"][li], k.w["ln_ffn_b"][li], gt, bt, dgb)

    stg_pos = [0]

    def load_cast(src_ap, dst_ap, ddst, eng):
        i = stg_pos[0]
        stg_pos[0] = (i + 1) % NSTG
        s = stg[i]
        sv = s if len(src_ap.shape) == 2 else r3(s, a=src_ap.shape[1])
        S.dma("sync", lambda e: e.dma_start(out=sv, in_=src_ap), writes=[dstg[i]])
        S.op(eng, lambda e: e.tensor_copy(out=dst_ap, in_=sv), reads=[dstg[i]], writes=[ddst])

    wgate = k.w["moe_w_gate"][li]
    wup = k.w["moe_w_up"][li]
    wdown = k.w["moe_w_down"][li]

    for half in range(NH):
        t0 = half * TH
        for t in range(TH):
            S.dma("sync", lambda e, t=t, t0=t0: e.dma_start(out=HTv[:, t].rearrange("p c x -> p (c x)"), in_=k.hT[t0 + t]),
                  reads=[k.d_hT[t0 + t]], writes=[dHT])
        for t in range(TH):
            S.dma("sync", lambda e, t=t, t0=t0: e.dma_start(out=yacc[t], in_=h_in[(t0 + t) * 128:(t0 + t + 1) * 128, :]),
                  reads=[k.d_h[t0 + t]], writes=[dy[t]])
            S.op("gpsimd", lambda e, t=t: e.tensor_scalar(out=yacc[t], in0=yacc[t], scalar1=DN_ALPHA, scalar2=None, op0=ALU.mult),
                 reads=[dy[t]], writes=[dy[t]])
        for t in range(TH):
            bank = 6 + (t % 2)
            ps, dps = k.ps[bank], k.dps[bank]
            for kc in range(16):
                S.op("tensor", lambda e, t=t, kc=kc, ps=ps: e.matmul(ps[:, 0:36], lhsT=HTv[:, t, kc, :], rhs=wrv[:, kc, :], start=(kc == 0), stop=(kc == 15)),
                     reads=[dHT, dwr], writes=[dps], inc=(kc == 15))
            lg = rt[:, 0:36]; gmax = rt[:, 40:41]; gexp = rt[:, 44:48]; gsum = rt[:, 48:49]; gone = rt[:, 52:56]
            elc = rt[:, 56:64]; mx8 = rt[:, 64:72]; nm1 = rt[:, 72:73]; ew = rt[:, 80:88]; selm = rt[:, 88:96]
            den = rt[:, 96:97]; coef = rt[:, 100:104]; ngmax = rt[:, 104:105]; gp = rt[:, 105:106]; within = rt[:, 112:120]
            V = lambda fn, rd=(), wr_=(): S.op("vector", fn, reads=[drt] + list(rd), writes=[drt] + list(wr_))
            V(lambda e, ps=ps: e.tensor_tensor(out=lg, in0=ps[:, 0:36], in1=rb, op=ALU.add), rd=[dps, drb])
            V(lambda e: e.tensor_reduce(out=gmax, in_=lg[:, 0:4], axis=AX.X, op=ALU.max))
            V(lambda e: e.tensor_scalar(out=ngmax, in0=gmax, scalar1=-1.0, scalar2=None, op0=ALU.mult))
            S.op("scalar", lambda e: e.activation(out=gexp, in_=lg[:, 0:4], func=AF.Exp, bias=ngmax, scale=1.0, accum_out=gsum), reads=[drt], writes=[drt])
            V(lambda e: e.reciprocal(out=gp, in_=gsum))
            V(lambda e: e.tensor_scalar(out=gone, in0=lg[:, 0:4], scalar1=gmax, scalar2=None, op0=ALU.is_equal))
            V(lambda e: e.tensor_scalar(out=elc, in0=lg[:, 4:12], scalar1=gone[:, 0:1], scalar2=None, op0=ALU.mult))
            for g in range(1, 4):
                V(lambda e, g=g: e.scalar_tensor_tensor(out=elc, in0=lg[:, 4 + 8 * g:12 + 8 * g], scalar=gone[:, g:g + 1], in1=elc, op0=ALU.mult, op1=ALU.add))
            V(lambda e: e.max(out=mx8, in_=elc))
            V(lambda e: e.tensor_scalar(out=nm1, in0=mx8[:, 0:1], scalar1=-1.0, scalar2=None, op0=ALU.mult))
            S.op("scalar", lambda e: e.activation(out=ew, in_=elc, func=AF.Exp, bias=nm1, scale=1.0), reads=[drt], writes=[drt])
            V(lambda e: e.tensor_scalar(out=selm, in0=elc, scalar1=mx8[:, 1:2], scalar2=None, op0=ALU.is_ge))
            V(lambda e: e.tensor_tensor(out=ew, in0=ew, in1=selm, op=ALU.mult))
            V(lambda e: e.tensor_reduce(out=den, in_=ew, axis=AX.X, op=ALU.add))
            V(lambda e: e.reciprocal(out=den, in_=den))
            V(lambda e: e.tensor_scalar(out=within, in0=ew, scalar1=den, scalar2=None, op0=ALU.mult))
            V(lambda e: e.tensor_scalar(out=coef, in0=gone, scalar1=gp, scalar2=None, op0=ALU.mult))
            for g in range(4):
                V(lambda e, g=g, t=t: e.tensor_scalar(out=gv[:, t, g * 8:(g + 1) * 8], in0=within, scalar1=coef[:, g:g + 1], scalar2=None, op0=ALU.mult),
                  wr_=[dgates[t]])
        for ex in range(getattr(k, 'ne_limit', NE)):
            for hf in range(2):
                load_cast(wgate[ex, hf * 1024:(hf + 1) * 1024, :].rearrange("(c p) f -> p c f", p=128), wgv[:, hf * 8:(hf + 1) * 8, :], dwg[hf], "gpsimd")
                load_cast(wup[ex, hf * 1024:(hf + 1) * 1024, :].rearrange("(c p) f -> p c f", p=128), wuv[:, hf * 8:(hf + 1) * 8, :], dwu[hf], "gpsimd")
            for tt in range(2):
                for fc in range(2):
                    pg, dpg = k.ps[fc], k.dps[fc]
                    pu, dpu = k.ps[2 + fc], k.dps[2 + fc]
                    for kc in range(16):
                        S.op("tensor", lambda e, kc=kc, fc=fc, tt=tt, pg=pg: e.matmul(pg, lhsT=wgv[:, kc, fc * 128:(fc + 1) * 128], rhs=HTv[:, tt * 4:(tt + 1) * 4, kc, :], start=(kc == 0), stop=(kc == 15)),
                             reads=[dHT, dwg[kc // 8]], writes=[dpg], inc=(kc == 15))
                    for kc in range(16):
                        S.op("tensor", lambda e, kc=kc, fc=fc, tt=tt, pu=pu: e.matmul(pu, lhsT=wuv[:, kc, fc * 128:(fc + 1) * 128], rhs=HTv[:, tt * 4:(tt + 1) * 4, kc, :], start=(kc == 0), stop=(kc == 15)),
                             reads=[dHT, dwu[kc // 8]], writes=[dpu], inc=(kc == 15))
                    S.op("scalar", lambda e, fc=fc, pg=pg: e.activation(out=sl[fc], in_=pg, func=AF.Silu), reads=[dpg], writes=[dsl[fc]])
                    S.op("vector", lambda e, fc=fc, tt=tt, pu=pu: e.tensor_tensor(out=hhv[:, tt, fc, :], in0=sl[fc], in1=pu, op=ALU.mult),
                         reads=[dsl[fc], dpu], writes=[dhh[tt][fc]])
            for hf in range(2):
                load_cast(wdown[ex, hf * 128:(hf + 1) * 128, :], wdv[:, hf, :], dwd[hf], "gpsimd")
            cnt = 0
            for tt in range(2):
                for sub in range(4):
                    t = tt * 4 + sub
                    for ds in range(4):
                        bank = 4 + (cnt % 2)
                        cnt += 1
                        po, dpo = k.ps[bank], k.dps[bank]
                        for fc in range(2):
                            S.op("tensor", lambda e, fc=fc, tt=tt, sub=sub, ds=ds, po=po: e.matmul(po, lhsT=hhv[:, tt, fc, sub * 128:(sub + 1) * 128], rhs=wdv[:, fc, ds * 512:(ds + 1) * 512], start=(fc == 0), stop=(fc == 1)),
                                 reads=[dhh[tt][fc], dwd[fc]], writes=[dpo], inc=(fc == 1))
                        S.op("vector", lambda e, t=t, ds=ds, po=po, ex=ex: e.scalar_tensor_tensor(out=yacc[t][:, ds * 512:(ds + 1) * 512], in0=po, scalar=gv[:, t, ex:ex + 1], in1=yacc[t][:, ds * 512:(ds + 1) * 512], op0=ALU.mult, op1=ALU.add),
                             reads=[dpo, dgates[t], dy[t]], writes=[dy[t]])
        for t in range(TH):
            ln_tile(k, yacc[t], dy[t], gt, bt, dgb, t0 + t, h_out, k.hT, lnst, dlnst, write_hT=write_hT)
    barrier(k)
    A.reset(m0)
def MM(k, out, lhsT, rhs, start, stop, reads, writes, inc):
    k.S.op("tensor", lambda e: e.matmul(out, lhsT=lhsT, rhs=rhs, start=start, stop=stop), reads=reads, writes=writes, inc=inc)


def TR(k, out, in_, ident, reads, writes, inc):
    k.S.op("tensor", lambda e: e.transpose(out, in_, ident), reads=reads, writes=writes, inc=inc)


def ACT(k, out, in_, func, reads, writes, bias=None, scale=None, accum_out=None):
    kw = {}
    if bias is not None:
        kw["bias"] = bias
    if scale is not None:
        kw["scale"] = scale
    if accum_out is not None:
        kw["accum_out"] = accum_out
    k.S.op("scalar", lambda e: e.activation(out=out, in_=in_, func=func, **kw), reads=reads, writes=writes)


def TS(k, eng, out, in0, s1, s2, op0, op1, reads, writes):
    if op1 is None:
        k.S.op(eng, lambda e: e.tensor_scalar(out=out, in0=in0, scalar1=s1, scalar2=None, op0=op0), reads=reads, writes=writes)
    else:
        k.S.op(eng, lambda e: e.tensor_scalar(out=out, in0=in0, scalar1=s1, scalar2=s2, op0=op0, op1=op1), reads=reads, writes=writes)


def TT(k, eng, out, in0, in1, op, reads, writes):
    k.S.op(eng, lambda e: e.tensor_tensor(out=out, in0=in0, in1=in1, op=op), reads=reads, writes=writes)


def STT(k, eng, out, in0, scalar, in1, op0, op1, reads, writes):
    k.S.op(eng, lambda e: e.scalar_tensor_tensor(out=out, in0=in0, scalar=scalar, in1=in1, op0=op0, op1=op1), reads=reads, writes=writes)


def CP(k, eng, out, in_, reads, writes):
    k.S.op(eng, lambda e: e.tensor_copy(out=out, in_=in_), reads=reads, writes=writes)


def DMA(k, q, out, in_, reads, writes, slow=False):
    if slow:
        k.S.dma(q, lambda e: e.dma_start(out=out, in_=in_, allow_slow_non_contiguous=True), reads=reads, writes=writes)
    else:
        k.S.dma(q, lambda e: e.dma_start(out=out, in_=in_), reads=reads, writes=writes)


class Stager:
    def __init__(self, k, nbuf, nelem):
        self.k = k
        self.bufs = [k.A.f32(nelem) for _ in range(nbuf)]
        self.deps = [Dep() for _ in range(nbuf)]
        self.pos = 0
        self.nelem = nelem
        self.engs = ["gpsimd", "vector"]
        self.epos = 0

    def load(self, src_ap, dst_ap, ddst, eng=None, slow=False):
        i = self.pos
        self.pos = (i + 1) % len(self.bufs)
        n = 1
        for d_ in src_ap.shape[1:]:
            n *= d_
        assert n <= self.nelem, (n, self.nelem)
        s = self.bufs[i][:, 0:n]
        if len(src_ap.shape) == 3:
            s = s.rearrange("p (a b) -> p a b", a=src_ap.shape[1])
        elif len(src_ap.shape) == 4:
            s = s.rearrange("p (a b c) -> p a b c", a=src_ap.shape[1], b=src_ap.shape[2])
        s = s[0:src_ap.shape[0]]
        DMA(self.k, "sync", s, src_ap, [], [self.deps[i]], slow=slow)
        if eng is None:
            eng = self.engs[self.epos]
            self.epos = (self.epos + 1) % len(self.engs)
        CP(self.k, eng, dst_ap, s, [self.deps[i]], [ddst])


def phase_outproj_ln(k, srcT, d_src, w_ap, g_ap, b_ap, h_in, h_out):
    S = k.S
    A = k.A
    m0 = A.mark()
    k.hTt = [A.bf16(2048), A.bf16(2048)]
    wo = A.bf16(16 * D)
    wov = r3(wo, a=16)
    dwo = [Dep() for _ in range(16)]
    stg = Stager(k, 3, 2048)
    gt = A.f32(D); bt = A.f32(D); dgb = Dep()
    src = [A.bf16(2048), A.bf16(2048)]
    dsrc = [Dep(), Dep()]
    at = [A.f32(D), A.f32(D)]
    dat = [Dep(), Dep()]
    lnst_raw = A.f32(8 + D)
    lnst = (lnst_raw[:, 0:1], lnst_raw[:, 1:2], lnst_raw[:, 2:3], lnst_raw[:, 3:4], lnst_raw[:, 4:5], lnst_raw[:, 8:8 + D])
    dlnst = Dep()
    ln_load_params(k, g_ap, b_ap, gt, bt, dgb)
    for kc in range(16):
        stg.load(w_ap[kc * 128:(kc + 1) * 128, :], wov[:, kc, :], dwo[kc])
    for tt in range(NT):
        b = tt % 2
        DMA(k, "sync", src[b], srcT[tt], [d_src[tt]], [dsrc[b]])
        DMA(k, "sync", at[b], h_in[tt * 128:(tt + 1) * 128, :], [k.d_h[tt]], [dat[b]])
        sv = r3(src[b], a=16)
        for ns in range(4):
            ps, dps = k.ps[ns], k.dps[ns]
            for kc in range(16):
                MM(k, ps, sv[:, kc, :], wov[:, kc, ns * 512:(ns + 1) * 512], kc == 0, kc == 15, [dsrc[b], dwo[kc]], [dps], kc == 15)
            STT(k, "vector", at[b][:, ns * 512:(ns + 1) * 512], at[b][:, ns * 512:(ns + 1) * 512], DN_ALPHA, ps, ALU.mult, ALU.add, [dps, dat[b]], [dat[b]])
        ln_tile(k, at[b], dat[b], gt, bt, dgb, tt, h_out, k.hT, lnst, dlnst, write_hT=True)
    barrier(k)
    A.reset(m0)


def rel_bucket_np(n):
    n = np.maximum(n, 0)
    nf = np.maximum(n, 1).astype(np.float32)
    large = 16 + (np.log(nf / np.float32(16)) / np.float32(np.log(128 / 16)) * np.float32(16)).astype(np.int32)
    large = np.minimum(large, 31)
    return np.where(n < 16, n, large)


def dsa_consts():
    ql = np.arange(128)[:, None]
    x = np.arange(256)[None, :]
    dist = np.where(x < 128, 128 + ql - x, ql - (x - 128))
    bk = rel_bucket_np(dist)
    oh = np.zeros((128, 32, 256), np.float32)
    for b in range(32):
        oh[:, b, :] = (bk == b)
    caus = np.where(np.arange(128)[None, :] <= np.arange(128)[:, None], 0.0, NEG).astype(np.float32)
    return {"c_ohb": oh.reshape(128, 32 * 256), "c_caus": caus}


def phase_dsa(k, j, li, h_in, h_out):
    S = k.S
    A = k.A
    W = k.w
    NH_ = 16
    att_scale = 128 ** -0.5
    widx_scale = (16 ** -0.5) * (128 ** -0.5)
    QT_d = k.dsa_QT; QIT_d = k.dsa_QIT; OT_d = k.dsa_OT
    d_OT = [Dep() for _ in range(NT)]
    d_QT = Dep(); d_QIT = Dep()
    m_phase = A.mark()
    CQT = A.bf16(4 * L); CQTv = r3(CQT, a=4); dCQT = Dep()
    CKVT = A.bf16(4 * L); CKVTv = r3(CKVT, a=4); dCKVT = Dep()
    KIT = A.bf16(L); dKIT = Dep()
    WI = A.f32(NT * 16); WIv = r3(WI, a=NT); dWI = Dep()
    identb = A.bf16(128); didb = Dep()
    CP(k, "vector", identb, k.ident, [], [didb])
    mA = A.mark()
    HTb = [A.bf16(2048), A.bf16(2048)]; dHTb = [Dep(), Dep()]
    win = A.bf16(16 * 1168); winv = r3(win, a=16); dwin = [Dep() for _ in range(16)]
    stg = Stager(k, 3, 2048)
    qg = A.f32(512); kg = A.f32(512); dqg = Dep()
    DMA(k, "sync", qg, W["dsa_q_norm"][j].rearrange("(o d) -> o d", o=1).partition_broadcast(128), [], [dqg])
    DMA(k, "sync", kg, W["dsa_kv_norm"][j].rearrange("(o d) -> o d", o=1).partition_broadcast(128), [], [dqg])
    for kc in range(16):
        stg.load(W["dsa_w_in"][j, kc * 128:(kc + 1) * 128, :], winv[:, kc, :], dwin[kc])
    pj = [A.f32(1168), A.f32(1168)]; dpj = [Dep(), Dep()]
    sm = A.f32(16); dsm = Dep()
    junk = A.f32(512)
    for t in range(NT):
        b = t % 2
        DMA(k, "sync", HTb[b], k.hT[t], [k.d_hT[t]], [dHTb[b]])
        HTt = r3(HTb[b], a=16)
        for ns, (c0, c1) in enumerate([(0, 512), (512, 1024), (1024, 1168)]):
            ps, dps = k.ps[ns], k.dps[ns]
            for kc in range(16):
                MM(k, ps[:, 0:c1 - c0], HTt[:, kc, :], winv[:, kc, c0:c1], kc == 0, kc == 15, [dHTb[b], dwin[kc]], [dps], kc == 15)
            CP(k, "vector", pj[b][:, c0:c1], ps[:, 0:c1 - c0], [dps], [dpj[b]])
        for qi, (c0, gain) in enumerate([(0, qg), (512, kg)]):
            ss = sm[:, qi * 4:qi * 4 + 1]; rs = sm[:, qi * 4 + 1:qi * 4 + 2]
            ACT(k, junk, pj[b][:, c0:c0 + 512], AF.Square, [dpj[b]], [dsm], accum_out=ss)
            TS(k, "vector", rs, ss, 1.0 / 512, RMS_EPS, ALU.mult, ALU.add, [dsm], [dsm])
            ACT(k, rs, rs, AF.Sqrt, [dsm], [dsm])
            k.S.op("vector", lambda e, rs=rs: e.reciprocal(out=rs, in_=rs), reads=[dsm], writes=[dsm])
            STT(k, "vector", pj[b][:, c0:c0 + 512], pj[b][:, c0:c0 + 512], rs, gain, ALU.mult, ALU.mult, [dsm, dpj[b], dqg], [dpj[b]])
        TS(k, "vector", WIv[:, t, :], pj[b][:, 1152:1168], widx_scale, None, ALU.mult, None, [dpj[b]], [dWI])
        for grp, (c0, dstv, ddst) in enumerate([(0, CQTv, dCQT), (512, CKVTv, dCKVT)]):
            ps, dps = k.ps[4 + grp], k.dps[4 + grp]
            for kc in range(4):
                TR(k, ps[:, kc * 128:(kc + 1) * 128], pj[b][:, c0 + kc * 128:c0 + (kc + 1) * 128], k.ident, [dpj[b]], [dps], kc == 3)
            ACT(k, dstv[:, :, t * 128:(t + 1) * 128], ps.rearrange("p (a b) -> p a b", a=4), AF.Copy, [dps], [ddst])
        ps, dps = k.ps[6], k.dps[6]
        TR(k, ps[:, 0:128], pj[b][:, 1024:1152], k.ident, [dpj[b]], [dps], True)
        ACT(k, KIT[:, t * 128:(t + 1) * 128], ps[:, 0:128], AF.Copy, [dps], [dKIT])
    barrier(k)
    A.reset(mA)
    if getattr(k, 'dsa_stop', '') == 'A':
        return
    mB = A.mark()
    wq = A.bf16(4 * 2048); wqv = r3(wq, a=4); dwq = [Dep() for _ in range(4)]
    stg = Stager(k, 3, 2048)
    ev = [A.bf16(512), A.bf16(512)]; dev_ = [Dep(), Dep()]
    cnt = 0
    for wname, dst_d, ddst in [("dsa_w_uq", QT_d, d_QT), ("dsa_w_qidx", QIT_d, d_QIT)]:
        for kc in range(4):
            stg.load(W[wname][j, kc * 128:(kc + 1) * 128, :], wqv[:, kc, :], dwq[kc])
        for h in range(NH_):
            for sl_ in range(4):
                ps, dps = k.ps[cnt % 4], k.dps[cnt % 4]
                b = cnt % 2
                cnt += 1
                for kc in range(4):
                    MM(k, ps, wqv[:, kc, h * 128:(h + 1) * 128], CQTv[:, kc, sl_ * 512:(sl_ + 1) * 512], kc == 0, kc == 3, [dwq[kc], dCQT], [dps], kc == 3)
                if b == 0:
                    ACT(k, ev[b], ps, AF.Copy, [dps], [dev_[b]])
                else:
                    CP(k, "vector", ev[b], ps, [dps], [dev_[b]])
                DMA(k, "gpsimd", dst_d[:, h, sl_ * 512:(sl_ + 1) * 512], ev[b], [dev_[b]], [ddst])
    barrier(k)
    A.reset(mB)
    if getattr(k, 'dsa_stop', '') == 'B':
        return
    KT_d = k.dsa_KT; V_d = k.dsa_V
    dKT = Dep(); dV = Dep()
    mC = A.mark()
    evc = [A.bf16(512), A.bf16(512)]; devc = [Dep(), Dep()]
    stg = Stager(k, 3, 2048)
    wukT = A.bf16(4 * 2048); wukTv = wukT.rearrange("p (c h d) -> p c h d", c=4, h=NH_); dwukT = Dep()
    wuv = A.bf16(4 * 2048); wuvv = wuv.rearrange("p (c h d) -> p c h d", c=4, h=NH_); dwuv = Dep()
    uk = [A.f32(512), A.f32(512)]; duk = [Dep(), Dep()]
    for h in range(NH_):
        b = h % 2
        DMA(k, "sync", uk[b], W["dsa_w_uk"][j, h], [], [duk[b]])
        ps, dps = k.ps[4 + b], k.dps[4 + b]
        for kc in range(4):
            TR(k, ps[:, kc * 128:(kc + 1) * 128], uk[b][:, kc * 128:(kc + 1) * 128], k.ident, [duk[b]], [dps], kc == 3)
        ACT(k, wukTv[:, :, h, :], ps.rearrange("p (a b) -> p a b", a=4), AF.Copy, [dps], [dwukT])
    for h in range(NH_):
        stg.load(W["dsa_w_uv"][j, h].rearrange("(c p) d -> p c d", p=128), wuvv[:, :, h, :], dwuv)
    cnt = 0
    for h in range(NH_):
        for sl_ in range(4):
            ps, dps = k.ps[cnt % 4], k.dps[cnt % 4]
            cnt += 1
            for kc in range(4):
                MM(k, ps, wukTv[:, kc, h, :], CKVTv[:, kc, sl_ * 512:(sl_ + 1) * 512], kc == 0, kc == 3, [dwukT, dCKVT], [dps], kc == 3)
            b = cnt % 2
            if b == 0:
                ACT(k, evc[b], ps, AF.Copy, [dps], [devc[b]])
            else:
                CP(k, "vector", evc[b], ps, [dps], [devc[b]])
            DMA(k, "gpsimd", KT_d[h, :, sl_ * 512:(sl_ + 1) * 512], evc[b], [devc[b]], [dKT])
    for st_ in range(NT):
        for hg in range(4):
            ps, dps = k.ps[cnt % 4], k.dps[cnt % 4]
            cnt += 1
            for kc in range(4):
                MM(k, ps, CKVTv[:, kc, st_ * 128:(st_ + 1) * 128], wuvv[:, kc, hg * 4:(hg + 1) * 4, :], kc == 0, kc == 3, [dwuv, dCKVT], [dps], kc == 3)
            b = cnt % 2
            if b == 0:
                ACT(k, evc[b], ps, AF.Copy, [dps], [devc[b]])
            else:
                CP(k, "vector", evc[b], ps, [dps], [devc[b]])
            DMA(k, "gpsimd", V_d[hg * 4:(hg + 1) * 4, :, st_ * 128:(st_ + 1) * 128].rearrange("h p d -> p h d"), r3(evc[b], a=4), [devc[b]], [dV])
    barrier(k)
    A.reset(mC)
    if getattr(k, 'dsa_stop', '') == 'C':
        return
    Tn = A.f32(NH_ * 256); Tnv = r3(Tn, a=NH_); dTn = Dep()
    caus = A.f32(128); dcaus = Dep()
    DMA(k, "sync", caus, k.c["c_caus"], [], [dcaus])
    mT = A.mark()
    ohb = A.f32(32 * 256); ohbv = r3(ohb, a=32); dohb = Dep()
    rbB = A.f32(512); drbB = Dep()
    DMA(k, "sync", ohb, k.c["c_ohb"], [], [dohb])
    DMA(k, "sync", rbB, W["rel_bias"].rearrange("(o b) h -> o (b h)", o=1).partition_broadcast(128), [], [drbB])
    for h in range(NH_):
        eng = "vector"
        TS(k, eng, Tnv[:, h, :], ohbv[:, 0, :], rbB[:, h:h + 1], None, ALU.mult, None, [dohb, drbB], [dTn])
        for b_ in range(1, 32):
            STT(k, eng, Tnv[:, h, :], ohbv[:, b_, :], rbB[:, b_ * 16 + h:b_ * 16 + h + 1], Tnv[:, h, :], ALU.mult, ALU.add, [dohb, drbB, dTn], [dTn])
        TS(k, eng, Tnv[:, h, :], Tnv[:, h, :], rbB[:, 31 * 16 + h:31 * 16 + h + 1], None, ALU.subtract, None, [drbB, dTn], [dTn])
    barrier(k)
    A.reset(mT)
    accs = [A.f32(L), A.f32(L)]; daccs = [Dep(), Dep()]
    tmp = [A.f32(512), A.f32(512)]; dtmp = [Dep(), Dep()]
    madds = [A.bf16(L), A.bf16(L)]; dmadds = [Dep(), Dep()]
    Xs = [A.f32(L) for _ in range(3)]; dXs = [Dep() for _ in range(3)]
    Ps = [A.bf16(L) for _ in range(3)]; dPs = [Dep() for _ in range(3)]
    PTs = [A.bf16(NT * 128) for _ in range(3)]; dPTs = [Dep() for _ in range(3)]
    QIbs = [A.bf16(NH_ * 128), A.bf16(NH_ * 128)]; dQIbs = [Dep(), Dep()]
    QTbs = [A.bf16(NH_ * 128), A.bf16(NH_ * 128)]; dQTbs = [Dep(), Dep()]
    OT = [A.bf16(NH_ * 128), A.bf16(NH_ * 128)]; dOTs = [Dep(), Dep()]
    mx8 = A.f32(8); dmx = Dep()
    sm2s = [A.f32(8) for _ in range(3)]; dsm2s = [Dep() for _ in range(3)]
    dgrs = [A.bf16(128) for _ in range(3)]; ddgrs = [Dep() for _ in range(3)]
    Kh = [A.bf16(L) for _ in range(3)]; dKh = [Dep() for _ in range(3)]
    Vh = [A.bf16(L) for _ in range(3)]; dVh = [Dep() for _ in range(3)]
    z1 = A.f32(1); dz1 = Dep()
    k.S.op("vector", lambda e: e.memset(z1, 0.0), reads=[], writes=[dz1])

    def pre_thunks(jb):
        SL = (jb + 1) * 128
        nbk = (SL + 511) // 512
        pb = jb % 2
        acc, dacc = accs[pb], daccs[pb]
        madd, dmadd = madds[pb], dmadds[pb]
        QIbv = r3(QIbs[pb], a=NH_); dQIb = dQIbs[pb]
        th_ = []

        def t_load():
            DMA(k, "sync", QIbv, QIT_d[:, :, jb * 128:(jb + 1) * 128], [d_QIT], [dQIb])
        th_.append(t_load)
        cnt = [0]
        for h in range(NH_):
            def t_head(h=h):
                for bk in range(nbk):
                    w_ = min(512, SL - bk * 512)
                    bank = 4
                    ps, dps = k.ps[bank], k.dps[bank]
                    tb = cnt[0] % 2
                    cnt[0] += 1
                    MM(k, ps[:, 0:w_], QIbv[:, h, :], KIT[:, bk * 512:bk * 512 + w_], True, True, [dQIb, dKIT], [dps], True)
                    if h == 0:
                        TS(k, "vector", acc[:, bk * 512:bk * 512 + w_], ps[:, 0:w_], z1, WIv[:, jb, h:h + 1], ALU.max, ALU.mult, [dps, dWI, dz1], [dacc])
                    else:
                        TS(k, "vector", tmp[tb][:, 0:w_], ps[:, 0:w_], z1, WIv[:, jb, h:h + 1], ALU.max, ALU.mult, [dps, dWI, dz1], [dtmp[tb]])
                        TT(k, "gpsimd", acc[:, bk * 512:bk * 512 + w_], acc[:, bk * 512:bk * 512 + w_], tmp[tb][:, 0:w_], ALU.add, [dtmp[tb], dacc], [dacc])
            th_.append(t_head)

        def t_caus():
            TT(k, "gpsimd", acc[:, jb * 128:SL], acc[:, jb * 128:SL], caus, ALU.add, [dacc, dcaus], [dacc])
        th_.append(t_caus)
        if SL > 256:
            for r in range(32):
                def t_round():
                    k.S.op("vector", lambda e: e.max(out=mx8, in_=acc[:, 0:SL]), reads=[dacc], writes=[dmx])
                    k.S.op("vector", lambda e: e.match_replace(out=acc[:, 0:SL], in_to_replace=mx8, in_values=acc[:, 0:SL], imm_value=-2.0e30), reads=[dacc, dmx], writes=[dacc])
                th_.append(t_round)

            def t_fin():
                TS(k, "vector", madd[:, 0:SL], acc[:, 0:SL], -1.5e30, NEG, ALU.is_gt, ALU.mult, [dacc], [dmadd])
        else:
            def t_fin():
                TS(k, "vector", madd[:, 0:SL], acc[:, 0:SL], -1.0e29, NEG, ALU.is_lt, ALU.mult, [dacc], [dmadd])
        th_.append(t_fin)
        return th_

    NB3 = 3
    dPV = [Dep(), Dep()]

    def bufs(i):
        b3 = i % NB3
        return (Xs[b3], dXs[b3], Ps[b3], dPs[b3], r3(PTs[b3], a=NT), dPTs[b3], sm2s[b3], dsm2s[b3], dgrs[b3], ddgrs[b3], Kh[b3], dKh[b3], Vh[b3], dVh[b3])

    def stageA(i):
        jb, h = divmod(i, NH_)
        SL = (jb + 1) * 128
        nbk = (SL + 511) // 512
        pb = jb % 2
        madd, dmadd = madds[pb], dmadds[pb]
        QTbv = r3(QTbs[pb], a=NH_); dQTb = dQTbs[pb]
        X, dX, P, dP, PTv, dPT, sm2, dsm2, dgr, ddgr, Kb, dKb, Vb, dVb = bufs(i)
        if h == 0:
            DMA(k, "sync", QTbv, QT_d[:, :, jb * 128:(jb + 1) * 128], [d_QT], [dQTb])
        DMA(k, "sync", Kb[:, 0:SL], KT_d[h, :, 0:SL], [dKT], [dKb])
        DMA(k, "sync", Vb[:, 0:SL], V_d[h, :, 0:SL], [dV], [dVb])
        for bk in range(nbk):
            w_ = min(512, SL - bk * 512)
            bank = (bk % 2) + 2 * (i % 2)
            ps, dps = k.ps[bank], k.dps[bank]
            MM(k, ps[:, 0:w_], QTbv[:, h, :], Kb[:, bk * 512:bk * 512 + w_], True, True, [dQTb, dKb], [dps], True)
            STT(k, "vector", X[:, bk * 512:bk * 512 + w_], ps[:, 0:w_], att_scale, madd[:, bk * 512:bk * 512 + w_], ALU.mult, ALU.add, [dps, dmadd], [dX])
        lo = max(0, jb - 1) * 128
        tlo = 0 if jb >= 1 else 128
        TT(k, "gpsimd", X[:, lo:SL], X[:, lo:SL], Tnv[:, h, tlo:256], ALU.add, [dX, dTn], [dX])
        rmax = sm2[:, 0:1]; nmax = sm2[:, 1:2]; rsum = sm2[:, 2:3]
        k.S.op("vector", lambda e: e.tensor_reduce(out=rmax, in_=X[:, 0:SL], axis=AX.X, op=ALU.max), reads=[dX], writes=[dsm2])
        TS(k, "vector", nmax, rmax, -1.0, None, ALU.mult, None, [dsm2], [dsm2])
        ACT(k, P[:, 0:SL], X[:, 0:SL], AF.Exp, [dX, dsm2], [dP, dsm2], bias=nmax, scale=1.0, accum_out=rsum)

    def stageB(i):
        jb, h = divmod(i, NH_)
        X, dX, P, dP, PTv, dPT, sm2, dsm2, dgr, ddgr, Kb, dKb, Vb, dVb = bufs(i)
        rsum = sm2[:, 2:3]; rinv = sm2[:, 3:4]
        k.S.op("vector", lambda e: e.reciprocal(out=rinv, in_=rsum), reads=[dsm2], writes=[dsm2])
        TS(k, "vector", dgr, identb, rinv, None, ALU.mult, None, [dsm2, didb], [ddgr])
        for st_ in range(jb + 1):
            bank = 6 + (st_ // 4) % 2
            ps, dps = k.ps[bank], k.dps[bank]
            last = (st_ % 4 == 3) or (st_ == jb)
            MM(k, ps[:, (st_ % 4) * 128:(st_ % 4 + 1) * 128], P[:, st_ * 128:(st_ + 1) * 128], dgr, True, True, [dP, ddgr], [dps], last)
            if last:
                s0 = (st_ // 4) * 4
                n_ = st_ - s0 + 1
                ACT(k, PTv[:, s0:s0 + n_, :], ps[:, 0:n_ * 128].rearrange("p (a b) -> p a b", a=n_), AF.Copy, [dps], [dPT])
        Vhv = r3(Vb, a=NT)
        pv = k.ps[5][:, (i % 2) * 128:(i % 2 + 1) * 128]
        for st_ in range(jb + 1):
            MM(k, pv, Vhv[:, st_, :], PTv[:, st_, :], st_ == 0, st_ == jb, [dVb, dPT], [dPV[i % 2]], st_ == jb)

    def stageC(i):
        jb, h = divmod(i, NH_)
        pb = jb % 2
        ot, dot = OT[pb], dOTs[pb]
        otv = r3(ot, a=NH_)
        pv = k.ps[5][:, (i % 2) * 128:(i % 2 + 1) * 128]
        CP(k, "vector", otv[:, h, :], pv, [dPV[i % 2]], [dot])
        if h == NH_ - 1:
            DMA(k, "gpsimd", OT_d[jb], ot, [dot], [d_OT[jb]])

    for t_ in pre_thunks(0):
        t_()
    NHEADS = NT * NH_
    nxt = []
    pos = 0
    per = 0
    for i in range(NHEADS + 2):
        if i < NHEADS:
            jb, h = divmod(i, NH_)
            if h == 0:
                for t_ in nxt[pos:]:
                    t_()
                nxt = pre_thunks(jb + 1) if jb + 1 < NT else []
                per = (len(nxt) + NH_ - 1) // NH_
                pos = 0
            stageA(i)
        if 0 <= i - 1 < NHEADS:
            stageB(i - 1)
        if 0 <= i - 2 < NHEADS:
            stageC(i - 2)
        if i < NHEADS:
            for t_ in nxt[pos:pos + per]:
                t_()
            pos += per
    barrier(k)
    A.reset(m_phase)
    if getattr(k, 'dsa_stop', '') == 'D':
        return
    phase_outproj_ln(k, OT_d, d_OT, W["dsa_w_out"][j], W["ln_mix_g"][li], W["ln_mix_b"][li], h_in, h_out)
import math as _math

S5_KVEC = [0, -1, -2, -3, -4, -5, -6, -7, 7, 6, 5, 4, 3, 2, 1, 0, 0, 1, 2, 3, 4, 5, 6, 7, 1, 2, 3, 4, 5, 6, 7, 8, 8, 16, 32, 64, 128, 256, 512, 1024]
NK = 40


def s5_consts():
    kv = np.tile(np.array(S5_KVEC, np.float32)[None, :], (128, 1))
    s_ = (np.arange(128) // 16)[:, None]
    t_ = (np.arange(128) // 16)[None, :]
    msk = (t_ >= s_).astype(np.float32)
    import ml_dtypes
    sel = np.zeros((128, 64, 128), np.float32)
    for g8 in range(8):
        for s in range(8):
            for p_ in range(16):
                sel[g8 * 16 + p_, g8 * 8 + s, s * 16 + p_] = 1.0
    selT = np.ascontiguousarray(sel.transpose(2, 1, 0))
    return {"c_kv40": kv, "c_s5mask": msk,
            "c_sel": sel.reshape(128, 8192).astype(ml_dtypes.bfloat16),
            "c_selT": selT.reshape(128, 8192).astype(ml_dtypes.bfloat16)}


def bc(ap, axis, shape):
    return ap.unsqueeze(axis).to_broadcast(list(shape))


def phase_s5(k, sj, li, h_in, h_out):
    S = k.S
    A = k.A
    W = k.w
    M_d, W1_d, W2_d, ZT_d = k.s5_M, k.s5_W1, k.s5_W2, k.s5_ZT
    dM = Dep(); dW1 = Dep(); dW2 = Dep()
    d_ZT = [Dep() for _ in range(NT)]
    TWO_PI = 2.0 * _math.pi
    m_phase = A.mark()
    DCOL = A.f32(128); dDCOL = Dep()
    ASr = A.f32(512); ASi = A.f32(512); ASn = A.f32(512); dAS = Dep()
    ASrv = r3(ASr, a=64); ASiv = r3(ASi, a=64); ASnv = r3(ASn, a=64)
    mP = A.mark()
    KV = A.f32(NK); dKV = Dep()
    DMA(k, "sync", KV, k.c["c_kv40"], [], [dKV])
    MASK = A.f32(128); dMASK = Dep()
    DMA(k, "sync", MASK, k.c["c_s5mask"], [], [dMASK])
    PAre = A.f32(64); PAim = A.f32(64); PDT = A.f32(64); dPA = Dep()
    PBre = A.f32(1024); PBim = A.f32(1024); PCre = A.f32(1024); PCim = A.f32(1024); dPB = Dep(); dPC = Dep()
    PBrev = r3(PBre, a=64); PBimv = r3(PBim, a=64); PCrev = r3(PCre, a=64); PCimv = r3(PCim, a=64)
    ld = [A.f32(2048), A.f32(2048)]; dld = [Dep(), Dep()]
    ld2 = A.f32(2048); dld2 = Dep()
    id64 = k.ident[0:64, 0:64]
    DMA(k, "sync", ld[0][:, 0:16], W["s5_d"][sj], [], [dld[0]])
    CP(k, "vector", ld[0][:, 16:144].rearrange("p (t q) -> p t q", t=8), bc(ld[0][:, 0:16], 1, [128, 8, 16]), [dld[0]], [dld[0]])
    TR(k, k.ps[0][:, 0:128], ld[0][:, 16:144], k.ident, [dld[0]], [k.dps[0]], True)
    CP(k, "vector", DCOL, k.ps[0][:, 0:128], [k.dps[0]], [dDCOL])
    DMA(k, "sync", ld[1][0:64, 0:128], W["s5_a_re"][sj].rearrange("(j g2) n -> j (g2 n)", g2=2), [], [dld[1]])
    DMA(k, "sync", ld[1][0:64, 128:256], W["s5_a_im"][sj].rearrange("(j g2) n -> j (g2 n)", g2=2), [], [dld[1]])
    DMA(k, "sync", ld[1][0:64, 256:258], W["s5_log_dt"][sj].rearrange("(j g2) -> j g2", g2=2), [], [dld[1]])
    CP(k, "vector", ld[1][0:64, 384:512].rearrange("p (g n) -> p g n", g=2), bc(ld[1][0:64, 256:258], 2, [64, 2, 64]), [dld[1]], [dld[1]])
    for i_, (c0, dst) in enumerate([(0, PAre), (128, PAim), (384, PDT)]):
        TR(k, k.ps[1][:, i_ * 64:(i_ + 1) * 64], ld[1][0:64, c0:c0 + 128], id64, [dld[1]], [k.dps[1]], True)
        CP(k, "vector", dst, k.ps[1][:, i_ * 64:(i_ + 1) * 64], [k.dps[1]], [dPA])
    cnt_ = 0
    for name, dstv, ddst, is_c in [("s5_b_re", PBrev, dPB, False), ("s5_b_im", PBimv, dPB, False), ("s5_c_re", PCrev, dPC, True), ("s5_c_im", PCimv, dPC, True)]:
        lb = ld[cnt_ % 2]; dlb = dld[cnt_ % 2]
        cnt_ += 1
        if is_c:
            DMA(k, "sync", lb[0:64, :], W[name][sj].rearrange("(j g2) p n -> j (g2 p n)", g2=2), [], [dlb])
            lb2 = ld2[0:64, :]
            CP(k, "vector", lb2.rearrange("j (p g n) -> j p g n", p=16, g=2), lb[0:64, :].rearrange("j (g p n) -> j p g n", g=2, p=16), [dlb, dld2], [dld2])
            lv = lb2.rearrange("j (p gn) -> j p gn", p=16)
        else:
            DMA(k, "sync", lb[0:64, :], W[name][sj].rearrange("(j g2) n p -> j (g2 n p)", g2=2), [], [dlb])
            lv = lb[0:64, :].rearrange("j (gn p) -> j gn p", p=16)
        for q4 in range(2):
            bank = 2 + q4
            ps, dps = k.ps[bank], k.dps[bank]
            for p8 in range(8):
                p_ = q4 * 8 + p8
                src = lv[:, p_, :] if is_c else lv[:, :, p_]
                TR(k, ps[:, p8 * 64:(p8 + 1) * 64], src, id64, [dlb, dld2], [dps], p8 == 7)
            CP(k, "vector", dstv[:, :, q4 * 8:(q4 + 1) * 8].rearrange("p j q -> p q j"), ps.rearrange("p (q j) -> p q j", q=8), [dps], [ddst])
    if getattr(k, 's5_stop', '') == 'P1':
        barrier(k)
        return
    dE = Dep()
    lr = A.f32(64); ldr = A.f32(64); th = A.f32(64); dtt = A.f32(64)
    TS(k, "vector", lr, PAre, -1.0e-4, None, ALU.min, None, [dPA], [dE])
    ACT(k, dtt, PDT, AF.Exp, [dPA], [dE])
    TT(k, "vector", ldr, lr, dtt, ALU.mult, [dE], [dE])
    TT(k, "vector", th, PAim, dtt, ALU.mult, [dE, dPA], [dE])
    NKK = 64 * NK
    shp = [128, 64, NK]
    ARG = A.f32(NKK); PHI = A.f32(NKK); RHO = A.f32(NKK); QF = A.f32(NKK); MSK2 = A.f32(NKK)
    ARE = A.f32(NKK); AIM = A.f32(NKK)
    QI = A.f32(NKK).bitcast(I32)
    v3 = lambda t_: r3(t_, a=64)
    TT(k, "vector", v3(ARG), bc(ldr, 2, shp), bc(KV, 1, shp), ALU.mult, [dE, dKV], [dE])
    ACT(k, RHO, ARG, AF.Exp, [dE], [dE])
    TT(k, "vector", v3(PHI), bc(th, 2, shp), bc(KV, 1, shp), ALU.mult, [dE, dKV], [dE])

    def sin_of(dst, off):
        TS(k, "vector", ARG, PHI, off, None, ALU.add, None, [dE], [dE])
        TS(k, "vector", QF, ARG, 1.0 / TWO_PI, None, ALU.mult, None, [dE], [dE])
        CP(k, "vector", QI, QF, [dE], [dE])
        CP(k, "vector", QF, QI, [dE], [dE])
        STT(k, "vector", ARG, QF, -TWO_PI, ARG, ALU.mult, ALU.add, [dE], [dE])
        TS(k, "vector", MSK2, ARG, _math.pi, -TWO_PI, ALU.is_gt, ALU.mult, [dE], [dE])
        TT(k, "vector", ARG, ARG, MSK2, ALU.add, [dE], [dE])
        TS(k, "vector", MSK2, ARG, -_math.pi, TWO_PI, ALU.is_lt, ALU.mult, [dE], [dE])
        TT(k, "vector", ARG, ARG, MSK2, ALU.add, [dE], [dE])
        ACT(k, dst, ARG, AF.Sin, [dE], [dE])

    sin_of(AIM, 64.0 * _math.pi)
    sin_of(ARE, 64.5 * _math.pi)
    TT(k, "vector", AIM, AIM, RHO, ALU.mult, [dE], [dE])
    TT(k, "vector", ARE, ARE, RHO, ALU.mult, [dE], [dE])
    AREv = v3(ARE); AIMv = v3(AIM)
    CP(k, "vector", ASrv, AREv[:, :, 32:40], [dE], [dAS])
    CP(k, "vector", ASiv, AIMv[:, :, 32:40], [dE], [dAS])
    TS(k, "vector", ASnv, AIMv[:, :, 32:40], -1.0, None, ALU.mult, None, [dE], [dAS])
    er = A.f32(64); ei = A.f32(64); qr = A.f32(64); qi_ = A.f32(64); den = A.f32(64); t1 = A.f32(64); fr = A.f32(64); fi = A.f32(64)
    TS(k, "vector", er, AREv[:, :, 24], -1.0, None, ALU.add, None, [dE], [dE])
    CP(k, "vector", ei, AIMv[:, :, 24], [dE], [dE])
    TT(k, "vector", qr, er, lr, ALU.mult, [dE], [dE])
    TT(k, "vector", t1, ei, PAim, ALU.mult, [dE], [dE])
    TT(k, "vector", qr, qr, t1, ALU.add, [dE], [dE])
    TT(k, "vector", qi_, ei, lr, ALU.mult, [dE], [dE])
    TT(k, "vector", t1, er, PAim, ALU.mult, [dE], [dE])
    TT(k, "vector", qi_, qi_, t1, ALU.subtract, [dE], [dE])
    TT(k, "vector", den, lr, lr, ALU.mult, [dE], [dE])
    TT(k, "vector", t1, PAim, PAim, ALU.mult, [dE], [dE])
    TT(k, "vector", den, den, t1, ALU.add, [dE], [dE])
    k.S.op("vector", lambda e: e.reciprocal(out=den, in_=den), reads=[dE], writes=[dE])
    TT(k, "vector", fr, qr, den, ALU.mult, [dE], [dE])
    TT(k, "vector", fi, qi_, den, ALU.mult, [dE], [dE])
    BBre = A.f32(1024); BBim = A.f32(1024); tb_ = A.f32(1024)
    BBrev = r3(BBre, a=64); BBimv = r3(BBim, a=64); tbv = r3(tb_, a=64)
    s16 = [128, 64, 16]
    TT(k, "vector", BBrev, bc(fr, 2, s16), PBrev, ALU.mult, [dE, dPB], [dE])
    TT(k, "vector", tbv, bc(fi, 2, s16), PBimv, ALU.mult, [dE, dPB], [dE])
    TT(k, "vector", BBre, BBre, tb_, ALU.subtract, [dE], [dE])
    TT(k, "vector", BBimv, bc(fr, 2, s16), PBimv, ALU.mult, [dE, dPB], [dE])
    TT(k, "vector", tbv, bc(fi, 2, s16), PBrev, ALU.mult, [dE, dPB], [dE])
    TT(k, "vector", BBim, BBim, tb_, ALU.add, [dE], [dE])
    if getattr(k, 's5_stop', '') == 'P2':
        barrier(k)
        return
    PB_ = 8
    PM = A.f32(2); dPM = Dep()
    k.S.op("vector", lambda e: e.memset(PM, 0.0), reads=[], writes=[dPM])
    k.S.op("vector", lambda e: e.memset(PM[0:64, 0:1], 1.0), reads=[dPM], writes=[dPM])
    k.S.op("vector", lambda e: e.memset(PM[64:128, 1:2], 1.0), reads=[dPM], writes=[dPM])
    LM = [[A.bf16(1024), A.bf16(1024)], [A.bf16(1024), A.bf16(1024)]]
    T = [A.f32(1024) for _ in range(4)]
    Tv = [t_.rearrange("p (j s q) -> p j s q", j=PB_, s=8) for t_ in T]
    RREb = A.bf16(1024); RIMb = A.bf16(1024)
    L2RE = A.f32(1024); L2IM = A.f32(1024)
    W2o = A.bf16(4096)
    W2ov = W2o.rearrange("p (j r m) -> p j r m", j=PB_, r=4)
    Mout = [A.bf16(512), A.bf16(512)]; dMout = [Dep(), Dep()]
    TAB = [A.bf16(512), A.bf16(512)]; dTAB = [Dep(), Dep()]
    for tb2 in TAB:
        k.S.op("vector", lambda e, tb2=tb2: e.memset(tb2, 0.0), reads=[], writes=[dE])
    dCh = Dep()
    s4 = [128, PB_, 8, 16]
    j8 = lambda t_: r3(t_, a=PB_)

    def products(blk, Bre, Bim, j0):
        a0 = blk * 8
        Ar = AREv[:, j0:j0 + PB_, a0:a0 + 8]; Ai = AIMv[:, j0:j0 + PB_, a0:a0 + 8]
        br = Bre[:, j0:j0 + PB_, :]; bi = Bim[:, j0:j0 + PB_, :]
        TT(k, "vector", Tv[0], bc(Ar, 3, s4), bc(br, 2, s4), ALU.mult, [dE, dPC, dCh], [dCh])
        TT(k, "vector", Tv[1], bc(Ai, 3, s4), bc(bi, 2, s4), ALU.mult, [dE, dPC, dCh], [dCh])
        TT(k, "vector", Tv[2], bc(Ai, 3, s4), bc(br, 2, s4), ALU.mult, [dE, dPC, dCh], [dCh])
        TT(k, "vector", Tv[3], bc(Ar, 3, s4), bc(bi, 2, s4), ALU.mult, [dE, dPC, dCh], [dCh])

    mcnt = 0
    for ch in range(64 // PB_):
        j0 = ch * PB_
        products(0, BBrev, BBimv, j0)
        TT(k, "vector", T[0], T[0], T[1], ALU.subtract, [dCh], [dCh])
        TT(k, "vector", T[2], T[2], T[3], ALU.add, [dCh], [dCh])
        for g2 in range(2):
            TS(k, "vector", LM[g2][0], T[0], PM[:, g2:g2 + 1], None, ALU.mult, None, [dCh, dPM], [dCh])
            TS(k, "vector", LM[g2][1], T[2], PM[:, g2:g2 + 1], None, ALU.mult, None, [dCh, dPM], [dCh])
        products(2, PCrev, PCimv, j0)
        TT(k, "vector", RREb, T[0], T[1], ALU.subtract, [dCh], [dCh])
        STT(k, "vector", RIMb, T[2], -1.0, T[3], ALU.mult, ALU.subtract, [dCh], [dCh])
        for half in range(PB_ // 2):
            bank = mcnt % 2
            mo, dmo = Mout[mcnt % 2], dMout[mcnt % 2]
            mcnt += 1
            ps, dps = k.ps[bank], k.dps[bank]
            for q in range(4):
                jj = half * 2 + q // 2
                g2 = q % 2
                MM(k, ps[:, q * 128:(q + 1) * 128], j8(LM[g2][0])[:, jj, :], j8(RREb)[:, jj, :], True, False, [dCh], [dps], False)
                MM(k, ps[:, q * 128:(q + 1) * 128], j8(LM[g2][1])[:, jj, :], j8(RIMb)[:, jj, :], False, True, [dCh], [dps], q == 3)
            TT(k, "vector", r3(mo, a=4), r3(ps, a=4), bc(MASK, 1, [128, 4, 128]), ALU.mult, [dps, dMASK], [dmo])
            g0 = (j0 + half * 2) * 2
            for q in range(4):
                DMA(k, "gpsimd", M_d[g0 + q], mo[:, q * 128:(q + 1) * 128], [dmo], [dM])
        if getattr(k, 's5_stop', '') == 'P3a':
            continue
        products(1, BBrev, BBimv, j0)
        TT(k, "vector", L2RE, T[0], T[1], ALU.subtract, [dCh], [dCh])
        TT(k, "vector", L2IM, T[2], T[3], ALU.add, [dCh], [dCh])
        for jj in range(PB_):
            bank = 2 + jj % 2
            ps, dps = k.ps[bank], k.dps[bank]
            tab, dtab = TAB[jj % 2], dTAB[jj % 2]
            TR(k, ps[:, 0:128], j8(L2RE)[:, jj, :], k.ident, [dCh], [dps], False)
            TR(k, ps[:, 128:256], j8(L2IM)[:, jj, :], k.ident, [dCh], [dps], True)
            tabv = tab.rearrange("p (r a m) -> p r a m", r=2, a=2)
            psv = ps[:, 0:256].rearrange("p (r m) -> p r m", r=2)
            CP(k, "vector", tabv[:, :, 0, 0:64], psv[:, :, 0:64], [dps], [dtab])
            CP(k, "vector", tabv[:, :, 1, 64:128], psv[:, :, 64:128], [dps], [dtab])
            DMA(k, "gpsimd", W1_d[j0 + jj], tab, [dtab], [dW1])
        if getattr(k, 's5_stop', '') == 'P3b':
            continue
        products(3, PCrev, PCimv, j0)
        TT(k, "vector", T[0], T[0], T[1], ALU.subtract, [dCh], [dCh])
        STT(k, "vector", T[2], T[2], -1.0, T[3], ALU.mult, ALU.subtract, [dCh], [dCh])
        for g2 in range(2):
            TS(k, "vector", W2ov[:, :, 2 * g2, :], j8(T[0]), PM[:, g2:g2 + 1], None, ALU.mult, None, [dCh, dPM], [dCh])
            TS(k, "vector", W2ov[:, :, 2 * g2 + 1, :], j8(T[2]), PM[:, g2:g2 + 1], None, ALU.mult, None, [dCh, dPM], [dCh])
        for jj in range(PB_):
            DMA(k, "gpsimd", W2_d[j0 + jj], W2o[:, jj * 512:(jj + 1) * 512], [dCh], [dW2, dCh])
    barrier(k)
    A.reset(mP)
    if getattr(k, 's5_stop', '') in ('P', 'P3a', 'P3b'):
        return
    R1 = A.bf16(16 * 2048)
    R1v = R1.rearrange("p (b s c) -> p b s c", b=16, s=8)
    dR1 = Dep()
    mR2 = A.mark()
    HT = A.bf16(NT * 2048); HTv = HT.rearrange("p (t c x) -> p t c x", t=NT, c=16); dHT = Dep()
    for t in range(NT):
        DMA(k, "sync", HTv[:, t].rearrange("p c x -> p (c x)"), k.hT[t], [k.d_hT[t]], [dHT])
    stg = Stager(k, 3, 2048)
    wcb = [A.bf16(2048), A.bf16(2048)]; dwcb = [Dep(), Dep()]
    cnt = 0
    for chb in range(16):
        b = chb % 2
        stg.load(W["s5_w_in"][sj].rearrange("(kc p) n -> p kc n", p=128)[:, :, chb * 128:(chb + 1) * 128], r3(wcb[b], a=16), dwcb[b])
        for sl_ in range(4):
            ps, dps = k.ps[cnt % 4], k.dps[cnt % 4]
            cnt += 1
            for kc in range(16):
                MM(k, ps, r3(wcb[b], a=16)[:, kc, :], HTv[:, sl_ * 4:(sl_ + 1) * 4, kc, :], kc == 0, kc == 15, [dwcb[b], dHT], [dps], kc == 15)
            src = ps.rearrange("p (c s) -> p s c", s=8)
            dst = R1v[:, chb, :, sl_ * 64:(sl_ + 1) * 64]
            if cnt % 2 == 0:
                ACT(k, dst, src, AF.Copy, [dps], [dR1])
            else:
                CP(k, "vector", dst, src, [dps], [dR1])
    barrier(k)
    A.reset(mR2)
    if getattr(k, 's5_stop', '') == 'U':
        return
    R2 = A.bf16(128 * 256)
    Xv = r3(R2, a=128)
    dX = Dep()
    SEL = A.bf16(8192); SELv = r3(SEL, a=64); SELT = A.bf16(8192); SELTv = r3(SELT, a=64); dSEL = Dep()
    DMA(k, "sync", SEL, k.c["c_sel"], [], [dSEL])
    DMA(k, "sync", SELT, k.c["c_selT"], [], [dSEL])
    for g0 in range(0, 128, 2):
        bank = (g0 // 2) % 4
        ps, dps = k.ps[bank], k.dps[bank]
        for gi in range(2):
            g = g0 + gi
            for s_ in range(8):
                MM(k, ps[:, gi * 256:(gi + 1) * 256], SELv[:, (g % 8) * 8 + s_, :], R1v[:, g // 8, s_, :], s_ == 0, s_ == 7, [dSEL, dR1], [dps], (gi == 1 and s_ == 7))
        if (g0 // 2) % 2 == 0:
            ACT(k, Xv[:, g0:g0 + 2, :], r3(ps, a=2), AF.Copy, [dps], [dX])
        else:
            CP(k, "vector", Xv[:, g0:g0 + 2, :], r3(ps, a=2), [dps], [dX])
    barrier(k)
    if getattr(k, 's5_stop', '') == 'X':
        return
    mL = A.mark()
    dZF = Dep()
    Mg = [A.bf16(256), A.bf16(256)]; W1t = [A.bf16(512), A.bf16(512)]; W2t = [A.bf16(512), A.bf16(512)]
    dMg = [Dep(), Dep()]; dW1t = [Dep(), Dep()]; dW2t = [Dep(), Dep()]
    REb = [A.f32(384), A.f32(384)]; IMb = [A.f32(384), A.f32(384)]
    dSC = Dep()
    for t_ in REb + IMb:
        k.S.op("vector", lambda e, t_=t_: e.memset(t_, 0.0), reads=[], writes=[dSC])
    HRE = A.bf16(256); HIM = A.bf16(256); dH = Dep()
    yb = [A.f32(256), A.f32(256)]; y2b = [A.f32(256), A.f32(256)]; dyb = [Dep(), Dep()]
    Zall = [A.bf16(8 * 256), A.bf16(8 * 256)]; dZall = [Dep(), Dep()]
    for j in range(64):
        b = j % 2
        for g2 in range(2):
            DMA(k, "sync", Mg[b][:, g2 * 128:(g2 + 1) * 128], M_d[2 * j + g2], [dM], [dMg[b]])
        DMA(k, "sync", W1t[b], W1_d[j], [dW1], [dW1t[b]])
        DMA(k, "sync", W2t[b], W2_d[j], [dW2], [dW2t[b]])
        ps, dps = k.ps[b], k.dps[b]
        for ri in range(2):
            MM(k, ps[:, ri * 256:(ri + 1) * 256], W1t[b][:, (2 * ri) * 128:(2 * ri + 1) * 128], Xv[:, 2 * j, :], True, False, [dW1t[b], dX], [dps], False)
            MM(k, ps[:, ri * 256:(ri + 1) * 256], W1t[b][:, (2 * ri + 1) * 128:(2 * ri + 2) * 128], Xv[:, 2 * j + 1, :], False, True, [dW1t[b], dX], [dps], ri == 1)
        ACT(k, REb[0][:, 128:384], ps[:, 0:256], AF.Copy, [dps], [dSC])
        ACT(k, IMb[0][:, 128:384], ps[:, 256:512], AF.Copy, [dps], [dSC])
        cur = 0
        for i in range(8):
            sft = 1 << i
            ra, ia = REb[cur], IMb[cur]
            rb_, ib_ = REb[1 - cur], IMb[1 - cur]
            ar = ASrv[:, j, i:i + 1]; ai = ASiv[:, j, i:i + 1]; an = ASnv[:, j, i:i + 1]
            STT(k, "vector", rb_[:, 128:384], ra[:, 128 - sft:384 - sft], ar, ra[:, 128:384], ALU.mult, ALU.add, [dSC, dAS], [dSC])
            STT(k, "vector", rb_[:, 128:384], ia[:, 128 - sft:384 - sft], an, rb_[:, 128:384], ALU.mult, ALU.add, [dSC, dAS], [dSC])
            STT(k, "vector", ib_[:, 128:384], ia[:, 128 - sft:384 - sft], ar, ia[:, 128:384], ALU.mult, ALU.add, [dSC, dAS], [dSC])
            STT(k, "vector", ib_[:, 128:384], ra[:, 128 - sft:384 - sft], ai, ib_[:, 128:384], ALU.mult, ALU.add, [dSC, dAS], [dSC])
            cur = 1 - cur
        ACT(k, HRE, REb[cur][:, 127:383], AF.Copy, [dSC], [dH])
        ACT(k, HIM, IMb[cur][:, 127:383], AF.Copy, [dSC], [dH])
        for g2 in range(2):
            g = 2 * j + g2
            prt = slice(g2 * 64, (g2 + 1) * 64)
            py, dpy = k.ps[2 + g2], k.dps[2 + g2]
            MM(k, py[:, 0:256], r3(Mg[b], a=2)[:, g2, :], Xv[:, g, :], True, False, [dMg[b], dX], [dpy], False)
            MM(k, py[:, 0:256], W2t[b][:, (2 * g2) * 128:(2 * g2 + 1) * 128], HRE, False, False, [dW2t[b], dH], [dpy], False)
            MM(k, py[:, 0:256], W2t[b][:, (2 * g2 + 1) * 128:(2 * g2 + 2) * 128], HIM, False, True, [dW2t[b], dH], [dpy], True)
            y = yb[g2]; y2 = y2b[g2]; dy_ = dyb[g2]
            STT(k, "vector", y, Xv[:, g, :], DCOL[:, g:g + 1], py[:, 0:256], ALU.mult, ALU.add, [dpy, dX, dDCOL], [dy_])
            TT(k, "gpsimd", y2, y, y, ALU.mult, [dy_], [dy_])
            TS(k, "gpsimd", y2, y2, 0.044715, 1.0, ALU.mult, ALU.add, [dy_], [dy_])
            TT(k, "gpsimd", y2, y2, y, ALU.mult, [dy_], [dy_])
            ACT(k, y2, y2, AF.Sigmoid, [dy_], [dy_], scale=1.5957691216057308)
            zb = (g // 8) % 2
            TT(k, "gpsimd", r3(Zall[zb], a=8)[:, g % 8, :], y, y2, ALU.mult, [dy_, dZall[zb]], [dZall[zb]])
        if j % 4 == 3:
            chb = j // 4
            zb = chb % 2
            for sp in range(4):
                bank = 4 + sp % 4
                ps2, dps2 = k.ps[bank], k.dps[bank]
                for si in range(2):
                    s_ = sp * 2 + si
                    for g8 in range(8):
                        MM(k, ps2[:, si * 256:(si + 1) * 256], SELTv[:, g8 * 8 + s_, :], r3(Zall[zb], a=8)[:, g8, :], g8 == 0, g8 == 7, [dSEL, dZall[zb]], [dps2], (si == 1 and g8 == 7))
                if sp % 2 == 0:
                    ACT(k, R1v[:, chb, sp * 2:sp * 2 + 2, :], r3(ps2, a=2), AF.Copy, [dps2], [dZF])
                else:
                    CP(k, "vector", R1v[:, chb, sp * 2:sp * 2 + 2, :], r3(ps2, a=2), [dps2], [dZF])
    barrier(k)
    A.reset(mR2)
    if getattr(k, 's5_stop', '') == 'L':
        return
    Z2N = A.bf16(NT * 2048)
    Z2Nv = Z2N.rearrange("p (t c l s) -> p t c l s", t=NT, c=16, l=16)
    dZ2 = Dep()
    stg = Stager(k, 3, 2048)
    wgb = [A.bf16(2048), A.bf16(2048)]; dwgb = [Dep(), Dep()]
    sg = [A.f32(512), A.f32(512)]; dsg = [Dep(), Dep()]
    cnt = 0
    for nb in range(16):
        b = nb % 2
        stg.load(W["s5_w_glu"][sj].rearrange("(kc p) n -> p kc n", p=128)[:, :, nb * 128:(nb + 1) * 128], r3(wgb[b], a=16), dwgb[b])
        for q in range(4):
            ps, dps = k.ps[cnt % 4], k.dps[cnt % 4]
            sb_ = cnt % 2
            cnt += 1
            zsl = lambda kc: R1v[:, kc, 2 * q:2 * q + 2, :]
            for kc in range(16):
                MM(k, ps, r3(wgb[b], a=16)[:, kc, :], zsl(kc), kc == 0, kc == 15, [dwgb[b], dZF], [dps], kc == 15)
            ACT(k, sg[sb_], ps, AF.Sigmoid, [dps], [dsg[sb_]])
            dst = Z2Nv[:, :, nb, :, 2 * q:2 * q + 2].rearrange("p t l s -> p s t l")
            in0 = sg[sb_].rearrange("p (s t l) -> p s t l", s=2, t=16)
            in1 = R1v[:, nb, 2 * q:2 * q + 2, :].rearrange("p s (t l) -> p s t l", t=16)
            TT(k, "vector", dst, in0, in1, ALU.mult, [dsg[sb_], dZF], [dZ2])
    for t in range(NT):
        DMA(k, "gpsimd", ZT_d[t], Z2N[:, t * 2048:(t + 1) * 2048], [dZ2], [d_ZT[t]])
    barrier(k)
    A.reset(m_phase)
    phase_outproj_ln(k, ZT_d, d_ZT, W["s5_w_out"][sj], W["ln_mix_g"][li], W["ln_mix_b"][li], h_in, h_out)
W_SPECS = [
    ("rel_bias", [32, 16]),
    ("s5_w_in", [2, 2048, 2048]), ("s5_a_re", [2, 128, 64]), ("s5_a_im", [2, 128, 64]), ("s5_log_dt", [2, 128]),
    ("s5_b_re", [2, 128, 64, 16]), ("s5_b_im", [2, 128, 64, 16]), ("s5_c_re", [2, 128, 16, 64]), ("s5_c_im", [2, 128, 16, 64]),
    ("s5_d", [2, 128, 16]), ("s5_w_glu", [2, 2048, 2048]), ("s5_w_out", [2, 2048, 2048]),
    ("dsa_w_in", [2, 2048, 1168]), ("dsa_q_norm", [2, 512]), ("dsa_kv_norm", [2, 512]),
    ("dsa_w_uq", [2, 512, 2048]), ("dsa_w_qidx", [2, 512, 2048]), ("dsa_w_uk", [2, 16, 128, 512]),
    ("dsa_w_uv", [2, 16, 512, 128]), ("dsa_w_out", [2, 2048, 2048]),
    ("moe_w_group", [4, 2048, 4]), ("moe_b_group", [4, 4]), ("moe_w_expert", [4, 2048, 32]), ("moe_b_expert", [4, 32]),
    ("moe_w_gate", [4, 32, 2048, 256]), ("moe_w_up", [4, 32, 2048, 256]), ("moe_w_down", [4, 32, 256, 2048]),
    ("ln_mix_g", [4, 2048]), ("ln_mix_b", [4, 2048]), ("ln_ffn_g", [4, 2048]), ("ln_ffn_b", [4, 2048]),
]


def host_consts():
    c = {}
    c["c_ident"] = np.eye(128, dtype=np.float32)
    c.update(dsa_consts())
    c.update(s5_consts())
    return c


def build_nc(mode="full", used=None, **kw):
    nc = bass.Bass("TRN2", target_bir_lowering=False)
    k = K()
    for a_, b_ in kw.items():
        setattr(k, a_, b_)
    k.nc = nc
    k.x = nc.dram_tensor("x", [L, D], F32, kind="ExternalInput").ap()
    k.w = {}
    for name, shp in W_SPECS:
        if used is not None and name not in used:
            continue
        k.w[name] = nc.dram_tensor(name, getattr(k, 'wshape', {}).get(name, shp), F32, kind="ExternalInput").ap()
    k.c = {}
    for name, arr in host_consts().items():
        k.c[name] = nc.dram_tensor(name, list(arr.shape), F32 if arr.dtype == np.float32 else BF16, kind="ExternalInput").ap()
    k.out = nc.dram_tensor("out", [L, D], F32, kind="ExternalOutput").ap()
    k.hA = nc.dram_tensor("hA", [L, D], F32).ap()
    k.hB = nc.dram_tensor("hB", [L, D], F32).ap()
    k.hT = nc.dram_tensor("hT", [NT, 128, 16 * 128], BF16).ap()
    k.s5_M = nc.dram_tensor("s5_M", [128, 128, 128], BF16).ap()
    k.s5_W1 = nc.dram_tensor("s5_W1", [64, 128, 512], BF16).ap()
    k.s5_W2 = nc.dram_tensor("s5_W2", [64, 128, 512], BF16).ap()
    k.s5_ZT = nc.dram_tensor("s5_ZT", [NT, 128, 16 * 128], BF16).ap()
    k.dsa_QT = nc.dram_tensor("dsa_QT", [128, 16, L], BF16).ap()
    k.dsa_QIT = nc.dram_tensor("dsa_QIT", [128, 16, L], BF16).ap()
    k.dsa_OT = nc.dram_tensor("dsa_OT", [NT, 128, 16 * 128], BF16).ap()
    k.dsa_KT = nc.dram_tensor("dsa_KT", [16, 128, L], BF16).ap()
    k.dsa_V = nc.dram_tensor("dsa_V", [16, 128, L], BF16).ap()
    k.d_h = [Dep() for _ in range(NT)]
    k.d_hT = [Dep() for _ in range(NT)]
    with ExitStack() as st:
        k.S = Sched(nc, st)
        k.A = Arena(nc, st, 52000)
        k.ps = []
        k.dps = []
        for i in range(8):
            k.ps.append(st.enter_context(nc.psum_tensor("ps%d" % i, [128, 512], F32))[:])
            k.dps.append(Dep())
        A = k.A
        k.ident = A.f32(128)
        d_id = Dep()
        k.S.dma("sync", lambda e: e.dma_start(out=k.ident, in_=k.c["c_ident"]), writes=[d_id])
        k.d_hTt = [Dep(), Dep()]
        k.hTt_pos = 0
        barrier(k)
        if mode == "dsa_only":
            phase_prep(k)
            phase_dsa(k, 0, 1, k.x, k.out)
        elif mode == "s5_only":
            phase_prep(k)
            phase_s5(k, 0, 0, k.x, k.out)
        elif mode == "moe_only":
            phase_prep(k)
            phase_moe(k, 0, k.x, k.out, write_hT=False)
        elif mode == "full":
            phase_prep(k)
            h_in = k.x
            bufs = [k.hA, k.hB]
            bi = 0
            for li in range(DEPTH):
                hm = bufs[bi]; bi ^= 1
                if li % 2 == 0:
                    phase_s5(k, li // 2, li, h_in, hm)
                else:
                    phase_dsa(k, li // 2, li, h_in, hm)
                last = (li == DEPTH - 1)
                hf = k.out if last else bufs[bi]
                bi ^= 1
                phase_moe(k, li, hm, hf, write_hT=not last)
                h_in = hf
        k.S.finish(k.d_h)
        k.S.emit()
    return nc


def kernel(**inputs):
    nc = build_nc("full")
    consts = host_consts()
    x = np.ascontiguousarray(inputs["x"], dtype=np.float32)
    shared = {name: np.ascontiguousarray(inputs[name], dtype=np.float32) for name, _ in W_SPECS}
    shared.update(consts)
    in_maps = []
    for c in range(8):
        m = dict(shared)
        m["x"] = x[c]
        in_maps.append(m)
    res = run_bass_kernel_spmd(nc, in_maps, core_ids=list(range(8)))
    return np.stack([np.asarray(r["out"], dtype=np.float32) for r in res.results], axis=0)
```

```python
from concourse.bass_utils import run_bass_kernel_spmd
import numpy as np
import concourse.bass as bass
import concourse.mybir as mybir
from contextlib import ExitStack

F32 = mybir.dt.float32
BF16 = mybir.dt.bfloat16
I32 = mybir.dt.int32
ALU = mybir.AluOpType
AF = mybir.ActivationFunctionType
AX = mybir.AxisListType

ENGINES = ("tensor", "vector", "scalar", "gpsimd", "sync")
DMA_RING = 8


class Dep:
    __slots__ = ("name", "w", "r")

    def __init__(self, name=""):
        self.name = name
        self.w = None
        self.r = {}


class Sched:
    def __init__(self, nc, stack, same_engine_sync=True):
        self.nc = nc
        self.stack = stack
        self.streams = {e: [] for e in ENGINES}
        self.count = {e: 0 for e in ENGINES}
        self.seen = {e: {} for e in ENGINES}
        self.sems = {}
        for e in ENGINES:
            self.sems[e] = stack.enter_context(nc.semaphore("s_" + e))
        self.ring = {}
        self.ring_cnt = {}
        self.ring_pos = {}
        for q in ("sync", "gpsimd", "scalar"):
            self.ring[q] = []
            for i in range(DMA_RING):
                key = "d_%s_%d" % (q, i)
                self.sems[key] = stack.enter_context(nc.semaphore(key))
                self.ring[q].append(key)
            self.ring_cnt[q] = [0] * DMA_RING
            self.ring_pos[q] = 0
        self.same_engine_sync = same_engine_sync
        self.out_deps = []

    def _collect(self, reads, writes):
        need = {}

        def add(kv):
            if kv is None:
                return
            k, v = kv
            if need.get(k, 0) < v:
                need[k] = v
        for d in reads:
            add(d.w)
        for d in writes:
            add(d.w)
            for k, v in d.r.items():
                add((k, v))
        return need

    def _waits(self, eng, need, skip_self):
        ws = []
        seen = self.seen[eng]
        for k, v in need.items():
            if k == eng and skip_self:
                continue
            if seen.get(k, 0) >= v:
                continue
            seen[k] = v
            ws.append((k, v))
        return ws

    def _update(self, reads, writes, ticket):
        k, v = ticket
        for d in writes:
            d.w = ticket
            d.r = {}
        for d in reads:
            if d.r.get(k, 0) < v:
                d.r[k] = v

    def op(self, eng, fn, reads=(), writes=(), inc=True):
        need = self._collect(reads, writes)
        skip_self = (eng == "tensor") or (not self.same_engine_sync)
        ws = self._waits(eng, need, skip_self)
        if inc:
            self.count[eng] += 1
            ticket = (eng, self.count[eng])
        else:
            ticket = (eng, self.count[eng] + 1)
        self.streams[eng].append((ws, fn, (eng, 1) if inc else None))
        self._update(reads, writes, ticket)
        return ticket

    def dma(self, q, fn, reads=(), writes=()):
        need = self._collect(reads, writes)
        pos = self.ring_pos[q]
        self.ring_pos[q] = (pos + 1) % DMA_RING
        key = self.ring[q][pos]
        prev = self.ring_cnt[q][pos]
        if prev > 0:
            if need.get(key, 0) < prev * 16:
                need[key] = prev * 16
        ws = self._waits(q, need, False)
        self.ring_cnt[q][pos] = prev + 1
        ticket = (key, (prev + 1) * 16)
        self.streams[q].append((ws, fn, (key, 16)))
        self._update(reads, writes, ticket)
        return ticket

    def finish(self, deps):
        need = self._collect(deps, deps)
        ws = self._waits("sync", need, False)
        self.streams["sync"].append((ws, None, None))

    def emit(self):
        nc = self.nc
        sems = self.sems
        streams = self.streams

        def run(engh, name):
            for ws, fn, inc in streams[name]:
                for k, v in ws:
                    engh.wait_ge(sems[k], v)
                if fn is not None:
                    ins = fn(engh)
                    if inc is not None:
                        ins.then_inc(sems[inc[0]], inc[1])

        with nc.Block() as block:
            @block.tensor
            def _(e):
                run(e, "tensor")

            @block.vector
            def _(e):
                run(e, "vector")

            @block.scalar
            def _(e):
                run(e, "scalar")

            @block.gpsimd
            def _(e):
                run(e, "gpsimd")

            @block.sync
            def _(e):
                run(e, "sync")
D = 2048
L = 2048
NT = 16
DEPTH = 4
DN_ALPHA = (2 * DEPTH) ** 0.25
LN_EPS = 1e-5
RMS_EPS = 1e-6
NE = 32
FF = 256
NEG = -1.0e30


class Arena:
    def __init__(self, nc, stack, nelem):
        self.t = stack.enter_context(nc.sbuf_tensor("arena", [128, nelem], F32))
        self.n = nelem
        self.off = 0

    def mark(self):
        return self.off

    def reset(self, m):
        self.off = m

    def f32(self, n, shape=None):
        assert self.off + n <= self.n, ("arena overflow", self.off, n, self.n)
        v = self.t[:, self.off:self.off + n]
        self.off += n
        return v

    def bf16(self, n):
        m = (n + 1) // 2
        return self.f32(m).bitcast(BF16)[:, 0:n]


class K:
    pass


def r3(ap, **kw):
    return ap.rearrange("p (a b) -> p a b", **kw)


def barrier(k):
    S = k.S
    cur = {}
    for e in ENGINES:
        if S.count[e] > 0:
            cur[e] = S.count[e]
    for q in S.ring:
        for i, key in enumerate(S.ring[q]):
            if S.ring_cnt[q][i] > 0:
                cur[key] = S.ring_cnt[q][i] * 16
    for e in ENGINES:
        ws = S._waits(e, dict(cur), False)
        if ws:
            S.streams[e].append((ws, None, None))


def ln_load_params(k, g_ap, b_ap, gt, bt, dgb):
    S = k.S
    S.dma("sync", lambda e: e.dma_start(out=gt, in_=g_ap.rearrange("(o d) -> o d", o=1).partition_broadcast(128)), writes=[dgb])
    S.dma("sync", lambda e: e.dma_start(out=bt, in_=b_ap.rearrange("(o d) -> o d", o=1).partition_broadcast(128)), writes=[dgb])


def ln_tile(k, a, da, gt, bt, dgb, tt, h_out, hT_out, st, dst, write_hT=True):
    S = k.S
    s1, s2, mean, var, rstd, junk = st
    S.op("scalar", lambda e: e.activation(out=junk, in_=a, func=AF.Identity, accum_out=s1), reads=[da], writes=[dst])
    S.op("scalar", lambda e: e.activation(out=junk, in_=a, func=AF.Square, accum_out=s2), reads=[da], writes=[dst])
    S.op("vector", lambda e: e.tensor_scalar(out=mean, in0=s1, scalar1=1.0 / D, scalar2=None, op0=ALU.mult), reads=[dst], writes=[dst])
    S.op("vector", lambda e: e.tensor_tensor(out=var, in0=mean, in1=mean, op=ALU.mult), reads=[dst], writes=[dst])
    S.op("vector", lambda e: e.scalar_tensor_tensor(out=var, in0=s2, scalar=1.0 / D, in1=var, op0=ALU.mult, op1=ALU.subtract), reads=[dst], writes=[dst])
    S.op("vector", lambda e: e.tensor_scalar(out=var, in0=var, scalar1=LN_EPS, scalar2=None, op0=ALU.add), reads=[dst], writes=[dst])
    S.op("scalar", lambda e: e.activation(out=var, in_=var, func=AF.Sqrt), reads=[dst], writes=[dst])
    S.op("vector", lambda e: e.reciprocal(out=rstd, in_=var), reads=[dst], writes=[dst])
    S.op("vector", lambda e: e.tensor_scalar(out=a, in0=a, scalar1=mean, scalar2=rstd, op0=ALU.subtract, op1=ALU.mult), reads=[dst, da], writes=[da])
    S.op("vector", lambda e: e.tensor_tensor(out=a, in0=a, in1=gt, op=ALU.mult), reads=[da, dgb], writes=[da])
    S.op("vector", lambda e: e.tensor_tensor(out=a, in0=a, in1=bt, op=ALU.add), reads=[da, dgb], writes=[da])
    S.dma("gpsimd", lambda e: e.dma_start(out=h_out[tt * 128:(tt + 1) * 128, :], in_=a), reads=[da], writes=[k.d_h[tt]])
    if write_hT:
        emit_hT(k, a, da, tt, hT_out)


def emit_hT(k, a, da, tt, hT_out):
    S = k.S
    slot = k.hTt_pos
    k.hTt_pos = (slot + 1) % 2
    hTt, dhTt = k.hTt[slot], k.d_hTt[slot]
    for q in range(4):
        bank = 6 + (q % 2)
        ps, dps = k.ps[bank], k.dps[bank]
        for j in range(4):
            kc = q * 4 + j
            S.op("tensor", lambda e, kc=kc, j=j, ps=ps: e.transpose(ps[:, j * 128:(j + 1) * 128], a[:, kc * 128:(kc + 1) * 128], k.ident),
                 reads=[da], writes=[dps], inc=(j == 3))
        S.op("scalar", lambda e, q=q, ps=ps: e.activation(out=hTt[:, q * 512:(q + 1) * 512], in_=ps, func=AF.Copy),
             reads=[dps], writes=[dhTt])
    S.dma("gpsimd", lambda e: e.dma_start(out=hT_out[tt], in_=hTt), reads=[dhTt], writes=[k.d_hT[tt]])


def phase_prep(k):
    S = k.S
    A = k.A
    m = A.mark()
    k.hTt = [A.bf16(2048), A.bf16(2048)]
    xt = [A.f32(D), A.f32(D)]
    dxt = [Dep(), Dep()]
    for tt in range(NT):
        b = tt % 2
        S.dma("sync", lambda e, tt=tt, b=b: e.dma_start(out=xt[b], in_=k.x[tt * 128:(tt + 1) * 128, :]), writes=[dxt[b]])
        emit_hT(k, xt[b], dxt[b], tt, k.hT)
    barrier(k)
    A.reset(m)


def phase_moe(k, li, h_in, h_out, write_hT=True):
    S = k.S
    A = k.A
    m0 = A.mark()
    k.hTt = [A.bf16(2048), A.bf16(2048)]
    NH = 2
    TH = NT // NH
    HT = A.bf16(TH * 16 * 128)
    HTv = HT.rearrange("p (t c x) -> p t c x", t=TH, c=16)
    dHT = Dep()
    yacc = [A.f32(D) for _ in range(TH)]
    dy = [Dep() for _ in range(TH)]
    wg = A.bf16(16 * FF); wu = A.bf16(16 * FF); wd = A.bf16(2 * D)
    wgv = r3(wg, a=16); wuv = r3(wu, a=16); wdv = r3(wd, a=2)
    dwg = [Dep(), Dep()]; dwu = [Dep(), Dep()]; dwd = [Dep(), Dep()]
    NSTG = 4
    stg = [A.f32(2048) for _ in range(NSTG)]
    dstg = [Dep() for _ in range(NSTG)]
    gt = A.f32(D); bt = A.f32(D); dgb = Dep()
    hh = A.bf16(2 * 2 * 512)
    hhv = hh.rearrange("p (t f x) -> p t f x", t=2, f=2)
    dhh = [[Dep(), Dep()], [Dep(), Dep()]]
    sl = [A.f32(512), A.f32(512)]
    dsl = [Dep(), Dep()]
    wr_s = A.f32(16 * 36); wr = A.bf16(16 * 36); dwr = Dep()
    wr_sv = r3(wr_s, a=16); wrv = r3(wr, a=16)
    rb = A.f32(36); drb = Dep()
    gates = A.f32(TH * NE); dgates = [Dep() for _ in range(TH)]
    gv = r3(gates, a=TH)
    rt = A.f32(256); drt = Dep()
    lnst_raw = A.f32(8 + D)
    lnst = (lnst_raw[:, 0:1], lnst_raw[:, 1:2], lnst_raw[:, 2:3], lnst_raw[:, 3:4], lnst_raw[:, 4:5], lnst_raw[:, 8:8 + D])
    dlnst = Dep()

    nc = k.nc
    S.dma("sync", lambda e: e.dma_start(out=wr_sv[:, :, 0:4], in_=k.w["moe_w_group"][li].rearrange("(c p) g -> p c g", p=128)), writes=[dwr])
    S.dma("sync", lambda e: e.dma_start(out=wr_sv[:, :, 4:36], in_=k.w["moe_w_expert"][li].rearrange("(c p) g -> p c g", p=128)), writes=[dwr])
    S.op("vector", lambda e: e.tensor_copy(out=wr, in_=wr_s), reads=[dwr], writes=[dwr])
    S.dma("sync", lambda e: e.dma_start(out=rb[:, 0:4], in_=k.w["moe_b_group"][li].rearrange("(o g) -> o g", o=1).partition_broadcast(128)), writes=[drb])
    S.dma("sync", lambda e: e.dma_start(out=rb[:, 4:36], in_=k.w["moe_b_expert"][li].rearrange("(o g) -> o g", o=1).partition_broadcast(128)), writes=[drb])
    ln_load_params(k, k.w["ln_ffn_g"][li], k.w["ln_ffn_b"][li], gt, bt, dgb)

    stg_pos = [0, 0]

    def load_cast(src_ap, dst_ap, ddst, eng):
        i = stg_pos[0]
        stg_pos[0] = (i + 1) % NSTG
        s = stg[i]
        sv = s if len(src_ap.shape) == 2 else r3(s, a=src_ap.shape[1])
        if not getattr(k, 'skip_wdma', False) or stg_pos[1] < 8:
            S.dma("sync", lambda e: e.dma_start(out=sv, in_=src_ap), writes=[dstg[i]])
        stg_pos[1] += 1
        if eng == "scalar":
            S.op(eng, lambda e: e.activation(out=dst_ap, in_=sv, func=AF.Copy), reads=[dstg[i]], writes=[ddst])
        else:
            S.op(eng, lambda e: e.tensor_copy(out=dst_ap, in_=sv), reads=[dstg[i]], writes=[ddst])

    wgate = k.w["moe_w_gate"][li]
    wup = k.w["moe_w_up"][li]
    wdown = k.w["moe_w_down"][li]

    for half in range(NH):
        t0 = half * TH
        for t in range(TH):
            S.dma("sync", lambda e, t=t, t0=t0: e.dma_start(out=HTv[:, t].rearrange("p c x -> p (c x)"), in_=k.hT[t0 + t]),
                  reads=[k.d_hT[t0 + t]], writes=[dHT])
        for t in range(TH):
            S.dma("sync", lambda e, t=t, t0=t0: e.dma_start(out=yacc[t], in_=h_in[(t0 + t) * 128:(t0 + t + 1) * 128, :]),
                  reads=[k.d_h[t0 + t]], writes=[dy[t]])
            S.op("scalar", lambda e, t=t: e.activation(out=yacc[t], in_=yacc[t], func=AF.Copy, scale=DN_ALPHA),
                 reads=[dy[t]], writes=[dy[t]])
        for t in range(TH):
            bank = 6 + (t % 2)
            ps, dps = k.ps[bank], k.dps[bank]
            for kc in range(16):
                S.op("tensor", lambda e, t=t, kc=kc, ps=ps: e.matmul(ps[:, 0:36], lhsT=HTv[:, t, kc, :], rhs=wrv[:, kc, :], start=(kc == 0), stop=(kc == 15)),
                     reads=[dHT, dwr], writes=[dps], inc=(kc == 15))
            lg = rt[:, 0:36]; gmax = rt[:, 40:41]; gexp = rt[:, 44:48]; gsum = rt[:, 48:49]; gone = rt[:, 52:56]
            elc = rt[:, 56:64]; mx8 = rt[:, 64:72]; nm1 = rt[:, 72:73]; ew = rt[:, 80:88]; selm = rt[:, 88:96]
            den = rt[:, 96:97]; coef = rt[:, 100:104]; ngmax = rt[:, 104:105]; gp = rt[:, 105:106]; within = rt[:, 112:120]
            V = lambda fn, rd=(), wr_=(): S.op("vector", fn, reads=[drt] + list(rd), writes=[drt] + list(wr_))
            V(lambda e, ps=ps: e.tensor_tensor(out=lg, in0=ps[:, 0:36], in1=rb, op=ALU.add), rd=[dps, drb])
            V(lambda e: e.tensor_reduce(out=gmax, in_=lg[:, 0:4], axis=AX.X, op=ALU.max))
            V(lambda e: e.tensor_scalar(out=ngmax, in0=gmax, scalar1=-1.0, scalar2=None, op0=ALU.mult))
            S.op("scalar", lambda e: e.activation(out=gexp, in_=lg[:, 0:4], func=AF.Exp, bias=ngmax, scale=1.0, accum_out=gsum), reads=[drt], writes=[drt])
            V(lambda e: e.reciprocal(out=gp, in_=gsum))
            V(lambda e: e.tensor_scalar(out=gone, in0=lg[:, 0:4], scalar1=gmax, scalar2=None, op0=ALU.is_equal))
            V(lambda e: e.tensor_scalar(out=elc, in0=lg[:, 4:12], scalar1=gone[:, 0:1], scalar2=None, op0=ALU.mult))
            for g in range(1, 4):
                V(lambda e, g=g: e.scalar_tensor_tensor(out=elc, in0=lg[:, 4 + 8 * g:12 + 8 * g], scalar=gone[:, g:g + 1], in1=elc, op0=ALU.mult, op1=ALU.add))
            V(lambda e: e.max(out=mx8, in_=elc))
            V(lambda e: e.tensor_scalar(out=nm1, in0=mx8[:, 0:1], scalar1=-1.0, scalar2=None, op0=ALU.mult))
            S.op("scalar", lambda e: e.activation(out=ew, in_=elc, func=AF.Exp, bias=nm1, scale=1.0), reads=[drt], writes=[drt])
            V(lambda e: e.tensor_scalar(out=selm, in0=elc, scalar1=mx8[:, 1:2], scalar2=None, op0=ALU.is_ge))
            V(lambda e: e.tensor_tensor(out=ew, in0=ew, in1=selm, op=ALU.mult))
            V(lambda e: e.tensor_reduce(out=den, in_=ew, axis=AX.X, op=ALU.add))
            V(lambda e: e.reciprocal(out=den, in_=den))
            V(lambda e: e.tensor_scalar(out=within, in0=ew, scalar1=den, scalar2=None, op0=ALU.mult))
            V(lambda e: e.tensor_scalar(out=coef, in0=gone, scalar1=gp, scalar2=None, op0=ALU.mult))
            for g in range(4):
                V(lambda e, g=g, t=t: e.tensor_scalar(out=gv[:, t, g * 8:(g + 1) * 8], in0=within, scalar1=coef[:, g:g + 1], scalar2=None, op0=ALU.mult),
                  wr_=[dgates[t]])
        for ex in range(getattr(k, 'ne_limit', NE)):
            for hf in range(2):
                load_cast(wgate[ex, hf * 1024:(hf + 1) * 1024, :].rearrange("(c p) f -> p c f", p=128), wgv[:, hf * 8:(hf + 1) * 8, :], dwg[hf], "scalar")
                load_cast(wup[ex, hf * 1024:(hf + 1) * 1024, :].rearrange("(c p) f -> p c f", p=128), wuv[:, hf * 8:(hf + 1) * 8, :], dwu[hf], "scalar")
            for tt in range(2):
                for fc in range(2):
                    pg, dpg = k.ps[fc], k.dps[fc]
                    pu, dpu = k.ps[2 + fc], k.dps[2 + fc]
                    for kc in range(16):
                        S.op("tensor", lambda e, kc=kc, fc=fc, tt=tt, pg=pg: e.matmul(pg, lhsT=wgv[:, kc, fc * 128:(fc + 1) * 128], rhs=HTv[:, tt * 4:(tt + 1) * 4, kc, :], start=(kc == 0), stop=(kc == 15)),
                             reads=[dHT, dwg[kc // 8]], writes=[dpg], inc=(kc == 15))
                    for kc in range(16):
                        S.op("tensor", lambda e, kc=kc, fc=fc, tt=tt, pu=pu: e.matmul(pu, lhsT=wuv[:, kc, fc * 128:(fc + 1) * 128], rhs=HTv[:, tt * 4:(tt + 1) * 4, kc, :], start=(kc == 0), stop=(kc == 15)),
                             reads=[dHT, dwu[kc // 8]], writes=[dpu], inc=(kc == 15))
                    S.op("scalar", lambda e, fc=fc, pg=pg: e.activation(out=sl[fc], in_=pg, func=AF.Silu), reads=[dpg], writes=[dsl[fc]])
                    S.op("vector", lambda e, fc=fc, tt=tt, pu=pu: e.tensor_tensor(out=hhv[:, tt, fc, :], in0=sl[fc], in1=pu, op=ALU.mult),
                         reads=[dsl[fc], dpu], writes=[dhh[tt][fc]])
            for hf in range(2):
                load_cast(wdown[ex, hf * 128:(hf + 1) * 128, :], wdv[:, hf, :], dwd[hf], "scalar")
            cnt = 0
            for tt in range(2):
                for sub in range(4):
                    t = tt * 4 + sub
                    for ds in range(4):
                        bank = 4 + (cnt % 2)
                        cnt += 1
                        po, dpo = k.ps[bank], k.dps[bank]
                        for fc in range(2):
                            S.op("tensor", lambda e, fc=fc, tt=tt, sub=sub, ds=ds, po=po: e.matmul(po, lhsT=hhv[:, tt, fc, sub * 128:(sub + 1) * 128], rhs=wdv[:, fc, ds * 512:(ds + 1) * 512], start=(fc == 0), stop=(fc == 1)),
                                 reads=[dhh[tt][fc], dwd[fc]], writes=[dpo], inc=(fc == 1))
                        S.op("vector", lambda e, t=t, ds=ds, po=po, ex=ex: e.scalar_tensor_tensor(out=yacc[t][:, ds * 512:(ds + 1) * 512], in0=po, scalar=gv[:, t, ex:ex + 1], in1=yacc[t][:, ds * 512:(ds + 1) * 512], op0=ALU.mult, op1=ALU.add),
                             reads=[dpo, dgates[t], dy[t]], writes=[dy[t]])
        for t in range(TH):
            ln_tile(k, yacc[t], dy[t], gt, bt, dgb, t0 + t, h_out, k.hT, lnst, dlnst, write_hT=write_hT)
    barrier(k)
    A.reset(m0)
def MM(k, out, lhsT, rhs, start, stop, reads, writes, inc):
    k.S.op("tensor", lambda e: e.matmul(out, lhsT=lhsT, rhs=rhs, start=start, stop=stop), reads=reads, writes=writes, inc=inc)


def TR(k, out, in_, ident, reads, writes, inc):
    k.S.op("tensor", lambda e: e.transpose(out, in_, ident), reads=reads, writes=writes, inc=inc)


def ACT(k, out, in_, func, reads, writes, bias=None, scale=None, accum_out=None):
    kw = {}
    if bias is not None:
        kw["bias"] = bias
    if scale is not None:
        kw["scale"] = scale
    if accum_out is not None:
        kw["accum_out"] = accum_out
    k.S.op("scalar", lambda e: e.activation(out=out, in_=in_, func=func, **kw), reads=reads, writes=writes)


def TS(k, eng, out, in0, s1, s2, op0, op1, reads, writes):
    if op1 is None:
        k.S.op(eng, lambda e: e.tensor_scalar(out=out, in0=in0, scalar1=s1, scalar2=None, op0=op0), reads=reads, writes=writes)
    else:
        k.S.op(eng, lambda e: e.tensor_scalar(out=out, in0=in0, scalar1=s1, scalar2=s2, op0=op0, op1=op1), reads=reads, writes=writes)


def TT(k, eng, out, in0, in1, op, reads, writes):
    k.S.op(eng, lambda e: e.tensor_tensor(out=out, in0=in0, in1=in1, op=op), reads=reads, writes=writes)


def STT(k, eng, out, in0, scalar, in1, op0, op1, reads, writes):
    k.S.op(eng, lambda e: e.scalar_tensor_tensor(out=out, in0=in0, scalar=scalar, in1=in1, op0=op0, op1=op1), reads=reads, writes=writes)


def CP(k, eng, out, in_, reads, writes):
    if eng == "scalar":
        k.S.op(eng, lambda e: e.activation(out=out, in_=in_, func=AF.Copy), reads=reads, writes=writes)
    else:
        k.S.op(eng, lambda e: e.tensor_copy(out=out, in_=in_), reads=reads, writes=writes)


def DMA(k, q, out, in_, reads, writes, slow=False):
    if slow:
        k.S.dma(q, lambda e: e.dma_start(out=out, in_=in_, allow_slow_non_contiguous=True), reads=reads, writes=writes)
    else:
        k.S.dma(q, lambda e: e.dma_start(out=out, in_=in_), reads=reads, writes=writes)


class Stager:
    def __init__(self, k, nbuf, nelem):
        self.k = k
        self.bufs = [k.A.f32(nelem) for _ in range(nbuf)]
        self.deps = [Dep() for _ in range(nbuf)]
        self.pos = 0
        self.nelem = nelem
        self.engs = ["scalar", "vector"]
        self.epos = 0

    def load(self, src_ap, dst_ap, ddst, eng=None, slow=False):
        i = self.pos
        self.pos = (i + 1) % len(self.bufs)
        n = 1
        for d_ in src_ap.shape[1:]:
            n *= d_
        assert n <= self.nelem, (n, self.nelem)
        s = self.bufs[i][:, 0:n]
        if len(src_ap.shape) == 3:
            s = s.rearrange("p (a b) -> p a b", a=src_ap.shape[1])
        elif len(src_ap.shape) == 4:
            s = s.rearrange("p (a b c) -> p a b c", a=src_ap.shape[1], b=src_ap.shape[2])
        s = s[0:src_ap.shape[0]]
        DMA(self.k, "sync", s, src_ap, [], [self.deps[i]], slow=slow)
        if eng is None:
            eng = self.engs[self.epos]
            self.epos = (self.epos + 1) % len(self.engs)
        CP(self.k, eng, dst_ap, s, [self.deps[i]], [ddst])


def phase_outproj_ln(k, srcT, d_src, w_ap, g_ap, b_ap, h_in, h_out):
    S = k.S
    A = k.A
    m0 = A.mark()
    k.hTt = [A.bf16(2048), A.bf16(2048)]
    wo = A.bf16(16 * D)
    wov = r3(wo, a=16)
    dwo = [Dep() for _ in range(16)]
    stg = Stager(k, 3, 2048)
    gt = A.f32(D); bt = A.f32(D); dgb = Dep()
    src = [A.bf16(2048), A.bf16(2048)]
    dsrc = [Dep(), Dep()]
    at = [A.f32(D), A.f32(D)]
    dat = [Dep(), Dep()]
    lnst_raw = A.f32(8 + D)
    lnst = (lnst_raw[:, 0:1], lnst_raw[:, 1:2], lnst_raw[:, 2:3], lnst_raw[:, 3:4], lnst_raw[:, 4:5], lnst_raw[:, 8:8 + D])
    dlnst = Dep()
    ln_load_params(k, g_ap, b_ap, gt, bt, dgb)
    for kc in range(16):
        stg.load(w_ap[kc * 128:(kc + 1) * 128, :], wov[:, kc, :], dwo[kc])
    for tt in range(NT):
        b = tt % 2
        DMA(k, "sync", src[b], srcT[tt], [d_src[tt]], [dsrc[b]])
        DMA(k, "sync", at[b], h_in[tt * 128:(tt + 1) * 128, :], [k.d_h[tt]], [dat[b]])
        sv = r3(src[b], a=16)
        for ns in range(4):
            ps, dps = k.ps[ns], k.dps[ns]
            for kc in range(16):
                MM(k, ps, sv[:, kc, :], wov[:, kc, ns * 512:(ns + 1) * 512], kc == 0, kc == 15, [dsrc[b], dwo[kc]], [dps], kc == 15)
            STT(k, "vector", at[b][:, ns * 512:(ns + 1) * 512], at[b][:, ns * 512:(ns + 1) * 512], DN_ALPHA, ps, ALU.mult, ALU.add, [dps, dat[b]], [dat[b]])
        ln_tile(k, at[b], dat[b], gt, bt, dgb, tt, h_out, k.hT, lnst, dlnst, write_hT=True)
    barrier(k)
    A.reset(m0)


def rel_bucket_np(n):
    n = np.maximum(n, 0)
    nf = np.maximum(n, 1).astype(np.float32)
    large = 16 + (np.log(nf / np.float32(16)) / np.float32(np.log(128 / 16)) * np.float32(16)).astype(np.int32)
    large = np.minimum(large, 31)
    return np.where(n < 16, n, large)


def dsa_consts():
    ql = np.arange(128)[:, None]
    x = np.arange(256)[None, :]
    dist = np.where(x < 128, 128 + ql - x, ql - (x - 128))
    bk = rel_bucket_np(dist)
    oh = np.zeros((128, 32, 256), np.float32)
    for b in range(32):
        oh[:, b, :] = (bk == b)
    caus = np.where(np.arange(128)[None, :] <= np.arange(128)[:, None], 0.0, NEG).astype(np.float32)
    return {"c_ohb": oh.reshape(128, 32 * 256), "c_caus": caus}


def phase_dsa(k, j, li, h_in, h_out):
    S = k.S
    A = k.A
    W = k.w
    NH_ = 16
    att_scale = 128 ** -0.5
    widx_scale = (16 ** -0.5) * (128 ** -0.5)
    QT_d = k.dsa_QT; QIT_d = k.dsa_QIT; OT_d = k.dsa_OT
    d_OT = [Dep() for _ in range(NT)]
    d_QT = Dep(); d_QIT = Dep()
    m_phase = A.mark()
    CQT = A.bf16(4 * L); CQTv = r3(CQT, a=4); dCQT = Dep()
    CKVT = A.bf16(4 * L); CKVTv = r3(CKVT, a=4); dCKVT = Dep()
    KIT = A.bf16(L); dKIT = Dep()
    WI = A.f32(NT * 16); WIv = r3(WI, a=NT); dWI = Dep()
    identb = A.bf16(128); didb = Dep()
    CP(k, "vector", identb, k.ident, [], [didb])
    mA = A.mark()
    HTb = [A.bf16(2048), A.bf16(2048)]; dHTb = [Dep(), Dep()]
    win = A.bf16(16 * 1168); winv = r3(win, a=16); dwin = [Dep() for _ in range(16)]
    stg = Stager(k, 3, 2048)
    qg = A.f32(512); kg = A.f32(512); dqg = Dep()
    DMA(k, "sync", qg, W["dsa_q_norm"][j].rearrange("(o d) -> o d", o=1).partition_broadcast(128), [], [dqg])
    DMA(k, "sync", kg, W["dsa_kv_norm"][j].rearrange("(o d) -> o d", o=1).partition_broadcast(128), [], [dqg])
    for kc in range(16):
        stg.load(W["dsa_w_in"][j, kc * 128:(kc + 1) * 128, :], winv[:, kc, :], dwin[kc])
    pj = [A.f32(1168), A.f32(1168)]; dpj = [Dep(), Dep()]
    sm = A.f32(16); dsm = Dep()
    junk = A.f32(512)
    for t in range(NT):
        b = t % 2
        DMA(k, "sync", HTb[b], k.hT[t], [k.d_hT[t]], [dHTb[b]])
        HTt = r3(HTb[b], a=16)
        for ns, (c0, c1) in enumerate([(0, 512), (512, 1024), (1024, 1168)]):
            ps, dps = k.ps[ns], k.dps[ns]
            for kc in range(16):
                MM(k, ps[:, 0:c1 - c0], HTt[:, kc, :], winv[:, kc, c0:c1], kc == 0, kc == 15, [dHTb[b], dwin[kc]], [dps], kc == 15)
            CP(k, "vector", pj[b][:, c0:c1], ps[:, 0:c1 - c0], [dps], [dpj[b]])
        for qi, (c0, gain) in enumerate([(0, qg), (512, kg)]):
            ss = sm[:, qi * 4:qi * 4 + 1]; rs = sm[:, qi * 4 + 1:qi * 4 + 2]
            ACT(k, junk, pj[b][:, c0:c0 + 512], AF.Square, [dpj[b]], [dsm], accum_out=ss)
            TS(k, "vector", rs, ss, 1.0 / 512, RMS_EPS, ALU.mult, ALU.add, [dsm], [dsm])
            ACT(k, rs, rs, AF.Sqrt, [dsm], [dsm])
            k.S.op("vector", lambda e, rs=rs: e.reciprocal(out=rs, in_=rs), reads=[dsm], writes=[dsm])
            STT(k, "vector", pj[b][:, c0:c0 + 512], pj[b][:, c0:c0 + 512], rs, gain, ALU.mult, ALU.mult, [dsm, dpj[b], dqg], [dpj[b]])
        TS(k, "vector", WIv[:, t, :], pj[b][:, 1152:1168], widx_scale, None, ALU.mult, None, [dpj[b]], [dWI])
        for grp, (c0, dstv, ddst) in enumerate([(0, CQTv, dCQT), (512, CKVTv, dCKVT)]):
            ps, dps = k.ps[4 + grp], k.dps[4 + grp]
            for kc in range(4):
                TR(k, ps[:, kc * 128:(kc + 1) * 128], pj[b][:, c0 + kc * 128:c0 + (kc + 1) * 128], k.ident, [dpj[b]], [dps], kc == 3)
            ACT(k, dstv[:, :, t * 128:(t + 1) * 128], ps.rearrange("p (a b) -> p a b", a=4), AF.Copy, [dps], [ddst])
        ps, dps = k.ps[6], k.dps[6]
        TR(k, ps[:, 0:128], pj[b][:, 1024:1152], k.ident, [dpj[b]], [dps], True)
        ACT(k, KIT[:, t * 128:(t + 1) * 128], ps[:, 0:128], AF.Copy, [dps], [dKIT])
    barrier(k)
    A.reset(mA)
    if getattr(k, 'dsa_stop', '') == 'A':
        return
    mB = A.mark()
    wq = A.bf16(4 * 2048); wqv = r3(wq, a=4); dwq = [Dep() for _ in range(4)]
    stg = Stager(k, 3, 2048)
    ev = [A.bf16(512), A.bf16(512)]; dev_ = [Dep(), Dep()]
    cnt = 0
    for wname, dst_d, ddst in [("dsa_w_uq", QT_d, d_QT), ("dsa_w_qidx", QIT_d, d_QIT)]:
        for kc in range(4):
            stg.load(W[wname][j, kc * 128:(kc + 1) * 128, :], wqv[:, kc, :], dwq[kc])
        for h in range(NH_):
            for sl_ in range(4):
                ps, dps = k.ps[cnt % 4], k.dps[cnt % 4]
                b = cnt % 2
                cnt += 1
                for kc in range(4):
                    MM(k, ps, wqv[:, kc, h * 128:(h + 1) * 128], CQTv[:, kc, sl_ * 512:(sl_ + 1) * 512], kc == 0, kc == 3, [dwq[kc], dCQT], [dps], kc == 3)
                if b == 0:
                    ACT(k, ev[b], ps, AF.Copy, [dps], [dev_[b]])
                else:
                    CP(k, "vector", ev[b], ps, [dps], [dev_[b]])
                DMA(k, "gpsimd", dst_d[:, h, sl_ * 512:(sl_ + 1) * 512], ev[b], [dev_[b]], [ddst])
    barrier(k)
    A.reset(mB)
    if getattr(k, 'dsa_stop', '') == 'B':
        return
    KT_d = k.dsa_KT; V_d = k.dsa_V
    dKT = Dep(); dV = Dep()
    mC = A.mark()
    evc = [A.bf16(512), A.bf16(512)]; devc = [Dep(), Dep()]
    stg = Stager(k, 3, 2048)
    wukT = A.bf16(4 * 2048); wukTv = wukT.rearrange("p (c h d) -> p c h d", c=4, h=NH_); dwukT = Dep()
    wuv = A.bf16(4 * 2048); wuvv = wuv.rearrange("p (c h d) -> p c h d", c=4, h=NH_); dwuv = Dep()
    uk = [A.f32(512), A.f32(512)]; duk = [Dep(), Dep()]
    for h in range(NH_):
        b = h % 2
        DMA(k, "sync", uk[b], W["dsa_w_uk"][j, h], [], [duk[b]])
        ps, dps = k.ps[4 + b], k.dps[4 + b]
        for kc in range(4):
            TR(k, ps[:, kc * 128:(kc + 1) * 128], uk[b][:, kc * 128:(kc + 1) * 128], k.ident, [duk[b]], [dps], kc == 3)
        ACT(k, wukTv[:, :, h, :], ps.rearrange("p (a b) -> p a b", a=4), AF.Copy, [dps], [dwukT])
    for h in range(NH_):
        stg.load(W["dsa_w_uv"][j, h].rearrange("(c p) d -> p c d", p=128), wuvv[:, :, h, :], dwuv)
    cnt = 0
    for h in range(NH_):
        for sl_ in range(4):
            ps, dps = k.ps[cnt % 4], k.dps[cnt % 4]
            cnt += 1
            for kc in range(4):
                MM(k, ps, wukTv[:, kc, h, :], CKVTv[:, kc, sl_ * 512:(sl_ + 1) * 512], kc == 0, kc == 3, [dwukT, dCKVT], [dps], kc == 3)
            b = cnt % 2
            if b == 0:
                ACT(k, evc[b], ps, AF.Copy, [dps], [devc[b]])
            else:
                CP(k, "vector", evc[b], ps, [dps], [devc[b]])
            DMA(k, "gpsimd", KT_d[h, :, sl_ * 512:(sl_ + 1) * 512], evc[b], [devc[b]], [dKT])
    for st_ in range(NT):
        for hg in range(4):
            ps, dps = k.ps[cnt % 4], k.dps[cnt % 4]
            cnt += 1
            for kc in range(4):
                MM(k, ps, CKVTv[:, kc, st_ * 128:(st_ + 1) * 128], wuvv[:, kc, hg * 4:(hg + 1) * 4, :], kc == 0, kc == 3, [dwuv, dCKVT], [dps], kc == 3)
            b = cnt % 2
            if b == 0:
                ACT(k, evc[b], ps, AF.Copy, [dps], [devc[b]])
            else:
                CP(k, "vector", evc[b], ps, [dps], [devc[b]])
            DMA(k, "gpsimd", V_d[hg * 4:(hg + 1) * 4, :, st_ * 128:(st_ + 1) * 128].rearrange("h p d -> p h d"), r3(evc[b], a=4), [devc[b]], [dV])
    barrier(k)
    A.reset(mC)
    if getattr(k, 'dsa_stop', '') == 'C':
        return
    Tn = A.f32(NH_ * 256); Tnv = r3(Tn, a=NH_); dTn = Dep()
    caus = A.f32(128); dcaus = Dep()
    DMA(k, "sync", caus, k.c["c_caus"], [], [dcaus])
    mT = A.mark()
    ohb = A.f32(32 * 256); ohbv = r3(ohb, a=32); dohb = Dep()
    rbB = A.f32(512); drbB = Dep()
    DMA(k, "sync", ohb, k.c["c_ohb"], [], [dohb])
    DMA(k, "sync", rbB, W["rel_bias"].rearrange("(o b) h -> o (b h)", o=1).partition_broadcast(128), [], [drbB])
    for h in range(NH_):
        eng = "vector"
        TS(k, eng, Tnv[:, h, :], ohbv[:, 0, :], rbB[:, h:h + 1], None, ALU.mult, None, [dohb, drbB], [dTn])
        for b_ in range(1, 32):
            STT(k, eng, Tnv[:, h, :], ohbv[:, b_, :], rbB[:, b_ * 16 + h:b_ * 16 + h + 1], Tnv[:, h, :], ALU.mult, ALU.add, [dohb, drbB, dTn], [dTn])
        TS(k, eng, Tnv[:, h, :], Tnv[:, h, :], rbB[:, 31 * 16 + h:31 * 16 + h + 1], None, ALU.subtract, None, [drbB, dTn], [dTn])
    barrier(k)
    A.reset(mT)
    accs = [A.f32(L), A.f32(L)]; daccs = [Dep(), Dep()]
    tmp = [A.f32(512), A.f32(512)]; dtmp = [Dep(), Dep()]
    madds = [A.bf16(L), A.bf16(L)]; dmadds = [Dep(), Dep()]
    Xs = [A.f32(L) for _ in range(3)]; dXs = [Dep() for _ in range(3)]
    Ps = [A.bf16(L) for _ in range(3)]; dPs = [Dep() for _ in range(3)]
    PTs = [A.bf16(NT * 128) for _ in range(3)]; dPTs = [Dep() for _ in range(3)]
    QIbs = [A.bf16(NH_ * 128), A.bf16(NH_ * 128)]; dQIbs = [Dep(), Dep()]
    QTbs = [A.bf16(NH_ * 128), A.bf16(NH_ * 128)]; dQTbs = [Dep(), Dep()]
    OT = [A.bf16(NH_ * 128), A.bf16(NH_ * 128)]; dOTs = [Dep(), Dep()]
    mx8 = A.f32(8); dmx = Dep()
    sm2s = [A.f32(8) for _ in range(3)]; dsm2s = [Dep() for _ in range(3)]
    dgrs = [A.bf16(128) for _ in range(3)]; ddgrs = [Dep() for _ in range(3)]
    Kh = [A.bf16(L) for _ in range(3)]; dKh = [Dep() for _ in range(3)]
    Vh = [A.bf16(L) for _ in range(3)]; dVh = [Dep() for _ in range(3)]
    z1 = A.f32(1); dz1 = Dep()
    k.S.op("vector", lambda e: e.memset(z1, 0.0), reads=[], writes=[dz1])

    def pre_thunks(jb):
        SL = (jb + 1) * 128
        nbk = (SL + 511) // 512
        pb = jb % 2
        acc, dacc = accs[pb], daccs[pb]
        madd, dmadd = madds[pb], dmadds[pb]
        QIbv = r3(QIbs[pb], a=NH_); dQIb = dQIbs[pb]
        th_ = []

        def t_load():
            DMA(k, "sync", QIbv, QIT_d[:, :, jb * 128:(jb + 1) * 128], [d_QIT], [dQIb])
        th_.append(t_load)
        cnt = [0]
        for h in range(NH_):
            def t_head(h=h):
                for bk in range(nbk):
                    w_ = min(512, SL - bk * 512)
                    bank = 4
                    ps, dps = k.ps[bank], k.dps[bank]
                    tb = cnt[0] % 2
                    cnt[0] += 1
                    MM(k, ps[:, 0:w_], QIbv[:, h, :], KIT[:, bk * 512:bk * 512 + w_], True, True, [dQIb, dKIT], [dps], True)
                    if h == 0:
                        TS(k, "vector", acc[:, bk * 512:bk * 512 + w_], ps[:, 0:w_], z1, WIv[:, jb, h:h + 1], ALU.max, ALU.mult, [dps, dWI, dz1], [dacc])
                    else:
                        TS(k, "vector", tmp[tb][:, 0:w_], ps[:, 0:w_], z1, WIv[:, jb, h:h + 1], ALU.max, ALU.mult, [dps, dWI, dz1], [dtmp[tb]])
                        TT(k, "gpsimd", acc[:, bk * 512:bk * 512 + w_], acc[:, bk * 512:bk * 512 + w_], tmp[tb][:, 0:w_], ALU.add, [dtmp[tb], dacc], [dacc])
            th_.append(t_head)

        def t_caus():
            TT(k, "gpsimd", acc[:, jb * 128:SL], acc[:, jb * 128:SL], caus, ALU.add, [dacc, dcaus], [dacc])
        th_.append(t_caus)
        if SL > 256:
            for r in range(32):
                def t_round():
                    k.S.op("vector", lambda e: e.max(out=mx8, in_=acc[:, 0:SL]), reads=[dacc], writes=[dmx])
                    k.S.op("vector", lambda e: e.match_replace(out=acc[:, 0:SL], in_to_replace=mx8, in_values=acc[:, 0:SL], imm_value=-2.0e30), reads=[dacc, dmx], writes=[dacc])
                th_.append(t_round)

            def t_fin():
                TS(k, "vector", madd[:, 0:SL], acc[:, 0:SL], -1.5e30, NEG, ALU.is_gt, ALU.mult, [dacc], [dmadd])
        else:
            def t_fin():
                TS(k, "vector", madd[:, 0:SL], acc[:, 0:SL], -1.0e29, NEG, ALU.is_lt, ALU.mult, [dacc], [dmadd])
        th_.append(t_fin)
        return th_

    NB3 = 3
    dPV = [Dep(), Dep()]

    def bufs(i):
        b3 = i % NB3
        return (Xs[b3], dXs[b3], Ps[b3], dPs[b3], r3(PTs[b3], a=NT), dPTs[b3], sm2s[b3], dsm2s[b3], dgrs[b3], ddgrs[b3], Kh[b3], dKh[b3], Vh[b3], dVh[b3])

    def stageA(i):
        jb, h = divmod(i, NH_)
        SL = (jb + 1) * 128
        nbk = (SL + 511) // 512
        pb = jb % 2
        madd, dmadd = madds[pb], dmadds[pb]
        QTbv = r3(QTbs[pb], a=NH_); dQTb = dQTbs[pb]
        X, dX, P, dP, PTv, dPT, sm2, dsm2, dgr, ddgr, Kb, dKb, Vb, dVb = bufs(i)
        if h == 0:
            DMA(k, "sync", QTbv, QT_d[:, :, jb * 128:(jb + 1) * 128], [d_QT], [dQTb])
        DMA(k, "sync", Kb[:, 0:SL], KT_d[h, :, 0:SL], [dKT], [dKb])
        DMA(k, "sync", Vb[:, 0:SL], V_d[h, :, 0:SL], [dV], [dVb])
        for bk in range(nbk):
            w_ = min(512, SL - bk * 512)
            bank = (bk % 2) + 2 * (i % 2)
            ps, dps = k.ps[bank], k.dps[bank]
            MM(k, ps[:, 0:w_], QTbv[:, h, :], Kb[:, bk * 512:bk * 512 + w_], True, True, [dQTb, dKb], [dps], True)
            STT(k, "vector", X[:, bk * 512:bk * 512 + w_], ps[:, 0:w_], att_scale, madd[:, bk * 512:bk * 512 + w_], ALU.mult, ALU.add, [dps, dmadd], [dX])
        lo = max(0, jb - 1) * 128
        tlo = 0 if jb >= 1 else 128
        TT(k, "vector", X[:, lo:SL], X[:, lo:SL], Tnv[:, h, tlo:256], ALU.add, [dX, dTn], [dX])
        rmax = sm2[:, 0:1]; nmax = sm2[:, 1:2]; rsum = sm2[:, 2:3]
        k.S.op("vector", lambda e: e.tensor_reduce(out=rmax, in_=X[:, 0:SL], axis=AX.X, op=ALU.max), reads=[dX], writes=[dsm2])
        TS(k, "vector", nmax, rmax, -1.0, None, ALU.mult, None, [dsm2], [dsm2])
        ACT(k, P[:, 0:SL], X[:, 0:SL], AF.Exp, [dX, dsm2], [dP, dsm2], bias=nmax, scale=1.0, accum_out=rsum)

    def stageB(i):
        jb, h = divmod(i, NH_)
        X, dX, P, dP, PTv, dPT, sm2, dsm2, dgr, ddgr, Kb, dKb, Vb, dVb = bufs(i)
        rsum = sm2[:, 2:3]; rinv = sm2[:, 3:4]
        k.S.op("vector", lambda e: e.reciprocal(out=rinv, in_=rsum), reads=[dsm2], writes=[dsm2])
        TS(k, "vector", dgr, identb, rinv, None, ALU.mult, None, [dsm2, didb], [ddgr])
        for st_ in range(jb + 1):
            bank = 6 + (st_ // 4) % 2
            ps, dps = k.ps[bank], k.dps[bank]
            last = (st_ % 4 == 3) or (st_ == jb)
            MM(k, ps[:, (st_ % 4) * 128:(st_ % 4 + 1) * 128], P[:, st_ * 128:(st_ + 1) * 128], dgr, True, True, [dP, ddgr], [dps], last)
            if last:
                s0 = (st_ // 4) * 4
                n_ = st_ - s0 + 1
                ACT(k, PTv[:, s0:s0 + n_, :], ps[:, 0:n_ * 128].rearrange("p (a b) -> p a b", a=n_), AF.Copy, [dps], [dPT])
        Vhv = r3(Vb, a=NT)
        pv = k.ps[5][:, (i % 2) * 128:(i % 2 + 1) * 128]
        for st_ in range(jb + 1):
            MM(k, pv, Vhv[:, st_, :], PTv[:, st_, :], st_ == 0, st_ == jb, [dVb, dPT], [dPV[i % 2]], st_ == jb)

    def stageC(i):
        jb, h = divmod(i, NH_)
        pb = jb % 2
        ot, dot = OT[pb], dOTs[pb]
        otv = r3(ot, a=NH_)
        pv = k.ps[5][:, (i % 2) * 128:(i % 2 + 1) * 128]
        CP(k, "vector", otv[:, h, :], pv, [dPV[i % 2]], [dot])
        if h == NH_ - 1:
            DMA(k, "gpsimd", OT_d[jb], ot, [dot], [d_OT[jb]])

    for t_ in pre_thunks(0):
        t_()
    NHEADS = NT * NH_
    nxt = []
    pos = 0
    per = 0
    for i in range(NHEADS + 2):
        if i < NHEADS:
            jb, h = divmod(i, NH_)
            if h == 0:
                for t_ in nxt[pos:]:
                    t_()
                nxt = pre_thunks(jb + 1) if jb + 1 < NT else []
                per = (len(nxt) + NH_ - 1) // NH_
                pos = 0
            stageA(i)
        if 0 <= i - 1 < NHEADS:
            stageB(i - 1)
        if 0 <= i - 2 < NHEADS:
            stageC(i - 2)
        if i < NHEADS:
            for t_ in nxt[pos:pos + per]:
                t_()
            pos += per
    barrier(k)
    A.reset(m_phase)
    if getattr(k, 'dsa_stop', '') == 'D':
        return
    phase_outproj_ln(k, OT_d, d_OT, W["dsa_w_out"][j], W["ln_mix_g"][li], W["ln_mix_b"][li], h_in, h_out)
import math as _math

S5_KVEC = [0, -1, -2, -3, -4, -5, -6, -7, 7, 6, 5, 4, 3, 2, 1, 0, 0, 1, 2, 3, 4, 5, 6, 7, 1, 2, 3, 4, 5, 6, 7, 8, 8, 16, 32, 64, 128, 256, 512, 1024]
NK = 40


def s5_consts():
    kv = np.tile(np.array(S5_KVEC, np.float32)[None, :], (128, 1))
    s_ = (np.arange(128) // 16)[:, None]
    t_ = (np.arange(128) // 16)[None, :]
    msk = (t_ >= s_).astype(np.float32)
    import ml_dtypes
    sel = np.zeros((128, 64, 128), np.float32)
    for g8 in range(8):
        for s in range(8):
            for p_ in range(16):
                sel[g8 * 16 + p_, g8 * 8 + s, s * 16 + p_] = 1.0
    selT = np.ascontiguousarray(sel.transpose(2, 1, 0))
    return {"c_kv40": kv, "c_s5mask": msk,
            "c_sel": sel.reshape(128, 8192).astype(ml_dtypes.bfloat16),
            "c_selT": selT.reshape(128, 8192).astype(ml_dtypes.bfloat16)}


def bc(ap, axis, shape):
    return ap.unsqueeze(axis).to_broadcast(list(shape))


def phase_s5(k, sj, li, h_in, h_out):
    S = k.S
    A = k.A
    W = k.w
    M_d, W1_d, W2_d, ZT_d = k.s5_M, k.s5_W1, k.s5_W2, k.s5_ZT
    dM = Dep(); dW1 = Dep(); dW2 = Dep()
    d_ZT = [Dep() for _ in range(NT)]
    TWO_PI = 2.0 * _math.pi
    m_phase = A.mark()
    DCOL = A.f32(128); dDCOL = Dep()
    ASr = A.f32(512); ASi = A.f32(512); ASn = A.f32(512); dAS = Dep()
    ASrv = r3(ASr, a=64); ASiv = r3(ASi, a=64); ASnv = r3(ASn, a=64)
    mP = A.mark()
    KV = A.f32(NK); dKV = Dep()
    DMA(k, "sync", KV, k.c["c_kv40"], [], [dKV])
    MASK = A.f32(128); dMASK = Dep()
    DMA(k, "sync", MASK, k.c["c_s5mask"], [], [dMASK])
    PAre = A.f32(64); PAim = A.f32(64); PDT = A.f32(64); dPA = Dep()
    PBre = A.f32(1024); PBim = A.f32(1024); PCre = A.f32(1024); PCim = A.f32(1024); dPB = Dep(); dPC = Dep()
    PBrev = r3(PBre, a=64); PBimv = r3(PBim, a=64); PCrev = r3(PCre, a=64); PCimv = r3(PCim, a=64)
    ld = [A.f32(2048), A.f32(2048)]; dld = [Dep(), Dep()]
    ld2 = A.f32(2048); dld2 = Dep()
    id64 = k.ident[0:64, 0:64]
    DMA(k, "sync", ld[0][:, 0:16], W["s5_d"][sj], [], [dld[0]])
    CP(k, "vector", ld[0][:, 16:144].rearrange("p (t q) -> p t q", t=8), bc(ld[0][:, 0:16], 1, [128, 8, 16]), [dld[0]], [dld[0]])
    TR(k, k.ps[0][:, 0:128], ld[0][:, 16:144], k.ident, [dld[0]], [k.dps[0]], True)
    CP(k, "vector", DCOL, k.ps[0][:, 0:128], [k.dps[0]], [dDCOL])
    DMA(k, "sync", ld[1][0:64, 0:128], W["s5_a_re"][sj].rearrange("(j g2) n -> j (g2 n)", g2=2), [], [dld[1]])
    DMA(k, "sync", ld[1][0:64, 128:256], W["s5_a_im"][sj].rearrange("(j g2) n -> j (g2 n)", g2=2), [], [dld[1]])
    DMA(k, "sync", ld[1][0:64, 256:258], W["s5_log_dt"][sj].rearrange("(j g2) -> j g2", g2=2), [], [dld[1]])
    CP(k, "vector", ld[1][0:64, 384:512].rearrange("p (g n) -> p g n", g=2), bc(ld[1][0:64, 256:258], 2, [64, 2, 64]), [dld[1]], [dld[1]])
    for i_, (c0, dst) in enumerate([(0, PAre), (128, PAim), (384, PDT)]):
        TR(k, k.ps[1][:, i_ * 64:(i_ + 1) * 64], ld[1][0:64, c0:c0 + 128], id64, [dld[1]], [k.dps[1]], True)
        CP(k, "vector", dst, k.ps[1][:, i_ * 64:(i_ + 1) * 64], [k.dps[1]], [dPA])
    cnt_ = 0
    for name, dstv, ddst, is_c in [("s5_b_re", PBrev, dPB, False), ("s5_b_im", PBimv, dPB, False), ("s5_c_re", PCrev, dPC, True), ("s5_c_im", PCimv, dPC, True)]:
        lb = ld[cnt_ % 2]; dlb = dld[cnt_ % 2]
        cnt_ += 1
        if is_c:
            DMA(k, "sync", lb[0:64, :], W[name][sj].rearrange("(j g2) p n -> j (g2 p n)", g2=2), [], [dlb])
            lb2 = ld2[0:64, :]
            CP(k, "vector", lb2.rearrange("j (p g n) -> j p g n", p=16, g=2), lb[0:64, :].rearrange("j (g p n) -> j p g n", g=2, p=16), [dlb, dld2], [dld2])
            lv = lb2.rearrange("j (p gn) -> j p gn", p=16)
        else:
            DMA(k, "sync", lb[0:64, :], W[name][sj].rearrange("(j g2) n p -> j (g2 n p)", g2=2), [], [dlb])
            lv = lb[0:64, :].rearrange("j (gn p) -> j gn p", p=16)
        for q4 in range(2):
            bank = 2 + q4
            ps, dps = k.ps[bank], k.dps[bank]
            for p8 in range(8):
                p_ = q4 * 8 + p8
                src = lv[:, p_, :] if is_c else lv[:, :, p_]
                TR(k, ps[:, p8 * 64:(p8 + 1) * 64], src, id64, [dlb, dld2], [dps], p8 == 7)
            CP(k, "vector", dstv[:, :, q4 * 8:(q4 + 1) * 8].rearrange("p j q -> p q j"), ps.rearrange("p (q j) -> p q j", q=8), [dps], [ddst])
    if getattr(k, 's5_stop', '') == 'P1':
        barrier(k)
        return
    dE = Dep()
    lr = A.f32(64); ldr = A.f32(64); th = A.f32(64); dtt = A.f32(64)
    TS(k, "vector", lr, PAre, -1.0e-4, None, ALU.min, None, [dPA], [dE])
    ACT(k, dtt, PDT, AF.Exp, [dPA], [dE])
    TT(k, "vector", ldr, lr, dtt, ALU.mult, [dE], [dE])
    TT(k, "vector", th, PAim, dtt, ALU.mult, [dE, dPA], [dE])
    NKK = 64 * NK
    shp = [128, 64, NK]
    ARG = A.f32(NKK); PHI = A.f32(NKK); RHO = A.f32(NKK); QF = A.f32(NKK); MSK2 = A.f32(NKK)
    ARE = A.f32(NKK); AIM = A.f32(NKK)
    QI = A.f32(NKK).bitcast(I32)
    v3 = lambda t_: r3(t_, a=64)
    TT(k, "vector", v3(ARG), bc(ldr, 2, shp), bc(KV, 1, shp), ALU.mult, [dE, dKV], [dE])
    ACT(k, RHO, ARG, AF.Exp, [dE], [dE])
    TT(k, "vector", v3(PHI), bc(th, 2, shp), bc(KV, 1, shp), ALU.mult, [dE, dKV], [dE])

    def sin_of(dst, off):
        TS(k, "vector", ARG, PHI, off, None, ALU.add, None, [dE], [dE])
        TS(k, "vector", QF, ARG, 1.0 / TWO_PI, None, ALU.mult, None, [dE], [dE])
        CP(k, "vector", QI, QF, [dE], [dE])
        CP(k, "vector", QF, QI, [dE], [dE])
        STT(k, "vector", ARG, QF, -TWO_PI, ARG, ALU.mult, ALU.add, [dE], [dE])
        TS(k, "vector", MSK2, ARG, _math.pi, -TWO_PI, ALU.is_gt, ALU.mult, [dE], [dE])
        TT(k, "vector", ARG, ARG, MSK2, ALU.add, [dE], [dE])
        TS(k, "vector", MSK2, ARG, -_math.pi, TWO_PI, ALU.is_lt, ALU.mult, [dE], [dE])
        TT(k, "vector", ARG, ARG, MSK2, ALU.add, [dE], [dE])
        ACT(k, dst, ARG, AF.Sin, [dE], [dE])

    sin_of(AIM, 64.0 * _math.pi)
    sin_of(ARE, 64.5 * _math.pi)
    TT(k, "vector", AIM, AIM, RHO, ALU.mult, [dE], [dE])
    TT(k, "vector", ARE, ARE, RHO, ALU.mult, [dE], [dE])
    AREv = v3(ARE); AIMv = v3(AIM)
    CP(k, "vector", ASrv, AREv[:, :, 32:40], [dE], [dAS])
    CP(k, "vector", ASiv, AIMv[:, :, 32:40], [dE], [dAS])
    TS(k, "vector", ASnv, AIMv[:, :, 32:40], -1.0, None, ALU.mult, None, [dE], [dAS])
    er = A.f32(64); ei = A.f32(64); qr = A.f32(64); qi_ = A.f32(64); den = A.f32(64); t1 = A.f32(64); fr = A.f32(64); fi = A.f32(64)
    TS(k, "vector", er, AREv[:, :, 24], -1.0, None, ALU.add, None, [dE], [dE])
    CP(k, "vector", ei, AIMv[:, :, 24], [dE], [dE])
    TT(k, "vector", qr, er, lr, ALU.mult, [dE], [dE])
    TT(k, "vector", t1, ei, PAim, ALU.mult, [dE], [dE])
    TT(k, "vector", qr, qr, t1, ALU.add, [dE], [dE])
    TT(k, "vector", qi_, ei, lr, ALU.mult, [dE], [dE])
    TT(k, "vector", t1, er, PAim, ALU.mult, [dE], [dE])
    TT(k, "vector", qi_, qi_, t1, ALU.subtract, [dE], [dE])
    TT(k, "vector", den, lr, lr, ALU.mult, [dE], [dE])
    TT(k, "vector", t1, PAim, PAim, ALU.mult, [dE], [dE])
    TT(k, "vector", den, den, t1, ALU.add, [dE], [dE])
    k.S.op("vector", lambda e: e.reciprocal(out=den, in_=den), reads=[dE], writes=[dE])
    TT(k, "vector", fr, qr, den, ALU.mult, [dE], [dE])
    TT(k, "vector", fi, qi_, den, ALU.mult, [dE], [dE])
    BBre = A.f32(1024); BBim = A.f32(1024); tb_ = A.f32(1024)
    BBrev = r3(BBre, a=64); BBimv = r3(BBim, a=64); tbv = r3(tb_, a=64)
    s16 = [128, 64, 16]
    TT(k, "vector", BBrev, bc(fr, 2, s16), PBrev, ALU.mult, [dE, dPB], [dE])
    TT(k, "vector", tbv, bc(fi, 2, s16), PBimv, ALU.mult, [dE, dPB], [dE])
    TT(k, "vector", BBre, BBre, tb_, ALU.subtract, [dE], [dE])
    TT(k, "vector", BBimv, bc(fr, 2, s16), PBimv, ALU.mult, [dE, dPB], [dE])
    TT(k, "vector", tbv, bc(fi, 2, s16), PBrev, ALU.mult, [dE, dPB], [dE])
    TT(k, "vector", BBim, BBim, tb_, ALU.add, [dE], [dE])
    if getattr(k, 's5_stop', '') == 'P2':
        barrier(k)
        return
    PB_ = 8
    PM = A.f32(2); dPM = Dep()
    k.S.op("vector", lambda e: e.memset(PM, 0.0), reads=[], writes=[dPM])
    k.S.op("vector", lambda e: e.memset(PM[0:64, 0:1], 1.0), reads=[dPM], writes=[dPM])
    k.S.op("vector", lambda e: e.memset(PM[64:128, 1:2], 1.0), reads=[dPM], writes=[dPM])
    LM = [[A.bf16(1024), A.bf16(1024)], [A.bf16(1024), A.bf16(1024)]]
    T = [A.f32(1024) for _ in range(4)]
    Tv = [t_.rearrange("p (j s q) -> p j s q", j=PB_, s=8) for t_ in T]
    RREb = A.bf16(1024); RIMb = A.bf16(1024)
    L2RE = A.f32(1024); L2IM = A.f32(1024)
    W2o = A.bf16(4096)
    W2ov = W2o.rearrange("p (j r m) -> p j r m", j=PB_, r=4)
    Mout = [A.bf16(512), A.bf16(512)]; dMout = [Dep(), Dep()]
    TAB = [A.bf16(512), A.bf16(512)]; dTAB = [Dep(), Dep()]
    for tb2 in TAB:
        k.S.op("vector", lambda e, tb2=tb2: e.memset(tb2, 0.0), reads=[], writes=[dE])
    dCh = Dep()
    s4 = [128, PB_, 8, 16]
    j8 = lambda t_: r3(t_, a=PB_)

    def products(blk, Bre, Bim, j0):
        a0 = blk * 8
        Ar = AREv[:, j0:j0 + PB_, a0:a0 + 8]; Ai = AIMv[:, j0:j0 + PB_, a0:a0 + 8]
        br = Bre[:, j0:j0 + PB_, :]; bi = Bim[:, j0:j0 + PB_, :]
        TT(k, "vector", Tv[0], bc(Ar, 3, s4), bc(br, 2, s4), ALU.mult, [dE, dPC, dCh], [dCh])
        TT(k, "vector", Tv[1], bc(Ai, 3, s4), bc(bi, 2, s4), ALU.mult, [dE, dPC, dCh], [dCh])
        TT(k, "vector", Tv[2], bc(Ai, 3, s4), bc(br, 2, s4), ALU.mult, [dE, dPC, dCh], [dCh])
        TT(k, "vector", Tv[3], bc(Ar, 3, s4), bc(bi, 2, s4), ALU.mult, [dE, dPC, dCh], [dCh])

    mcnt = 0
    for ch in range(64 // PB_):
        j0 = ch * PB_
        products(0, BBrev, BBimv, j0)
        TT(k, "vector", T[0], T[0], T[1], ALU.subtract, [dCh], [dCh])
        TT(k, "vector", T[2], T[2], T[3], ALU.add, [dCh], [dCh])
        for g2 in range(2):
            TS(k, "vector", LM[g2][0], T[0], PM[:, g2:g2 + 1], None, ALU.mult, None, [dCh, dPM], [dCh])
            TS(k, "vector", LM[g2][1], T[2], PM[:, g2:g2 + 1], None, ALU.mult, None, [dCh, dPM], [dCh])
        products(2, PCrev, PCimv, j0)
        TT(k, "vector", RREb, T[0], T[1], ALU.subtract, [dCh], [dCh])
        STT(k, "vector", RIMb, T[2], -1.0, T[3], ALU.mult, ALU.subtract, [dCh], [dCh])
        for half in range(PB_ // 2):
            bank = mcnt % 2
            mo, dmo = Mout[mcnt % 2], dMout[mcnt % 2]
            mcnt += 1
            ps, dps = k.ps[bank], k.dps[bank]
            for q in range(4):
                jj = half * 2 + q // 2
                g2 = q % 2
                MM(k, ps[:, q * 128:(q + 1) * 128], j8(LM[g2][0])[:, jj, :], j8(RREb)[:, jj, :], True, False, [dCh], [dps], False)
                MM(k, ps[:, q * 128:(q + 1) * 128], j8(LM[g2][1])[:, jj, :], j8(RIMb)[:, jj, :], False, True, [dCh], [dps], q == 3)
            TT(k, "vector", r3(mo, a=4), r3(ps, a=4), bc(MASK, 1, [128, 4, 128]), ALU.mult, [dps, dMASK], [dmo])
            g0 = (j0 + half * 2) * 2
            for q in range(4):
                DMA(k, "gpsimd", M_d[g0 + q], mo[:, q * 128:(q + 1) * 128], [dmo], [dM])
        if getattr(k, 's5_stop', '') == 'P3a':
            continue
        products(1, BBrev, BBimv, j0)
        TT(k, "vector", L2RE, T[0], T[1], ALU.subtract, [dCh], [dCh])
        TT(k, "vector", L2IM, T[2], T[3], ALU.add, [dCh], [dCh])
        for jj in range(PB_):
            bank = 2 + jj % 2
            ps, dps = k.ps[bank], k.dps[bank]
            tab, dtab = TAB[jj % 2], dTAB[jj % 2]
            TR(k, ps[:, 0:128], j8(L2RE)[:, jj, :], k.ident, [dCh], [dps], False)
            TR(k, ps[:, 128:256], j8(L2IM)[:, jj, :], k.ident, [dCh], [dps], True)
            tabv = tab.rearrange("p (r a m) -> p r a m", r=2, a=2)
            psv = ps[:, 0:256].rearrange("p (r m) -> p r m", r=2)
            CP(k, "vector", tabv[:, :, 0, 0:64], psv[:, :, 0:64], [dps], [dtab])
            CP(k, "vector", tabv[:, :, 1, 64:128], psv[:, :, 64:128], [dps], [dtab])
            DMA(k, "gpsimd", W1_d[j0 + jj], tab, [dtab], [dW1])
        if getattr(k, 's5_stop', '') == 'P3b':
            continue
        products(3, PCrev, PCimv, j0)
        TT(k, "vector", T[0], T[0], T[1], ALU.subtract, [dCh], [dCh])
        STT(k, "vector", T[2], T[2], -1.0, T[3], ALU.mult, ALU.subtract, [dCh], [dCh])
        for g2 in range(2):
            TS(k, "vector", W2ov[:, :, 2 * g2, :], j8(T[0]), PM[:, g2:g2 + 1], None, ALU.mult, None, [dCh, dPM], [dCh])
            TS(k, "vector", W2ov[:, :, 2 * g2 + 1, :], j8(T[2]), PM[:, g2:g2 + 1], None, ALU.mult, None, [dCh, dPM], [dCh])
        for jj in range(PB_):
            DMA(k, "gpsimd", W2_d[j0 + jj], W2o[:, jj * 512:(jj + 1) * 512], [dCh], [dW2, dCh])
    barrier(k)
    A.reset(mP)
    if getattr(k, 's5_stop', '') in ('P', 'P3a', 'P3b'):
        return
    R1 = A.bf16(16 * 2048)
    R1v = R1.rearrange("p (b s c) -> p b s c", b=16, s=8)
    dR1 = Dep()
    mR2 = A.mark()
    HT = A.bf16(NT * 2048); HTv = HT.rearrange("p (t c x) -> p t c x", t=NT, c=16); dHT = Dep()
    for t in range(NT):
        DMA(k, "sync", HTv[:, t].rearrange("p c x -> p (c x)"), k.hT[t], [k.d_hT[t]], [dHT])
    stg = Stager(k, 3, 2048)
    wcb = [A.bf16(2048), A.bf16(2048)]; dwcb = [Dep(), Dep()]
    cnt = 0
    for chb in range(16):
        b = chb % 2
        stg.load(W["s5_w_in"][sj].rearrange("(kc p) n -> p kc n", p=128)[:, :, chb * 128:(chb + 1) * 128], r3(wcb[b], a=16), dwcb[b])
        for sl_ in range(4):
            ps, dps = k.ps[cnt % 4], k.dps[cnt % 4]
            cnt += 1
            for kc in range(16):
                MM(k, ps, r3(wcb[b], a=16)[:, kc, :], HTv[:, sl_ * 4:(sl_ + 1) * 4, kc, :], kc == 0, kc == 15, [dwcb[b], dHT], [dps], kc == 15)
            src = ps.rearrange("p (c s) -> p s c", s=8)
            dst = R1v[:, chb, :, sl_ * 64:(sl_ + 1) * 64]
            if cnt % 2 == 0:
                ACT(k, dst, src, AF.Copy, [dps], [dR1])
            else:
                CP(k, "vector", dst, src, [dps], [dR1])
    barrier(k)
    A.reset(mR2)
    if getattr(k, 's5_stop', '') == 'U':
        return
    R2 = A.bf16(128 * 256)
    Xv = r3(R2, a=128)
    dX = Dep()
    SEL = A.bf16(8192); SELv = r3(SEL, a=64); SELT = A.bf16(8192); SELTv = r3(SELT, a=64); dSEL = Dep()
    DMA(k, "sync", SEL, k.c["c_sel"], [], [dSEL])
    DMA(k, "sync", SELT, k.c["c_selT"], [], [dSEL])
    for g0 in range(0, 128, 2):
        bank = (g0 // 2) % 4
        ps, dps = k.ps[bank], k.dps[bank]
        for gi in range(2):
            g = g0 + gi
            for s_ in range(8):
                MM(k, ps[:, gi * 256:(gi + 1) * 256], SELv[:, (g % 8) * 8 + s_, :], R1v[:, g // 8, s_, :], s_ == 0, s_ == 7, [dSEL, dR1], [dps], (gi == 1 and s_ == 7))
        if (g0 // 2) % 2 == 0:
            ACT(k, Xv[:, g0:g0 + 2, :], r3(ps, a=2), AF.Copy, [dps], [dX])
        else:
            CP(k, "vector", Xv[:, g0:g0 + 2, :], r3(ps, a=2), [dps], [dX])
    barrier(k)
    if getattr(k, 's5_stop', '') == 'X':
        return
    mL = A.mark()
    dZF = Dep()
    Mg = [A.bf16(256), A.bf16(256)]; W1t = [A.bf16(512), A.bf16(512)]; W2t = [A.bf16(512), A.bf16(512)]
    dMg = [Dep(), Dep()]; dW1t = [Dep(), Dep()]; dW2t = [Dep(), Dep()]
    REb = [A.f32(384), A.f32(384)]; IMb = [A.f32(384), A.f32(384)]
    dSC = Dep()
    for t_ in REb + IMb:
        k.S.op("vector", lambda e, t_=t_: e.memset(t_, 0.0), reads=[], writes=[dSC])
    HRE = A.bf16(256); HIM = A.bf16(256); dH = Dep()
    yb = [A.f32(256), A.f32(256)]; y2b = [A.f32(256), A.f32(256)]; dyb = [Dep(), Dep()]
    Zall = [A.bf16(8 * 256), A.bf16(8 * 256)]; dZall = [Dep(), Dep()]
    for j in range(64):
        b = j % 2
        for g2 in range(2):
            DMA(k, "sync", Mg[b][:, g2 * 128:(g2 + 1) * 128], M_d[2 * j + g2], [dM], [dMg[b]])
        DMA(k, "sync", W1t[b], W1_d[j], [dW1], [dW1t[b]])
        DMA(k, "sync", W2t[b], W2_d[j], [dW2], [dW2t[b]])
        ps, dps = k.ps[b], k.dps[b]
        for ri in range(2):
            MM(k, ps[:, ri * 256:(ri + 1) * 256], W1t[b][:, (2 * ri) * 128:(2 * ri + 1) * 128], Xv[:, 2 * j, :], True, False, [dW1t[b], dX], [dps], False)
            MM(k, ps[:, ri * 256:(ri + 1) * 256], W1t[b][:, (2 * ri + 1) * 128:(2 * ri + 2) * 128], Xv[:, 2 * j + 1, :], False, True, [dW1t[b], dX], [dps], ri == 1)
        ACT(k, REb[0][:, 128:384], ps[:, 0:256], AF.Copy, [dps], [dSC])
        ACT(k, IMb[0][:, 128:384], ps[:, 256:512], AF.Copy, [dps], [dSC])
        cur = 0
        for i in range(8):
            sft = 1 << i
            ra, ia = REb[cur], IMb[cur]
            rb_, ib_ = REb[1 - cur], IMb[1 - cur]
            ar = ASrv[:, j, i:i + 1]; ai = ASiv[:, j, i:i + 1]; an = ASnv[:, j, i:i + 1]
            STT(k, "vector", rb_[:, 128:384], ra[:, 128 - sft:384 - sft], ar, ra[:, 128:384], ALU.mult, ALU.add, [dSC, dAS], [dSC])
            STT(k, "vector", rb_[:, 128:384], ia[:, 128 - sft:384 - sft], an, rb_[:, 128:384], ALU.mult, ALU.add, [dSC, dAS], [dSC])
            STT(k, "vector", ib_[:, 128:384], ia[:, 128 - sft:384 - sft], ar, ia[:, 128:384], ALU.mult, ALU.add, [dSC, dAS], [dSC])
            STT(k, "vector", ib_[:, 128:384], ra[:, 128 - sft:384 - sft], ai, ib_[:, 128:384], ALU.mult, ALU.add, [dSC, dAS], [dSC])
            cur = 1 - cur
        ACT(k, HRE, REb[cur][:, 127:383], AF.Copy, [dSC], [dH])
        ACT(k, HIM, IMb[cur][:, 127:383], AF.Copy, [dSC], [dH])
        for g2 in range(2):
            g = 2 * j + g2
            prt = slice(g2 * 64, (g2 + 1) * 64)
            py, dpy = k.ps[2 + g2], k.dps[2 + g2]
            MM(k, py[:, 0:256], r3(Mg[b], a=2)[:, g2, :], Xv[:, g, :], True, False, [dMg[b], dX], [dpy], False)
            MM(k, py[:, 0:256], W2t[b][:, (2 * g2) * 128:(2 * g2 + 1) * 128], HRE, False, False, [dW2t[b], dH], [dpy], False)
            MM(k, py[:, 0:256], W2t[b][:, (2 * g2 + 1) * 128:(2 * g2 + 2) * 128], HIM, False, True, [dW2t[b], dH], [dpy], True)
            y = yb[g2]; y2 = y2b[g2]; dy_ = dyb[g2]
            STT(k, "vector", y, Xv[:, g, :], DCOL[:, g:g + 1], py[:, 0:256], ALU.mult, ALU.add, [dpy, dX, dDCOL], [dy_])
            TT(k, "gpsimd", y2, y, y, ALU.mult, [dy_], [dy_])
            TS(k, "gpsimd", y2, y2, 0.044715, 1.0, ALU.mult, ALU.add, [dy_], [dy_])
            TT(k, "gpsimd", y2, y2, y, ALU.mult, [dy_], [dy_])
            ACT(k, y2, y2, AF.Sigmoid, [dy_], [dy_], scale=1.5957691216057308)
            zb = (g // 8) % 2
            TT(k, "gpsimd", r3(Zall[zb], a=8)[:, g % 8, :], y, y2, ALU.mult, [dy_, dZall[zb]], [dZall[zb]])
        if j % 4 == 3:
            chb = j // 4
            zb = chb % 2
            for sp in range(4):
                bank = 4 + sp % 4
                ps2, dps2 = k.ps[bank], k.dps[bank]
                for si in range(2):
                    s_ = sp * 2 + si
                    for g8 in range(8):
                        MM(k, ps2[:, si * 256:(si + 1) * 256], SELTv[:, g8 * 8 + s_, :], r3(Zall[zb], a=8)[:, g8, :], g8 == 0, g8 == 7, [dSEL, dZall[zb]], [dps2], (si == 1 and g8 == 7))
                if sp % 2 == 0:
                    ACT(k, R1v[:, chb, sp * 2:sp * 2 + 2, :], r3(ps2, a=2), AF.Copy, [dps2], [dZF])
                else:
                    CP(k, "vector", R1v[:, chb, sp * 2:sp * 2 + 2, :], r3(ps2, a=2), [dps2], [dZF])
    barrier(k)
    A.reset(mR2)
    if getattr(k, 's5_stop', '') == 'L':
        return
    Z2N = A.bf16(NT * 2048)
    Z2Nv = Z2N.rearrange("p (t c l s) -> p t c l s", t=NT, c=16, l=16)
    dZ2 = Dep()
    stg = Stager(k, 3, 2048)
    wgb = [A.bf16(2048), A.bf16(2048)]; dwgb = [Dep(), Dep()]
    sg = [A.f32(512), A.f32(512)]; dsg = [Dep(), Dep()]
    cnt = 0
    for nb in range(16):
        b = nb % 2
        stg.load(W["s5_w_glu"][sj].rearrange("(kc p) n -> p kc n", p=128)[:, :, nb * 128:(nb + 1) * 128], r3(wgb[b], a=16), dwgb[b])
        for q in range(4):
            ps, dps = k.ps[cnt % 4], k.dps[cnt % 4]
            sb_ = cnt % 2
            cnt += 1
            zsl = lambda kc: R1v[:, kc, 2 * q:2 * q + 2, :]
            for kc in range(16):
                MM(k, ps, r3(wgb[b], a=16)[:, kc, :], zsl(kc), kc == 0, kc == 15, [dwgb[b], dZF], [dps], kc == 15)
            ACT(k, sg[sb_], ps, AF.Sigmoid, [dps], [dsg[sb_]])
            dst = Z2Nv[:, :, nb, :, 2 * q:2 * q + 2].rearrange("p t l s -> p s t l")
            in0 = sg[sb_].rearrange("p (s t l) -> p s t l", s=2, t=16)
            in1 = R1v[:, nb, 2 * q:2 * q + 2, :].rearrange("p s (t l) -> p s t l", t=16)
            TT(k, "vector", dst, in0, in1, ALU.mult, [dsg[sb_], dZF], [dZ2])
    for t in range(NT):
        DMA(k, "gpsimd", ZT_d[t], Z2N[:, t * 2048:(t + 1) * 2048], [dZ2], [d_ZT[t]])
    barrier(k)
    A.reset(m_phase)
    phase_outproj_ln(k, ZT_d, d_ZT, W["s5_w_out"][sj], W["ln_mix_g"][li], W["ln_mix_b"][li], h_in, h_out)
W_SPECS = [
    ("rel_bias", [32, 16]),
    ("s5_w_in", [2, 2048, 2048]), ("s5_a_re", [2, 128, 64]), ("s5_a_im", [2, 128, 64]), ("s5_log_dt", [2, 128]),
    ("s5_b_re", [2, 128, 64, 16]), ("s5_b_im", [2, 128, 64, 16]), ("s5_c_re", [2, 128, 16, 64]), ("s5_c_im", [2, 128, 16, 64]),
    ("s5_d", [2, 128, 16]), ("s5_w_glu", [2, 2048, 2048]), ("s5_w_out", [2, 2048, 2048]),
    ("dsa_w_in", [2, 2048, 1168]), ("dsa_q_norm", [2, 512]), ("dsa_kv_norm", [2, 512]),
    ("dsa_w_uq", [2, 512, 2048]), ("dsa_w_qidx", [2, 512, 2048]), ("dsa_w_uk", [2, 16, 128, 512]),
    ("dsa_w_uv", [2, 16, 512, 128]), ("dsa_w_out", [2, 2048, 2048]),
    ("moe_w_group", [4, 2048, 4]), ("moe_b_group", [4, 4]), ("moe_w_expert", [4, 2048, 32]), ("moe_b_expert", [4, 32]),
    ("moe_w_gate", [4, 32, 2048, 256]), ("moe_w_up", [4, 32, 2048, 256]), ("moe_w_down", [4, 32, 256, 2048]),
    ("ln_mix_g", [4, 2048]), ("ln_mix_b", [4, 2048]), ("ln_ffn_g", [4, 2048]), ("ln_ffn_b", [4, 2048]),
]


def host_consts():
    c = {}
    c["c_ident"] = np.eye(128, dtype=np.float32)
    c.update(dsa_consts())
    c.update(s5_consts())
    return c


def build_nc(mode="full", used=None, **kw):
    nc = bass.Bass("TRN2", target_bir_lowering=False)
    k = K()
    for a_, b_ in kw.items():
        setattr(k, a_, b_)
    k.nc = nc
    k.x = nc.dram_tensor("x", [L, D], F32, kind="ExternalInput").ap()
    k.w = {}
    for name, shp in W_SPECS:
        if used is not None and name not in used:
            continue
        k.w[name] = nc.dram_tensor(name, getattr(k, 'wshape', {}).get(name, shp), F32, kind="ExternalInput").ap()
    k.c = {}
    for name, arr in host_consts().items():
        k.c[name] = nc.dram_tensor(name, list(arr.shape), F32 if arr.dtype == np.float32 else BF16, kind="ExternalInput").ap()
    k.out = nc.dram_tensor("out", [L, D], F32, kind="ExternalOutput").ap()
    k.hA = nc.dram_tensor("hA", [L, D], F32).ap()
    k.hB = nc.dram_tensor("hB", [L, D], F32).ap()
    k.hT = nc.dram_tensor("hT", [NT, 128, 16 * 128], BF16).ap()
    k.s5_M = nc.dram_tensor("s5_M", [128, 128, 128], BF16).ap()
    k.s5_W1 = nc.dram_tensor("s5_W1", [64, 128, 512], BF16).ap()
    k.s5_W2 = nc.dram_tensor("s5_W2", [64, 128, 512], BF16).ap()
    k.s5_ZT = nc.dram_tensor("s5_ZT", [NT, 128, 16 * 128], BF16).ap()
    k.dsa_QT = nc.dram_tensor("dsa_QT", [128, 16, L], BF16).ap()
    k.dsa_QIT = nc.dram_tensor("dsa_QIT", [128, 16, L], BF16).ap()
    k.dsa_OT = nc.dram_tensor("dsa_OT", [NT, 128, 16 * 128], BF16).ap()
    k.dsa_KT = nc.dram_tensor("dsa_KT", [16, 128, L], BF16).ap()
    k.dsa_V = nc.dram_tensor("dsa_V", [16, 128, L], BF16).ap()
    k.d_h = [Dep() for _ in range(NT)]
    k.d_hT = [Dep() for _ in range(NT)]
    with ExitStack() as st:
        k.S = Sched(nc, st)
        k.A = Arena(nc, st, 52000)
        k.ps = []
        k.dps = []
        for i in range(8):
            k.ps.append(st.enter_context(nc.psum_tensor("ps%d" % i, [128, 512], F32))[:])
            k.dps.append(Dep())
        A = k.A
        k.ident = A.f32(128)
        d_id = Dep()
        k.S.dma("sync", lambda e: e.dma_start(out=k.ident, in_=k.c["c_ident"]), writes=[d_id])
        k.d_hTt = [Dep(), Dep()]
        k.hTt_pos = 0
        barrier(k)
        if mode == "dsa_only":
            phase_prep(k)
            phase_dsa(k, 0, 1, k.x, k.out)
        elif mode == "s5_only":
            phase_prep(k)
            phase_s5(k, 0, 0, k.x, k.out)
        elif mode == "moe_only":
            phase_prep(k)
            phase_moe(k, 0, k.x, k.out, write_hT=False)
        elif mode == "full":
            phase_prep(k)
            h_in = k.x
            bufs = [k.hA, k.hB]
            bi = 0
            for li in range(DEPTH):
                hm = bufs[bi]; bi ^= 1
                if li % 2 == 0:
                    phase_s5(k, li // 2, li, h_in, hm)
                else:
                    phase_dsa(k, li // 2, li, h_in, hm)
                last = (li == DEPTH - 1)
                hf = k.out if last else bufs[bi]
                bi ^= 1
                phase_moe(k, li, hm, hf, write_hT=not last)
                h_in = hf
        k.S.finish(k.d_h)
        k.S.emit()
    return nc


def kernel(**inputs):
    nc = build_nc("full")
    consts = host_consts()
    x = np.ascontiguousarray(inputs["x"], dtype=np.float32)
    shared = {name: np.ascontiguousarray(inputs[name], dtype=np.float32) for name, _ in W_SPECS}
    shared.update(consts)
    in_maps = []
    for c in range(8):
        m = dict(shared)
        m["x"] = x[c]
        in_maps.append(m)
    res = run_bass_kernel_spmd(nc, in_maps, core_ids=list(range(8)))
    return np.stack([np.asarray(r["out"], dtype=np.float32) for r in res.results], axis=0)
```

```python
from concourse.bass_utils import run_bass_kernel_spmd
import numpy as np
import concourse.bass as bass
import concourse.mybir as mybir
from contextlib import ExitStack

F32 = mybir.dt.float32
BF16 = mybir.dt.bfloat16
I32 = mybir.dt.int32
ALU = mybir.AluOpType
AF = mybir.ActivationFunctionType
AX = mybir.AxisListType

ENGINES = ("tensor", "vector", "scalar", "gpsimd", "sync")
DMA_RING = 8


class Dep:
    __slots__ = ("name", "w", "r")

    def __init__(self, name=""):
        self.name = name
        self.w = None
        self.r = {}


class Sched:
    def __init__(self, nc, stack, same_engine_sync=True):
        self.nc = nc
        self.stack = stack
        self.streams = {e: [] for e in ENGINES}
        self.count = {e: 0 for e in ENGINES}
        self.seen = {e: {} for e in ENGINES}
        self.sems = {}
        for e in ENGINES:
            self.sems[e] = stack.enter_context(nc.semaphore("s_" + e))
        self.ring = {}
        self.ring_cnt = {}
        self.ring_pos = {}
        for q in ("sync", "gpsimd", "scalar"):
            self.ring[q] = []
            for i in range(DMA_RING):
                key = "d_%s_%d" % (q, i)
                self.sems[key] = stack.enter_context(nc.semaphore(key))
                self.ring[q].append(key)
            self.ring_cnt[q] = [0] * DMA_RING
            self.ring_pos[q] = 0
        self.same_engine_sync = same_engine_sync
        self.out_deps = []

    def _collect(self, reads, writes):
        need = {}

        def add(kv):
            if kv is None:
                return
            k, v = kv
            if need.get(k, 0) < v:
                need[k] = v
        for d in reads:
            add(d.w)
        for d in writes:
            add(d.w)
            for k, v in d.r.items():
                add((k, v))
        return need

    def _waits(self, eng, need, skip_self):
        ws = []
        seen = self.seen[eng]
        for k, v in need.items():
            if k == eng and skip_self:
                continue
            if seen.get(k, 0) >= v:
                continue
            seen[k] = v
            ws.append((k, v))
        return ws

    def _update(self, reads, writes, ticket):
        k, v = ticket
        for d in writes:
            d.w = ticket
            d.r = {}
        for d in reads:
            if d.r.get(k, 0) < v:
                d.r[k] = v

    def op(self, eng, fn, reads=(), writes=(), inc=True):
        need = self._collect(reads, writes)
        skip_self = (eng == "tensor") or (not self.same_engine_sync)
        ws = self._waits(eng, need, skip_self)
        if inc:
            self.count[eng] += 1
            ticket = (eng, self.count[eng])
        else:
            ticket = (eng, self.count[eng] + 1)
        self.streams[eng].append((ws, fn, (eng, 1) if inc else None))
        self._update(reads, writes, ticket)
        return ticket

    def dma(self, q, fn, reads=(), writes=()):
        need = self._collect(reads, writes)
        pos = self.ring_pos[q]
        self.ring_pos[q] = (pos + 1) % DMA_RING
        key = self.ring[q][pos]
        prev = self.ring_cnt[q][pos]
        if prev > 0:
            if need.get(key, 0) < prev * 16:
                need[key] = prev * 16
        ws = self._waits(q, need, False)
        self.ring_cnt[q][pos] = prev + 1
        ticket = (key, (prev + 1) * 16)
        self.streams[q].append((ws, fn, (key, 16)))
        self._update(reads, writes, ticket)
        return ticket

    def finish(self, deps):
        need = self._collect(deps, deps)
        ws = self._waits("sync", need, False)
        self.streams["sync"].append((ws, None, None))

    def emit(self):
        nc = self.nc
        sems = self.sems
        streams = self.streams

        def run(engh, name):
            for ws, fn, inc in streams[name]:
                for k, v in ws:
                    engh.wait_ge(sems[k], v)
                if fn is not None:
                    ins = fn(engh)
                    if inc is not None:
                        ins.then_inc(sems[inc[0]], inc[1])

        with nc.Block() as block:
            @block.tensor
            def _(e):
                run(e, "tensor")

            @block.vector
            def _(e):
                run(e, "vector")

            @block.scalar
            def _(e):
                run(e, "scalar")

            @block.gpsimd
            def _(e):
                run(e, "gpsimd")

            @block.sync
            def _(e):
                run(e, "sync")
D = 2048
L = 2048
NT = 16
DEPTH = 4
DN_ALPHA = (2 * DEPTH) ** 0.25
LN_EPS = 1e-5
RMS_EPS = 1e-6
NE = 32
FF = 256
NEG = -1.0e30


class Arena:
    def __init__(self, nc, stack, nelem):
        self.t = stack.enter_context(nc.sbuf_tensor("arena", [128, nelem], F32))
        self.n = nelem
        self.off = 0

    def mark(self):
        return self.off

    def reset(self, m):
        self.off = m

    def f32(self, n, shape=None):
        assert self.off + n <= self.n, ("arena overflow", self.off, n, self.n)
        v = self.t[:, self.off:self.off + n]
        self.off += n
        return v

    def bf16(self, n):
        m = (n + 1) // 2
        return self.f32(m).bitcast(BF16)[:, 0:n]


class K:
    pass


def r3(ap, **kw):
    return ap.rearrange("p (a b) -> p a b", **kw)


def barrier(k):
    S = k.S
    cur = {}
    for e in ENGINES:
        if S.count[e] > 0:
            cur[e] = S.count[e]
    for q in S.ring:
        for i, key in enumerate(S.ring[q]):
            if S.ring_cnt[q][i] > 0:
                cur[key] = S.ring_cnt[q][i] * 16
    for e in ENGINES:
        ws = S._waits(e, dict(cur), False)
        if ws:
            S.streams[e].append((ws, None, None))


def ln_load_params(k, g_ap, b_ap, gt, bt, dgb):
    S = k.S
    S.dma("sync", lambda e: e.dma_start(out=gt, in_=g_ap.rearrange("(o d) -> o d", o=1).partition_broadcast(128)), writes=[dgb])
    S.dma("sync", lambda e: e.dma_start(out=bt, in_=b_ap.rearrange("(o d) -> o d", o=1).partition_broadcast(128)), writes=[dgb])


def ln_tile(k, a, da, gt, bt, dgb, tt, h_out, hT_out, st, dst, write_hT=True):
    S = k.S
    s1, s2, mean, var, rstd, junk = st
    S.op("scalar", lambda e: e.activation(out=junk, in_=a, func=AF.Identity, accum_out=s1), reads=[da], writes=[dst])
    S.op("scalar", lambda e: e.activation(out=junk, in_=a, func=AF.Square, accum_out=s2), reads=[da], writes=[dst])
    S.op("vector", lambda e: e.tensor_scalar(out=mean, in0=s1, scalar1=1.0 / D, scalar2=None, op0=ALU.mult), reads=[dst], writes=[dst])
    S.op("vector", lambda e: e.tensor_tensor(out=var, in0=mean, in1=mean, op=ALU.mult), reads=[dst], writes=[dst])
    S.op("vector", lambda e: e.scalar_tensor_tensor(out=var, in0=s2, scalar=1.0 / D, in1=var, op0=ALU.mult, op1=ALU.subtract), reads=[dst], writes=[dst])
    S.op("vector", lambda e: e.tensor_scalar(out=var, in0=var, scalar1=LN_EPS, scalar2=None, op0=ALU.add), reads=[dst], writes=[dst])
    S.op("scalar", lambda e: e.activation(out=var, in_=var, func=AF.Sqrt), reads=[dst], writes=[dst])
    S.op("vector", lambda e: e.reciprocal(out=rstd, in_=var), reads=[dst], writes=[dst])
    S.op("vector", lambda e: e.tensor_scalar(out=a, in0=a, scalar1=mean, scalar2=rstd, op0=ALU.subtract, op1=ALU.mult), reads=[dst, da], writes=[da])
    S.op("vector", lambda e: e.tensor_tensor(out=a, in0=a, in1=gt, op=ALU.mult), reads=[da, dgb], writes=[da])
    S.op("vector", lambda e: e.tensor_tensor(out=a, in0=a, in1=bt, op=ALU.add), reads=[da, dgb], writes=[da])
    S.dma("gpsimd", lambda e: e.dma_start(out=h_out[tt * 128:(tt + 1) * 128, :], in_=a), reads=[da], writes=[k.d_h[tt]])
    if write_hT:
        emit_hT(k, a, da, tt, hT_out)


def emit_hT(k, a, da, tt, hT_out):
    S = k.S
    slot = k.hTt_pos
    k.hTt_pos = (slot + 1) % 2
    hTt, dhTt = k.hTt[slot], k.d_hTt[slot]
    for q in range(4):
        bank = 6 + (q % 2)
        ps, dps = k.ps[bank], k.dps[bank]
        for j in range(4):
            kc = q * 4 + j
            S.op("tensor", lambda e, kc=kc, j=j, ps=ps: e.transpose(ps[:, j * 128:(j + 1) * 128], a[:, kc * 128:(kc + 1) * 128], k.ident),
                 reads=[da], writes=[dps], inc=(j == 3))
        S.op("scalar", lambda e, q=q, ps=ps: e.activation(out=hTt[:, q * 512:(q + 1) * 512], in_=ps, func=AF.Copy),
             reads=[dps], writes=[dhTt])
    S.dma("gpsimd", lambda e: e.dma_start(out=hT_out[tt], in_=hTt), reads=[dhTt], writes=[k.d_hT[tt]])


def phase_prep(k):
    S = k.S
    A = k.A
    m = A.mark()
    k.hTt = [A.bf16(2048), A.bf16(2048)]
    xt = [A.f32(D), A.f32(D)]
    dxt = [Dep(), Dep()]
    for tt in range(NT):
        b = tt % 2
        S.dma("sync", lambda e, tt=tt, b=b: e.dma_start(out=xt[b], in_=k.x[tt * 128:(tt + 1) * 128, :]), writes=[dxt[b]])
        emit_hT(k, xt[b], dxt[b], tt, k.hT)
    barrier(k)
    A.reset(m)


def phase_moe(k, li, h_in, h_out, write_hT=True):
    S = k.S
    A = k.A
    m0 = A.mark()
    k.hTt = [A.bf16(2048), A.bf16(2048)]
    NH = 2
    TH = NT // NH
    HT = A.bf16(TH * 16 * 128)
    HTv = HT.rearrange("p (t c x) -> p t c x", t=TH, c=16)
    dHT = Dep()
    yacc = [A.f32(D) for _ in range(TH)]
    dy = [Dep() for _ in range(TH)]
    wg = A.bf16(16 * FF); wu = A.bf16(16 * FF); wd = A.bf16(2 * D)
    wgv = r3(wg, a=16); wuv = r3(wu, a=16); wdv = r3(wd, a=2)
    dwg = [Dep(), Dep()]; dwu = [Dep(), Dep()]; dwd = [Dep(), Dep()]
    NSTG = 4
    stg = [A.f32(2048) for _ in range(NSTG)]
    dstg = [Dep() for _ in range(NSTG)]
    gt = A.f32(D); bt = A.f32(D); dgb = Dep()
    hh = A.bf16(2 * 2 * 512)
    hhv = hh.rearrange("p (t f x) -> p t f x", t=2, f=2)
    dhh = [[Dep(), Dep()], [Dep(), Dep()]]
    sl = [A.f32(512), A.f32(512)]
    dsl = [Dep(), Dep()]
    wr_s = A.f32(16 * 36); wr = A.bf16(16 * 36); dwr = Dep()
    wr_sv = r3(wr_s, a=16); wrv = r3(wr, a=16)
    rb = A.f32(36); drb = Dep()
    gates = A.f32(TH * NE); dgates = [Dep() for _ in range(TH)]
    gv = r3(gates, a=TH)
    RT = A.f32(1024); drt = Dep()
    lnst_raw = A.f32(8 + D)
    lnst = (lnst_raw[:, 0:1], lnst_raw[:, 1:2], lnst_raw[:, 2:3], lnst_raw[:, 3:4], lnst_raw[:, 4:5], lnst_raw[:, 8:8 + D])
    dlnst = Dep()

    nc = k.nc
    S.dma("sync", lambda e: e.dma_start(out=wr_sv[:, :, 0:4], in_=k.w["moe_w_group"][li].rearrange("(c p) g -> p c g", p=128)), writes=[dwr])
    S.dma("sync", lambda e: e.dma_start(out=wr_sv[:, :, 4:36], in_=k.w["moe_w_expert"][li].rearrange("(c p) g -> p c g", p=128)), writes=[dwr])
    S.op("vector", lambda e: e.tensor_copy(out=wr, in_=wr_s), reads=[dwr], writes=[dwr])
    S.dma("sync", lambda e: e.dma_start(out=rb[:, 0:4], in_=k.w["moe_b_group"][li].rearrange("(o g) -> o g", o=1).partition_broadcast(128)), writes=[drb])
    S.dma("sync", lambda e: e.dma_start(out=rb[:, 4:36], in_=k.w["moe_b_expert"][li].rearrange("(o g) -> o g", o=1).partition_broadcast(128)), writes=[drb])
    ln_load_params(k, k.w["ln_ffn_g"][li], k.w["ln_ffn_b"][li], gt, bt, dgb)

    stg_pos = [0, 0]

    def load_cast(src_ap, dst_ap, ddst, eng):
        i = stg_pos[0]
        stg_pos[0] = (i + 1) % NSTG
        s = stg[i]
        sv = s if len(src_ap.shape) == 2 else r3(s, a=src_ap.shape[1])
        if not getattr(k, 'skip_wdma', False) or stg_pos[1] < 8:
            S.dma("sync", lambda e: e.dma_start(out=sv, in_=src_ap), writes=[dstg[i]])
        stg_pos[1] += 1
        if eng == "scalar":
            S.op(eng, lambda e: e.activation(out=dst_ap, in_=sv, func=AF.Copy), reads=[dstg[i]], writes=[ddst])
        else:
            S.op(eng, lambda e: e.tensor_copy(out=dst_ap, in_=sv), reads=[dstg[i]], writes=[ddst])

    wgate = k.w["moe_w_gate"][li]
    wup = k.w["moe_w_up"][li]
    wdown = k.w["moe_w_down"][li]

    for half in range(NH):
        t0 = half * TH
        for t in range(TH):
            S.dma("sync", lambda e, t=t, t0=t0: e.dma_start(out=HTv[:, t].rearrange("p c x -> p (c x)"), in_=k.hT[t0 + t]),
                  reads=[k.d_hT[t0 + t]], writes=[dHT])
        for t in range(TH):
            S.dma("sync", lambda e, t=t, t0=t0: e.dma_start(out=yacc[t], in_=h_in[(t0 + t) * 128:(t0 + t + 1) * 128, :]),
                  reads=[k.d_h[t0 + t]], writes=[dy[t]])
            S.op("scalar", lambda e, t=t: e.activation(out=yacc[t], in_=yacc[t], func=AF.Copy, scale=DN_ALPHA),
                 reads=[dy[t]], writes=[dy[t]])
        ps, dps = k.ps[6], k.dps[6]
        for t in range(TH):
            for kc in range(16):
                MM(k, ps[:, t * 36:(t + 1) * 36], HTv[:, t, kc, :], wrv[:, kc, :], kc == 0, kc == 15, [dHT, dwr], [dps], (t == TH - 1 and kc == 15))
        lg3 = RT[:, 0:TH * 36].rearrange("p (t x) -> p t x", t=TH)
        o_ = TH * 36
        gmax = RT[:, o_:o_ + TH]; o_ += TH
        gsum = RT[:, o_:o_ + TH]; o_ += TH
        gp = RT[:, o_:o_ + TH]; o_ += TH
        m1 = RT[:, o_:o_ + TH]; o_ += TH
        m2 = RT[:, o_:o_ + TH]; o_ += TH
        den = RT[:, o_:o_ + TH]; o_ += TH
        gexp3 = RT[:, o_:o_ + TH * 4].rearrange("p (t x) -> p t x", t=TH); o_ += TH * 4
        gone3 = RT[:, o_:o_ + TH * 4].rearrange("p (t x) -> p t x", t=TH); o_ += TH * 4
        coef3 = RT[:, o_:o_ + TH * 4].rearrange("p (t x) -> p t x", t=TH); o_ += TH * 4
        elc3 = RT[:, o_:o_ + TH * 8].rearrange("p (t x) -> p t x", t=TH); o_ += TH * 8
        tmp3 = RT[:, o_:o_ + TH * 8].rearrange("p (t x) -> p t x", t=TH); o_ += TH * 8
        ew3 = RT[:, o_:o_ + TH * 8].rearrange("p (t x) -> p t x", t=TH); o_ += TH * 8
        sel3 = RT[:, o_:o_ + TH * 8].rearrange("p (t x) -> p t x", t=TH); o_ += TH * 8
        assert o_ <= 1024
        s4_ = [128, TH, 4]; s8_ = [128, TH, 8]
        rd = [drt]; wr_ = [drt]
        TT(k, "vector", lg3, ps[:, 0:TH * 36].rearrange("p (t x) -> p t x", t=TH), rb.unsqueeze(1).to_broadcast([128, TH, 36]), ALU.add, [dps, drb, drt], wr_)
        k.S.op("vector", lambda e: e.tensor_reduce(out=gmax, in_=lg3[:, :, 0:4], axis=AX.X, op=ALU.max), reads=rd, writes=wr_)
        TT(k, "vector", gexp3, lg3[:, :, 0:4], gmax.unsqueeze(2).to_broadcast(s4_), ALU.subtract, rd, wr_)
        ACT(k, gexp3, gexp3, AF.Exp, rd, wr_)
        k.S.op("vector", lambda e: e.tensor_reduce(out=gsum, in_=gexp3, axis=AX.X, op=ALU.add), reads=rd, writes=wr_)
        k.S.op("vector", lambda e: e.reciprocal(out=gp, in_=gsum), reads=rd, writes=wr_)
        TT(k, "vector", gone3, lg3[:, :, 0:4], gmax.unsqueeze(2).to_broadcast(s4_), ALU.is_equal, rd, wr_)
        for g in range(4):
            dst_ = elc3 if g == 0 else tmp3
            TT(k, "vector", dst_, lg3[:, :, 4 + 8 * g:12 + 8 * g], gone3[:, :, g].unsqueeze(2).to_broadcast(s8_), ALU.mult, rd, wr_)
            if g > 0:
                TT(k, "vector", elc3, elc3, tmp3, ALU.add, rd, wr_)
        k.S.op("vector", lambda e: e.tensor_reduce(out=m1, in_=elc3, axis=AX.X, op=ALU.max), reads=rd, writes=wr_)
        TT(k, "vector", tmp3, elc3, m1.unsqueeze(2).to_broadcast(s8_), ALU.is_equal, rd, wr_)
        STT(k, "vector", tmp3, tmp3, NEG, elc3, ALU.mult, ALU.add, rd, wr_)
        k.S.op("vector", lambda e: e.tensor_reduce(out=m2, in_=tmp3, axis=AX.X, op=ALU.max), reads=rd, writes=wr_)
        TT(k, "vector", ew3, elc3, m1.unsqueeze(2).to_broadcast(s8_), ALU.subtract, rd, wr_)
        ACT(k, ew3, ew3, AF.Exp, rd, wr_)
        TT(k, "vector", sel3, elc3, m2.unsqueeze(2).to_broadcast(s8_), ALU.is_ge, rd, wr_)
        TT(k, "vector", ew3, ew3, sel3, ALU.mult, rd, wr_)
        k.S.op("vector", lambda e: e.tensor_reduce(out=den, in_=ew3, axis=AX.X, op=ALU.add), reads=rd, writes=wr_)
        k.S.op("vector", lambda e: e.reciprocal(out=den, in_=den), reads=rd, writes=wr_)
        TT(k, "vector", ew3, ew3, den.unsqueeze(2).to_broadcast(s8_), ALU.mult, rd, wr_)
        TT(k, "vector", coef3, gone3, gp.unsqueeze(2).to_broadcast(s4_), ALU.mult, rd, wr_)
        for g in range(4):
            TT(k, "vector", gv[:, :, g * 8:(g + 1) * 8], ew3, coef3[:, :, g].unsqueeze(2).to_broadcast(s8_), ALU.mult, rd, [drt] + dgates)
        for ex in range(getattr(k, 'ne_limit', NE)):
            for hf in range(2):
                load_cast(wgate[ex, hf * 1024:(hf + 1) * 1024, :].rearrange("(c p) f -> p c f", p=128), wgv[:, hf * 8:(hf + 1) * 8, :], dwg[hf], "scalar")
                load_cast(wup[ex, hf * 1024:(hf + 1) * 1024, :].rearrange("(c p) f -> p c f", p=128), wuv[:, hf * 8:(hf + 1) * 8, :], dwu[hf], "scalar")
            for tt in range(2):
                for fc in range(2):
                    pg, dpg = k.ps[fc], k.dps[fc]
                    pu, dpu = k.ps[2 + fc], k.dps[2 + fc]
                    for kc in range(16):
                        S.op("tensor", lambda e, kc=kc, fc=fc, tt=tt, pg=pg: e.matmul(pg, lhsT=wgv[:, kc, fc * 128:(fc + 1) * 128], rhs=HTv[:, tt * 4:(tt + 1) * 4, kc, :], start=(kc == 0), stop=(kc == 15)),
                             reads=[dHT, dwg[kc // 8]], writes=[dpg], inc=(kc == 15))
                    for kc in range(16):
                        S.op("tensor", lambda e, kc=kc, fc=fc, tt=tt, pu=pu: e.matmul(pu, lhsT=wuv[:, kc, fc * 128:(fc + 1) * 128], rhs=HTv[:, tt * 4:(tt + 1) * 4, kc, :], start=(kc == 0), stop=(kc == 15)),
                             reads=[dHT, dwu[kc // 8]], writes=[dpu], inc=(kc == 15))
                    S.op("scalar", lambda e, fc=fc, pg=pg: e.activation(out=sl[fc], in_=pg, func=AF.Silu), reads=[dpg], writes=[dsl[fc]])
                    S.op("vector", lambda e, fc=fc, tt=tt, pu=pu: e.tensor_tensor(out=hhv[:, tt, fc, :], in0=sl[fc], in1=pu, op=ALU.mult),
                         reads=[dsl[fc], dpu], writes=[dhh[tt][fc]])
            for hf in range(2):
                load_cast(wdown[ex, hf * 128:(hf + 1) * 128, :], wdv[:, hf, :], dwd[hf], "scalar")
            cnt = 0
            for tt in range(2):
                for sub in range(4):
                    t = tt * 4 + sub
                    for ds in range(4):
                        bank = 4 + (cnt % 2)
                        cnt += 1
                        po, dpo = k.ps[bank], k.dps[bank]
                        for fc in range(2):
                            S.op("tensor", lambda e, fc=fc, tt=tt, sub=sub, ds=ds, po=po: e.matmul(po, lhsT=hhv[:, tt, fc, sub * 128:(sub + 1) * 128], rhs=wdv[:, fc, ds * 512:(ds + 1) * 512], start=(fc == 0), stop=(fc == 1)),
                                 reads=[dhh[tt][fc], dwd[fc]], writes=[dpo], inc=(fc == 1))
                        S.op("vector", lambda e, t=t, ds=ds, po=po, ex=ex: e.scalar_tensor_tensor(out=yacc[t][:, ds * 512:(ds + 1) * 512], in0=po, scalar=gv[:, t, ex:ex + 1], in1=yacc[t][:, ds * 512:(ds + 1) * 512], op0=ALU.mult, op1=ALU.add),
                             reads=[dpo, dgates[t], dy[t]], writes=[dy[t]])
        for t in range(TH):
            ln_tile(k, yacc[t], dy[t], gt, bt, dgb, t0 + t, h_out, k.hT, lnst, dlnst, write_hT=write_hT)
    barrier(k)
    A.reset(m0)
def MM(k, out, lhsT, rhs, start, stop, reads, writes, inc):
    k.S.op("tensor", lambda e: e.matmul(out, lhsT=lhsT, rhs=rhs, start=start, stop=stop), reads=reads, writes=writes, inc=inc)


def TR(k, out, in_, ident, reads, writes, inc):
    k.S.op("tensor", lambda e: e.transpose(out, in_, ident), reads=reads, writes=writes, inc=inc)


def ACT(k, out, in_, func, reads, writes, bias=None, scale=None, accum_out=None):
    kw = {}
    if bias is not None:
        kw["bias"] = bias
    if scale is not None:
        kw["scale"] = scale
    if accum_out is not None:
        kw["accum_out"] = accum_out
    k.S.op("scalar", lambda e: e.activation(out=out, in_=in_, func=func, **kw), reads=reads, writes=writes)


def TS(k, eng, out, in0, s1, s2, op0, op1, reads, writes):
    if op1 is None:
        k.S.op(eng, lambda e: e.tensor_scalar(out=out, in0=in0, scalar1=s1, scalar2=None, op0=op0), reads=reads, writes=writes)
    else:
        k.S.op(eng, lambda e: e.tensor_scalar(out=out, in0=in0, scalar1=s1, scalar2=s2, op0=op0, op1=op1), reads=reads, writes=writes)


def TT(k, eng, out, in0, in1, op, reads, writes):
    k.S.op(eng, lambda e: e.tensor_tensor(out=out, in0=in0, in1=in1, op=op), reads=reads, writes=writes)


def STT(k, eng, out, in0, scalar, in1, op0, op1, reads, writes):
    k.S.op(eng, lambda e: e.scalar_tensor_tensor(out=out, in0=in0, scalar=scalar, in1=in1, op0=op0, op1=op1), reads=reads, writes=writes)


def CP(k, eng, out, in_, reads, writes):
    if eng == "scalar":
        k.S.op(eng, lambda e: e.activation(out=out, in_=in_, func=AF.Copy), reads=reads, writes=writes)
    else:
        k.S.op(eng, lambda e: e.tensor_copy(out=out, in_=in_), reads=reads, writes=writes)


def DMA(k, q, out, in_, reads, writes, slow=False):
    if slow:
        k.S.dma(q, lambda e: e.dma_start(out=out, in_=in_, allow_slow_non_contiguous=True), reads=reads, writes=writes)
    else:
        k.S.dma(q, lambda e: e.dma_start(out=out, in_=in_), reads=reads, writes=writes)


class Stager:
    def __init__(self, k, nbuf, nelem):
        self.k = k
        self.bufs = [k.A.f32(nelem) for _ in range(nbuf)]
        self.deps = [Dep() for _ in range(nbuf)]
        self.pos = 0
        self.nelem = nelem
        self.engs = ["scalar", "vector"]
        self.epos = 0

    def load(self, src_ap, dst_ap, ddst, eng=None, slow=False):
        i = self.pos
        self.pos = (i + 1) % len(self.bufs)
        n = 1
        for d_ in src_ap.shape[1:]:
            n *= d_
        assert n <= self.nelem, (n, self.nelem)
        s = self.bufs[i][:, 0:n]
        if len(src_ap.shape) == 3:
            s = s.rearrange("p (a b) -> p a b", a=src_ap.shape[1])
        elif len(src_ap.shape) == 4:
            s = s.rearrange("p (a b c) -> p a b c", a=src_ap.shape[1], b=src_ap.shape[2])
        s = s[0:src_ap.shape[0]]
        DMA(self.k, "sync", s, src_ap, [], [self.deps[i]], slow=slow)
        if eng is None:
            eng = self.engs[self.epos]
            self.epos = (self.epos + 1) % len(self.engs)
        CP(self.k, eng, dst_ap, s, [self.deps[i]], [ddst])


def phase_outproj_ln(k, srcT, d_src, w_ap, g_ap, b_ap, h_in, h_out):
    S = k.S
    A = k.A
    m0 = A.mark()
    k.hTt = [A.bf16(2048), A.bf16(2048)]
    wo = A.bf16(16 * D)
    wov = r3(wo, a=16)
    dwo = [Dep() for _ in range(16)]
    stg = Stager(k, 3, 2048)
    gt = A.f32(D); bt = A.f32(D); dgb = Dep()
    src = [A.bf16(2048), A.bf16(2048)]
    dsrc = [Dep(), Dep()]
    at = [A.f32(D), A.f32(D)]
    dat = [Dep(), Dep()]
    lnst_raw = A.f32(8 + D)
    lnst = (lnst_raw[:, 0:1], lnst_raw[:, 1:2], lnst_raw[:, 2:3], lnst_raw[:, 3:4], lnst_raw[:, 4:5], lnst_raw[:, 8:8 + D])
    dlnst = Dep()
    ln_load_params(k, g_ap, b_ap, gt, bt, dgb)
    for kc in range(16):
        stg.load(w_ap[kc * 128:(kc + 1) * 128, :], wov[:, kc, :], dwo[kc])
    for tt in range(NT):
        b = tt % 2
        DMA(k, "sync", src[b], srcT[tt], [d_src[tt]], [dsrc[b]])
        DMA(k, "sync", at[b], h_in[tt * 128:(tt + 1) * 128, :], [k.d_h[tt]], [dat[b]])
        sv = r3(src[b], a=16)
        for ns in range(4):
            ps, dps = k.ps[ns], k.dps[ns]
            for kc in range(16):
                MM(k, ps, sv[:, kc, :], wov[:, kc, ns * 512:(ns + 1) * 512], kc == 0, kc == 15, [dsrc[b], dwo[kc]], [dps], kc == 15)
            STT(k, "vector", at[b][:, ns * 512:(ns + 1) * 512], at[b][:, ns * 512:(ns + 1) * 512], DN_ALPHA, ps, ALU.mult, ALU.add, [dps, dat[b]], [dat[b]])
        ln_tile(k, at[b], dat[b], gt, bt, dgb, tt, h_out, k.hT, lnst, dlnst, write_hT=True)
    barrier(k)
    A.reset(m0)


def rel_bucket_np(n):
    n = np.maximum(n, 0)
    nf = np.maximum(n, 1).astype(np.float32)
    large = 16 + (np.log(nf / np.float32(16)) / np.float32(np.log(128 / 16)) * np.float32(16)).astype(np.int32)
    large = np.minimum(large, 31)
    return np.where(n < 16, n, large)


def dsa_consts():
    ql = np.arange(128)[:, None]
    x = np.arange(256)[None, :]
    dist = np.where(x < 128, 128 + ql - x, ql - (x - 128))
    bk = rel_bucket_np(dist)
    oh = np.zeros((128, 32, 256), np.float32)
    for b in range(32):
        oh[:, b, :] = (bk == b)
    caus = np.where(np.arange(128)[None, :] <= np.arange(128)[:, None], 0.0, NEG).astype(np.float32)
    return {"c_ohb": oh.reshape(128, 32 * 256), "c_caus": caus}


def phase_dsa(k, j, li, h_in, h_out):
    S = k.S
    A = k.A
    W = k.w
    NH_ = 16
    att_scale = 128 ** -0.5
    widx_scale = (16 ** -0.5) * (128 ** -0.5)
    QT_d = k.dsa_QT; QIT_d = k.dsa_QIT; OT_d = k.dsa_OT
    d_OT = [Dep() for _ in range(NT)]
    d_QT = Dep(); d_QIT = Dep()
    m_phase = A.mark()
    CQT = A.bf16(4 * L); CQTv = r3(CQT, a=4); dCQT = Dep()
    CKVT = A.bf16(4 * L); CKVTv = r3(CKVT, a=4); dCKVT = Dep()
    KIT = A.bf16(L); dKIT = Dep()
    WI = A.f32(NT * 16); WIv = r3(WI, a=NT); dWI = Dep()
    identb = A.bf16(128); didb = Dep()
    CP(k, "vector", identb, k.ident, [], [didb])
    mA = A.mark()
    HTb = [A.bf16(2048), A.bf16(2048)]; dHTb = [Dep(), Dep()]
    win = A.bf16(16 * 1168); winv = r3(win, a=16); dwin = [Dep() for _ in range(16)]
    stg = Stager(k, 3, 2048)
    qg = A.f32(512); kg = A.f32(512); dqg = Dep()
    DMA(k, "sync", qg, W["dsa_q_norm"][j].rearrange("(o d) -> o d", o=1).partition_broadcast(128), [], [dqg])
    DMA(k, "sync", kg, W["dsa_kv_norm"][j].rearrange("(o d) -> o d", o=1).partition_broadcast(128), [], [dqg])
    for kc in range(16):
        stg.load(W["dsa_w_in"][j, kc * 128:(kc + 1) * 128, :], winv[:, kc, :], dwin[kc])
    pj = [A.f32(1168), A.f32(1168)]; dpj = [Dep(), Dep()]
    sm = A.f32(16); dsm = Dep()
    junk = A.f32(512)
    for t in range(NT):
        b = t % 2
        DMA(k, "sync", HTb[b], k.hT[t], [k.d_hT[t]], [dHTb[b]])
        HTt = r3(HTb[b], a=16)
        for ns, (c0, c1) in enumerate([(0, 512), (512, 1024), (1024, 1168)]):
            ps, dps = k.ps[ns], k.dps[ns]
            for kc in range(16):
                MM(k, ps[:, 0:c1 - c0], HTt[:, kc, :], winv[:, kc, c0:c1], kc == 0, kc == 15, [dHTb[b], dwin[kc]], [dps], kc == 15)
            CP(k, "vector", pj[b][:, c0:c1], ps[:, 0:c1 - c0], [dps], [dpj[b]])
        for qi, (c0, gain) in enumerate([(0, qg), (512, kg)]):
            ss = sm[:, qi * 4:qi * 4 + 1]; rs = sm[:, qi * 4 + 1:qi * 4 + 2]
            ACT(k, junk, pj[b][:, c0:c0 + 512], AF.Square, [dpj[b]], [dsm], accum_out=ss)
            TS(k, "vector", rs, ss, 1.0 / 512, RMS_EPS, ALU.mult, ALU.add, [dsm], [dsm])
            ACT(k, rs, rs, AF.Sqrt, [dsm], [dsm])
            k.S.op("vector", lambda e, rs=rs: e.reciprocal(out=rs, in_=rs), reads=[dsm], writes=[dsm])
            STT(k, "vector", pj[b][:, c0:c0 + 512], pj[b][:, c0:c0 + 512], rs, gain, ALU.mult, ALU.mult, [dsm, dpj[b], dqg], [dpj[b]])
        TS(k, "vector", WIv[:, t, :], pj[b][:, 1152:1168], widx_scale, None, ALU.mult, None, [dpj[b]], [dWI])
        for grp, (c0, dstv, ddst) in enumerate([(0, CQTv, dCQT), (512, CKVTv, dCKVT)]):
            ps, dps = k.ps[4 + grp], k.dps[4 + grp]
            for kc in range(4):
                TR(k, ps[:, kc * 128:(kc + 1) * 128], pj[b][:, c0 + kc * 128:c0 + (kc + 1) * 128], k.ident, [dpj[b]], [dps], kc == 3)
            ACT(k, dstv[:, :, t * 128:(t + 1) * 128], ps.rearrange("p (a b) -> p a b", a=4), AF.Copy, [dps], [ddst])
        ps, dps = k.ps[6], k.dps[6]
        TR(k, ps[:, 0:128], pj[b][:, 1024:1152], k.ident, [dpj[b]], [dps], True)
        ACT(k, KIT[:, t * 128:(t + 1) * 128], ps[:, 0:128], AF.Copy, [dps], [dKIT])
    barrier(k)
    A.reset(mA)
    if getattr(k, 'dsa_stop', '') == 'A':
        return
    mB = A.mark()
    wq = A.bf16(4 * 2048); wqv = r3(wq, a=4); dwq = [Dep() for _ in range(4)]
    stg = Stager(k, 3, 2048)
    ev = [A.bf16(512), A.bf16(512)]; dev_ = [Dep(), Dep()]
    cnt = 0
    for wname, dst_d, ddst in [("dsa_w_uq", QT_d, d_QT), ("dsa_w_qidx", QIT_d, d_QIT)]:
        for kc in range(4):
            stg.load(W[wname][j, kc * 128:(kc + 1) * 128, :], wqv[:, kc, :], dwq[kc])
        for h in range(NH_):
            for sl_ in range(4):
                ps, dps = k.ps[cnt % 4], k.dps[cnt % 4]
                b = cnt % 2
                cnt += 1
                for kc in range(4):
                    MM(k, ps, wqv[:, kc, h * 128:(h + 1) * 128], CQTv[:, kc, sl_ * 512:(sl_ + 1) * 512], kc == 0, kc == 3, [dwq[kc], dCQT], [dps], kc == 3)
                if b == 0:
                    ACT(k, ev[b], ps, AF.Copy, [dps], [dev_[b]])
                else:
                    CP(k, "vector", ev[b], ps, [dps], [dev_[b]])
                DMA(k, "gpsimd", dst_d[:, h, sl_ * 512:(sl_ + 1) * 512], ev[b], [dev_[b]], [ddst])
    barrier(k)
    A.reset(mB)
    if getattr(k, 'dsa_stop', '') == 'B':
        return
    KT_d = k.dsa_KT; V_d = k.dsa_V
    dKT = Dep(); dV = Dep()
    mC = A.mark()
    evc = [A.bf16(512), A.bf16(512)]; devc = [Dep(), Dep()]
    stg = Stager(k, 3, 2048)
    wukT = A.bf16(4 * 2048); wukTv = wukT.rearrange("p (c h d) -> p c h d", c=4, h=NH_); dwukT = Dep()
    wuv = A.bf16(4 * 2048); wuvv = wuv.rearrange("p (c h d) -> p c h d", c=4, h=NH_); dwuv = Dep()
    uk = [A.f32(512), A.f32(512)]; duk = [Dep(), Dep()]
    for h in range(NH_):
        b = h % 2
        DMA(k, "sync", uk[b], W["dsa_w_uk"][j, h], [], [duk[b]])
        ps, dps = k.ps[4 + b], k.dps[4 + b]
        for kc in range(4):
            TR(k, ps[:, kc * 128:(kc + 1) * 128], uk[b][:, kc * 128:(kc + 1) * 128], k.ident, [duk[b]], [dps], kc == 3)
        ACT(k, wukTv[:, :, h, :], ps.rearrange("p (a b) -> p a b", a=4), AF.Copy, [dps], [dwukT])
    for h in range(NH_):
        stg.load(W["dsa_w_uv"][j, h].rearrange("(c p) d -> p c d", p=128), wuvv[:, :, h, :], dwuv)
    cnt = 0
    for h in range(NH_):
        for sl_ in range(4):
            ps, dps = k.ps[cnt % 4], k.dps[cnt % 4]
            cnt += 1
            for kc in range(4):
                MM(k, ps, wukTv[:, kc, h, :], CKVTv[:, kc, sl_ * 512:(sl_ + 1) * 512], kc == 0, kc == 3, [dwukT, dCKVT], [dps], kc == 3)
            b = cnt % 2
            if b == 0:
                ACT(k, evc[b], ps, AF.Copy, [dps], [devc[b]])
            else:
                CP(k, "vector", evc[b], ps, [dps], [devc[b]])
            DMA(k, "gpsimd", KT_d[h, :, sl_ * 512:(sl_ + 1) * 512], evc[b], [devc[b]], [dKT])
    for st_ in range(NT):
        for hg in range(4):
            ps, dps = k.ps[cnt % 4], k.dps[cnt % 4]
            cnt += 1
            for kc in range(4):
                MM(k, ps, CKVTv[:, kc, st_ * 128:(st_ + 1) * 128], wuvv[:, kc, hg * 4:(hg + 1) * 4, :], kc == 0, kc == 3, [dwuv, dCKVT], [dps], kc == 3)
            b = cnt % 2
            if b == 0:
                ACT(k, evc[b], ps, AF.Copy, [dps], [devc[b]])
            else:
                CP(k, "vector", evc[b], ps, [dps], [devc[b]])
            DMA(k, "gpsimd", V_d[hg * 4:(hg + 1) * 4, :, st_ * 128:(st_ + 1) * 128].rearrange("h p d -> p h d"), r3(evc[b], a=4), [devc[b]], [dV])
    barrier(k)
    A.reset(mC)
    if getattr(k, 'dsa_stop', '') == 'C':
        return
    Tn = A.f32(NH_ * 256); Tnv = r3(Tn, a=NH_); dTn = Dep()
    caus = A.f32(128); dcaus = Dep()
    DMA(k, "sync", caus, k.c["c_caus"], [], [dcaus])
    mT = A.mark()
    ohb = A.f32(32 * 256); ohbv = r3(ohb, a=32); dohb = Dep()
    rbB = A.f32(512); drbB = Dep()
    DMA(k, "sync", ohb, k.c["c_ohb"], [], [dohb])
    DMA(k, "sync", rbB, W["rel_bias"].rearrange("(o b) h -> o (b h)", o=1).partition_broadcast(128), [], [drbB])
    for h in range(NH_):
        eng = "vector"
        TS(k, eng, Tnv[:, h, :], ohbv[:, 0, :], rbB[:, h:h + 1], None, ALU.mult, None, [dohb, drbB], [dTn])
        for b_ in range(1, 32):
            STT(k, eng, Tnv[:, h, :], ohbv[:, b_, :], rbB[:, b_ * 16 + h:b_ * 16 + h + 1], Tnv[:, h, :], ALU.mult, ALU.add, [dohb, drbB, dTn], [dTn])
        TS(k, eng, Tnv[:, h, :], Tnv[:, h, :], rbB[:, 31 * 16 + h:31 * 16 + h + 1], None, ALU.subtract, None, [drbB, dTn], [dTn])
    barrier(k)
    A.reset(mT)
    accs = [A.f32(L), A.f32(L)]; daccs = [Dep(), Dep()]
    tmp = [A.f32(512), A.f32(512)]; dtmp = [Dep(), Dep()]
    madds = [A.bf16(L), A.bf16(L)]; dmadds = [Dep(), Dep()]
    Xs = [A.f32(L) for _ in range(3)]; dXs = [Dep() for _ in range(3)]
    Ps = [A.bf16(L) for _ in range(3)]; dPs = [Dep() for _ in range(3)]
    PTs = [A.bf16(NT * 128) for _ in range(3)]; dPTs = [Dep() for _ in range(3)]
    QIbs = [A.bf16(NH_ * 128), A.bf16(NH_ * 128)]; dQIbs = [Dep(), Dep()]
    QTbs = [A.bf16(NH_ * 128), A.bf16(NH_ * 128)]; dQTbs = [Dep(), Dep()]
    OT = [A.bf16(NH_ * 128), A.bf16(NH_ * 128)]; dOTs = [Dep(), Dep()]
    mx8 = A.f32(8); dmx = Dep()
    sm2s = [A.f32(8) for _ in range(3)]; dsm2s = [Dep() for _ in range(3)]
    dgrs = [A.bf16(128) for _ in range(3)]; ddgrs = [Dep() for _ in range(3)]
    Kh = [A.bf16(L) for _ in range(3)]; dKh = [Dep() for _ in range(3)]
    Vh = [A.bf16(L) for _ in range(3)]; dVh = [Dep() for _ in range(3)]
    z1 = A.f32(1); dz1 = Dep()
    k.S.op("vector", lambda e: e.memset(z1, 0.0), reads=[], writes=[dz1])

    def pre_thunks(jb):
        SL = (jb + 1) * 128
        nbk = (SL + 511) // 512
        pb = jb % 2
        acc, dacc = accs[pb], daccs[pb]
        madd, dmadd = madds[pb], dmadds[pb]
        QIbv = r3(QIbs[pb], a=NH_); dQIb = dQIbs[pb]
        th_ = []

        def t_load():
            DMA(k, "sync", QIbv, QIT_d[:, :, jb * 128:(jb + 1) * 128], [d_QIT], [dQIb])
        th_.append(t_load)
        cnt = [0]
        for h in range(NH_):
            def t_head(h=h):
                for bk in range(nbk):
                    w_ = min(512, SL - bk * 512)
                    bank = 4
                    ps, dps = k.ps[bank], k.dps[bank]
                    tb = cnt[0] % 2
                    cnt[0] += 1
                    MM(k, ps[:, 0:w_], QIbv[:, h, :], KIT[:, bk * 512:bk * 512 + w_], True, True, [dQIb, dKIT], [dps], True)
                    if h == 0:
                        TS(k, "vector", acc[:, bk * 512:bk * 512 + w_], ps[:, 0:w_], z1, WIv[:, jb, h:h + 1], ALU.max, ALU.mult, [dps, dWI, dz1], [dacc])
                    else:
                        TS(k, "vector", tmp[tb][:, 0:w_], ps[:, 0:w_], z1, WIv[:, jb, h:h + 1], ALU.max, ALU.mult, [dps, dWI, dz1], [dtmp[tb]])
                        TT(k, "gpsimd", acc[:, bk * 512:bk * 512 + w_], acc[:, bk * 512:bk * 512 + w_], tmp[tb][:, 0:w_], ALU.add, [dtmp[tb], dacc], [dacc])
            th_.append(t_head)

        def t_caus():
            TT(k, "gpsimd", acc[:, jb * 128:SL], acc[:, jb * 128:SL], caus, ALU.add, [dacc, dcaus], [dacc])
        th_.append(t_caus)
        if SL > 256:
            for r in range(32):
                def t_round():
                    k.S.op("vector", lambda e: e.max(out=mx8, in_=acc[:, 0:SL]), reads=[dacc], writes=[dmx])
                    k.S.op("vector", lambda e: e.match_replace(out=acc[:, 0:SL], in_to_replace=mx8, in_values=acc[:, 0:SL], imm_value=-2.0e30), reads=[dacc, dmx], writes=[dacc])
                th_.append(t_round)

            def t_fin():
                TS(k, "vector", madd[:, 0:SL], acc[:, 0:SL], -1.5e30, NEG, ALU.is_gt, ALU.mult, [dacc], [dmadd])
        else:
            def t_fin():
                TS(k, "vector", madd[:, 0:SL], acc[:, 0:SL], -1.0e29, NEG, ALU.is_lt, ALU.mult, [dacc], [dmadd])
        th_.append(t_fin)
        return th_

    NB3 = 3
    dPV = [Dep(), Dep()]

    def bufs(i):
        b3 = i % NB3
        return (Xs[b3], dXs[b3], Ps[b3], dPs[b3], r3(PTs[b3], a=NT), dPTs[b3], sm2s[b3], dsm2s[b3], dgrs[b3], ddgrs[b3], Kh[b3], dKh[b3], Vh[b3], dVh[b3])

    def stageA(i):
        jb, h = divmod(i, NH_)
        SL = (jb + 1) * 128
        nbk = (SL + 511) // 512
        pb = jb % 2
        madd, dmadd = madds[pb], dmadds[pb]
        QTbv = r3(QTbs[pb], a=NH_); dQTb = dQTbs[pb]
        X, dX, P, dP, PTv, dPT, sm2, dsm2, dgr, ddgr, Kb, dKb, Vb, dVb = bufs(i)
        if h == 0:
            DMA(k, "sync", QTbv, QT_d[:, :, jb * 128:(jb + 1) * 128], [d_QT], [dQTb])
        DMA(k, "sync", Kb[:, 0:SL], KT_d[h, :, 0:SL], [dKT], [dKb])
        DMA(k, "sync", Vb[:, 0:SL], V_d[h, :, 0:SL], [dV], [dVb])
        for bk in range(nbk):
            w_ = min(512, SL - bk * 512)
            bank = (bk % 2) + 2 * (i % 2)
            ps, dps = k.ps[bank], k.dps[bank]
            MM(k, ps[:, 0:w_], QTbv[:, h, :], Kb[:, bk * 512:bk * 512 + w_], True, True, [dQTb, dKb], [dps], True)
            STT(k, "vector", X[:, bk * 512:bk * 512 + w_], ps[:, 0:w_], att_scale, madd[:, bk * 512:bk * 512 + w_], ALU.mult, ALU.add, [dps, dmadd], [dX])
        lo = max(0, jb - 1) * 128
        tlo = 0 if jb >= 1 else 128
        TT(k, "vector", X[:, lo:SL], X[:, lo:SL], Tnv[:, h, tlo:256], ALU.add, [dX, dTn], [dX])
        rmax = sm2[:, 0:1]; nmax = sm2[:, 1:2]; rsum = sm2[:, 2:3]
        k.S.op("vector", lambda e: e.tensor_reduce(out=rmax, in_=X[:, 0:SL], axis=AX.X, op=ALU.max), reads=[dX], writes=[dsm2])
        TS(k, "vector", nmax, rmax, -1.0, None, ALU.mult, None, [dsm2], [dsm2])
        ACT(k, P[:, 0:SL], X[:, 0:SL], AF.Exp, [dX, dsm2], [dP, dsm2], bias=nmax, scale=1.0, accum_out=rsum)

    def stageB(i):
        jb, h = divmod(i, NH_)
        X, dX, P, dP, PTv, dPT, sm2, dsm2, dgr, ddgr, Kb, dKb, Vb, dVb = bufs(i)
        rsum = sm2[:, 2:3]; rinv = sm2[:, 3:4]
        k.S.op("vector", lambda e: e.reciprocal(out=rinv, in_=rsum), reads=[dsm2], writes=[dsm2])
        TS(k, "vector", dgr, identb, rinv, None, ALU.mult, None, [dsm2, didb], [ddgr])
        for st_ in range(jb + 1):
            bank = 6 + (st_ // 4) % 2
            ps, dps = k.ps[bank], k.dps[bank]
            last = (st_ % 4 == 3) or (st_ == jb)
            MM(k, ps[:, (st_ % 4) * 128:(st_ % 4 + 1) * 128], P[:, st_ * 128:(st_ + 1) * 128], dgr, True, True, [dP, ddgr], [dps], last)
            if last:
                s0 = (st_ // 4) * 4
                n_ = st_ - s0 + 1
                ACT(k, PTv[:, s0:s0 + n_, :], ps[:, 0:n_ * 128].rearrange("p (a b) -> p a b", a=n_), AF.Copy, [dps], [dPT])
        Vhv = r3(Vb, a=NT)
        pv = k.ps[5][:, (i % 2) * 128:(i % 2 + 1) * 128]
        for st_ in range(jb + 1):
            MM(k, pv, Vhv[:, st_, :], PTv[:, st_, :], st_ == 0, st_ == jb, [dVb, dPT], [dPV[i % 2]], st_ == jb)

    def stageC(i):
        jb, h = divmod(i, NH_)
        pb = jb % 2
        ot, dot = OT[pb], dOTs[pb]
        otv = r3(ot, a=NH_)
        pv = k.ps[5][:, (i % 2) * 128:(i % 2 + 1) * 128]
        CP(k, "vector", otv[:, h, :], pv, [dPV[i % 2]], [dot])
        if h == NH_ - 1:
            DMA(k, "gpsimd", OT_d[jb], ot, [dot], [d_OT[jb]])

    for t_ in pre_thunks(0):
        t_()
    NHEADS = NT * NH_
    nxt = []
    pos = 0
    per = 0
    for i in range(NHEADS + 2):
        if i < NHEADS:
            jb, h = divmod(i, NH_)
            if h == 0:
                for t_ in nxt[pos:]:
                    t_()
                nxt = pre_thunks(jb + 1) if jb + 1 < NT else []
                per = (len(nxt) + NH_ - 1) // NH_
                pos = 0
            stageA(i)
        if 0 <= i - 1 < NHEADS:
            stageB(i - 1)
        if 0 <= i - 2 < NHEADS:
            stageC(i - 2)
        if i < NHEADS:
            for t_ in nxt[pos:pos + per]:
                t_()
            pos += per
    barrier(k)
    A.reset(m_phase)
    if getattr(k, 'dsa_stop', '') == 'D':
        return
    phase_outproj_ln(k, OT_d, d_OT, W["dsa_w_out"][j], W["ln_mix_g"][li], W["ln_mix_b"][li], h_in, h_out)
import math as _math

S5_KVEC = [0, -1, -2, -3, -4, -5, -6, -7, 7, 6, 5, 4, 3, 2, 1, 0, 0, 1, 2, 3, 4, 5, 6, 7, 1, 2, 3, 4, 5, 6, 7, 8, 8, 16, 32, 64, 128, 256, 512, 1024]
NK = 40


def s5_consts():
    kv = np.tile(np.array(S5_KVEC, np.float32)[None, :], (128, 1))
    s_ = (np.arange(128) // 16)[:, None]
    t_ = (np.arange(128) // 16)[None, :]
    msk = (t_ >= s_).astype(np.float32)
    import ml_dtypes
    sel = np.zeros((128, 64, 128), np.float32)
    for g8 in range(8):
        for s in range(8):
            for p_ in range(16):
                sel[g8 * 16 + p_, g8 * 8 + s, s * 16 + p_] = 1.0
    selT = np.ascontiguousarray(sel.transpose(2, 1, 0))
    return {"c_kv40": kv, "c_s5mask": msk,
            "c_sel": sel.reshape(128, 8192).astype(ml_dtypes.bfloat16),
            "c_selT": selT.reshape(128, 8192).astype(ml_dtypes.bfloat16)}


def bc(ap, axis, shape):
    return ap.unsqueeze(axis).to_broadcast(list(shape))


def phase_s5(k, sj, li, h_in, h_out):
    S = k.S
    A = k.A
    W = k.w
    M_d, W1_d, W2_d, ZT_d = k.s5_M, k.s5_W1, k.s5_W2, k.s5_ZT
    dM = Dep(); dW1 = Dep(); dW2 = Dep()
    d_ZT = [Dep() for _ in range(NT)]
    TWO_PI = 2.0 * _math.pi
    m_phase = A.mark()
    DCOL = A.f32(128); dDCOL = Dep()
    ASr = A.f32(512); ASi = A.f32(512); ASn = A.f32(512); dAS = Dep()
    ASrv = r3(ASr, a=64); ASiv = r3(ASi, a=64); ASnv = r3(ASn, a=64)
    mP = A.mark()
    KV = A.f32(NK); dKV = Dep()
    DMA(k, "sync", KV, k.c["c_kv40"], [], [dKV])
    MASK = A.f32(128); dMASK = Dep()
    DMA(k, "sync", MASK, k.c["c_s5mask"], [], [dMASK])
    PAre = A.f32(64); PAim = A.f32(64); PDT = A.f32(64); dPA = Dep()
    PBre = A.f32(1024); PBim = A.f32(1024); PCre = A.f32(1024); PCim = A.f32(1024); dPB = Dep(); dPC = Dep()
    PBrev = r3(PBre, a=64); PBimv = r3(PBim, a=64); PCrev = r3(PCre, a=64); PCimv = r3(PCim, a=64)
    ld = [A.f32(2048), A.f32(2048)]; dld = [Dep(), Dep()]
    ld2 = A.f32(2048); dld2 = Dep()
    id64 = k.ident[0:64, 0:64]
    DMA(k, "sync", ld[0][:, 0:16], W["s5_d"][sj], [], [dld[0]])
    CP(k, "vector", ld[0][:, 16:144].rearrange("p (t q) -> p t q", t=8), bc(ld[0][:, 0:16], 1, [128, 8, 16]), [dld[0]], [dld[0]])
    TR(k, k.ps[0][:, 0:128], ld[0][:, 16:144], k.ident, [dld[0]], [k.dps[0]], True)
    CP(k, "vector", DCOL, k.ps[0][:, 0:128], [k.dps[0]], [dDCOL])
    DMA(k, "sync", ld[1][0:64, 0:128], W["s5_a_re"][sj].rearrange("(j g2) n -> j (g2 n)", g2=2), [], [dld[1]])
    DMA(k, "sync", ld[1][0:64, 128:256], W["s5_a_im"][sj].rearrange("(j g2) n -> j (g2 n)", g2=2), [], [dld[1]])
    DMA(k, "sync", ld[1][0:64, 256:258], W["s5_log_dt"][sj].rearrange("(j g2) -> j g2", g2=2), [], [dld[1]])
    CP(k, "vector", ld[1][0:64, 384:512].rearrange("p (g n) -> p g n", g=2), bc(ld[1][0:64, 256:258], 2, [64, 2, 64]), [dld[1]], [dld[1]])
    for i_, (c0, dst) in enumerate([(0, PAre), (128, PAim), (384, PDT)]):
        TR(k, k.ps[1][:, i_ * 64:(i_ + 1) * 64], ld[1][0:64, c0:c0 + 128], id64, [dld[1]], [k.dps[1]], True)
        CP(k, "vector", dst, k.ps[1][:, i_ * 64:(i_ + 1) * 64], [k.dps[1]], [dPA])
    cnt_ = 0
    for name, dstv, ddst, is_c in [("s5_b_re", PBrev, dPB, False), ("s5_b_im", PBimv, dPB, False), ("s5_c_re", PCrev, dPC, True), ("s5_c_im", PCimv, dPC, True)]:
        lb = ld[cnt_ % 2]; dlb = dld[cnt_ % 2]
        cnt_ += 1
        if is_c:
            DMA(k, "sync", lb[0:64, :], W[name][sj].rearrange("(j g2) p n -> j (g2 p n)", g2=2), [], [dlb])
            lb2 = ld2[0:64, :]
            CP(k, "vector", lb2.rearrange("j (p g n) -> j p g n", p=16, g=2), lb[0:64, :].rearrange("j (g p n) -> j p g n", g=2, p=16), [dlb, dld2], [dld2])
            lv = lb2.rearrange("j (p gn) -> j p gn", p=16)
        else:
            DMA(k, "sync", lb[0:64, :], W[name][sj].rearrange("(j g2) n p -> j (g2 n p)", g2=2), [], [dlb])
            lv = lb[0:64, :].rearrange("j (gn p) -> j gn p", p=16)
        for q4 in range(2):
            bank = 2 + q4
            ps, dps = k.ps[bank], k.dps[bank]
            for p8 in range(8):
                p_ = q4 * 8 + p8
                src = lv[:, p_, :] if is_c else lv[:, :, p_]
                TR(k, ps[:, p8 * 64:(p8 + 1) * 64], src, id64, [dlb, dld2], [dps], p8 == 7)
            CP(k, "vector", dstv[:, :, q4 * 8:(q4 + 1) * 8].rearrange("p j q -> p q j"), ps.rearrange("p (q j) -> p q j", q=8), [dps], [ddst])
    if getattr(k, 's5_stop', '') == 'P1':
        barrier(k)
        return
    dE = Dep()
    lr = A.f32(64); ldr = A.f32(64); th = A.f32(64); dtt = A.f32(64)
    TS(k, "vector", lr, PAre, -1.0e-4, None, ALU.min, None, [dPA], [dE])
    ACT(k, dtt, PDT, AF.Exp, [dPA], [dE])
    TT(k, "vector", ldr, lr, dtt, ALU.mult, [dE], [dE])
    TT(k, "vector", th, PAim, dtt, ALU.mult, [dE, dPA], [dE])
    NKK = 64 * NK
    shp = [128, 64, NK]
    ARG = A.f32(NKK); PHI = A.f32(NKK); RHO = A.f32(NKK); QF = A.f32(NKK); MSK2 = A.f32(NKK)
    ARE = A.f32(NKK); AIM = A.f32(NKK)
    QI = A.f32(NKK).bitcast(I32)
    v3 = lambda t_: r3(t_, a=64)
    TT(k, "vector", v3(ARG), bc(ldr, 2, shp), bc(KV, 1, shp), ALU.mult, [dE, dKV], [dE])
    ACT(k, RHO, ARG, AF.Exp, [dE], [dE])
    TT(k, "vector", v3(PHI), bc(th, 2, shp), bc(KV, 1, shp), ALU.mult, [dE, dKV], [dE])

    def sin_of(dst, off):
        TS(k, "vector", ARG, PHI, off, None, ALU.add, None, [dE], [dE])
        TS(k, "vector", QF, ARG, 1.0 / TWO_PI, None, ALU.mult, None, [dE], [dE])
        CP(k, "vector", QI, QF, [dE], [dE])
        CP(k, "vector", QF, QI, [dE], [dE])
        STT(k, "vector", ARG, QF, -TWO_PI, ARG, ALU.mult, ALU.add, [dE], [dE])
        TS(k, "vector", MSK2, ARG, _math.pi, -TWO_PI, ALU.is_gt, ALU.mult, [dE], [dE])
        TT(k, "vector", ARG, ARG, MSK2, ALU.add, [dE], [dE])
        TS(k, "vector", MSK2, ARG, -_math.pi, TWO_PI, ALU.is_lt, ALU.mult, [dE], [dE])
        TT(k, "vector", ARG, ARG, MSK2, ALU.add, [dE], [dE])
        ACT(k, dst, ARG, AF.Sin, [dE], [dE])

    sin_of(AIM, 64.0 * _math.pi)
    sin_of(ARE, 64.5 * _math.pi)
    TT(k, "vector", AIM, AIM, RHO, ALU.mult, [dE], [dE])
    TT(k, "vector", ARE, ARE, RHO, ALU.mult, [dE], [dE])
    AREv = v3(ARE); AIMv = v3(AIM)
    CP(k, "vector", ASrv, AREv[:, :, 32:40], [dE], [dAS])
    CP(k, "vector", ASiv, AIMv[:, :, 32:40], [dE], [dAS])
    TS(k, "vector", ASnv, AIMv[:, :, 32:40], -1.0, None, ALU.mult, None, [dE], [dAS])
    er = A.f32(64); ei = A.f32(64); qr = A.f32(64); qi_ = A.f32(64); den = A.f32(64); t1 = A.f32(64); fr = A.f32(64); fi = A.f32(64)
    TS(k, "vector", er, AREv[:, :, 24], -1.0, None, ALU.add, None, [dE], [dE])
    CP(k, "vector", ei, AIMv[:, :, 24], [dE], [dE])
    TT(k, "vector", qr, er, lr, ALU.mult, [dE], [dE])
    TT(k, "vector", t1, ei, PAim, ALU.mult, [dE], [dE])
    TT(k, "vector", qr, qr, t1, ALU.add, [dE], [dE])
    TT(k, "vector", qi_, ei, lr, ALU.mult, [dE], [dE])
    TT(k, "vector", t1, er, PAim, ALU.mult, [dE], [dE])
    TT(k, "vector", qi_, qi_, t1, ALU.subtract, [dE], [dE])
    TT(k, "vector", den, lr, lr, ALU.mult, [dE], [dE])
    TT(k, "vector", t1, PAim, PAim, ALU.mult, [dE], [dE])
    TT(k, "vector", den, den, t1, ALU.add, [dE], [dE])
    k.S.op("vector", lambda e: e.reciprocal(out=den, in_=den), reads=[dE], writes=[dE])
    TT(k, "vector", fr, qr, den, ALU.mult, [dE], [dE])
    TT(k, "vector", fi, qi_, den, ALU.mult, [dE], [dE])
    BBre = A.f32(1024); BBim = A.f32(1024); tb_ = A.f32(1024)
    BBrev = r3(BBre, a=64); BBimv = r3(BBim, a=64); tbv = r3(tb_, a=64)
    s16 = [128, 64, 16]
    TT(k, "vector", BBrev, bc(fr, 2, s16), PBrev, ALU.mult, [dE, dPB], [dE])
    TT(k, "vector", tbv, bc(fi, 2, s16), PBimv, ALU.mult, [dE, dPB], [dE])
    TT(k, "vector", BBre, BBre, tb_, ALU.subtract, [dE], [dE])
    TT(k, "vector", BBimv, bc(fr, 2, s16), PBimv, ALU.mult, [dE, dPB], [dE])
    TT(k, "vector", tbv, bc(fi, 2, s16), PBrev, ALU.mult, [dE, dPB], [dE])
    TT(k, "vector", BBim, BBim, tb_, ALU.add, [dE], [dE])
    if getattr(k, 's5_stop', '') == 'P2':
        barrier(k)
        return
    PB_ = 8
    PM = A.f32(2); dPM = Dep()
    k.S.op("vector", lambda e: e.memset(PM, 0.0), reads=[], writes=[dPM])
    k.S.op("vector", lambda e: e.memset(PM[0:64, 0:1], 1.0), reads=[dPM], writes=[dPM])
    k.S.op("vector", lambda e: e.memset(PM[64:128, 1:2], 1.0), reads=[dPM], writes=[dPM])
    LM = [[A.bf16(1024), A.bf16(1024)], [A.bf16(1024), A.bf16(1024)]]
    T = [A.f32(1024) for _ in range(4)]
    Tv = [t_.rearrange("p (j s q) -> p j s q", j=PB_, s=8) for t_ in T]
    RREb = A.bf16(1024); RIMb = A.bf16(1024)
    L2RE = A.f32(1024); L2IM = A.f32(1024)
    W2o = A.bf16(4096)
    W2ov = W2o.rearrange("p (j r m) -> p j r m", j=PB_, r=4)
    Mout = [A.bf16(512), A.bf16(512)]; dMout = [Dep(), Dep()]
    TAB = [A.bf16(512), A.bf16(512)]; dTAB = [Dep(), Dep()]
    for tb2 in TAB:
        k.S.op("vector", lambda e, tb2=tb2: e.memset(tb2, 0.0), reads=[], writes=[dE])
    dCh = Dep()
    s4 = [128, PB_, 8, 16]
    j8 = lambda t_: r3(t_, a=PB_)

    def products(blk, Bre, Bim, j0):
        a0 = blk * 8
        Ar = AREv[:, j0:j0 + PB_, a0:a0 + 8]; Ai = AIMv[:, j0:j0 + PB_, a0:a0 + 8]
        br = Bre[:, j0:j0 + PB_, :]; bi = Bim[:, j0:j0 + PB_, :]
        TT(k, "vector", Tv[0], bc(Ar, 3, s4), bc(br, 2, s4), ALU.mult, [dE, dPC, dCh], [dCh])
        TT(k, "vector", Tv[1], bc(Ai, 3, s4), bc(bi, 2, s4), ALU.mult, [dE, dPC, dCh], [dCh])
        TT(k, "vector", Tv[2], bc(Ai, 3, s4), bc(br, 2, s4), ALU.mult, [dE, dPC, dCh], [dCh])
        TT(k, "vector", Tv[3], bc(Ar, 3, s4), bc(bi, 2, s4), ALU.mult, [dE, dPC, dCh], [dCh])

    mcnt = 0
    for ch in range(64 // PB_):
        j0 = ch * PB_
        products(0, BBrev, BBimv, j0)
        TT(k, "vector", T[0], T[0], T[1], ALU.subtract, [dCh], [dCh])
        TT(k, "vector", T[2], T[2], T[3], ALU.add, [dCh], [dCh])
        for g2 in range(2):
            TS(k, "vector", LM[g2][0], T[0], PM[:, g2:g2 + 1], None, ALU.mult, None, [dCh, dPM], [dCh])
            TS(k, "vector", LM[g2][1], T[2], PM[:, g2:g2 + 1], None, ALU.mult, None, [dCh, dPM], [dCh])
        products(2, PCrev, PCimv, j0)
        TT(k, "vector", RREb, T[0], T[1], ALU.subtract, [dCh], [dCh])
        STT(k, "vector", RIMb, T[2], -1.0, T[3], ALU.mult, ALU.subtract, [dCh], [dCh])
        for half in range(PB_ // 2):
            bank = mcnt % 2
            mo, dmo = Mout[mcnt % 2], dMout[mcnt % 2]
            mcnt += 1
            ps, dps = k.ps[bank], k.dps[bank]
            for q in range(4):
                jj = half * 2 + q // 2
                g2 = q % 2
                MM(k, ps[:, q * 128:(q + 1) * 128], j8(LM[g2][0])[:, jj, :], j8(RREb)[:, jj, :], True, False, [dCh], [dps], False)
                MM(k, ps[:, q * 128:(q + 1) * 128], j8(LM[g2][1])[:, jj, :], j8(RIMb)[:, jj, :], False, True, [dCh], [dps], q == 3)
            TT(k, "vector", r3(mo, a=4), r3(ps, a=4), bc(MASK, 1, [128, 4, 128]), ALU.mult, [dps, dMASK], [dmo])
            g0 = (j0 + half * 2) * 2
            for q in range(4):
                DMA(k, "gpsimd", M_d[g0 + q], mo[:, q * 128:(q + 1) * 128], [dmo], [dM])
        if getattr(k, 's5_stop', '') == 'P3a':
            continue
        products(1, BBrev, BBimv, j0)
        TT(k, "vector", L2RE, T[0], T[1], ALU.subtract, [dCh], [dCh])
        TT(k, "vector", L2IM, T[2], T[3], ALU.add, [dCh], [dCh])
        for jj in range(PB_):
            bank = 2 + jj % 2
            ps, dps = k.ps[bank], k.dps[bank]
            tab, dtab = TAB[jj % 2], dTAB[jj % 2]
            TR(k, ps[:, 0:128], j8(L2RE)[:, jj, :], k.ident, [dCh], [dps], False)
            TR(k, ps[:, 128:256], j8(L2IM)[:, jj, :], k.ident, [dCh], [dps], True)
            tabv = tab.rearrange("p (r a m) -> p r a m", r=2, a=2)
            psv = ps[:, 0:256].rearrange("p (r m) -> p r m", r=2)
            CP(k, "vector", tabv[:, :, 0, 0:64], psv[:, :, 0:64], [dps], [dtab])
            CP(k, "vector", tabv[:, :, 1, 64:128], psv[:, :, 64:128], [dps], [dtab])
            DMA(k, "gpsimd", W1_d[j0 + jj], tab, [dtab], [dW1])
        if getattr(k, 's5_stop', '') == 'P3b':
            continue
        products(3, PCrev, PCimv, j0)
        TT(k, "vector", T[0], T[0], T[1], ALU.subtract, [dCh], [dCh])
        STT(k, "vector", T[2], T[2], -1.0, T[3], ALU.mult, ALU.subtract, [dCh], [dCh])
        for g2 in range(2):
            TS(k, "vector", W2ov[:, :, 2 * g2, :], j8(T[0]), PM[:, g2:g2 + 1], None, ALU.mult, None, [dCh, dPM], [dCh])
            TS(k, "vector", W2ov[:, :, 2 * g2 + 1, :], j8(T[2]), PM[:, g2:g2 + 1], None, ALU.mult, None, [dCh, dPM], [dCh])
        for jj in range(PB_):
            DMA(k, "gpsimd", W2_d[j0 + jj], W2o[:, jj * 512:(jj + 1) * 512], [dCh], [dW2, dCh])
    barrier(k)
    A.reset(mP)
    if getattr(k, 's5_stop', '') in ('P', 'P3a', 'P3b'):
        return
    R1 = A.bf16(16 * 2048)
    R1v = R1.rearrange("p (b s c) -> p b s c", b=16, s=8)
    dR1 = Dep()
    mR2 = A.mark()
    HT = A.bf16(NT * 2048); HTv = HT.rearrange("p (t c x) -> p t c x", t=NT, c=16); dHT = Dep()
    for t in range(NT):
        DMA(k, "sync", HTv[:, t].rearrange("p c x -> p (c x)"), k.hT[t], [k.d_hT[t]], [dHT])
    stg = Stager(k, 3, 2048)
    wcb = [A.bf16(2048), A.bf16(2048)]; dwcb = [Dep(), Dep()]
    cnt = 0
    for chb in range(16):
        b = chb % 2
        stg.load(W["s5_w_in"][sj].rearrange("(kc p) n -> p kc n", p=128)[:, :, chb * 128:(chb + 1) * 128], r3(wcb[b], a=16), dwcb[b])
        for sl_ in range(4):
            ps, dps = k.ps[cnt % 4], k.dps[cnt % 4]
            cnt += 1
            for kc in range(16):
                MM(k, ps, r3(wcb[b], a=16)[:, kc, :], HTv[:, sl_ * 4:(sl_ + 1) * 4, kc, :], kc == 0, kc == 15, [dwcb[b], dHT], [dps], kc == 15)
            src = ps.rearrange("p (c s) -> p s c", s=8)
            dst = R1v[:, chb, :, sl_ * 64:(sl_ + 1) * 64]
            if cnt % 2 == 0:
                ACT(k, dst, src, AF.Copy, [dps], [dR1])
            else:
                CP(k, "vector", dst, src, [dps], [dR1])
    barrier(k)
    A.reset(mR2)
    if getattr(k, 's5_stop', '') == 'U':
        return
    R2 = A.bf16(128 * 256)
    Xv = r3(R2, a=128)
    dX = Dep()
    SEL = A.bf16(8192); SELv = r3(SEL, a=64); SELT = A.bf16(8192); SELTv = r3(SELT, a=64); dSEL = Dep()
    DMA(k, "sync", SEL, k.c["c_sel"], [], [dSEL])
    DMA(k, "sync", SELT, k.c["c_selT"], [], [dSEL])
    for g0 in range(0, 128, 2):
        bank = (g0 // 2) % 4
        ps, dps = k.ps[bank], k.dps[bank]
        for gi in range(2):
            g = g0 + gi
            for s_ in range(8):
                MM(k, ps[:, gi * 256:(gi + 1) * 256], SELv[:, (g % 8) * 8 + s_, :], R1v[:, g // 8, s_, :], s_ == 0, s_ == 7, [dSEL, dR1], [dps], (gi == 1 and s_ == 7))
        if (g0 // 2) % 2 == 0:
            ACT(k, Xv[:, g0:g0 + 2, :], r3(ps, a=2), AF.Copy, [dps], [dX])
        else:
            CP(k, "vector", Xv[:, g0:g0 + 2, :], r3(ps, a=2), [dps], [dX])
    barrier(k)
    if getattr(k, 's5_stop', '') == 'X':
        return
    mL = A.mark()
    dZF = Dep()
    Mg = [A.bf16(256) for _ in range(4)]; W1t = [A.bf16(512), A.bf16(512)]; W2t = [A.bf16(512) for _ in range(4)]
    dMg = [Dep() for _ in range(4)]; dW1t = [Dep(), Dep()]; dW2t = [Dep() for _ in range(4)]
    REs = [[A.f32(384), A.f32(384)] for _ in range(2)]; IMs = [[A.f32(384), A.f32(384)] for _ in range(2)]
    dSCs = [Dep(), Dep()]
    for sl_ in range(2):
        for t_ in REs[sl_] + IMs[sl_]:
            k.S.op("vector", lambda e, t_=t_: e.memset(t_, 0.0), reads=[], writes=[dSCs[sl_]])
    HREs = [A.bf16(256), A.bf16(256)]; HIMs = [A.bf16(256), A.bf16(256)]; dHs = [Dep(), Dep()]
    ybs = [[A.f32(256), A.f32(256)] for _ in range(2)]; y2bs = [[A.f32(256), A.f32(256)] for _ in range(2)]
    dybs = [[Dep(), Dep()], [Dep(), Dep()]]
    Zall = [A.bf16(8 * 256), A.bf16(8 * 256)]; dZall = [Dep(), Dep()]

    def st_S(j):
        b = j % 2
        b4 = j % 4
        for g2 in range(2):
            DMA(k, "sync", Mg[b4][:, g2 * 128:(g2 + 1) * 128], M_d[2 * j + g2], [dM], [dMg[b4]])
        DMA(k, "sync", W1t[b], W1_d[j], [dW1], [dW1t[b]])
        DMA(k, "sync", W2t[b4], W2_d[j], [dW2], [dW2t[b4]])
        ps, dps = k.ps[b], k.dps[b]
        for ri in range(2):
            MM(k, ps[:, ri * 256:(ri + 1) * 256], W1t[b][:, (2 * ri) * 128:(2 * ri + 1) * 128], Xv[:, 2 * j, :], True, False, [dW1t[b], dX], [dps], False)
            MM(k, ps[:, ri * 256:(ri + 1) * 256], W1t[b][:, (2 * ri + 1) * 128:(2 * ri + 2) * 128], Xv[:, 2 * j + 1, :], False, True, [dW1t[b], dX], [dps], ri == 1)
        ACT(k, REs[b][0][:, 128:384], ps[:, 0:256], AF.Copy, [dps], [dSCs[b]])
        ACT(k, IMs[b][0][:, 128:384], ps[:, 256:512], AF.Copy, [dps], [dSCs[b]])

    def st_scan_step(j, i):
        b = j % 2
        cur = i % 2
        sft = 1 << i
        ra, ia = REs[b][cur], IMs[b][cur]
        rb_, ib_ = REs[b][1 - cur], IMs[b][1 - cur]
        ar = ASrv[:, j, i:i + 1]; ai = ASiv[:, j, i:i + 1]; an = ASnv[:, j, i:i + 1]
        d_ = dSCs[b]
        STT(k, "vector", rb_[:, 128:384], ra[:, 128 - sft:384 - sft], ar, ra[:, 128:384], ALU.mult, ALU.add, [d_, dAS], [d_])
        STT(k, "vector", rb_[:, 128:384], ia[:, 128 - sft:384 - sft], an, rb_[:, 128:384], ALU.mult, ALU.add, [d_, dAS], [d_])
        STT(k, "vector", ib_[:, 128:384], ia[:, 128 - sft:384 - sft], ar, ia[:, 128:384], ALU.mult, ALU.add, [d_, dAS], [d_])
        STT(k, "vector", ib_[:, 128:384], ra[:, 128 - sft:384 - sft], ai, ib_[:, 128:384], ALU.mult, ALU.add, [d_, dAS], [d_])

    def st_cast(j):
        b = j % 2
        ACT(k, HREs[b], REs[b][0][:, 127:383], AF.Copy, [dSCs[b]], [dHs[b]])
        ACT(k, HIMs[b], IMs[b][0][:, 127:383], AF.Copy, [dSCs[b]], [dHs[b]])

    def st_Y(j):
        b = j % 2
        for g2 in range(2):
            g = 2 * j + g2
            py, dpy = k.ps[2 + g2], k.dps[2 + g2]
            b4 = j % 4
            MM(k, py[:, 0:256], r3(Mg[b4], a=2)[:, g2, :], Xv[:, g, :], True, False, [dMg[b4], dX], [dpy], False)
            MM(k, py[:, 0:256], W2t[b4][:, (2 * g2) * 128:(2 * g2 + 1) * 128], HREs[b], False, False, [dW2t[b4], dHs[b]], [dpy], False)
            MM(k, py[:, 0:256], W2t[b4][:, (2 * g2 + 1) * 128:(2 * g2 + 2) * 128], HIMs[b], False, True, [dW2t[b4], dHs[b]], [dpy], True)
            y = ybs[b][g2]; y2 = y2bs[b][g2]; dy_ = dybs[b][g2]
            STT(k, "vector", y, Xv[:, g, :], DCOL[:, g:g + 1], py[:, 0:256], ALU.mult, ALU.add, [dpy, dX, dDCOL], [dy_])
            TT(k, "gpsimd", y2, y, y, ALU.mult, [dy_], [dy_])
            TS(k, "gpsimd", y2, y2, 0.044715, 1.0, ALU.mult, ALU.add, [dy_], [dy_])
            TT(k, "gpsimd", y2, y2, y, ALU.mult, [dy_], [dy_])
            ACT(k, y2, y2, AF.Sigmoid, [dy_], [dy_], scale=1.5957691216057308)
            zb = (g // 8) % 2
            TT(k, "gpsimd", r3(Zall[zb], a=8)[:, g % 8, :], y, y2, ALU.mult, [dy_, dZall[zb]], [dZall[zb]])
        if j % 4 == 3:
            chb = j // 4
            zb = chb % 2
            for sp in range(4):
                bank = 4 + sp % 4
                ps2, dps2 = k.ps[bank], k.dps[bank]
                for si in range(2):
                    s_ = sp * 2 + si
                    for g8 in range(8):
                        MM(k, ps2[:, si * 256:(si + 1) * 256], SELTv[:, g8 * 8 + s_, :], r3(Zall[zb], a=8)[:, g8, :], g8 == 0, g8 == 7, [dSEL, dZall[zb]], [dps2], (si == 1 and g8 == 7))
                if sp % 2 == 0:
                    ACT(k, R1v[:, chb, sp * 2:sp * 2 + 2, :], r3(ps2, a=2), AF.Copy, [dps2], [dZF])
                else:
                    CP(k, "vector", R1v[:, chb, sp * 2:sp * 2 + 2, :], r3(ps2, a=2), [dps2], [dZF])

    for c in range(0, 64, 2):
        st_S(c)
        st_S(c + 1)
        if c >= 2:
            st_Y(c - 2)
            st_Y(c - 1)
        for i in range(8):
            st_scan_step(c, i)
            st_scan_step(c + 1, i)
        st_cast(c)
        st_cast(c + 1)
    st_Y(62)
    st_Y(63)
    barrier(k)
    A.reset(mR2)
    if getattr(k, 's5_stop', '') == 'L':
        return
    Z2N = A.bf16(NT * 2048)
    Z2Nv = Z2N.rearrange("p (t c l s) -> p t c l s", t=NT, c=16, l=16)
    dZ2 = Dep()
    stg = Stager(k, 3, 2048)
    wgb = [A.bf16(2048), A.bf16(2048)]; dwgb = [Dep(), Dep()]
    sg = [A.f32(512), A.f32(512)]; dsg = [Dep(), Dep()]
    cnt = 0
    for nb in range(16):
        b = nb % 2
        stg.load(W["s5_w_glu"][sj].rearrange("(kc p) n -> p kc n", p=128)[:, :, nb * 128:(nb + 1) * 128], r3(wgb[b], a=16), dwgb[b])
        for q in range(4):
            ps, dps = k.ps[cnt % 4], k.dps[cnt % 4]
            sb_ = cnt % 2
            cnt += 1
            zsl = lambda kc: R1v[:, kc, 2 * q:2 * q + 2, :]
            for kc in range(16):
                MM(k, ps, r3(wgb[b], a=16)[:, kc, :], zsl(kc), kc == 0, kc == 15, [dwgb[b], dZF], [dps], kc == 15)
            ACT(k, sg[sb_], ps, AF.Sigmoid, [dps], [dsg[sb_]])
            dst = Z2Nv[:, :, nb, :, 2 * q:2 * q + 2].rearrange("p t l s -> p s t l")
            in0 = sg[sb_].rearrange("p (s t l) -> p s t l", s=2, t=16)
            in1 = R1v[:, nb, 2 * q:2 * q + 2, :].rearrange("p s (t l) -> p s t l", t=16)
            TT(k, "vector", dst, in0, in1, ALU.mult, [dsg[sb_], dZF], [dZ2])
    for t in range(NT):
        DMA(k, "gpsimd", ZT_d[t], Z2N[:, t * 2048:(t + 1) * 2048], [dZ2], [d_ZT[t]])
    barrier(k)
    A.reset(m_phase)
    phase_outproj_ln(k, ZT_d, d_ZT, W["s5_w_out"][sj], W["ln_mix_g"][li], W["ln_mix_b"][li], h_in, h_out)
W_SPECS = [
    ("rel_bias", [32, 16]),
    ("s5_w_in", [2, 2048, 2048]), ("s5_a_re", [2, 128, 64]), ("s5_a_im", [2, 128, 64]), ("s5_log_dt", [2, 128]),
    ("s5_b_re", [2, 128, 64, 16]), ("s5_b_im", [2, 128, 64, 16]), ("s5_c_re", [2, 128, 16, 64]), ("s5_c_im", [2, 128, 16, 64]),
    ("s5_d", [2, 128, 16]), ("s5_w_glu", [2, 2048, 2048]), ("s5_w_out", [2, 2048, 2048]),
    ("dsa_w_in", [2, 2048, 1168]), ("dsa_q_norm", [2, 512]), ("dsa_kv_norm", [2, 512]),
    ("dsa_w_uq", [2, 512, 2048]), ("dsa_w_qidx", [2, 512, 2048]), ("dsa_w_uk", [2, 16, 128, 512]),
    ("dsa_w_uv", [2, 16, 512, 128]), ("dsa_w_out", [2, 2048, 2048]),
    ("moe_w_group", [4, 2048, 4]), ("moe_b_group", [4, 4]), ("moe_w_expert", [4, 2048, 32]), ("moe_b_expert", [4, 32]),
    ("moe_w_gate", [4, 32, 2048, 256]), ("moe_w_up", [4, 32, 2048, 256]), ("moe_w_down", [4, 32, 256, 2048]),
    ("ln_mix_g", [4, 2048]), ("ln_mix_b", [4, 2048]), ("ln_ffn_g", [4, 2048]), ("ln_ffn_b", [4, 2048]),
]


def host_consts():
    c = {}
    c["c_ident"] = np.eye(128, dtype=np.float32)
    c.update(dsa_consts())
    c.update(s5_consts())
    return c


def build_nc(mode="full", used=None, **kw):
    nc = bass.Bass("TRN2", target_bir_lowering=False)
    k = K()
    for a_, b_ in kw.items():
        setattr(k, a_, b_)
    k.nc = nc
    k.x = nc.dram_tensor("x", [L, D], F32, kind="ExternalInput").ap()
    k.w = {}
    for name, shp in W_SPECS:
        if used is not None and name not in used:
            continue
        k.w[name] = nc.dram_tensor(name, getattr(k, 'wshape', {}).get(name, shp), F32, kind="ExternalInput").ap()
    k.c = {}
    for name, arr in host_consts().items():
        k.c[name] = nc.dram_tensor(name, list(arr.shape), F32 if arr.dtype == np.float32 else BF16, kind="ExternalInput").ap()
    k.out = nc.dram_tensor("out", [L, D], F32, kind="ExternalOutput").ap()
    k.hA = nc.dram_tensor("hA", [L, D], F32).ap()
    k.hB = nc.dram_tensor("hB", [L, D], F32).ap()
    k.hT = nc.dram_tensor("hT", [NT, 128, 16 * 128], BF16).ap()
    k.s5_M = nc.dram_tensor("s5_M", [128, 128, 128], BF16).ap()
    k.s5_W1 = nc.dram_tensor("s5_W1", [64, 128, 512], BF16).ap()
    k.s5_W2 = nc.dram_tensor("s5_W2", [64, 128, 512], BF16).ap()
    k.s5_ZT = nc.dram_tensor("s5_ZT", [NT, 128, 16 * 128], BF16).ap()
    k.dsa_QT = nc.dram_tensor("dsa_QT", [128, 16, L], BF16).ap()
    k.dsa_QIT = nc.dram_tensor("dsa_QIT", [128, 16, L], BF16).ap()
    k.dsa_OT = nc.dram_tensor("dsa_OT", [NT, 128, 16 * 128], BF16).ap()
    k.dsa_KT = nc.dram_tensor("dsa_KT", [16, 128, L], BF16).ap()
    k.dsa_V = nc.dram_tensor("dsa_V", [16, 128, L], BF16).ap()
    k.d_h = [Dep() for _ in range(NT)]
    k.d_hT = [Dep() for _ in range(NT)]
    with ExitStack() as st:
        k.S = Sched(nc, st)
        k.A = Arena(nc, st, 53000)
        k.ps = []
        k.dps = []
        for i in range(8):
            k.ps.append(st.enter_context(nc.psum_tensor("ps%d" % i, [128, 512], F32))[:])
            k.dps.append(Dep())
        A = k.A
        k.ident = A.f32(128)
        d_id = Dep()
        k.S.dma("sync", lambda e: e.dma_start(out=k.ident, in_=k.c["c_ident"]), writes=[d_id])
        k.d_hTt = [Dep(), Dep()]
        k.hTt_pos = 0
        barrier(k)
        if mode == "dsa_only":
            phase_prep(k)
            phase_dsa(k, 0, 1, k.x, k.out)
        elif mode == "s5_only":
            phase_prep(k)
            phase_s5(k, 0, 0, k.x, k.out)
        elif mode == "moe_only":
            phase_prep(k)
            phase_moe(k, 0, k.x, k.out, write_hT=False)
        elif mode == "full":
            phase_prep(k)
            h_in = k.x
            bufs = [k.hA, k.hB]
            bi = 0
            for li in range(DEPTH):
                hm = bufs[bi]; bi ^= 1
                if li % 2 == 0:
                    phase_s5(k, li // 2, li, h_in, hm)
                else:
                    phase_dsa(k, li // 2, li, h_in, hm)
                last = (li == DEPTH - 1)
                hf = k.out if last else bufs[bi]
                bi ^= 1
                phase_moe(k, li, hm, hf, write_hT=not last)
                h_in = hf
        k.S.finish(k.d_h)
        k.S.emit()
    return nc


def kernel(**inputs):
    nc = build_nc("full")
    consts = host_consts()
    x = np.ascontiguousarray(inputs["x"], dtype=np.float32)
    shared = {name: np.ascontiguousarray(inputs[name], dtype=np.float32) for name, _ in W_SPECS}
    shared.update(consts)
    in_maps = []
    for c in range(8):
        m = dict(shared)
        m["x"] = x[c]
        in_maps.append(m)
    res = run_bass_kernel_spmd(nc, in_maps, core_ids=list(range(8)))
    return np.stack([np.asarray(r["out"], dtype=np.float32) for r in res.results], axis=0)
```

```python
from concourse.bass_utils import run_bass_kernel_spmd
import numpy as np
import concourse.bass as bass
import concourse.mybir as mybir
from contextlib import ExitStack

F32 = mybir.dt.float32
BF16 = mybir.dt.bfloat16
I32 = mybir.dt.int32
ALU = mybir.AluOpType
AF = mybir.ActivationFunctionType
AX = mybir.AxisListType

ENGINES = ("tensor", "vector", "scalar", "gpsimd", "sync")
DMA_RING = 8


class Dep:
    __slots__ = ("name", "w", "r")

    def __init__(self, name=""):
        self.name = name
        self.w = None
        self.r = {}


class Sched:
    def __init__(self, nc, stack, same_engine_sync=True):
        self.nc = nc
        self.stack = stack
        self.streams = {e: [] for e in ENGINES}
        self.count = {e: 0 for e in ENGINES}
        self.seen = {e: {} for e in ENGINES}
        self.sems = {}
        for e in ENGINES:
            self.sems[e] = stack.enter_context(nc.semaphore("s_" + e))
        self.ring = {}
        self.ring_cnt = {}
        self.ring_pos = {}
        for q in ("sync", "gpsimd", "scalar"):
            self.ring[q] = []
            for i in range(DMA_RING):
                key = "d_%s_%d" % (q, i)
                self.sems[key] = stack.enter_context(nc.semaphore(key))
                self.ring[q].append(key)
            self.ring_cnt[q] = [0] * DMA_RING
            self.ring_pos[q] = 0
        self.same_engine_sync = same_engine_sync
        self.out_deps = []

    def _collect(self, reads, writes):
        need = {}

        def add(kv):
            if kv is None:
                return
            k, v = kv
            if need.get(k, 0) < v:
                need[k] = v
        for d in reads:
            add(d.w)
        for d in writes:
            add(d.w)
            for k, v in d.r.items():
                add((k, v))
        return need

    def _waits(self, eng, need, skip_self):
        ws = []
        seen = self.seen[eng]
        for k, v in need.items():
            if k == eng and skip_self:
                continue
            if seen.get(k, 0) >= v:
                continue
            seen[k] = v
            ws.append((k, v))
        return ws

    def _update(self, reads, writes, ticket):
        k, v = ticket
        for d in writes:
            d.w = ticket
            d.r = {}
        for d in reads:
            if d.r.get(k, 0) < v:
                d.r[k] = v

    def op(self, eng, fn, reads=(), writes=(), inc=True):
        need = self._collect(reads, writes)
        skip_self = (eng == "tensor") or (not self.same_engine_sync)
        ws = self._waits(eng, need, skip_self)
        if inc:
            self.count[eng] += 1
            ticket = (eng, self.count[eng])
        else:
            ticket = (eng, self.count[eng] + 1)
        self.streams[eng].append((ws, fn, (eng, 1) if inc else None))
        self._update(reads, writes, ticket)
        return ticket

    def dma(self, q, fn, reads=(), writes=()):
        need = self._collect(reads, writes)
        pos = self.ring_pos[q]
        self.ring_pos[q] = (pos + 1) % DMA_RING
        key = self.ring[q][pos]
        prev = self.ring_cnt[q][pos]
        if prev > 0:
            if need.get(key, 0) < prev * 16:
                need[key] = prev * 16
        ws = self._waits(q, need, False)
        self.ring_cnt[q][pos] = prev + 1
        ticket = (key, (prev + 1) * 16)
        self.streams[q].append((ws, fn, (key, 16)))
        self._update(reads, writes, ticket)
        return ticket

    def finish(self, deps):
        need = self._collect(deps, deps)
        ws = self._waits("sync", need, False)
        self.streams["sync"].append((ws, None, None))

    def emit(self):
        nc = self.nc
        sems = self.sems
        streams = self.streams

        def run(engh, name):
            for ws, fn, inc in streams[name]:
                for k, v in ws:
                    engh.wait_ge(sems[k], v)
                if fn is not None:
                    ins = fn(engh)
                    if inc is not None:
                        ins.then_inc(sems[inc[0]], inc[1])

        with nc.Block() as block:
            @block.tensor
            def _(e):
                run(e, "tensor")

            @block.vector
            def _(e):
                run(e, "vector")

            @block.scalar
            def _(e):
                run(e, "scalar")

            @block.gpsimd
            def _(e):
                run(e, "gpsimd")

            @block.sync
            def _(e):
                run(e, "sync")
D = 2048
L = 2048
NT = 16
DEPTH = 4
DN_ALPHA = (2 * DEPTH) ** 0.25
LN_EPS = 1e-5
RMS_EPS = 1e-6
NE = 32
FF = 256
NEG = -1.0e30


class Arena:
    def __init__(self, nc, stack, nelem):
        self.t = stack.enter_context(nc.sbuf_tensor("arena", [128, nelem], F32))
        self.n = nelem
        self.off = 0

    def mark(self):
        return self.off

    def reset(self, m):
        self.off = m

    def f32(self, n, shape=None):
        assert self.off + n <= self.n, ("arena overflow", self.off, n, self.n)
        v = self.t[:, self.off:self.off + n]
        self.off += n
        return v

    def bf16(self, n):
        m = (n + 1) // 2
        return self.f32(m).bitcast(BF16)[:, 0:n]


class K:
    pass


def r3(ap, **kw):
    return ap.rearrange("p (a b) -> p a b", **kw)


def barrier(k):
    S = k.S
    cur = {}
    for e in ENGINES:
        if S.count[e] > 0:
            cur[e] = S.count[e]
    for q in S.ring:
        for i, key in enumerate(S.ring[q]):
            if S.ring_cnt[q][i] > 0:
                cur[key] = S.ring_cnt[q][i] * 16
    for e in ENGINES:
        ws = S._waits(e, dict(cur), False)
        if ws:
            S.streams[e].append((ws, None, None))


def ln_load_params(k, g_ap, b_ap, gt, bt, dgb):
    S = k.S
    S.dma("sync", lambda e: e.dma_start(out=gt, in_=g_ap.rearrange("(o d) -> o d", o=1).partition_broadcast(128)), writes=[dgb])
    S.dma("sync", lambda e: e.dma_start(out=bt, in_=b_ap.rearrange("(o d) -> o d", o=1).partition_broadcast(128)), writes=[dgb])


def ln_stats(k, a, da, st, dst):
    S = k.S
    s1, s2, mean, var, rstd, junk = st
    S.op("scalar", lambda e: e.activation(out=junk, in_=a, func=AF.Identity, accum_out=s1), reads=[da], writes=[dst])
    S.op("scalar", lambda e: e.activation(out=junk, in_=a, func=AF.Square, accum_out=s2), reads=[da], writes=[dst])


def ln_norm(k, a, da, gt, bt, dgb, st, dst):
    S = k.S
    s1, s2, mean, var, rstd, junk = st
    S.op("vector", lambda e: e.tensor_scalar(out=mean, in0=s1, scalar1=1.0 / D, scalar2=None, op0=ALU.mult), reads=[dst], writes=[dst])
    S.op("vector", lambda e: e.tensor_tensor(out=var, in0=mean, in1=mean, op=ALU.mult), reads=[dst], writes=[dst])
    S.op("vector", lambda e: e.scalar_tensor_tensor(out=var, in0=s2, scalar=1.0 / D, in1=var, op0=ALU.mult, op1=ALU.subtract), reads=[dst], writes=[dst])
    S.op("vector", lambda e: e.tensor_scalar(out=var, in0=var, scalar1=LN_EPS, scalar2=None, op0=ALU.add), reads=[dst], writes=[dst])
    S.op("scalar", lambda e: e.activation(out=var, in_=var, func=AF.Sqrt), reads=[dst], writes=[dst])
    S.op("vector", lambda e: e.reciprocal(out=rstd, in_=var), reads=[dst], writes=[dst])
    S.op("vector", lambda e: e.tensor_scalar(out=a, in0=a, scalar1=mean, scalar2=rstd, op0=ALU.subtract, op1=ALU.mult), reads=[dst, da], writes=[da])
    S.op("vector", lambda e: e.tensor_tensor(out=a, in0=a, in1=gt, op=ALU.mult), reads=[da, dgb], writes=[da])
    S.op("vector", lambda e: e.tensor_tensor(out=a, in0=a, in1=bt, op=ALU.add), reads=[da, dgb], writes=[da])


def ln_out(k, a, da, tt, h_out, hT_out, write_hT=True):
    k.S.dma("gpsimd", lambda e: e.dma_start(out=h_out[tt * 128:(tt + 1) * 128, :], in_=a), reads=[da], writes=[k.d_h[tt]])
    if write_hT:
        emit_hT(k, a, da, tt, hT_out)


def ln_tile(k, a, da, gt, bt, dgb, tt, h_out, hT_out, st, dst, write_hT=True):
    ln_stats(k, a, da, st, dst)
    ln_norm(k, a, da, gt, bt, dgb, st, dst)
    ln_out(k, a, da, tt, h_out, hT_out, write_hT)


def emit_hT(k, a, da, tt, hT_out):
    S = k.S
    slot = k.hTt_pos
    k.hTt_pos = (slot + 1) % 2
    hTt, dhTt = k.hTt[slot], k.d_hTt[slot]
    for q in range(4):
        bank = 6 + (q % 2)
        ps, dps = k.ps[bank], k.dps[bank]
        for j in range(4):
            kc = q * 4 + j
            S.op("tensor", lambda e, kc=kc, j=j, ps=ps: e.transpose(ps[:, j * 128:(j + 1) * 128], a[:, kc * 128:(kc + 1) * 128], k.ident),
                 reads=[da], writes=[dps], inc=(j == 3))
        S.op("scalar", lambda e, q=q, ps=ps: e.activation(out=hTt[:, q * 512:(q + 1) * 512], in_=ps, func=AF.Copy),
             reads=[dps], writes=[dhTt])
    S.dma("gpsimd", lambda e: e.dma_start(out=hT_out[tt], in_=hTt), reads=[dhTt], writes=[k.d_hT[tt]])


def phase_prep(k):
    S = k.S
    A = k.A
    m = A.mark()
    k.hTt = [A.bf16(2048), A.bf16(2048)]
    xt = [A.f32(D), A.f32(D)]
    dxt = [Dep(), Dep()]
    for tt in range(NT):
        b = tt % 2
        S.dma("sync", lambda e, tt=tt, b=b: e.dma_start(out=xt[b], in_=k.x[tt * 128:(tt + 1) * 128, :]), writes=[dxt[b]])
        emit_hT(k, xt[b], dxt[b], tt, k.hT)
    barrier(k)
    A.reset(m)


def phase_moe(k, li, h_in, h_out, write_hT=True):
    S = k.S
    A = k.A
    m0 = A.mark()
    k.hTt = [A.bf16(2048), A.bf16(2048)]
    NH = 2
    TH = NT // NH
    HT = A.bf16(TH * 16 * 128)
    HTv = HT.rearrange("p (t c x) -> p t c x", t=TH, c=16)
    dHT = Dep()
    yacc = [A.f32(D) for _ in range(TH)]
    dy = [Dep() for _ in range(TH)]
    wg = A.bf16(16 * FF); wu = A.bf16(16 * FF); wd = A.bf16(2 * D)
    wgv = r3(wg, a=16); wuv = r3(wu, a=16); wdv = r3(wd, a=2)
    dwg = [Dep(), Dep()]; dwu = [Dep(), Dep()]; dwd = [Dep(), Dep()]
    NSTG = 3
    stg = [A.f32(2048) for _ in range(NSTG)]
    dstg = [Dep() for _ in range(NSTG)]
    gt = A.f32(D); bt = A.f32(D); dgb = Dep()
    hh2 = [A.bf16(2 * 2 * 512), A.bf16(2 * 2 * 512)]
    hhv2 = [h_.rearrange("p (t f x) -> p t f x", t=2, f=2) for h_ in hh2]
    dhh2 = [[[Dep(), Dep()], [Dep(), Dep()]], [[Dep(), Dep()], [Dep(), Dep()]]]
    wd_b = A.bf16(2 * D)
    wdv2 = [wdv, r3(wd_b, a=2)]
    dwd2 = [dwd, [Dep(), Dep()]]
    sl = [A.f32(512), A.f32(512)]
    dsl = [Dep(), Dep()]
    wr_s = A.f32(16 * 36); wr = A.bf16(16 * 36); dwr = Dep()
    wr_sv = r3(wr_s, a=16); wrv = r3(wr, a=16)
    rb = A.f32(36); drb = Dep()
    gates = A.f32(TH * NE); dgates = [Dep() for _ in range(TH)]
    gv = r3(gates, a=TH)
    RT = A.f32(704); drt = Dep()
    lnst_raw = A.f32(8 * TH + D)
    lnsts = [(lnst_raw[:, 8 * t_ + 0:8 * t_ + 1], lnst_raw[:, 8 * t_ + 1:8 * t_ + 2], lnst_raw[:, 8 * t_ + 2:8 * t_ + 3], lnst_raw[:, 8 * t_ + 3:8 * t_ + 4], lnst_raw[:, 8 * t_ + 4:8 * t_ + 5], lnst_raw[:, 8 * TH:8 * TH + D]) for t_ in range(TH)]
    dlnsts = [Dep() for _ in range(TH)]

    nc = k.nc
    S.dma("sync", lambda e: e.dma_start(out=wr_sv[:, :, 0:4], in_=k.w["moe_w_group"][li].rearrange("(c p) g -> p c g", p=128)), writes=[dwr])
    S.dma("sync", lambda e: e.dma_start(out=wr_sv[:, :, 4:36], in_=k.w["moe_w_expert"][li].rearrange("(c p) g -> p c g", p=128)), writes=[dwr])
    S.op("vector", lambda e: e.tensor_copy(out=wr, in_=wr_s), reads=[dwr], writes=[dwr])
    S.dma("sync", lambda e: e.dma_start(out=rb[:, 0:4], in_=k.w["moe_b_group"][li].rearrange("(o g) -> o g", o=1).partition_broadcast(128)), writes=[drb])
    S.dma("sync", lambda e: e.dma_start(out=rb[:, 4:36], in_=k.w["moe_b_expert"][li].rearrange("(o g) -> o g", o=1).partition_broadcast(128)), writes=[drb])
    ln_load_params(k, k.w["ln_ffn_g"][li], k.w["ln_ffn_b"][li], gt, bt, dgb)

    stg_pos = [0, 0]

    def load_cast(src_ap, dst_ap, ddst, eng):
        i = stg_pos[0]
        stg_pos[0] = (i + 1) % NSTG
        s = stg[i]
        sv = s if len(src_ap.shape) == 2 else r3(s, a=src_ap.shape[1])
        if not getattr(k, 'skip_wdma', False) or stg_pos[1] < 8:
            S.dma("sync", lambda e: e.dma_start(out=sv, in_=src_ap), writes=[dstg[i]])
        stg_pos[1] += 1
        if eng == "scalar":
            S.op(eng, lambda e: e.activation(out=dst_ap, in_=sv, func=AF.Copy), reads=[dstg[i]], writes=[ddst])
        else:
            S.op(eng, lambda e: e.tensor_copy(out=dst_ap, in_=sv), reads=[dstg[i]], writes=[ddst])

    wgate = k.w["moe_w_gate"][li]
    wup = k.w["moe_w_up"][li]
    wdown = k.w["moe_w_down"][li]

    for half in range(NH):
        t0 = half * TH
        for t in range(TH):
            S.dma("sync", lambda e, t=t, t0=t0: e.dma_start(out=HTv[:, t].rearrange("p c x -> p (c x)"), in_=k.hT[t0 + t]),
                  reads=[k.d_hT[t0 + t]], writes=[dHT])
        for t in range(TH):
            S.dma("sync", lambda e, t=t, t0=t0: e.dma_start(out=yacc[t], in_=h_in[(t0 + t) * 128:(t0 + t + 1) * 128, :]),
                  reads=[k.d_h[t0 + t]], writes=[dy[t]])
            S.op("scalar", lambda e, t=t: e.activation(out=yacc[t], in_=yacc[t], func=AF.Copy, scale=DN_ALPHA),
                 reads=[dy[t]], writes=[dy[t]])
        ps, dps = k.ps[6], k.dps[6]
        for t in range(TH):
            for kc in range(16):
                MM(k, ps[:, t * 36:(t + 1) * 36], HTv[:, t, kc, :], wrv[:, kc, :], kc == 0, kc == 15, [dHT, dwr], [dps], (t == TH - 1 and kc == 15))
        lg3 = RT[:, 0:TH * 36].rearrange("p (t x) -> p t x", t=TH)
        o_ = TH * 36
        gmax = RT[:, o_:o_ + TH]; o_ += TH
        gsum = RT[:, o_:o_ + TH]; o_ += TH
        gp = RT[:, o_:o_ + TH]; o_ += TH
        m1 = RT[:, o_:o_ + TH]; o_ += TH
        m2 = RT[:, o_:o_ + TH]; o_ += TH
        den = RT[:, o_:o_ + TH]; o_ += TH
        gexp3 = RT[:, o_:o_ + TH * 4].rearrange("p (t x) -> p t x", t=TH); o_ += TH * 4
        gone3 = RT[:, o_:o_ + TH * 4].rearrange("p (t x) -> p t x", t=TH); o_ += TH * 4
        coef3 = RT[:, o_:o_ + TH * 4].rearrange("p (t x) -> p t x", t=TH); o_ += TH * 4
        elc3 = RT[:, o_:o_ + TH * 8].rearrange("p (t x) -> p t x", t=TH); o_ += TH * 8
        tmp3 = RT[:, o_:o_ + TH * 8].rearrange("p (t x) -> p t x", t=TH); o_ += TH * 8
        ew3 = RT[:, o_:o_ + TH * 8].rearrange("p (t x) -> p t x", t=TH); o_ += TH * 8
        sel3 = RT[:, o_:o_ + TH * 8].rearrange("p (t x) -> p t x", t=TH); o_ += TH * 8
        assert o_ <= 704
        s4_ = [128, TH, 4]; s8_ = [128, TH, 8]
        rd = [drt]; wr_ = [drt]
        TT(k, "vector", lg3, ps[:, 0:TH * 36].rearrange("p (t x) -> p t x", t=TH), rb.unsqueeze(1).to_broadcast([128, TH, 36]), ALU.add, [dps, drb, drt], wr_)
        k.S.op("vector", lambda e: e.tensor_reduce(out=gmax, in_=lg3[:, :, 0:4], axis=AX.X, op=ALU.max), reads=rd, writes=wr_)
        TT(k, "vector", gexp3, lg3[:, :, 0:4], gmax.unsqueeze(2).to_broadcast(s4_), ALU.subtract, rd, wr_)
        ACT(k, gexp3, gexp3, AF.Exp, rd, wr_)
        k.S.op("vector", lambda e: e.tensor_reduce(out=gsum, in_=gexp3, axis=AX.X, op=ALU.add), reads=rd, writes=wr_)
        k.S.op("vector", lambda e: e.reciprocal(out=gp, in_=gsum), reads=rd, writes=wr_)
        TT(k, "vector", gone3, lg3[:, :, 0:4], gmax.unsqueeze(2).to_broadcast(s4_), ALU.is_equal, rd, wr_)
        for g in range(4):
            dst_ = elc3 if g == 0 else tmp3
            TT(k, "vector", dst_, lg3[:, :, 4 + 8 * g:12 + 8 * g], gone3[:, :, g].unsqueeze(2).to_broadcast(s8_), ALU.mult, rd, wr_)
            if g > 0:
                TT(k, "vector", elc3, elc3, tmp3, ALU.add, rd, wr_)
        k.S.op("vector", lambda e: e.tensor_reduce(out=m1, in_=elc3, axis=AX.X, op=ALU.max), reads=rd, writes=wr_)
        TT(k, "vector", tmp3, elc3, m1.unsqueeze(2).to_broadcast(s8_), ALU.is_equal, rd, wr_)
        STT(k, "vector", tmp3, tmp3, NEG, elc3, ALU.mult, ALU.add, rd, wr_)
        k.S.op("vector", lambda e: e.tensor_reduce(out=m2, in_=tmp3, axis=AX.X, op=ALU.max), reads=rd, writes=wr_)
        TT(k, "vector", ew3, elc3, m1.unsqueeze(2).to_broadcast(s8_), ALU.subtract, rd, wr_)
        ACT(k, ew3, ew3, AF.Exp, rd, wr_)
        TT(k, "vector", sel3, elc3, m2.unsqueeze(2).to_broadcast(s8_), ALU.is_ge, rd, wr_)
        TT(k, "vector", ew3, ew3, sel3, ALU.mult, rd, wr_)
        k.S.op("vector", lambda e: e.tensor_reduce(out=den, in_=ew3, axis=AX.X, op=ALU.add), reads=rd, writes=wr_)
        k.S.op("vector", lambda e: e.reciprocal(out=den, in_=den), reads=rd, writes=wr_)
        TT(k, "vector", ew3, ew3, den.unsqueeze(2).to_broadcast(s8_), ALU.mult, rd, wr_)
        TT(k, "vector", coef3, gone3, gp.unsqueeze(2).to_broadcast(s4_), ALU.mult, rd, wr_)
        for g in range(4):
            TT(k, "vector", gv[:, :, g * 8:(g + 1) * 8], ew3, coef3[:, :, g].unsqueeze(2).to_broadcast(s8_), ALU.mult, rd, [drt] + dgates)
        NEX = getattr(k, 'ne_limit', NE)

        def gu_load(ex):
            for hf in range(2):
                load_cast(wgate[ex, hf * 1024:(hf + 1) * 1024, :].rearrange("(c p) f -> p c f", p=128), wgv[:, hf * 8:(hf + 1) * 8, :], dwg[hf], "scalar")
                load_cast(wup[ex, hf * 1024:(hf + 1) * 1024, :].rearrange("(c p) f -> p c f", p=128), wuv[:, hf * 8:(hf + 1) * 8, :], dwu[hf], "scalar")

        def gu_part(ex, tt, fc):
            eb = ex % 2
            pg, dpg = k.ps[fc], k.dps[fc]
            pu, dpu = k.ps[2 + fc], k.dps[2 + fc]
            for kc in range(16):
                MM(k, pg, wgv[:, kc, fc * 128:(fc + 1) * 128], HTv[:, tt * 4:(tt + 1) * 4, kc, :], kc == 0, kc == 15, [dHT, dwg[kc // 8]], [dpg], kc == 15)
            for kc in range(16):
                MM(k, pu, wuv[:, kc, fc * 128:(fc + 1) * 128], HTv[:, tt * 4:(tt + 1) * 4, kc, :], kc == 0, kc == 15, [dHT, dwu[kc // 8]], [dpu], kc == 15)
            ACT(k, sl[fc], pg, AF.Silu, [dpg], [dsl[fc]])
            TT(k, "vector", hhv2[eb][:, tt, fc, :], sl[fc], pu, ALU.mult, [dsl[fc], dpu], [dhh2[eb][tt][fc]])

        def dn_load(ex):
            wb = ex % 2
            for hf in range(2):
                load_cast(wdown[ex, hf * 128:(hf + 1) * 128, :], wdv2[wb][:, hf, :], dwd2[wb][hf], "scalar")

        dn_cnt = [0]

        def dn_part(ex, tt, sub):
            eb = ex % 2
            wb = ex % 2
            t = tt * 4 + sub
            for ds in range(4):
                bank = 4 + (dn_cnt[0] % 2)
                dn_cnt[0] += 1
                po, dpo = k.ps[bank], k.dps[bank]
                for fc in range(2):
                    MM(k, po, hhv2[eb][:, tt, fc, sub * 128:(sub + 1) * 128], wdv2[wb][:, fc, ds * 512:(ds + 1) * 512], fc == 0, fc == 1, [dhh2[eb][tt][fc], dwd2[wb][fc]], [dpo], fc == 1)
                STT(k, "vector", yacc[t][:, ds * 512:(ds + 1) * 512], po, gv[:, t, ex:ex + 1], yacc[t][:, ds * 512:(ds + 1) * 512], ALU.mult, ALU.add, [dpo, dgates[t], dy[t]], [dy[t]])

        gu_load(0)
        dn_load(0)
        for tt in range(2):
            for fc in range(2):
                gu_part(0, tt, fc)
        for ex in range(NEX):
            nx = ex + 1 if ex + 1 < NEX else None
            if nx is not None:
                gu_load(nx)
                dn_load(nx)
            parts = [(tt, sub) for tt in range(2) for sub in range(4)]
            gparts = [(tt, fc) for tt in range(2) for fc in range(2)]
            for p_ in parts[0:4]:
                dn_part(ex, *p_)
            if nx is not None:
                gu_part(nx, *gparts[0])
            for p_ in parts[4:6]:
                dn_part(ex, *p_)
            if nx is not None:
                gu_part(nx, *gparts[1])
            for p_ in parts[6:8]:
                dn_part(ex, *p_)
            if nx is not None:
                gu_part(nx, *gparts[2])
                gu_part(nx, *gparts[3])
        for t in range(TH):
            ln_stats(k, yacc[t], dy[t], lnsts[t], dlnsts[t])
        for t in range(TH):
            ln_norm(k, yacc[t], dy[t], gt, bt, dgb, lnsts[t], dlnsts[t])
            if t >= 1:
                ln_out(k, yacc[t - 1], dy[t - 1], t0 + t - 1, h_out, k.hT, write_hT)
        ln_out(k, yacc[TH - 1], dy[TH - 1], t0 + TH - 1, h_out, k.hT, write_hT)
    barrier(k)
    A.reset(m0)
def MM(k, out, lhsT, rhs, start, stop, reads, writes, inc):
    k.S.op("tensor", lambda e: e.matmul(out, lhsT=lhsT, rhs=rhs, start=start, stop=stop), reads=reads, writes=writes, inc=inc)


def TR(k, out, in_, ident, reads, writes, inc):
    k.S.op("tensor", lambda e: e.transpose(out, in_, ident), reads=reads, writes=writes, inc=inc)


def ACT(k, out, in_, func, reads, writes, bias=None, scale=None, accum_out=None):
    kw = {}
    if bias is not None:
        kw["bias"] = bias
    if scale is not None:
        kw["scale"] = scale
    if accum_out is not None:
        kw["accum_out"] = accum_out
    k.S.op("scalar", lambda e: e.activation(out=out, in_=in_, func=func, **kw), reads=reads, writes=writes)


def TS(k, eng, out, in0, s1, s2, op0, op1, reads, writes):
    if op1 is None:
        k.S.op(eng, lambda e: e.tensor_scalar(out=out, in0=in0, scalar1=s1, scalar2=None, op0=op0), reads=reads, writes=writes)
    else:
        k.S.op(eng, lambda e: e.tensor_scalar(out=out, in0=in0, scalar1=s1, scalar2=s2, op0=op0, op1=op1), reads=reads, writes=writes)


def TT(k, eng, out, in0, in1, op, reads, writes):
    k.S.op(eng, lambda e: e.tensor_tensor(out=out, in0=in0, in1=in1, op=op), reads=reads, writes=writes)


def STT(k, eng, out, in0, scalar, in1, op0, op1, reads, writes):
    k.S.op(eng, lambda e: e.scalar_tensor_tensor(out=out, in0=in0, scalar=scalar, in1=in1, op0=op0, op1=op1), reads=reads, writes=writes)


def CP(k, eng, out, in_, reads, writes):
    if eng == "scalar":
        k.S.op(eng, lambda e: e.activation(out=out, in_=in_, func=AF.Copy), reads=reads, writes=writes)
    else:
        k.S.op(eng, lambda e: e.tensor_copy(out=out, in_=in_), reads=reads, writes=writes)


def DMA(k, q, out, in_, reads, writes, slow=False):
    if slow:
        k.S.dma(q, lambda e: e.dma_start(out=out, in_=in_, allow_slow_non_contiguous=True), reads=reads, writes=writes)
    else:
        k.S.dma(q, lambda e: e.dma_start(out=out, in_=in_), reads=reads, writes=writes)


class Stager:
    def __init__(self, k, nbuf, nelem):
        self.k = k
        self.bufs = [k.A.f32(nelem) for _ in range(nbuf)]
        self.deps = [Dep() for _ in range(nbuf)]
        self.pos = 0
        self.nelem = nelem
        self.engs = ["scalar", "vector"]
        self.epos = 0

    def load(self, src_ap, dst_ap, ddst, eng=None, slow=False):
        i = self.pos
        self.pos = (i + 1) % len(self.bufs)
        n = 1
        for d_ in src_ap.shape[1:]:
            n *= d_
        assert n <= self.nelem, (n, self.nelem)
        s = self.bufs[i][:, 0:n]
        if len(src_ap.shape) == 3:
            s = s.rearrange("p (a b) -> p a b", a=src_ap.shape[1])
        elif len(src_ap.shape) == 4:
            s = s.rearrange("p (a b c) -> p a b c", a=src_ap.shape[1], b=src_ap.shape[2])
        s = s[0:src_ap.shape[0]]
        DMA(self.k, "sync", s, src_ap, [], [self.deps[i]], slow=slow)
        if eng is None:
            eng = self.engs[self.epos]
            self.epos = (self.epos + 1) % len(self.engs)
        CP(self.k, eng, dst_ap, s, [self.deps[i]], [ddst])


def phase_outproj_ln(k, srcT, d_src, w_ap, g_ap, b_ap, h_in, h_out):
    S = k.S
    A = k.A
    m0 = A.mark()
    k.hTt = [A.bf16(2048), A.bf16(2048)]
    wo = A.bf16(16 * D)
    wov = r3(wo, a=16)
    dwo = [Dep() for _ in range(16)]
    stg = Stager(k, 3, 2048)
    gt = A.f32(D); bt = A.f32(D); dgb = Dep()
    src = [A.bf16(2048) for _ in range(3)]
    dsrc = [Dep() for _ in range(3)]
    at = [A.f32(D) for _ in range(3)]
    dat = [Dep() for _ in range(3)]
    lnst_raw = A.f32(24 + D)
    lnsts = [(lnst_raw[:, 8 * t_ + 0:8 * t_ + 1], lnst_raw[:, 8 * t_ + 1:8 * t_ + 2], lnst_raw[:, 8 * t_ + 2:8 * t_ + 3], lnst_raw[:, 8 * t_ + 3:8 * t_ + 4], lnst_raw[:, 8 * t_ + 4:8 * t_ + 5], lnst_raw[:, 24:24 + D]) for t_ in range(3)]
    dlnsts = [Dep() for _ in range(3)]
    ln_load_params(k, g_ap, b_ap, gt, bt, dgb)
    for kc in range(16):
        stg.load(w_ap[kc * 128:(kc + 1) * 128, :], wov[:, kc, :], dwo[kc])
    def mm_stage(tt):
        b = tt % 3
        DMA(k, "sync", src[b], srcT[tt], [d_src[tt]], [dsrc[b]])
        DMA(k, "sync", at[b], h_in[tt * 128:(tt + 1) * 128, :], [k.d_h[tt]], [dat[b]])
        sv = r3(src[b], a=16)
        for ns in range(4):
            ps, dps = k.ps[ns], k.dps[ns]
            for kc in range(16):
                MM(k, ps, sv[:, kc, :], wov[:, kc, ns * 512:(ns + 1) * 512], kc == 0, kc == 15, [dsrc[b], dwo[kc]], [dps], kc == 15)
            STT(k, "vector", at[b][:, ns * 512:(ns + 1) * 512], at[b][:, ns * 512:(ns + 1) * 512], DN_ALPHA, ps, ALU.mult, ALU.add, [dps, dat[b]], [dat[b]])
        ln_stats(k, at[b], dat[b], lnsts[b], dlnsts[b])

    mm_stage(0)
    for tt in range(NT):
        b = tt % 3
        if tt + 1 < NT:
            mm_stage(tt + 1)
        ln_norm(k, at[b], dat[b], gt, bt, dgb, lnsts[b], dlnsts[b])
        ln_out(k, at[b], dat[b], tt, h_out, k.hT, True)
    barrier(k)
    A.reset(m0)


def rel_bucket_np(n):
    n = np.maximum(n, 0)
    nf = np.maximum(n, 1).astype(np.float32)
    large = 16 + (np.log(nf / np.float32(16)) / np.float32(np.log(128 / 16)) * np.float32(16)).astype(np.int32)
    large = np.minimum(large, 31)
    return np.where(n < 16, n, large)


def dsa_consts():
    ql = np.arange(128)[:, None]
    x = np.arange(256)[None, :]
    dist = np.where(x < 128, 128 + ql - x, ql - (x - 128))
    bk = rel_bucket_np(dist)
    oh = np.zeros((128, 32, 256), np.float32)
    for b in range(32):
        oh[:, b, :] = (bk == b)
    caus = np.where(np.arange(128)[None, :] <= np.arange(128)[:, None], 0.0, NEG).astype(np.float32)
    return {"c_ohb": oh.reshape(128, 32 * 256), "c_caus": caus}


def phase_dsa(k, j, li, h_in, h_out):
    S = k.S
    A = k.A
    W = k.w
    NH_ = 16
    att_scale = 128 ** -0.5
    widx_scale = (16 ** -0.5) * (128 ** -0.5)
    QT_d = k.dsa_QT; QIT_d = k.dsa_QIT; OT_d = k.dsa_OT
    d_OT = [Dep() for _ in range(NT)]
    d_QT = Dep(); d_QIT = Dep()
    m_phase = A.mark()
    CQT = A.bf16(4 * L); CQTv = r3(CQT, a=4); dCQT = Dep()
    CKVT = A.bf16(4 * L); CKVTv = r3(CKVT, a=4); dCKVT = Dep()
    KIT = A.bf16(L); dKIT = Dep()
    WI = A.f32(NT * 16); WIv = r3(WI, a=NT); dWI = Dep()
    identb = A.bf16(128); didb = Dep()
    CP(k, "vector", identb, k.ident, [], [didb])
    mA = A.mark()
    HTb = [A.bf16(2048), A.bf16(2048)]; dHTb = [Dep(), Dep()]
    win = A.bf16(16 * 1168); winv = r3(win, a=16); dwin = [Dep() for _ in range(16)]
    stg = Stager(k, 3, 2048)
    qg = A.f32(512); kg = A.f32(512); dqg = Dep()
    DMA(k, "sync", qg, W["dsa_q_norm"][j].rearrange("(o d) -> o d", o=1).partition_broadcast(128), [], [dqg])
    DMA(k, "sync", kg, W["dsa_kv_norm"][j].rearrange("(o d) -> o d", o=1).partition_broadcast(128), [], [dqg])
    for kc in range(16):
        stg.load(W["dsa_w_in"][j, kc * 128:(kc + 1) * 128, :], winv[:, kc, :], dwin[kc])
    pj = [A.f32(1168), A.f32(1168)]; dpj = [Dep(), Dep()]
    sm = A.f32(16); dsm = Dep()
    junk = A.f32(512)
    for t in range(NT):
        b = t % 2
        DMA(k, "sync", HTb[b], k.hT[t], [k.d_hT[t]], [dHTb[b]])
        HTt = r3(HTb[b], a=16)
        for ns, (c0, c1) in enumerate([(0, 512), (512, 1024), (1024, 1168)]):
            ps, dps = k.ps[ns], k.dps[ns]
            for kc in range(16):
                MM(k, ps[:, 0:c1 - c0], HTt[:, kc, :], winv[:, kc, c0:c1], kc == 0, kc == 15, [dHTb[b], dwin[kc]], [dps], kc == 15)
            CP(k, "vector", pj[b][:, c0:c1], ps[:, 0:c1 - c0], [dps], [dpj[b]])
        for qi, (c0, gain) in enumerate([(0, qg), (512, kg)]):
            ss = sm[:, qi * 4:qi * 4 + 1]; rs = sm[:, qi * 4 + 1:qi * 4 + 2]
            ACT(k, junk, pj[b][:, c0:c0 + 512], AF.Square, [dpj[b]], [dsm], accum_out=ss)
            TS(k, "vector", rs, ss, 1.0 / 512, RMS_EPS, ALU.mult, ALU.add, [dsm], [dsm])
            ACT(k, rs, rs, AF.Sqrt, [dsm], [dsm])
            k.S.op("vector", lambda e, rs=rs: e.reciprocal(out=rs, in_=rs), reads=[dsm], writes=[dsm])
            STT(k, "vector", pj[b][:, c0:c0 + 512], pj[b][:, c0:c0 + 512], rs, gain, ALU.mult, ALU.mult, [dsm, dpj[b], dqg], [dpj[b]])
        TS(k, "vector", WIv[:, t, :], pj[b][:, 1152:1168], widx_scale, None, ALU.mult, None, [dpj[b]], [dWI])
        for grp, (c0, dstv, ddst) in enumerate([(0, CQTv, dCQT), (512, CKVTv, dCKVT)]):
            ps, dps = k.ps[4 + grp], k.dps[4 + grp]
            for kc in range(4):
                TR(k, ps[:, kc * 128:(kc + 1) * 128], pj[b][:, c0 + kc * 128:c0 + (kc + 1) * 128], k.ident, [dpj[b]], [dps], kc == 3)
            ACT(k, dstv[:, :, t * 128:(t + 1) * 128], ps.rearrange("p (a b) -> p a b", a=4), AF.Copy, [dps], [ddst])
        ps, dps = k.ps[6], k.dps[6]
        TR(k, ps[:, 0:128], pj[b][:, 1024:1152], k.ident, [dpj[b]], [dps], True)
        ACT(k, KIT[:, t * 128:(t + 1) * 128], ps[:, 0:128], AF.Copy, [dps], [dKIT])
    barrier(k)
    A.reset(mA)
    if getattr(k, 'dsa_stop', '') == 'A':
        return
    mB = A.mark()
    wq = A.bf16(4 * 2048); wqv = r3(wq, a=4); dwq = [Dep() for _ in range(4)]
    stg = Stager(k, 3, 2048)
    ev = [A.bf16(512), A.bf16(512)]; dev_ = [Dep(), Dep()]
    cnt = 0
    for wname, dst_d, ddst in [("dsa_w_uq", QT_d, d_QT), ("dsa_w_qidx", QIT_d, d_QIT)]:
        for kc in range(4):
            stg.load(W[wname][j, kc * 128:(kc + 1) * 128, :], wqv[:, kc, :], dwq[kc])
        for h in range(NH_):
            for sl_ in range(4):
                ps, dps = k.ps[cnt % 4], k.dps[cnt % 4]
                b = cnt % 2
                cnt += 1
                for kc in range(4):
                    MM(k, ps, wqv[:, kc, h * 128:(h + 1) * 128], CQTv[:, kc, sl_ * 512:(sl_ + 1) * 512], kc == 0, kc == 3, [dwq[kc], dCQT], [dps], kc == 3)
                if b == 0:
                    ACT(k, ev[b], ps, AF.Copy, [dps], [dev_[b]])
                else:
                    CP(k, "vector", ev[b], ps, [dps], [dev_[b]])
                DMA(k, "gpsimd", dst_d[:, h, sl_ * 512:(sl_ + 1) * 512], ev[b], [dev_[b]], [ddst])
    barrier(k)
    A.reset(mB)
    if getattr(k, 'dsa_stop', '') == 'B':
        return
    KT_d = k.dsa_KT; V_d = k.dsa_V
    dKT = Dep(); dV = Dep()
    mC = A.mark()
    evc = [A.bf16(512), A.bf16(512)]; devc = [Dep(), Dep()]
    stg = Stager(k, 3, 2048)
    wukT = A.bf16(4 * 2048); wukTv = wukT.rearrange("p (c h d) -> p c h d", c=4, h=NH_); dwukT = Dep()
    wuv = A.bf16(4 * 2048); wuvv = wuv.rearrange("p (c h d) -> p c h d", c=4, h=NH_); dwuv = Dep()
    uk = [A.f32(512), A.f32(512)]; duk = [Dep(), Dep()]
    for h in range(NH_):
        b = h % 2
        DMA(k, "sync", uk[b], W["dsa_w_uk"][j, h], [], [duk[b]])
        ps, dps = k.ps[4 + b], k.dps[4 + b]
        for kc in range(4):
            TR(k, ps[:, kc * 128:(kc + 1) * 128], uk[b][:, kc * 128:(kc + 1) * 128], k.ident, [duk[b]], [dps], kc == 3)
        ACT(k, wukTv[:, :, h, :], ps.rearrange("p (a b) -> p a b", a=4), AF.Copy, [dps], [dwukT])
    for h in range(NH_):
        stg.load(W["dsa_w_uv"][j, h].rearrange("(c p) d -> p c d", p=128), wuvv[:, :, h, :], dwuv)
    cnt = 0
    for h in range(NH_):
        for sl_ in range(4):
            ps, dps = k.ps[cnt % 4], k.dps[cnt % 4]
            cnt += 1
            for kc in range(4):
                MM(k, ps, wukTv[:, kc, h, :], CKVTv[:, kc, sl_ * 512:(sl_ + 1) * 512], kc == 0, kc == 3, [dwukT, dCKVT], [dps], kc == 3)
            b = cnt % 2
            if b == 0:
                ACT(k, evc[b], ps, AF.Copy, [dps], [devc[b]])
            else:
                CP(k, "vector", evc[b], ps, [dps], [devc[b]])
            DMA(k, "gpsimd", KT_d[h, :, sl_ * 512:(sl_ + 1) * 512], evc[b], [devc[b]], [dKT])
    for st_ in range(NT):
        for hg in range(4):
            ps, dps = k.ps[cnt % 4], k.dps[cnt % 4]
            cnt += 1
            for kc in range(4):
                MM(k, ps, CKVTv[:, kc, st_ * 128:(st_ + 1) * 128], wuvv[:, kc, hg * 4:(hg + 1) * 4, :], kc == 0, kc == 3, [dwuv, dCKVT], [dps], kc == 3)
            b = cnt % 2
            if b == 0:
                ACT(k, evc[b], ps, AF.Copy, [dps], [devc[b]])
            else:
                CP(k, "vector", evc[b], ps, [dps], [devc[b]])
            DMA(k, "gpsimd", V_d[hg * 4:(hg + 1) * 4, :, st_ * 128:(st_ + 1) * 128].rearrange("h p d -> p h d"), r3(evc[b], a=4), [devc[b]], [dV])
    barrier(k)
    A.reset(mC)
    if getattr(k, 'dsa_stop', '') == 'C':
        return
    Tn = A.f32(NH_ * 256); Tnv = r3(Tn, a=NH_); dTn = Dep()
    caus = A.f32(128); dcaus = Dep()
    DMA(k, "sync", caus, k.c["c_caus"], [], [dcaus])
    mT = A.mark()
    ohb = A.f32(32 * 256); ohbv = r3(ohb, a=32); dohb = Dep()
    rbB = A.f32(512); drbB = Dep()
    DMA(k, "sync", ohb, k.c["c_ohb"], [], [dohb])
    DMA(k, "sync", rbB, W["rel_bias"].rearrange("(o b) h -> o (b h)", o=1).partition_broadcast(128), [], [drbB])
    for h in range(NH_):
        eng = "vector"
        TS(k, eng, Tnv[:, h, :], ohbv[:, 0, :], rbB[:, h:h + 1], None, ALU.mult, None, [dohb, drbB], [dTn])
        for b_ in range(1, 32):
            STT(k, eng, Tnv[:, h, :], ohbv[:, b_, :], rbB[:, b_ * 16 + h:b_ * 16 + h + 1], Tnv[:, h, :], ALU.mult, ALU.add, [dohb, drbB, dTn], [dTn])
        TS(k, eng, Tnv[:, h, :], Tnv[:, h, :], rbB[:, 31 * 16 + h:31 * 16 + h + 1], None, ALU.subtract, None, [drbB, dTn], [dTn])
    barrier(k)
    A.reset(mT)
    accs = [A.f32(L), A.f32(L)]; daccs = [Dep(), Dep()]
    tmp = [A.f32(512), A.f32(512)]; dtmp = [Dep(), Dep()]
    madds = [A.bf16(L), A.bf16(L)]; dmadds = [Dep(), Dep()]
    Xs = [A.f32(L) for _ in range(3)]; dXs = [Dep() for _ in range(3)]
    Ps = [A.bf16(L) for _ in range(3)]; dPs = [Dep() for _ in range(3)]
    PTs = [A.bf16(NT * 128) for _ in range(3)]; dPTs = [Dep() for _ in range(3)]
    QIbs = [A.bf16(NH_ * 128), A.bf16(NH_ * 128)]; dQIbs = [Dep(), Dep()]
    QTbs = [A.bf16(NH_ * 128), A.bf16(NH_ * 128)]; dQTbs = [Dep(), Dep()]
    OT = [A.bf16(NH_ * 128), A.bf16(NH_ * 128)]; dOTs = [Dep(), Dep()]
    mx8 = A.f32(8); dmx = Dep()
    sm2s = [A.f32(8) for _ in range(3)]; dsm2s = [Dep() for _ in range(3)]
    dgrs = [A.bf16(128) for _ in range(3)]; ddgrs = [Dep() for _ in range(3)]
    Kh = [A.bf16(L) for _ in range(3)]; dKh = [Dep() for _ in range(3)]
    Vh = [A.bf16(L) for _ in range(3)]; dVh = [Dep() for _ in range(3)]
    z1 = A.f32(1); dz1 = Dep()
    k.S.op("vector", lambda e: e.memset(z1, 0.0), reads=[], writes=[dz1])

    def pre_thunks(jb):
        SL = (jb + 1) * 128
        nbk = (SL + 511) // 512
        pb = jb % 2
        acc, dacc = accs[pb], daccs[pb]
        madd, dmadd = madds[pb], dmadds[pb]
        QIbv = r3(QIbs[pb], a=NH_); dQIb = dQIbs[pb]
        th_ = []

        def t_load():
            DMA(k, "sync", QIbv, QIT_d[:, :, jb * 128:(jb + 1) * 128], [d_QIT], [dQIb])
        th_.append(t_load)
        cnt = [0]
        for h in range(NH_):
            def t_head(h=h):
                for bk in range(nbk):
                    w_ = min(512, SL - bk * 512)
                    bank = 4
                    ps, dps = k.ps[bank], k.dps[bank]
                    tb = cnt[0] % 2
                    cnt[0] += 1
                    MM(k, ps[:, 0:w_], QIbv[:, h, :], KIT[:, bk * 512:bk * 512 + w_], True, True, [dQIb, dKIT], [dps], True)
                    if h == 0:
                        TS(k, "vector", acc[:, bk * 512:bk * 512 + w_], ps[:, 0:w_], z1, WIv[:, jb, h:h + 1], ALU.max, ALU.mult, [dps, dWI, dz1], [dacc])
                    else:
                        TS(k, "vector", tmp[tb][:, 0:w_], ps[:, 0:w_], z1, WIv[:, jb, h:h + 1], ALU.max, ALU.mult, [dps, dWI, dz1], [dtmp[tb]])
                        TT(k, "gpsimd", acc[:, bk * 512:bk * 512 + w_], acc[:, bk * 512:bk * 512 + w_], tmp[tb][:, 0:w_], ALU.add, [dtmp[tb], dacc], [dacc])
            th_.append(t_head)

        def t_caus():
            TT(k, "gpsimd", acc[:, jb * 128:SL], acc[:, jb * 128:SL], caus, ALU.add, [dacc, dcaus], [dacc])
        th_.append(t_caus)
        if SL > 256:
            for r in range(getattr(k, 'topk_rounds', 32)):
                def t_round():
                    k.S.op("vector", lambda e: e.max(out=mx8, in_=acc[:, 0:SL]), reads=[dacc], writes=[dmx])
                    k.S.op("vector", lambda e: e.match_replace(out=acc[:, 0:SL], in_to_replace=mx8, in_values=acc[:, 0:SL], imm_value=-2.0e30), reads=[dacc, dmx], writes=[dacc])
                th_.append(t_round)

            def t_fin():
                TS(k, "vector", madd[:, 0:SL], acc[:, 0:SL], -1.5e30, NEG, ALU.is_gt, ALU.mult, [dacc], [dmadd])
        else:
            def t_fin():
                TS(k, "vector", madd[:, 0:SL], acc[:, 0:SL], -1.0e29, NEG, ALU.is_lt, ALU.mult, [dacc], [dmadd])
        th_.append(t_fin)
        return th_

    NB3 = 3
    dPV = [Dep(), Dep()]

    def bufs(i):
        b3 = i % NB3
        return (Xs[b3], dXs[b3], Ps[b3], dPs[b3], r3(PTs[b3], a=NT), dPTs[b3], sm2s[b3], dsm2s[b3], dgrs[b3], ddgrs[b3], Kh[b3], dKh[b3], Vh[b3], dVh[b3])

    def stageA(i):
        jb, h = divmod(i, NH_)
        SL = (jb + 1) * 128
        nbk = (SL + 511) // 512
        pb = jb % 2
        madd, dmadd = madds[pb], dmadds[pb]
        QTbv = r3(QTbs[pb], a=NH_); dQTb = dQTbs[pb]
        X, dX, P, dP, PTv, dPT, sm2, dsm2, dgr, ddgr, Kb, dKb, Vb, dVb = bufs(i)
        if h == 0:
            DMA(k, "sync", QTbv, QT_d[:, :, jb * 128:(jb + 1) * 128], [d_QT], [dQTb])
        DMA(k, "sync", Kb[:, 0:SL], KT_d[h, :, 0:SL], [dKT], [dKb])
        DMA(k, "sync", Vb[:, 0:SL], V_d[h, :, 0:SL], [dV], [dVb])
        for bk in range(nbk):
            w_ = min(512, SL - bk * 512)
            bank = (bk % 2) + 2 * (i % 2)
            ps, dps = k.ps[bank], k.dps[bank]
            MM(k, ps[:, 0:w_], QTbv[:, h, :], Kb[:, bk * 512:bk * 512 + w_], True, True, [dQTb, dKb], [dps], True)
            STT(k, "vector", X[:, bk * 512:bk * 512 + w_], ps[:, 0:w_], att_scale, madd[:, bk * 512:bk * 512 + w_], ALU.mult, ALU.add, [dps, dmadd], [dX])
        lo = max(0, jb - 1) * 128
        tlo = 0 if jb >= 1 else 128
        TT(k, "vector", X[:, lo:SL], X[:, lo:SL], Tnv[:, h, tlo:256], ALU.add, [dX, dTn], [dX])
        rmax = sm2[:, 0:1]; nmax = sm2[:, 1:2]; rsum = sm2[:, 2:3]
        k.S.op("vector", lambda e: e.tensor_reduce(out=rmax, in_=X[:, 0:SL], axis=AX.X, op=ALU.max), reads=[dX], writes=[dsm2])
        TS(k, "vector", nmax, rmax, -1.0, None, ALU.mult, None, [dsm2], [dsm2])
        ACT(k, P[:, 0:SL], X[:, 0:SL], AF.Exp, [dX, dsm2], [dP, dsm2], bias=nmax, scale=1.0, accum_out=rsum)

    def stageB(i):
        jb, h = divmod(i, NH_)
        X, dX, P, dP, PTv, dPT, sm2, dsm2, dgr, ddgr, Kb, dKb, Vb, dVb = bufs(i)
        rsum = sm2[:, 2:3]; rinv = sm2[:, 3:4]
        k.S.op("vector", lambda e: e.reciprocal(out=rinv, in_=rsum), reads=[dsm2], writes=[dsm2])
        TS(k, "vector", dgr, identb, rinv, None, ALU.mult, None, [dsm2, didb], [ddgr])
        for st_ in range(jb + 1):
            bank = 6 + (st_ // 4) % 2
            ps, dps = k.ps[bank], k.dps[bank]
            last = (st_ % 4 == 3) or (st_ == jb)
            MM(k, ps[:, (st_ % 4) * 128:(st_ % 4 + 1) * 128], P[:, st_ * 128:(st_ + 1) * 128], dgr, True, True, [dP, ddgr], [dps], last)
            if last:
                s0 = (st_ // 4) * 4
                n_ = st_ - s0 + 1
                ACT(k, PTv[:, s0:s0 + n_, :], ps[:, 0:n_ * 128].rearrange("p (a b) -> p a b", a=n_), AF.Copy, [dps], [dPT])
        Vhv = r3(Vb, a=NT)
        pv = k.ps[5][:, (i % 2) * 128:(i % 2 + 1) * 128]
        for st_ in range(jb + 1):
            MM(k, pv, Vhv[:, st_, :], PTv[:, st_, :], st_ == 0, st_ == jb, [dVb, dPT], [dPV[i % 2]], st_ == jb)

    def stageC(i):
        jb, h = divmod(i, NH_)
        pb = jb % 2
        ot, dot = OT[pb], dOTs[pb]
        otv = r3(ot, a=NH_)
        pv = k.ps[5][:, (i % 2) * 128:(i % 2 + 1) * 128]
        CP(k, "scalar", otv[:, h, :], pv, [dPV[i % 2]], [dot])
        if h == NH_ - 1:
            DMA(k, "gpsimd", OT_d[jb], ot, [dot], [d_OT[jb]])

    for t_ in pre_thunks(0):
        t_()
    NHEADS = NT * NH_
    nxt = []
    pos = 0
    per = 0
    for i in range(NHEADS + 2):
        if i < NHEADS:
            jb, h = divmod(i, NH_)
            if h == 0:
                for t_ in nxt[pos:]:
                    t_()
                nxt = pre_thunks(jb + 1) if jb + 1 < NT else []
                per = (len(nxt) + NH_ - 1) // NH_
                pos = 0
            stageA(i)
        if 0 <= i - 1 < NHEADS:
            stageB(i - 1)
        if 0 <= i - 2 < NHEADS:
            stageC(i - 2)
        if i < NHEADS:
            for t_ in nxt[pos:pos + per]:
                t_()
            pos += per
    barrier(k)
    A.reset(m_phase)
    if getattr(k, 'dsa_stop', '') == 'D':
        return
    phase_outproj_ln(k, OT_d, d_OT, W["dsa_w_out"][j], W["ln_mix_g"][li], W["ln_mix_b"][li], h_in, h_out)
import math as _math

S5_KVEC = [0, -1, -2, -3, -4, -5, -6, -7, 7, 6, 5, 4, 3, 2, 1, 0, 0, 1, 2, 3, 4, 5, 6, 7, 1, 2, 3, 4, 5, 6, 7, 8, 8, 16, 32, 64, 128, 256, 512, 1024]
NK = 40


def s5_consts():
    kv = np.tile(np.array(S5_KVEC, np.float32)[None, :], (128, 1))
    s_ = (np.arange(128) // 16)[:, None]
    t_ = (np.arange(128) // 16)[None, :]
    msk = (t_ >= s_).astype(np.float32)
    import ml_dtypes
    sel = np.zeros((128, 64, 128), np.float32)
    for g8 in range(8):
        for s in range(8):
            for p_ in range(16):
                sel[g8 * 16 + p_, g8 * 8 + s, s * 16 + p_] = 1.0
    selT = np.ascontiguousarray(sel.transpose(2, 1, 0))
    return {"c_kv40": kv, "c_s5mask": msk,
            "c_sel": sel.reshape(128, 8192).astype(ml_dtypes.bfloat16),
            "c_selT": selT.reshape(128, 8192).astype(ml_dtypes.bfloat16)}


def bc(ap, axis, shape):
    return ap.unsqueeze(axis).to_broadcast(list(shape))


def phase_s5(k, sj, li, h_in, h_out):
    S = k.S
    A = k.A
    W = k.w
    M_d, W1_d, W2_d, ZT_d = k.s5_M, k.s5_W1, k.s5_W2, k.s5_ZT
    dM = Dep(); dW1 = Dep(); dW2 = Dep()
    d_ZT = [Dep() for _ in range(NT)]
    TWO_PI = 2.0 * _math.pi
    m_phase = A.mark()
    DCOL = A.f32(128); dDCOL = Dep()
    ASr = A.f32(512); ASi = A.f32(512); ASn = A.f32(512); dAS = Dep()
    ASrv = r3(ASr, a=64); ASiv = r3(ASi, a=64); ASnv = r3(ASn, a=64)
    mP = A.mark()
    KV = A.f32(NK); dKV = Dep()
    DMA(k, "sync", KV, k.c["c_kv40"], [], [dKV])
    MASK = A.f32(128); dMASK = Dep()
    DMA(k, "sync", MASK, k.c["c_s5mask"], [], [dMASK])
    PAre = A.f32(64); PAim = A.f32(64); PDT = A.f32(64); dPA = Dep()
    PBre = A.f32(1024); PBim = A.f32(1024); PCre = A.f32(1024); PCim = A.f32(1024); dPB = Dep(); dPC = Dep()
    PBrev = r3(PBre, a=64); PBimv = r3(PBim, a=64); PCrev = r3(PCre, a=64); PCimv = r3(PCim, a=64)
    ld = [A.f32(2048), A.f32(2048)]; dld = [Dep(), Dep()]
    ld2 = A.f32(2048); dld2 = Dep()
    id64 = k.ident[0:64, 0:64]
    DMA(k, "sync", ld[0][:, 0:16], W["s5_d"][sj], [], [dld[0]])
    CP(k, "vector", ld[0][:, 16:144].rearrange("p (t q) -> p t q", t=8), bc(ld[0][:, 0:16], 1, [128, 8, 16]), [dld[0]], [dld[0]])
    TR(k, k.ps[0][:, 0:128], ld[0][:, 16:144], k.ident, [dld[0]], [k.dps[0]], True)
    CP(k, "vector", DCOL, k.ps[0][:, 0:128], [k.dps[0]], [dDCOL])
    DMA(k, "sync", ld[1][0:64, 0:128], W["s5_a_re"][sj].rearrange("(j g2) n -> j (g2 n)", g2=2), [], [dld[1]])
    DMA(k, "sync", ld[1][0:64, 128:256], W["s5_a_im"][sj].rearrange("(j g2) n -> j (g2 n)", g2=2), [], [dld[1]])
    DMA(k, "sync", ld[1][0:64, 256:258], W["s5_log_dt"][sj].rearrange("(j g2) -> j g2", g2=2), [], [dld[1]])
    CP(k, "vector", ld[1][0:64, 384:512].rearrange("p (g n) -> p g n", g=2), bc(ld[1][0:64, 256:258], 2, [64, 2, 64]), [dld[1]], [dld[1]])
    for i_, (c0, dst) in enumerate([(0, PAre), (128, PAim), (384, PDT)]):
        TR(k, k.ps[1][:, i_ * 64:(i_ + 1) * 64], ld[1][0:64, c0:c0 + 128], id64, [dld[1]], [k.dps[1]], True)
        CP(k, "vector", dst, k.ps[1][:, i_ * 64:(i_ + 1) * 64], [k.dps[1]], [dPA])
    cnt_ = 0
    for name, dstv, ddst, is_c in [("s5_b_re", PBrev, dPB, False), ("s5_b_im", PBimv, dPB, False), ("s5_c_re", PCrev, dPC, True), ("s5_c_im", PCimv, dPC, True)]:
        lb = ld[cnt_ % 2]; dlb = dld[cnt_ % 2]
        cnt_ += 1
        if is_c:
            DMA(k, "sync", lb[0:64, :], W[name][sj].rearrange("(j g2) p n -> j (g2 p n)", g2=2), [], [dlb])
            lb2 = ld2[0:64, :]
            CP(k, "vector", lb2.rearrange("j (p g n) -> j p g n", p=16, g=2), lb[0:64, :].rearrange("j (g p n) -> j p g n", g=2, p=16), [dlb, dld2], [dld2])
            lv = lb2.rearrange("j (p gn) -> j p gn", p=16)
        else:
            DMA(k, "sync", lb[0:64, :], W[name][sj].rearrange("(j g2) n p -> j (g2 n p)", g2=2), [], [dlb])
            lv = lb[0:64, :].rearrange("j (gn p) -> j gn p", p=16)
        for q4 in range(2):
            bank = 2 + q4
            ps, dps = k.ps[bank], k.dps[bank]
            for p8 in range(8):
                p_ = q4 * 8 + p8
                src = lv[:, p_, :] if is_c else lv[:, :, p_]
                TR(k, ps[:, p8 * 64:(p8 + 1) * 64], src, id64, [dlb, dld2], [dps], p8 == 7)
            CP(k, "vector", dstv[:, :, q4 * 8:(q4 + 1) * 8].rearrange("p j q -> p q j"), ps.rearrange("p (q j) -> p q j", q=8), [dps], [ddst])
    if getattr(k, 's5_stop', '') == 'P1':
        barrier(k)
        return
    dE = Dep()
    lr = A.f32(64); ldr = A.f32(64); th = A.f32(64); dtt = A.f32(64)
    TS(k, "vector", lr, PAre, -1.0e-4, None, ALU.min, None, [dPA], [dE])
    ACT(k, dtt, PDT, AF.Exp, [dPA], [dE])
    TT(k, "vector", ldr, lr, dtt, ALU.mult, [dE], [dE])
    TT(k, "vector", th, PAim, dtt, ALU.mult, [dE, dPA], [dE])
    NKK = 64 * NK
    shp = [128, 64, NK]
    ARG = A.f32(NKK); PHI = A.f32(NKK); RHO = A.f32(NKK); QF = A.f32(NKK); MSK2 = A.f32(NKK)
    ARE = A.f32(NKK); AIM = A.f32(NKK)
    QI = A.f32(NKK).bitcast(I32)
    v3 = lambda t_: r3(t_, a=64)
    TT(k, "vector", v3(ARG), bc(ldr, 2, shp), bc(KV, 1, shp), ALU.mult, [dE, dKV], [dE])
    ACT(k, RHO, ARG, AF.Exp, [dE], [dE])
    TT(k, "vector", v3(PHI), bc(th, 2, shp), bc(KV, 1, shp), ALU.mult, [dE, dKV], [dE])

    def sin_of(dst, off):
        TS(k, "vector", ARG, PHI, off, None, ALU.add, None, [dE], [dE])
        TS(k, "vector", QF, ARG, 1.0 / TWO_PI, None, ALU.mult, None, [dE], [dE])
        CP(k, "vector", QI, QF, [dE], [dE])
        CP(k, "vector", QF, QI, [dE], [dE])
        STT(k, "vector", ARG, QF, -TWO_PI, ARG, ALU.mult, ALU.add, [dE], [dE])
        TS(k, "vector", MSK2, ARG, _math.pi, -TWO_PI, ALU.is_gt, ALU.mult, [dE], [dE])
        TT(k, "vector", ARG, ARG, MSK2, ALU.add, [dE], [dE])
        TS(k, "vector", MSK2, ARG, -_math.pi, TWO_PI, ALU.is_lt, ALU.mult, [dE], [dE])
        TT(k, "vector", ARG, ARG, MSK2, ALU.add, [dE], [dE])
        ACT(k, dst, ARG, AF.Sin, [dE], [dE])

    sin_of(AIM, 64.0 * _math.pi)
    sin_of(ARE, 64.5 * _math.pi)
    TT(k, "vector", AIM, AIM, RHO, ALU.mult, [dE], [dE])
    TT(k, "vector", ARE, ARE, RHO, ALU.mult, [dE], [dE])
    AREv = v3(ARE); AIMv = v3(AIM)
    CP(k, "vector", ASrv, AREv[:, :, 32:40], [dE], [dAS])
    CP(k, "vector", ASiv, AIMv[:, :, 32:40], [dE], [dAS])
    TS(k, "vector", ASnv, AIMv[:, :, 32:40], -1.0, None, ALU.mult, None, [dE], [dAS])
    er = A.f32(64); ei = A.f32(64); qr = A.f32(64); qi_ = A.f32(64); den = A.f32(64); t1 = A.f32(64); fr = A.f32(64); fi = A.f32(64)
    TS(k, "vector", er, AREv[:, :, 24], -1.0, None, ALU.add, None, [dE], [dE])
    CP(k, "vector", ei, AIMv[:, :, 24], [dE], [dE])
    TT(k, "vector", qr, er, lr, ALU.mult, [dE], [dE])
    TT(k, "vector", t1, ei, PAim, ALU.mult, [dE], [dE])
    TT(k, "vector", qr, qr, t1, ALU.add, [dE], [dE])
    TT(k, "vector", qi_, ei, lr, ALU.mult, [dE], [dE])
    TT(k, "vector", t1, er, PAim, ALU.mult, [dE], [dE])
    TT(k, "vector", qi_, qi_, t1, ALU.subtract, [dE], [dE])
    TT(k, "vector", den, lr, lr, ALU.mult, [dE], [dE])
    TT(k, "vector", t1, PAim, PAim, ALU.mult, [dE], [dE])
    TT(k, "vector", den, den, t1, ALU.add, [dE], [dE])
    k.S.op("vector", lambda e: e.reciprocal(out=den, in_=den), reads=[dE], writes=[dE])
    TT(k, "vector", fr, qr, den, ALU.mult, [dE], [dE])
    TT(k, "vector", fi, qi_, den, ALU.mult, [dE], [dE])
    BBre = A.f32(1024); BBim = A.f32(1024); tb_ = A.f32(1024)
    BBrev = r3(BBre, a=64); BBimv = r3(BBim, a=64); tbv = r3(tb_, a=64)
    s16 = [128, 64, 16]
    TT(k, "vector", BBrev, bc(fr, 2, s16), PBrev, ALU.mult, [dE, dPB], [dE])
    TT(k, "vector", tbv, bc(fi, 2, s16), PBimv, ALU.mult, [dE, dPB], [dE])
    TT(k, "vector", BBre, BBre, tb_, ALU.subtract, [dE], [dE])
    TT(k, "vector", BBimv, bc(fr, 2, s16), PBimv, ALU.mult, [dE, dPB], [dE])
    TT(k, "vector", tbv, bc(fi, 2, s16), PBrev, ALU.mult, [dE, dPB], [dE])
    TT(k, "vector", BBim, BBim, tb_, ALU.add, [dE], [dE])
    if getattr(k, 's5_stop', '') == 'P2':
        barrier(k)
        return
    PB_ = 8
    PM = A.f32(2); dPM = Dep()
    k.S.op("vector", lambda e: e.memset(PM, 0.0), reads=[], writes=[dPM])
    k.S.op("vector", lambda e: e.memset(PM[0:64, 0:1], 1.0), reads=[dPM], writes=[dPM])
    k.S.op("vector", lambda e: e.memset(PM[64:128, 1:2], 1.0), reads=[dPM], writes=[dPM])
    LM = [[A.bf16(1024), A.bf16(1024)], [A.bf16(1024), A.bf16(1024)]]
    T = [A.f32(1024) for _ in range(4)]
    Tv = [t_.rearrange("p (j s q) -> p j s q", j=PB_, s=8) for t_ in T]
    RREb = A.bf16(1024); RIMb = A.bf16(1024)
    L2RE = A.f32(1024); L2IM = A.f32(1024)
    W2o = A.bf16(4096)
    W2ov = W2o.rearrange("p (j r m) -> p j r m", j=PB_, r=4)
    Mout = [A.bf16(512), A.bf16(512)]; dMout = [Dep(), Dep()]
    TAB = [A.bf16(512), A.bf16(512)]; dTAB = [Dep(), Dep()]
    for tb2 in TAB:
        k.S.op("vector", lambda e, tb2=tb2: e.memset(tb2, 0.0), reads=[], writes=[dE])
    dCh = Dep()
    s4 = [128, PB_, 8, 16]
    j8 = lambda t_: r3(t_, a=PB_)

    def products(blk, Bre, Bim, j0):
        a0 = blk * 8
        Ar = AREv[:, j0:j0 + PB_, a0:a0 + 8]; Ai = AIMv[:, j0:j0 + PB_, a0:a0 + 8]
        br = Bre[:, j0:j0 + PB_, :]; bi = Bim[:, j0:j0 + PB_, :]
        TT(k, "vector", Tv[0], bc(Ar, 3, s4), bc(br, 2, s4), ALU.mult, [dE, dPC, dCh], [dCh])
        TT(k, "vector", Tv[1], bc(Ai, 3, s4), bc(bi, 2, s4), ALU.mult, [dE, dPC, dCh], [dCh])
        TT(k, "vector", Tv[2], bc(Ai, 3, s4), bc(br, 2, s4), ALU.mult, [dE, dPC, dCh], [dCh])
        TT(k, "vector", Tv[3], bc(Ar, 3, s4), bc(bi, 2, s4), ALU.mult, [dE, dPC, dCh], [dCh])

    mcnt = 0
    for ch in range(64 // PB_):
        j0 = ch * PB_
        products(0, BBrev, BBimv, j0)
        TT(k, "vector", T[0], T[0], T[1], ALU.subtract, [dCh], [dCh])
        TT(k, "vector", T[2], T[2], T[3], ALU.add, [dCh], [dCh])
        for g2 in range(2):
            TS(k, "vector", LM[g2][0], T[0], PM[:, g2:g2 + 1], None, ALU.mult, None, [dCh, dPM], [dCh])
            TS(k, "vector", LM[g2][1], T[2], PM[:, g2:g2 + 1], None, ALU.mult, None, [dCh, dPM], [dCh])
        products(2, PCrev, PCimv, j0)
        TT(k, "vector", RREb, T[0], T[1], ALU.subtract, [dCh], [dCh])
        STT(k, "vector", RIMb, T[2], -1.0, T[3], ALU.mult, ALU.subtract, [dCh], [dCh])
        for half in range(PB_ // 2):
            bank = mcnt % 2
            mo, dmo = Mout[mcnt % 2], dMout[mcnt % 2]
            mcnt += 1
            ps, dps = k.ps[bank], k.dps[bank]
            for q in range(4):
                jj = half * 2 + q // 2
                g2 = q % 2
                MM(k, ps[:, q * 128:(q + 1) * 128], j8(LM[g2][0])[:, jj, :], j8(RREb)[:, jj, :], True, False, [dCh], [dps], False)
                MM(k, ps[:, q * 128:(q + 1) * 128], j8(LM[g2][1])[:, jj, :], j8(RIMb)[:, jj, :], False, True, [dCh], [dps], q == 3)
            TT(k, "vector", r3(mo, a=4), r3(ps, a=4), bc(MASK, 1, [128, 4, 128]), ALU.mult, [dps, dMASK], [dmo])
            g0 = (j0 + half * 2) * 2
            for q in range(4):
                DMA(k, "gpsimd", M_d[g0 + q], mo[:, q * 128:(q + 1) * 128], [dmo], [dM])
        if getattr(k, 's5_stop', '') == 'P3a':
            continue
        products(1, BBrev, BBimv, j0)
        TT(k, "vector", L2RE, T[0], T[1], ALU.subtract, [dCh], [dCh])
        TT(k, "vector", L2IM, T[2], T[3], ALU.add, [dCh], [dCh])
        for jj in range(PB_):
            bank = 2 + jj % 2
            ps, dps = k.ps[bank], k.dps[bank]
            tab, dtab = TAB[jj % 2], dTAB[jj % 2]
            TR(k, ps[:, 0:128], j8(L2RE)[:, jj, :], k.ident, [dCh], [dps], False)
            TR(k, ps[:, 128:256], j8(L2IM)[:, jj, :], k.ident, [dCh], [dps], True)
            tabv = tab.rearrange("p (r a m) -> p r a m", r=2, a=2)
            psv = ps[:, 0:256].rearrange("p (r m) -> p r m", r=2)
            CP(k, "vector", tabv[:, :, 0, 0:64], psv[:, :, 0:64], [dps], [dtab])
            CP(k, "vector", tabv[:, :, 1, 64:128], psv[:, :, 64:128], [dps], [dtab])
            DMA(k, "gpsimd", W1_d[j0 + jj], tab, [dtab], [dW1])
        if getattr(k, 's5_stop', '') == 'P3b':
            continue
        products(3, PCrev, PCimv, j0)
        TT(k, "vector", T[0], T[0], T[1], ALU.subtract, [dCh], [dCh])
        STT(k, "vector", T[2], T[2], -1.0, T[3], ALU.mult, ALU.subtract, [dCh], [dCh])
        for g2 in range(2):
            TS(k, "vector", W2ov[:, :, 2 * g2, :], j8(T[0]), PM[:, g2:g2 + 1], None, ALU.mult, None, [dCh, dPM], [dCh])
            TS(k, "vector", W2ov[:, :, 2 * g2 + 1, :], j8(T[2]), PM[:, g2:g2 + 1], None, ALU.mult, None, [dCh, dPM], [dCh])
        for jj in range(PB_):
            DMA(k, "gpsimd", W2_d[j0 + jj], W2o[:, jj * 512:(jj + 1) * 512], [dCh], [dW2, dCh])
    barrier(k)
    A.reset(mP)
    if getattr(k, 's5_stop', '') in ('P', 'P3a', 'P3b'):
        return
    R1 = A.bf16(16 * 2048)
    R1v = R1.rearrange("p (b s c) -> p b s c", b=16, s=8)
    dR1 = Dep()
    mR2 = A.mark()
    HT = A.bf16(NT * 2048); HTv = HT.rearrange("p (t c x) -> p t c x", t=NT, c=16); dHT = Dep()
    for t in range(NT):
        DMA(k, "sync", HTv[:, t].rearrange("p c x -> p (c x)"), k.hT[t], [k.d_hT[t]], [dHT])
    stg = Stager(k, 3, 2048)
    wcb = [A.bf16(2048), A.bf16(2048)]; dwcb = [Dep(), Dep()]
    cnt = 0
    for chb in range(16):
        b = chb % 2
        stg.load(W["s5_w_in"][sj].rearrange("(kc p) n -> p kc n", p=128)[:, :, chb * 128:(chb + 1) * 128], r3(wcb[b], a=16), dwcb[b])
        for sl_ in range(4):
            ps, dps = k.ps[cnt % 4], k.dps[cnt % 4]
            cnt += 1
            for kc in range(16):
                MM(k, ps, r3(wcb[b], a=16)[:, kc, :], HTv[:, sl_ * 4:(sl_ + 1) * 4, kc, :], kc == 0, kc == 15, [dwcb[b], dHT], [dps], kc == 15)
            src = ps.rearrange("p (c s) -> p s c", s=8)
            dst = R1v[:, chb, :, sl_ * 64:(sl_ + 1) * 64]
            if cnt % 2 == 0:
                ACT(k, dst, src, AF.Copy, [dps], [dR1])
            else:
                CP(k, "vector", dst, src, [dps], [dR1])
    barrier(k)
    A.reset(mR2)
    if getattr(k, 's5_stop', '') == 'U':
        return
    R2 = A.bf16(128 * 256)
    Xv = r3(R2, a=128)
    dX = Dep()
    SEL = A.bf16(8192); SELv = r3(SEL, a=64); SELT = A.bf16(8192); SELTv = r3(SELT, a=64); dSEL = Dep()
    DMA(k, "sync", SEL, k.c["c_sel"], [], [dSEL])
    DMA(k, "sync", SELT, k.c["c_selT"], [], [dSEL])
    for g0 in range(0, 128, 2):
        bank = (g0 // 2) % 4
        ps, dps = k.ps[bank], k.dps[bank]
        for gi in range(2):
            g = g0 + gi
            for s_ in range(8):
                MM(k, ps[:, gi * 256:(gi + 1) * 256], SELv[:, (g % 8) * 8 + s_, :], R1v[:, g // 8, s_, :], s_ == 0, s_ == 7, [dSEL, dR1], [dps], (gi == 1 and s_ == 7))
        if (g0 // 2) % 2 == 0:
            ACT(k, Xv[:, g0:g0 + 2, :], r3(ps, a=2), AF.Copy, [dps], [dX])
        else:
            CP(k, "vector", Xv[:, g0:g0 + 2, :], r3(ps, a=2), [dps], [dX])
    barrier(k)
    if getattr(k, 's5_stop', '') == 'X':
        return
    mL = A.mark()
    dZF = Dep()
    Mg = [A.bf16(256) for _ in range(4)]; W1t = [A.bf16(512), A.bf16(512)]; W2t = [A.bf16(512) for _ in range(4)]
    dMg = [Dep() for _ in range(4)]; dW1t = [Dep(), Dep()]; dW2t = [Dep() for _ in range(4)]
    REs = [[A.f32(384), A.f32(384)] for _ in range(2)]; IMs = [[A.f32(384), A.f32(384)] for _ in range(2)]
    dSCs = [Dep(), Dep()]
    for sl_ in range(2):
        for t_ in REs[sl_] + IMs[sl_]:
            k.S.op("vector", lambda e, t_=t_: e.memset(t_, 0.0), reads=[], writes=[dSCs[sl_]])
    HREs = [A.bf16(256), A.bf16(256)]; HIMs = [A.bf16(256), A.bf16(256)]; dHs = [Dep(), Dep()]
    ybs = [[A.f32(256), A.f32(256)] for _ in range(2)]; y2bs = [[A.f32(256), A.f32(256)] for _ in range(2)]
    dybs = [[Dep(), Dep()], [Dep(), Dep()]]
    Zall = [A.bf16(8 * 256), A.bf16(8 * 256)]; dZall = [Dep(), Dep()]

    def st_S(j):
        b = j % 2
        b4 = j % 4
        for g2 in range(2):
            DMA(k, "sync", Mg[b4][:, g2 * 128:(g2 + 1) * 128], M_d[2 * j + g2], [dM], [dMg[b4]])
        DMA(k, "sync", W1t[b], W1_d[j], [dW1], [dW1t[b]])
        DMA(k, "sync", W2t[b4], W2_d[j], [dW2], [dW2t[b4]])
        ps, dps = k.ps[b], k.dps[b]
        for ri in range(2):
            MM(k, ps[:, ri * 256:(ri + 1) * 256], W1t[b][:, (2 * ri) * 128:(2 * ri + 1) * 128], Xv[:, 2 * j, :], True, False, [dW1t[b], dX], [dps], False)
            MM(k, ps[:, ri * 256:(ri + 1) * 256], W1t[b][:, (2 * ri + 1) * 128:(2 * ri + 2) * 128], Xv[:, 2 * j + 1, :], False, True, [dW1t[b], dX], [dps], ri == 1)
        ACT(k, REs[b][0][:, 128:384], ps[:, 0:256], AF.Copy, [dps], [dSCs[b]])
        ACT(k, IMs[b][0][:, 128:384], ps[:, 256:512], AF.Copy, [dps], [dSCs[b]])

    def st_scan_step(j, i):
        b = j % 2
        cur = i % 2
        sft = 1 << i
        ra, ia = REs[b][cur], IMs[b][cur]
        rb_, ib_ = REs[b][1 - cur], IMs[b][1 - cur]
        ar = ASrv[:, j, i:i + 1]; ai = ASiv[:, j, i:i + 1]; an = ASnv[:, j, i:i + 1]
        d_ = dSCs[b]
        STT(k, "vector", rb_[:, 128:384], ra[:, 128 - sft:384 - sft], ar, ra[:, 128:384], ALU.mult, ALU.add, [d_, dAS], [d_])
        STT(k, "vector", rb_[:, 128:384], ia[:, 128 - sft:384 - sft], an, rb_[:, 128:384], ALU.mult, ALU.add, [d_, dAS], [d_])
        STT(k, "vector", ib_[:, 128:384], ia[:, 128 - sft:384 - sft], ar, ia[:, 128:384], ALU.mult, ALU.add, [d_, dAS], [d_])
        STT(k, "vector", ib_[:, 128:384], ra[:, 128 - sft:384 - sft], ai, ib_[:, 128:384], ALU.mult, ALU.add, [d_, dAS], [d_])

    def st_cast(j):
        b = j % 2
        ACT(k, HREs[b], REs[b][0][:, 127:383], AF.Copy, [dSCs[b]], [dHs[b]])
        ACT(k, HIMs[b], IMs[b][0][:, 127:383], AF.Copy, [dSCs[b]], [dHs[b]])

    def st_Y(j):
        b = j % 2
        for g2 in range(2):
            g = 2 * j + g2
            py, dpy = k.ps[2 + g2], k.dps[2 + g2]
            b4 = j % 4
            MM(k, py[:, 0:256], r3(Mg[b4], a=2)[:, g2, :], Xv[:, g, :], True, False, [dMg[b4], dX], [dpy], False)
            MM(k, py[:, 0:256], W2t[b4][:, (2 * g2) * 128:(2 * g2 + 1) * 128], HREs[b], False, False, [dW2t[b4], dHs[b]], [dpy], False)
            MM(k, py[:, 0:256], W2t[b4][:, (2 * g2 + 1) * 128:(2 * g2 + 2) * 128], HIMs[b], False, True, [dW2t[b4], dHs[b]], [dpy], True)
            y = ybs[b][g2]; y2 = y2bs[b][g2]; dy_ = dybs[b][g2]
            STT(k, "vector", y, Xv[:, g, :], DCOL[:, g:g + 1], py[:, 0:256], ALU.mult, ALU.add, [dpy, dX, dDCOL], [dy_])
            TT(k, "gpsimd", y2, y, y, ALU.mult, [dy_], [dy_])
            TS(k, "gpsimd", y2, y2, 0.044715, 1.0, ALU.mult, ALU.add, [dy_], [dy_])
            TT(k, "gpsimd", y2, y2, y, ALU.mult, [dy_], [dy_])
            ACT(k, y2, y2, AF.Sigmoid, [dy_], [dy_], scale=1.5957691216057308)
            zb = (g // 8) % 2
            TT(k, "gpsimd", r3(Zall[zb], a=8)[:, g % 8, :], y, y2, ALU.mult, [dy_, dZall[zb]], [dZall[zb]])
        if j % 4 == 3:
            chb = j // 4
            zb = chb % 2
            for sp in range(4):
                bank = 4 + sp % 4
                ps2, dps2 = k.ps[bank], k.dps[bank]
                for si in range(2):
                    s_ = sp * 2 + si
                    for g8 in range(8):
                        MM(k, ps2[:, si * 256:(si + 1) * 256], SELTv[:, g8 * 8 + s_, :], r3(Zall[zb], a=8)[:, g8, :], g8 == 0, g8 == 7, [dSEL, dZall[zb]], [dps2], (si == 1 and g8 == 7))
                if sp % 2 == 0:
                    ACT(k, R1v[:, chb, sp * 2:sp * 2 + 2, :], r3(ps2, a=2), AF.Copy, [dps2], [dZF])
                else:
                    CP(k, "vector", R1v[:, chb, sp * 2:sp * 2 + 2, :], r3(ps2, a=2), [dps2], [dZF])

    for c in range(0, 64, 2):
        st_S(c)
        st_S(c + 1)
        if c >= 2:
            st_Y(c - 2)
            st_Y(c - 1)
        for i in range(8):
            st_scan_step(c, i)
            st_scan_step(c + 1, i)
        st_cast(c)
        st_cast(c + 1)
    st_Y(62)
    st_Y(63)
    barrier(k)
    A.reset(mR2)
    if getattr(k, 's5_stop', '') == 'L':
        return
    Z2N = A.bf16(NT * 2048)
    Z2Nv = Z2N.rearrange("p (t c l s) -> p t c l s", t=NT, c=16, l=16)
    dZ2 = Dep()
    stg = Stager(k, 3, 2048)
    wgb = [A.bf16(2048), A.bf16(2048)]; dwgb = [Dep(), Dep()]
    sg = [A.f32(512), A.f32(512)]; dsg = [Dep(), Dep()]
    cnt = 0
    for nb in range(16):
        b = nb % 2
        stg.load(W["s5_w_glu"][sj].rearrange("(kc p) n -> p kc n", p=128)[:, :, nb * 128:(nb + 1) * 128], r3(wgb[b], a=16), dwgb[b])
        for q in range(4):
            ps, dps = k.ps[cnt % 4], k.dps[cnt % 4]
            sb_ = cnt % 2
            cnt += 1
            zsl = lambda kc: R1v[:, kc, 2 * q:2 * q + 2, :]
            for kc in range(16):
                MM(k, ps, r3(wgb[b], a=16)[:, kc, :], zsl(kc), kc == 0, kc == 15, [dwgb[b], dZF], [dps], kc == 15)
            ACT(k, sg[sb_], ps, AF.Sigmoid, [dps], [dsg[sb_]])
            dst = Z2Nv[:, :, nb, :, 2 * q:2 * q + 2].rearrange("p t l s -> p s t l")
            in0 = sg[sb_].rearrange("p (s t l) -> p s t l", s=2, t=16)
            in1 = R1v[:, nb, 2 * q:2 * q + 2, :].rearrange("p s (t l) -> p s t l", t=16)
            TT(k, "vector", dst, in0, in1, ALU.mult, [dsg[sb_], dZF], [dZ2])
    for t in range(NT):
        DMA(k, "gpsimd", ZT_d[t], Z2N[:, t * 2048:(t + 1) * 2048], [dZ2], [d_ZT[t]])
    barrier(k)
    A.reset(m_phase)
    phase_outproj_ln(k, ZT_d, d_ZT, W["s5_w_out"][sj], W["ln_mix_g"][li], W["ln_mix_b"][li], h_in, h_out)
W_SPECS = [
    ("rel_bias", [32, 16]),
    ("s5_w_in", [2, 2048, 2048]), ("s5_a_re", [2, 128, 64]), ("s5_a_im", [2, 128, 64]), ("s5_log_dt", [2, 128]),
    ("s5_b_re", [2, 128, 64, 16]), ("s5_b_im", [2, 128, 64, 16]), ("s5_c_re", [2, 128, 16, 64]), ("s5_c_im", [2, 128, 16, 64]),
    ("s5_d", [2, 128, 16]), ("s5_w_glu", [2, 2048, 2048]), ("s5_w_out", [2, 2048, 2048]),
    ("dsa_w_in", [2, 2048, 1168]), ("dsa_q_norm", [2, 512]), ("dsa_kv_norm", [2, 512]),
    ("dsa_w_uq", [2, 512, 2048]), ("dsa_w_qidx", [2, 512, 2048]), ("dsa_w_uk", [2, 16, 128, 512]),
    ("dsa_w_uv", [2, 16, 512, 128]), ("dsa_w_out", [2, 2048, 2048]),
    ("moe_w_group", [4, 2048, 4]), ("moe_b_group", [4, 4]), ("moe_w_expert", [4, 2048, 32]), ("moe_b_expert", [4, 32]),
    ("moe_w_gate", [4, 32, 2048, 256]), ("moe_w_up", [4, 32, 2048, 256]), ("moe_w_down", [4, 32, 256, 2048]),
    ("ln_mix_g", [4, 2048]), ("ln_mix_b", [4, 2048]), ("ln_ffn_g", [4, 2048]), ("ln_ffn_b", [4, 2048]),
]


def host_consts():
    c = {}
    c["c_ident"] = np.eye(128, dtype=np.float32)
    c.update(dsa_consts())
    c.update(s5_consts())
    return c


def build_nc(mode="full", used=None, **kw):
    nc = bass.Bass("TRN2", target_bir_lowering=False)
    k = K()
    for a_, b_ in kw.items():
        setattr(k, a_, b_)
    k.nc = nc
    k.x = nc.dram_tensor("x", [L, D], F32, kind="ExternalInput").ap()
    k.w = {}
    for name, shp in W_SPECS:
        if used is not None and name not in used:
            continue
        k.w[name] = nc.dram_tensor(name, getattr(k, 'wshape', {}).get(name, shp), F32, kind="ExternalInput").ap()
    k.c = {}
    for name, arr in host_consts().items():
        k.c[name] = nc.dram_tensor(name, list(arr.shape), F32 if arr.dtype == np.float32 else BF16, kind="ExternalInput").ap()
    k.out = nc.dram_tensor("out", [L, D], F32, kind="ExternalOutput").ap()
    k.hA = nc.dram_tensor("hA", [L, D], F32).ap()
    k.hB = nc.dram_tensor("hB", [L, D], F32).ap()
    k.hT = nc.dram_tensor("hT", [NT, 128, 16 * 128], BF16).ap()
    k.s5_M = nc.dram_tensor("s5_M", [128, 128, 128], BF16).ap()
    k.s5_W1 = nc.dram_tensor("s5_W1", [64, 128, 512], BF16).ap()
    k.s5_W2 = nc.dram_tensor("s5_W2", [64, 128, 512], BF16).ap()
    k.s5_ZT = nc.dram_tensor("s5_ZT", [NT, 128, 16 * 128], BF16).ap()
    k.dsa_QT = nc.dram_tensor("dsa_QT", [128, 16, L], BF16).ap()
    k.dsa_QIT = nc.dram_tensor("dsa_QIT", [128, 16, L], BF16).ap()
    k.dsa_OT = nc.dram_tensor("dsa_OT", [NT, 128, 16 * 128], BF16).ap()
    k.dsa_KT = nc.dram_tensor("dsa_KT", [16, 128, L], BF16).ap()
    k.dsa_V = nc.dram_tensor("dsa_V", [16, 128, L], BF16).ap()
    k.d_h = [Dep() for _ in range(NT)]
    k.d_hT = [Dep() for _ in range(NT)]
    with ExitStack() as st:
        k.S = Sched(nc, st)
        k.A = Arena(nc, st, 53000)
        k.ps = []
        k.dps = []
        for i in range(8):
            k.ps.append(st.enter_context(nc.psum_tensor("ps%d" % i, [128, 512], F32))[:])
            k.dps.append(Dep())
        A = k.A
        k.ident = A.f32(128)
        d_id = Dep()
        k.S.dma("sync", lambda e: e.dma_start(out=k.ident, in_=k.c["c_ident"]), writes=[d_id])
        k.d_hTt = [Dep(), Dep()]
        k.hTt_pos = 0
        barrier(k)
        if mode == "dsa_only":
            phase_prep(k)
            phase_dsa(k, 0, 1, k.x, k.out)
        elif mode == "s5_only":
            phase_prep(k)
            phase_s5(k, 0, 0, k.x, k.out)
        elif mode == "moe_only":
            phase_prep(k)
            phase_moe(k, 0, k.x, k.out, write_hT=False)
        elif mode == "full":
            phase_prep(k)
            h_in = k.x
            bufs = [k.hA, k.hB]
            bi = 0
            for li in range(DEPTH):
                hm = bufs[bi]; bi ^= 1
                if li % 2 == 0:
                    phase_s5(k, li // 2, li, h_in, hm)
                else:
                    phase_dsa(k, li // 2, li, h_in, hm)
                last = (li == DEPTH - 1)
                hf = k.out if last else bufs[bi]
                bi ^= 1
                phase_moe(k, li, hm, hf, write_hT=not last)
                h_in = hf
        k.S.finish(k.d_h)
        k.S.emit()
    return nc


def kernel(**inputs):
    nc = build_nc("full")
    consts = host_consts()
    x = np.ascontiguousarray(inputs["x"], dtype=np.float32)
    shared = {name: np.ascontiguousarray(inputs[name], dtype=np.float32) for name, _ in W_SPECS}
    shared.update(consts)
    in_maps = []
    for c in range(8):
        m = dict(shared)
        m["x"] = x[c]
        in_maps.append(m)
    res = run_bass_kernel_spmd(nc, in_maps, core_ids=list(range(8)))
    return np.stack([np.asarray(r["out"], dtype=np.float32) for r in res.results], axis=0)
```

```python
from concourse.bass_utils import run_bass_kernel_spmd
import numpy as np
import concourse.bass as bass
import concourse.mybir as mybir
from contextlib import ExitStack

F32 = mybir.dt.float32
BF16 = mybir.dt.bfloat16
I32 = mybir.dt.int32
ALU = mybir.AluOpType
AF = mybir.ActivationFunctionType
AX = mybir.AxisListType

ENGINES = ("tensor", "vector", "scalar", "gpsimd", "sync")
DMA_RING = 8


class Dep:
    __slots__ = ("name", "w", "r")

    def __init__(self, name=""):
        self.name = name
        self.w = None
        self.r = {}


class Sched:
    def __init__(self, nc, stack, same_engine_sync=True):
        self.nc = nc
        self.stack = stack
        self.streams = {e: [] for e in ENGINES}
        self.count = {e: 0 for e in ENGINES}
        self.seen = {e: {} for e in ENGINES}
        self.sems = {}
        for e in ENGINES:
            self.sems[e] = stack.enter_context(nc.semaphore("s_" + e))
        self.ring = {}
        self.ring_cnt = {}
        self.ring_pos = {}
        for q in ("sync", "gpsimd", "scalar"):
            self.ring[q] = []
            for i in range(DMA_RING):
                key = "d_%s_%d" % (q, i)
                self.sems[key] = stack.enter_context(nc.semaphore(key))
                self.ring[q].append(key)
            self.ring_cnt[q] = [0] * DMA_RING
            self.ring_pos[q] = 0
        self.same_engine_sync = same_engine_sync
        self.out_deps = []

    def _collect(self, reads, writes):
        need = {}

        def add(kv):
            if kv is None:
                return
            k, v = kv
            if need.get(k, 0) < v:
                need[k] = v
        for d in reads:
            add(d.w)
        for d in writes:
            add(d.w)
            for k, v in d.r.items():
                add((k, v))
        return need

    def _waits(self, eng, need, skip_self):
        ws = []
        seen = self.seen[eng]
        for k, v in need.items():
            if k == eng and skip_self:
                continue
            if seen.get(k, 0) >= v:
                continue
            seen[k] = v
            ws.append((k, v))
        return ws

    def _update(self, reads, writes, ticket):
        k, v = ticket
        for d in writes:
            d.w = ticket
            d.r = {}
        for d in reads:
            if d.r.get(k, 0) < v:
                d.r[k] = v

    def op(self, eng, fn, reads=(), writes=(), inc=True):
        need = self._collect(reads, writes)
        skip_self = (eng == "tensor") or (not self.same_engine_sync)
        ws = self._waits(eng, need, skip_self)
        if inc:
            self.count[eng] += 1
            ticket = (eng, self.count[eng])
        else:
            ticket = (eng, self.count[eng] + 1)
        self.streams[eng].append((ws, fn, (eng, 1) if inc else None))
        self._update(reads, writes, ticket)
        return ticket

    def dma(self, q, fn, reads=(), writes=()):
        need = self._collect(reads, writes)
        pos = self.ring_pos[q]
        self.ring_pos[q] = (pos + 1) % DMA_RING
        key = self.ring[q][pos]
        prev = self.ring_cnt[q][pos]
        if prev > 0:
            if need.get(key, 0) < prev * 16:
                need[key] = prev * 16
        ws = self._waits(q, need, False)
        self.ring_cnt[q][pos] = prev + 1
        ticket = (key, (prev + 1) * 16)
        self.streams[q].append((ws, fn, (key, 16)))
        self._update(reads, writes, ticket)
        return ticket

    def finish(self, deps):
        need = self._collect(deps, deps)
        ws = self._waits("sync", need, False)
        self.streams["sync"].append((ws, None, None))

    def emit(self):
        nc = self.nc
        sems = self.sems
        streams = self.streams

        def run(engh, name):
            for ws, fn, inc in streams[name]:
                for k, v in ws:
                    engh.wait_ge(sems[k], v)
                if fn is not None:
                    ins = fn(engh)
                    if inc is not None:
                        ins.then_inc(sems[inc[0]], inc[1])

        with nc.Block() as block:
            @block.tensor
            def _(e):
                run(e, "tensor")

            @block.vector
            def _(e):
                run(e, "vector")

            @block.scalar
            def _(e):
                run(e, "scalar")

            @block.gpsimd
            def _(e):
                run(e, "gpsimd")

            @block.sync
            def _(e):
                run(e, "sync")
D = 2048
L = 2048
NT = 16
DEPTH = 4
DN_ALPHA = (2 * DEPTH) ** 0.25
LN_EPS = 1e-5
RMS_EPS = 1e-6
NE = 32
FF = 256
NEG = -1.0e30


class Arena:
    def __init__(self, nc, stack, nelem):
        self.t = stack.enter_context(nc.sbuf_tensor("arena", [128, nelem], F32))
        self.n = nelem
        self.off = 0

    def mark(self):
        return self.off

    def reset(self, m):
        self.off = m

    def f32(self, n, shape=None):
        assert self.off + n <= self.n, ("arena overflow", self.off, n, self.n)
        v = self.t[:, self.off:self.off + n]
        self.off += n
        return v

    def bf16(self, n):
        m = (n + 1) // 2
        return self.f32(m).bitcast(BF16)[:, 0:n]


class K:
    pass


def r3(ap, **kw):
    return ap.rearrange("p (a b) -> p a b", **kw)


def barrier(k):
    S = k.S
    cur = {}
    for e in ENGINES:
        if S.count[e] > 0:
            cur[e] = S.count[e]
    for q in S.ring:
        for i, key in enumerate(S.ring[q]):
            if S.ring_cnt[q][i] > 0:
                cur[key] = S.ring_cnt[q][i] * 16
    for e in ENGINES:
        ws = S._waits(e, dict(cur), False)
        if ws:
            S.streams[e].append((ws, None, None))


def ln_load_params(k, g_ap, b_ap, gt, bt, dgb):
    S = k.S
    S.dma("sync", lambda e: e.dma_start(out=gt, in_=g_ap.rearrange("(o d) -> o d", o=1).partition_broadcast(128)), writes=[dgb])
    S.dma("sync", lambda e: e.dma_start(out=bt, in_=b_ap.rearrange("(o d) -> o d", o=1).partition_broadcast(128)), writes=[dgb])


def ln_stats(k, a, da, st, dst):
    S = k.S
    s1, s2, mean, var, rstd, junk = st
    S.op("scalar", lambda e: e.activation(out=junk, in_=a, func=AF.Identity, accum_out=s1), reads=[da], writes=[dst])
    S.op("scalar", lambda e: e.activation(out=junk, in_=a, func=AF.Square, accum_out=s2), reads=[da], writes=[dst])


def ln_norm(k, a, da, gt, bt, dgb, st, dst):
    S = k.S
    s1, s2, mean, var, rstd, junk = st
    S.op("vector", lambda e: e.tensor_scalar(out=mean, in0=s1, scalar1=1.0 / D, scalar2=None, op0=ALU.mult), reads=[dst], writes=[dst])
    S.op("vector", lambda e: e.tensor_tensor(out=var, in0=mean, in1=mean, op=ALU.mult), reads=[dst], writes=[dst])
    S.op("vector", lambda e: e.scalar_tensor_tensor(out=var, in0=s2, scalar=1.0 / D, in1=var, op0=ALU.mult, op1=ALU.subtract), reads=[dst], writes=[dst])
    S.op("vector", lambda e: e.tensor_scalar(out=var, in0=var, scalar1=LN_EPS, scalar2=None, op0=ALU.add), reads=[dst], writes=[dst])
    S.op("scalar", lambda e: e.activation(out=var, in_=var, func=AF.Sqrt), reads=[dst], writes=[dst])
    S.op("vector", lambda e: e.reciprocal(out=rstd, in_=var), reads=[dst], writes=[dst])
    S.op("vector", lambda e: e.tensor_scalar(out=a, in0=a, scalar1=mean, scalar2=rstd, op0=ALU.subtract, op1=ALU.mult), reads=[dst, da], writes=[da])
    S.op("vector", lambda e: e.tensor_tensor(out=a, in0=a, in1=gt, op=ALU.mult), reads=[da, dgb], writes=[da])
    S.op("vector", lambda e: e.tensor_tensor(out=a, in0=a, in1=bt, op=ALU.add), reads=[da, dgb], writes=[da])


def ln_out(k, a, da, tt, h_out, hT_out, write_hT=True):
    k.S.dma("gpsimd", lambda e: e.dma_start(out=h_out[tt * 128:(tt + 1) * 128, :], in_=a), reads=[da], writes=[k.d_h[tt]])
    if write_hT:
        emit_hT(k, a, da, tt, hT_out)


def ln_tile(k, a, da, gt, bt, dgb, tt, h_out, hT_out, st, dst, write_hT=True):
    ln_stats(k, a, da, st, dst)
    ln_norm(k, a, da, gt, bt, dgb, st, dst)
    ln_out(k, a, da, tt, h_out, hT_out, write_hT)


def emit_hT(k, a, da, tt, hT_out):
    S = k.S
    slot = k.hTt_pos
    k.hTt_pos = (slot + 1) % 2
    hTt, dhTt = k.hTt[slot], k.d_hTt[slot]
    for q in range(4):
        bank = 6 + (q % 2)
        ps, dps = k.ps[bank], k.dps[bank]
        for j in range(4):
            kc = q * 4 + j
            S.op("tensor", lambda e, kc=kc, j=j, ps=ps: e.transpose(ps[:, j * 128:(j + 1) * 128], a[:, kc * 128:(kc + 1) * 128], k.ident),
                 reads=[da], writes=[dps], inc=(j == 3))
        S.op("scalar", lambda e, q=q, ps=ps: e.activation(out=hTt[:, q * 512:(q + 1) * 512], in_=ps, func=AF.Copy),
             reads=[dps], writes=[dhTt])
    S.dma("gpsimd", lambda e: e.dma_start(out=hT_out[tt], in_=hTt), reads=[dhTt], writes=[k.d_hT[tt]])


def phase_prep(k):
    S = k.S
    A = k.A
    m = A.mark()
    k.hTt = [A.bf16(2048), A.bf16(2048)]
    xt = [A.f32(D), A.f32(D)]
    dxt = [Dep(), Dep()]
    for tt in range(NT):
        b = tt % 2
        S.dma("sync", lambda e, tt=tt, b=b: e.dma_start(out=xt[b], in_=k.x[tt * 128:(tt + 1) * 128, :]), writes=[dxt[b]])
        emit_hT(k, xt[b], dxt[b], tt, k.hT)
    barrier(k)
    A.reset(m)


def phase_moe(k, li, h_in, h_out, write_hT=True):
    S = k.S
    A = k.A
    m0 = A.mark()
    k.hTt = [A.bf16(2048), A.bf16(2048)]
    NH = 2
    TH = NT // NH
    HT = A.bf16(TH * 16 * 128)
    HTv = HT.rearrange("p (t c x) -> p t c x", t=TH, c=16)
    dHT = Dep()
    yacc = [A.f32(D) for _ in range(TH)]
    dy = [Dep() for _ in range(TH)]
    wg = A.bf16(16 * FF); wu = A.bf16(16 * FF); wd = A.bf16(2 * D)
    wgv = r3(wg, a=16); wuv = r3(wu, a=16); wdv = r3(wd, a=2)
    dwg = [Dep(), Dep()]; dwu = [Dep(), Dep()]; dwd = [Dep(), Dep()]
    NSTG = 3
    stg = [A.f32(2048) for _ in range(NSTG)]
    dstg = [Dep() for _ in range(NSTG)]
    gt = A.f32(D); bt = A.f32(D); dgb = Dep()
    hh2 = [A.bf16(2 * 2 * 512), A.bf16(2 * 2 * 512)]
    hhv2 = [h_.rearrange("p (t f x) -> p t f x", t=2, f=2) for h_ in hh2]
    dhh2 = [[[Dep(), Dep()], [Dep(), Dep()]], [[Dep(), Dep()], [Dep(), Dep()]]]
    wd_b = A.bf16(2 * D)
    wdv2 = [wdv, r3(wd_b, a=2)]
    dwd2 = [dwd, [Dep(), Dep()]]
    sl = [A.f32(512), A.f32(512)]
    dsl = [Dep(), Dep()]
    wr_s = A.f32(16 * 36); wr = A.bf16(16 * 36); dwr = Dep()
    wr_sv = r3(wr_s, a=16); wrv = r3(wr, a=16)
    rb = A.f32(36); drb = Dep()
    gates = A.f32(TH * NE); dgates = [Dep() for _ in range(TH)]
    gv = r3(gates, a=TH)
    RT = A.f32(704); drt = Dep()
    lnst_raw = A.f32(8 * TH + D)
    lnsts = [(lnst_raw[:, 8 * t_ + 0:8 * t_ + 1], lnst_raw[:, 8 * t_ + 1:8 * t_ + 2], lnst_raw[:, 8 * t_ + 2:8 * t_ + 3], lnst_raw[:, 8 * t_ + 3:8 * t_ + 4], lnst_raw[:, 8 * t_ + 4:8 * t_ + 5], lnst_raw[:, 8 * TH:8 * TH + D]) for t_ in range(TH)]
    dlnsts = [Dep() for _ in range(TH)]

    nc = k.nc
    S.dma("sync", lambda e: e.dma_start(out=wr_sv[:, :, 0:4], in_=k.w["moe_w_group"][li].rearrange("(c p) g -> p c g", p=128)), writes=[dwr])
    S.dma("sync", lambda e: e.dma_start(out=wr_sv[:, :, 4:36], in_=k.w["moe_w_expert"][li].rearrange("(c p) g -> p c g", p=128)), writes=[dwr])
    S.op("vector", lambda e: e.tensor_copy(out=wr, in_=wr_s), reads=[dwr], writes=[dwr])
    S.dma("sync", lambda e: e.dma_start(out=rb[:, 0:4], in_=k.w["moe_b_group"][li].rearrange("(o g) -> o g", o=1).partition_broadcast(128)), writes=[drb])
    S.dma("sync", lambda e: e.dma_start(out=rb[:, 4:36], in_=k.w["moe_b_expert"][li].rearrange("(o g) -> o g", o=1).partition_broadcast(128)), writes=[drb])
    ln_load_params(k, k.w["ln_ffn_g"][li], k.w["ln_ffn_b"][li], gt, bt, dgb)

    stg_pos = [0, 0]

    def load_cast(src_ap, dst_ap, ddst, eng):
        i = stg_pos[0]
        stg_pos[0] = (i + 1) % NSTG
        s = stg[i]
        sv = s if len(src_ap.shape) == 2 else r3(s, a=src_ap.shape[1])
        if not getattr(k, 'skip_wdma', False) or stg_pos[1] < 8:
            S.dma("sync", lambda e: e.dma_start(out=sv, in_=src_ap), writes=[dstg[i]])
        stg_pos[1] += 1
        if eng == "scalar":
            S.op(eng, lambda e: e.activation(out=dst_ap, in_=sv, func=AF.Copy), reads=[dstg[i]], writes=[ddst])
        else:
            S.op(eng, lambda e: e.tensor_copy(out=dst_ap, in_=sv), reads=[dstg[i]], writes=[ddst])

    wgate = k.w["moe_w_gate"][li]
    wup = k.w["moe_w_up"][li]
    wdown = k.w["moe_w_down"][li]

    for half in range(NH):
        t0 = half * TH
        for t in range(TH):
            S.dma("sync", lambda e, t=t, t0=t0: e.dma_start(out=HTv[:, t].rearrange("p c x -> p (c x)"), in_=k.hT[t0 + t]),
                  reads=[k.d_hT[t0 + t]], writes=[dHT])
        for t in range(TH):
            S.dma("sync", lambda e, t=t, t0=t0: e.dma_start(out=yacc[t], in_=h_in[(t0 + t) * 128:(t0 + t + 1) * 128, :]),
                  reads=[k.d_h[t0 + t]], writes=[dy[t]])
            S.op("scalar", lambda e, t=t: e.activation(out=yacc[t], in_=yacc[t], func=AF.Copy, scale=DN_ALPHA),
                 reads=[dy[t]], writes=[dy[t]])
        ps, dps = k.ps[6], k.dps[6]
        for t in range(TH):
            for kc in range(16):
                MM(k, ps[:, t * 36:(t + 1) * 36], HTv[:, t, kc, :], wrv[:, kc, :], kc == 0, kc == 15, [dHT, dwr], [dps], (t == TH - 1 and kc == 15))
        lg3 = RT[:, 0:TH * 36].rearrange("p (t x) -> p t x", t=TH)
        o_ = TH * 36
        gmax = RT[:, o_:o_ + TH]; o_ += TH
        gsum = RT[:, o_:o_ + TH]; o_ += TH
        gp = RT[:, o_:o_ + TH]; o_ += TH
        m1 = RT[:, o_:o_ + TH]; o_ += TH
        m2 = RT[:, o_:o_ + TH]; o_ += TH
        den = RT[:, o_:o_ + TH]; o_ += TH
        gexp3 = RT[:, o_:o_ + TH * 4].rearrange("p (t x) -> p t x", t=TH); o_ += TH * 4
        gone3 = RT[:, o_:o_ + TH * 4].rearrange("p (t x) -> p t x", t=TH); o_ += TH * 4
        coef3 = RT[:, o_:o_ + TH * 4].rearrange("p (t x) -> p t x", t=TH); o_ += TH * 4
        elc3 = RT[:, o_:o_ + TH * 8].rearrange("p (t x) -> p t x", t=TH); o_ += TH * 8
        tmp3 = RT[:, o_:o_ + TH * 8].rearrange("p (t x) -> p t x", t=TH); o_ += TH * 8
        ew3 = RT[:, o_:o_ + TH * 8].rearrange("p (t x) -> p t x", t=TH); o_ += TH * 8
        sel3 = RT[:, o_:o_ + TH * 8].rearrange("p (t x) -> p t x", t=TH); o_ += TH * 8
        assert o_ <= 704
        s4_ = [128, TH, 4]; s8_ = [128, TH, 8]
        rd = [drt]; wr_ = [drt]
        TT(k, "vector", lg3, ps[:, 0:TH * 36].rearrange("p (t x) -> p t x", t=TH), rb.unsqueeze(1).to_broadcast([128, TH, 36]), ALU.add, [dps, drb, drt], wr_)
        k.S.op("vector", lambda e: e.tensor_reduce(out=gmax, in_=lg3[:, :, 0:4], axis=AX.X, op=ALU.max), reads=rd, writes=wr_)
        TT(k, "vector", gexp3, lg3[:, :, 0:4], gmax.unsqueeze(2).to_broadcast(s4_), ALU.subtract, rd, wr_)
        ACT(k, gexp3, gexp3, AF.Exp, rd, wr_)
        k.S.op("vector", lambda e: e.tensor_reduce(out=gsum, in_=gexp3, axis=AX.X, op=ALU.add), reads=rd, writes=wr_)
        k.S.op("vector", lambda e: e.reciprocal(out=gp, in_=gsum), reads=rd, writes=wr_)
        TT(k, "vector", gone3, lg3[:, :, 0:4], gmax.unsqueeze(2).to_broadcast(s4_), ALU.is_equal, rd, wr_)
        for g in range(4):
            dst_ = elc3 if g == 0 else tmp3
            TT(k, "vector", dst_, lg3[:, :, 4 + 8 * g:12 + 8 * g], gone3[:, :, g].unsqueeze(2).to_broadcast(s8_), ALU.mult, rd, wr_)
            if g > 0:
                TT(k, "vector", elc3, elc3, tmp3, ALU.add, rd, wr_)
        k.S.op("vector", lambda e: e.tensor_reduce(out=m1, in_=elc3, axis=AX.X, op=ALU.max), reads=rd, writes=wr_)
        TT(k, "vector", tmp3, elc3, m1.unsqueeze(2).to_broadcast(s8_), ALU.is_equal, rd, wr_)
        STT(k, "vector", tmp3, tmp3, NEG, elc3, ALU.mult, ALU.add, rd, wr_)
        k.S.op("vector", lambda e: e.tensor_reduce(out=m2, in_=tmp3, axis=AX.X, op=ALU.max), reads=rd, writes=wr_)
        TT(k, "vector", ew3, elc3, m1.unsqueeze(2).to_broadcast(s8_), ALU.subtract, rd, wr_)
        ACT(k, ew3, ew3, AF.Exp, rd, wr_)
        TT(k, "vector", sel3, elc3, m2.unsqueeze(2).to_broadcast(s8_), ALU.is_ge, rd, wr_)
        TT(k, "vector", ew3, ew3, sel3, ALU.mult, rd, wr_)
        k.S.op("vector", lambda e: e.tensor_reduce(out=den, in_=ew3, axis=AX.X, op=ALU.add), reads=rd, writes=wr_)
        k.S.op("vector", lambda e: e.reciprocal(out=den, in_=den), reads=rd, writes=wr_)
        TT(k, "vector", ew3, ew3, den.unsqueeze(2).to_broadcast(s8_), ALU.mult, rd, wr_)
        TT(k, "vector", coef3, gone3, gp.unsqueeze(2).to_broadcast(s4_), ALU.mult, rd, wr_)
        for g in range(4):
            TT(k, "vector", gv[:, :, g * 8:(g + 1) * 8], ew3, coef3[:, :, g].unsqueeze(2).to_broadcast(s8_), ALU.mult, rd, [drt] + dgates)
        NEX = getattr(k, 'ne_limit', NE)

        def gu_load(ex):
            for hf in range(2):
                load_cast(wgate[ex, hf * 1024:(hf + 1) * 1024, :].rearrange("(c p) f -> p c f", p=128), wgv[:, hf * 8:(hf + 1) * 8, :], dwg[hf], "scalar")
                load_cast(wup[ex, hf * 1024:(hf + 1) * 1024, :].rearrange("(c p) f -> p c f", p=128), wuv[:, hf * 8:(hf + 1) * 8, :], dwu[hf], "scalar")

        def gu_part(ex, tt, fc):
            eb = ex % 2
            pg, dpg = k.ps[fc], k.dps[fc]
            pu, dpu = k.ps[2 + fc], k.dps[2 + fc]
            for kc in range(16):
                MM(k, pg, wgv[:, kc, fc * 128:(fc + 1) * 128], HTv[:, tt * 4:(tt + 1) * 4, kc, :], kc == 0, kc == 15, [dHT, dwg[kc // 8]], [dpg], kc == 15)
            for kc in range(16):
                MM(k, pu, wuv[:, kc, fc * 128:(fc + 1) * 128], HTv[:, tt * 4:(tt + 1) * 4, kc, :], kc == 0, kc == 15, [dHT, dwu[kc // 8]], [dpu], kc == 15)
            ACT(k, sl[fc], pg, AF.Silu, [dpg], [dsl[fc]])
            TT(k, "vector", hhv2[eb][:, tt, fc, :], sl[fc], pu, ALU.mult, [dsl[fc], dpu], [dhh2[eb][tt][fc]])

        def dn_load(ex):
            wb = ex % 2
            for hf in range(2):
                load_cast(wdown[ex, hf * 128:(hf + 1) * 128, :], wdv2[wb][:, hf, :], dwd2[wb][hf], "scalar")

        dn_cnt = [0]

        def dn_part(ex, tt, sub):
            eb = ex % 2
            wb = ex % 2
            t = tt * 4 + sub
            for ds in range(4):
                bank = 4 + (dn_cnt[0] % 2)
                dn_cnt[0] += 1
                po, dpo = k.ps[bank], k.dps[bank]
                for fc in range(2):
                    MM(k, po, hhv2[eb][:, tt, fc, sub * 128:(sub + 1) * 128], wdv2[wb][:, fc, ds * 512:(ds + 1) * 512], fc == 0, fc == 1, [dhh2[eb][tt][fc], dwd2[wb][fc]], [dpo], fc == 1)
                STT(k, "vector", yacc[t][:, ds * 512:(ds + 1) * 512], po, gv[:, t, ex:ex + 1], yacc[t][:, ds * 512:(ds + 1) * 512], ALU.mult, ALU.add, [dpo, dgates[t], dy[t]], [dy[t]])

        gu_load(0)
        dn_load(0)
        for tt in range(2):
            for fc in range(2):
                gu_part(0, tt, fc)
        for ex in range(NEX):
            nx = ex + 1 if ex + 1 < NEX else None
            if nx is not None:
                gu_load(nx)
                dn_load(nx)
            parts = [(tt, sub) for tt in range(2) for sub in range(4)]
            gparts = [(tt, fc) for tt in range(2) for fc in range(2)]
            for p_ in parts[0:4]:
                dn_part(ex, *p_)
            if nx is not None:
                gu_part(nx, *gparts[0])
            for p_ in parts[4:6]:
                dn_part(ex, *p_)
            if nx is not None:
                gu_part(nx, *gparts[1])
            for p_ in parts[6:8]:
                dn_part(ex, *p_)
            if nx is not None:
                gu_part(nx, *gparts[2])
                gu_part(nx, *gparts[3])
        for t in range(TH):
            ln_stats(k, yacc[t], dy[t], lnsts[t], dlnsts[t])
        for t in range(TH):
            ln_norm(k, yacc[t], dy[t], gt, bt, dgb, lnsts[t], dlnsts[t])
            if t >= 1:
                ln_out(k, yacc[t - 1], dy[t - 1], t0 + t - 1, h_out, k.hT, write_hT)
        ln_out(k, yacc[TH - 1], dy[TH - 1], t0 + TH - 1, h_out, k.hT, write_hT)
    barrier(k)
    A.reset(m0)
def MM(k, out, lhsT, rhs, start, stop, reads, writes, inc):
    k.S.op("tensor", lambda e: e.matmul(out, lhsT=lhsT, rhs=rhs, start=start, stop=stop), reads=reads, writes=writes, inc=inc)


def TR(k, out, in_, ident, reads, writes, inc):
    k.S.op("tensor", lambda e: e.transpose(out, in_, ident), reads=reads, writes=writes, inc=inc)


def ACT(k, out, in_, func, reads, writes, bias=None, scale=None, accum_out=None):
    kw = {}
    if bias is not None:
        kw["bias"] = bias
    if scale is not None:
        kw["scale"] = scale
    if accum_out is not None:
        kw["accum_out"] = accum_out
    k.S.op("scalar", lambda e: e.activation(out=out, in_=in_, func=func, **kw), reads=reads, writes=writes)


def TS(k, eng, out, in0, s1, s2, op0, op1, reads, writes):
    if op1 is None:
        k.S.op(eng, lambda e: e.tensor_scalar(out=out, in0=in0, scalar1=s1, scalar2=None, op0=op0), reads=reads, writes=writes)
    else:
        k.S.op(eng, lambda e: e.tensor_scalar(out=out, in0=in0, scalar1=s1, scalar2=s2, op0=op0, op1=op1), reads=reads, writes=writes)


def TT(k, eng, out, in0, in1, op, reads, writes):
    k.S.op(eng, lambda e: e.tensor_tensor(out=out, in0=in0, in1=in1, op=op), reads=reads, writes=writes)


def STT(k, eng, out, in0, scalar, in1, op0, op1, reads, writes):
    k.S.op(eng, lambda e: e.scalar_tensor_tensor(out=out, in0=in0, scalar=scalar, in1=in1, op0=op0, op1=op1), reads=reads, writes=writes)


def CP(k, eng, out, in_, reads, writes):
    if eng == "scalar":
        k.S.op(eng, lambda e: e.activation(out=out, in_=in_, func=AF.Copy), reads=reads, writes=writes)
    else:
        k.S.op(eng, lambda e: e.tensor_copy(out=out, in_=in_), reads=reads, writes=writes)


def DMA(k, q, out, in_, reads, writes, slow=False):
    if slow:
        k.S.dma(q, lambda e: e.dma_start(out=out, in_=in_, allow_slow_non_contiguous=True), reads=reads, writes=writes)
    else:
        k.S.dma(q, lambda e: e.dma_start(out=out, in_=in_), reads=reads, writes=writes)


class Stager:
    def __init__(self, k, nbuf, nelem):
        self.k = k
        self.bufs = [k.A.f32(nelem) for _ in range(nbuf)]
        self.deps = [Dep() for _ in range(nbuf)]
        self.pos = 0
        self.nelem = nelem
        self.engs = ["scalar", "vector"]
        self.epos = 0

    def load(self, src_ap, dst_ap, ddst, eng=None, slow=False):
        i = self.pos
        self.pos = (i + 1) % len(self.bufs)
        n = 1
        for d_ in src_ap.shape[1:]:
            n *= d_
        assert n <= self.nelem, (n, self.nelem)
        s = self.bufs[i][:, 0:n]
        if len(src_ap.shape) == 3:
            s = s.rearrange("p (a b) -> p a b", a=src_ap.shape[1])
        elif len(src_ap.shape) == 4:
            s = s.rearrange("p (a b c) -> p a b c", a=src_ap.shape[1], b=src_ap.shape[2])
        s = s[0:src_ap.shape[0]]
        DMA(self.k, "sync", s, src_ap, [], [self.deps[i]], slow=slow)
        if eng is None:
            eng = self.engs[self.epos]
            self.epos = (self.epos + 1) % len(self.engs)
        CP(self.k, eng, dst_ap, s, [self.deps[i]], [ddst])


def phase_outproj_ln(k, srcT, d_src, w_ap, g_ap, b_ap, h_in, h_out):
    S = k.S
    A = k.A
    m0 = A.mark()
    k.hTt = [A.bf16(2048), A.bf16(2048)]
    wo = A.bf16(16 * D)
    wov = r3(wo, a=16)
    dwo = [Dep() for _ in range(16)]
    stg = Stager(k, 3, 2048)
    gt = A.f32(D); bt = A.f32(D); dgb = Dep()
    src = [A.bf16(2048) for _ in range(3)]
    dsrc = [Dep() for _ in range(3)]
    at = [A.f32(D) for _ in range(3)]
    dat = [Dep() for _ in range(3)]
    lnst_raw = A.f32(24 + D)
    lnsts = [(lnst_raw[:, 8 * t_ + 0:8 * t_ + 1], lnst_raw[:, 8 * t_ + 1:8 * t_ + 2], lnst_raw[:, 8 * t_ + 2:8 * t_ + 3], lnst_raw[:, 8 * t_ + 3:8 * t_ + 4], lnst_raw[:, 8 * t_ + 4:8 * t_ + 5], lnst_raw[:, 24:24 + D]) for t_ in range(3)]
    dlnsts = [Dep() for _ in range(3)]
    ln_load_params(k, g_ap, b_ap, gt, bt, dgb)
    for kc in range(16):
        stg.load(w_ap[kc * 128:(kc + 1) * 128, :], wov[:, kc, :], dwo[kc])
    def mm_stage(tt):
        b = tt % 3
        DMA(k, "sync", src[b], srcT[tt], [d_src[tt]], [dsrc[b]])
        DMA(k, "sync", at[b], h_in[tt * 128:(tt + 1) * 128, :], [k.d_h[tt]], [dat[b]])
        sv = r3(src[b], a=16)
        for ns in range(4):
            ps, dps = k.ps[ns], k.dps[ns]
            for kc in range(16):
                MM(k, ps, sv[:, kc, :], wov[:, kc, ns * 512:(ns + 1) * 512], kc == 0, kc == 15, [dsrc[b], dwo[kc]], [dps], kc == 15)
            STT(k, "vector", at[b][:, ns * 512:(ns + 1) * 512], at[b][:, ns * 512:(ns + 1) * 512], DN_ALPHA, ps, ALU.mult, ALU.add, [dps, dat[b]], [dat[b]])
        ln_stats(k, at[b], dat[b], lnsts[b], dlnsts[b])

    mm_stage(0)
    for tt in range(NT):
        b = tt % 3
        if tt + 1 < NT:
            mm_stage(tt + 1)
        ln_norm(k, at[b], dat[b], gt, bt, dgb, lnsts[b], dlnsts[b])
        ln_out(k, at[b], dat[b], tt, h_out, k.hT, True)
    barrier(k)
    A.reset(m0)


def rel_bucket_np(n):
    n = np.maximum(n, 0)
    nf = np.maximum(n, 1).astype(np.float32)
    large = 16 + (np.log(nf / np.float32(16)) / np.float32(np.log(128 / 16)) * np.float32(16)).astype(np.int32)
    large = np.minimum(large, 31)
    return np.where(n < 16, n, large)


def dsa_consts():
    ql = np.arange(128)[:, None]
    x = np.arange(256)[None, :]
    dist = np.where(x < 128, 128 + ql - x, ql - (x - 128))
    bk = rel_bucket_np(dist)
    oh = np.zeros((128, 32, 256), np.float32)
    for b in range(32):
        oh[:, b, :] = (bk == b)
    caus = np.where(np.arange(128)[None, :] <= np.arange(128)[:, None], 0.0, NEG).astype(np.float32)
    return {"c_ohb": oh.reshape(128, 32 * 256), "c_caus": caus}


def phase_dsa(k, j, li, h_in, h_out):
    S = k.S
    A = k.A
    W = k.w
    NH_ = 16
    att_scale = 128 ** -0.5
    widx_scale = (16 ** -0.5) * (128 ** -0.5)
    QT_d = k.dsa_QT; QIT_d = k.dsa_QIT; OT_d = k.dsa_OT
    d_OT = [Dep() for _ in range(NT)]
    d_QT = Dep(); d_QIT = Dep()
    m_phase = A.mark()
    CQT = A.bf16(4 * L); CQTv = r3(CQT, a=4); dCQT = Dep()
    CKVT = A.bf16(4 * L); CKVTv = r3(CKVT, a=4); dCKVT = Dep()
    KIT = A.bf16(L); dKIT = Dep()
    WI = A.f32(NT * 16); WIv = r3(WI, a=NT); dWI = Dep()
    identb = A.bf16(128); didb = Dep()
    CP(k, "vector", identb, k.ident, [], [didb])
    mA = A.mark()
    HTb = [A.bf16(2048), A.bf16(2048)]; dHTb = [Dep(), Dep()]
    win = A.bf16(16 * 1168); winv = r3(win, a=16); dwin = [Dep() for _ in range(16)]
    stg = Stager(k, 3, 2048)
    qg = A.f32(512); kg = A.f32(512); dqg = Dep()
    DMA(k, "sync", qg, W["dsa_q_norm"][j].rearrange("(o d) -> o d", o=1).partition_broadcast(128), [], [dqg])
    DMA(k, "sync", kg, W["dsa_kv_norm"][j].rearrange("(o d) -> o d", o=1).partition_broadcast(128), [], [dqg])
    for kc in range(16):
        stg.load(W["dsa_w_in"][j, kc * 128:(kc + 1) * 128, :], winv[:, kc, :], dwin[kc])
    pj = [A.f32(1168), A.f32(1168)]; dpj = [Dep(), Dep()]
    sm = A.f32(16); dsm = Dep()
    junk = A.f32(512)
    for t in range(NT):
        b = t % 2
        DMA(k, "sync", HTb[b], k.hT[t], [k.d_hT[t]], [dHTb[b]])
        HTt = r3(HTb[b], a=16)
        for ns, (c0, c1) in enumerate([(0, 512), (512, 1024), (1024, 1168)]):
            ps, dps = k.ps[ns], k.dps[ns]
            for kc in range(16):
                MM(k, ps[:, 0:c1 - c0], HTt[:, kc, :], winv[:, kc, c0:c1], kc == 0, kc == 15, [dHTb[b], dwin[kc]], [dps], kc == 15)
            CP(k, "vector", pj[b][:, c0:c1], ps[:, 0:c1 - c0], [dps], [dpj[b]])
        for qi, (c0, gain) in enumerate([(0, qg), (512, kg)]):
            ss = sm[:, qi * 4:qi * 4 + 1]; rs = sm[:, qi * 4 + 1:qi * 4 + 2]
            ACT(k, junk, pj[b][:, c0:c0 + 512], AF.Square, [dpj[b]], [dsm], accum_out=ss)
            TS(k, "vector", rs, ss, 1.0 / 512, RMS_EPS, ALU.mult, ALU.add, [dsm], [dsm])
            ACT(k, rs, rs, AF.Sqrt, [dsm], [dsm])
            k.S.op("vector", lambda e, rs=rs: e.reciprocal(out=rs, in_=rs), reads=[dsm], writes=[dsm])
            STT(k, "vector", pj[b][:, c0:c0 + 512], pj[b][:, c0:c0 + 512], rs, gain, ALU.mult, ALU.mult, [dsm, dpj[b], dqg], [dpj[b]])
        TS(k, "vector", WIv[:, t, :], pj[b][:, 1152:1168], widx_scale, None, ALU.mult, None, [dpj[b]], [dWI])
        for grp, (c0, dstv, ddst) in enumerate([(0, CQTv, dCQT), (512, CKVTv, dCKVT)]):
            ps, dps = k.ps[4 + grp], k.dps[4 + grp]
            for kc in range(4):
                TR(k, ps[:, kc * 128:(kc + 1) * 128], pj[b][:, c0 + kc * 128:c0 + (kc + 1) * 128], k.ident, [dpj[b]], [dps], kc == 3)
            ACT(k, dstv[:, :, t * 128:(t + 1) * 128], ps.rearrange("p (a b) -> p a b", a=4), AF.Copy, [dps], [ddst])
        ps, dps = k.ps[6], k.dps[6]
        TR(k, ps[:, 0:128], pj[b][:, 1024:1152], k.ident, [dpj[b]], [dps], True)
        ACT(k, KIT[:, t * 128:(t + 1) * 128], ps[:, 0:128], AF.Copy, [dps], [dKIT])
    barrier(k)
    A.reset(mA)
    if getattr(k, 'dsa_stop', '') == 'A':
        return
    mB = A.mark()
    wq = A.bf16(4 * 2048); wqv = r3(wq, a=4); dwq = [Dep() for _ in range(4)]
    stg = Stager(k, 3, 2048)
    ev = [A.bf16(512), A.bf16(512)]; dev_ = [Dep(), Dep()]
    cnt = 0
    for wname, dst_d, ddst in [("dsa_w_uq", QT_d, d_QT), ("dsa_w_qidx", QIT_d, d_QIT)]:
        for kc in range(4):
            stg.load(W[wname][j, kc * 128:(kc + 1) * 128, :], wqv[:, kc, :], dwq[kc])
        for h in range(NH_):
            for sl_ in range(4):
                ps, dps = k.ps[cnt % 4], k.dps[cnt % 4]
                b = cnt % 2
                cnt += 1
                for kc in range(4):
                    MM(k, ps, wqv[:, kc, h * 128:(h + 1) * 128], CQTv[:, kc, sl_ * 512:(sl_ + 1) * 512], kc == 0, kc == 3, [dwq[kc], dCQT], [dps], kc == 3)
                if b == 0:
                    ACT(k, ev[b], ps, AF.Copy, [dps], [dev_[b]])
                else:
                    CP(k, "vector", ev[b], ps, [dps], [dev_[b]])
                DMA(k, "gpsimd", dst_d[:, h, sl_ * 512:(sl_ + 1) * 512], ev[b], [dev_[b]], [ddst])
    barrier(k)
    A.reset(mB)
    if getattr(k, 'dsa_stop', '') == 'B':
        return
    KT_d = k.dsa_KT; V_d = k.dsa_V
    dKT = Dep(); dV = Dep()
    mC = A.mark()
    evc = [A.bf16(512), A.bf16(512)]; devc = [Dep(), Dep()]
    stg = Stager(k, 3, 2048)
    wukT = A.bf16(4 * 2048); wukTv = wukT.rearrange("p (c h d) -> p c h d", c=4, h=NH_); dwukT = Dep()
    wuv = A.bf16(4 * 2048); wuvv = wuv.rearrange("p (c h d) -> p c h d", c=4, h=NH_); dwuv = Dep()
    uk = [A.f32(512), A.f32(512)]; duk = [Dep(), Dep()]
    for h in range(NH_):
        b = h % 2
        DMA(k, "sync", uk[b], W["dsa_w_uk"][j, h], [], [duk[b]])
        ps, dps = k.ps[4 + b], k.dps[4 + b]
        for kc in range(4):
            TR(k, ps[:, kc * 128:(kc + 1) * 128], uk[b][:, kc * 128:(kc + 1) * 128], k.ident, [duk[b]], [dps], kc == 3)
        ACT(k, wukTv[:, :, h, :], ps.rearrange("p (a b) -> p a b", a=4), AF.Copy, [dps], [dwukT])
    for h in range(NH_):
        stg.load(W["dsa_w_uv"][j, h].rearrange("(c p) d -> p c d", p=128), wuvv[:, :, h, :], dwuv)
    cnt = 0
    for h in range(NH_):
        for sl_ in range(4):
            ps, dps = k.ps[cnt % 4], k.dps[cnt % 4]
            cnt += 1
            for kc in range(4):
                MM(k, ps, wukTv[:, kc, h, :], CKVTv[:, kc, sl_ * 512:(sl_ + 1) * 512], kc == 0, kc == 3, [dwukT, dCKVT], [dps], kc == 3)
            b = cnt % 2
            if b == 0:
                ACT(k, evc[b], ps, AF.Copy, [dps], [devc[b]])
            else:
                CP(k, "vector", evc[b], ps, [dps], [devc[b]])
            DMA(k, "gpsimd", KT_d[h, :, sl_ * 512:(sl_ + 1) * 512], evc[b], [devc[b]], [dKT])
    for st_ in range(NT):
        for hg in range(4):
            ps, dps = k.ps[cnt % 4], k.dps[cnt % 4]
            cnt += 1
            for kc in range(4):
                MM(k, ps, CKVTv[:, kc, st_ * 128:(st_ + 1) * 128], wuvv[:, kc, hg * 4:(hg + 1) * 4, :], kc == 0, kc == 3, [dwuv, dCKVT], [dps], kc == 3)
            b = cnt % 2
            if b == 0:
                ACT(k, evc[b], ps, AF.Copy, [dps], [devc[b]])
            else:
                CP(k, "vector", evc[b], ps, [dps], [devc[b]])
            DMA(k, "gpsimd", V_d[hg * 4:(hg + 1) * 4, :, st_ * 128:(st_ + 1) * 128].rearrange("h p d -> p h d"), r3(evc[b], a=4), [devc[b]], [dV])
    barrier(k)
    A.reset(mC)
    if getattr(k, 'dsa_stop', '') == 'C':
        return
    Tn = A.f32(NH_ * 256); Tnv = r3(Tn, a=NH_); dTn = Dep()
    caus = A.f32(128); dcaus = Dep()
    DMA(k, "sync", caus, k.c["c_caus"], [], [dcaus])
    mT = A.mark()
    ohb = A.f32(32 * 256); ohbv = r3(ohb, a=32); dohb = Dep()
    rbB = A.f32(512); drbB = Dep()
    DMA(k, "sync", ohb, k.c["c_ohb"], [], [dohb])
    DMA(k, "sync", rbB, W["rel_bias"].rearrange("(o b) h -> o (b h)", o=1).partition_broadcast(128), [], [drbB])
    for h in range(NH_):
        eng = "vector"
        TS(k, eng, Tnv[:, h, :], ohbv[:, 0, :], rbB[:, h:h + 1], None, ALU.mult, None, [dohb, drbB], [dTn])
        for b_ in range(1, 32):
            STT(k, eng, Tnv[:, h, :], ohbv[:, b_, :], rbB[:, b_ * 16 + h:b_ * 16 + h + 1], Tnv[:, h, :], ALU.mult, ALU.add, [dohb, drbB, dTn], [dTn])
        TS(k, eng, Tnv[:, h, :], Tnv[:, h, :], rbB[:, 31 * 16 + h:31 * 16 + h + 1], None, ALU.subtract, None, [drbB, dTn], [dTn])
    barrier(k)
    A.reset(mT)
    accs = [A.f32(L), A.f32(L)]; daccs = [Dep(), Dep()]
    tmp = [A.f32(512), A.f32(512)]; dtmp = [Dep(), Dep()]
    madds = [A.bf16(L), A.bf16(L)]; dmadds = [Dep(), Dep()]
    Xs = [A.f32(L) for _ in range(3)]; dXs = [Dep() for _ in range(3)]
    Ps = [A.bf16(L) for _ in range(3)]; dPs = [Dep() for _ in range(3)]
    PTs = [A.bf16(NT * 128) for _ in range(3)]; dPTs = [Dep() for _ in range(3)]
    QIbs = [A.bf16(NH_ * 128), A.bf16(NH_ * 128)]; dQIbs = [Dep(), Dep()]
    QTbs = [A.bf16(NH_ * 128), A.bf16(NH_ * 128)]; dQTbs = [Dep(), Dep()]
    OT = [A.bf16(NH_ * 128), A.bf16(NH_ * 128)]; dOTs = [Dep(), Dep()]
    mx8 = A.f32(8); dmx = Dep()
    sm2s = [A.f32(8) for _ in range(3)]; dsm2s = [Dep() for _ in range(3)]
    dgrs = [A.bf16(128) for _ in range(3)]; ddgrs = [Dep() for _ in range(3)]
    Kh = [A.bf16(L) for _ in range(3)]; dKh = [Dep() for _ in range(3)]
    Vh = [A.bf16(L) for _ in range(3)]; dVh = [Dep() for _ in range(3)]
    z1 = A.f32(1); dz1 = Dep()
    k.S.op("vector", lambda e: e.memset(z1, 0.0), reads=[], writes=[dz1])

    def pre_thunks(jb):
        SL = (jb + 1) * 128
        nbk = (SL + 511) // 512
        pb = jb % 2
        acc, dacc = accs[pb], daccs[pb]
        madd, dmadd = madds[pb], dmadds[pb]
        QIbv = r3(QIbs[pb], a=NH_); dQIb = dQIbs[pb]
        th_ = []

        def t_load():
            DMA(k, "sync", QIbv, QIT_d[:, :, jb * 128:(jb + 1) * 128], [d_QIT], [dQIb])
        th_.append(t_load)
        cnt = [0]
        for h in range(NH_):
            def t_head(h=h):
                for bk in range(nbk):
                    w_ = min(512, SL - bk * 512)
                    bank = 4
                    ps, dps = k.ps[bank], k.dps[bank]
                    tb = cnt[0] % 2
                    cnt[0] += 1
                    MM(k, ps[:, 0:w_], QIbv[:, h, :], KIT[:, bk * 512:bk * 512 + w_], True, True, [dQIb, dKIT], [dps], True)
                    ACT(k, tmp[tb][:, 0:w_], ps[:, 0:w_], AF.Relu, [dps], [dtmp[tb]])
                    if h == 0:
                        TS(k, "vector", acc[:, bk * 512:bk * 512 + w_], tmp[tb][:, 0:w_], WIv[:, jb, h:h + 1], None, ALU.mult, None, [dtmp[tb], dWI], [dacc])
                    else:
                        STT(k, "vector", acc[:, bk * 512:bk * 512 + w_], tmp[tb][:, 0:w_], WIv[:, jb, h:h + 1], acc[:, bk * 512:bk * 512 + w_], ALU.mult, ALU.add, [dtmp[tb], dWI, dacc], [dacc])
            th_.append(t_head)

        def t_caus():
            TT(k, "gpsimd", acc[:, jb * 128:SL], acc[:, jb * 128:SL], caus, ALU.add, [dacc, dcaus], [dacc])
        th_.append(t_caus)
        if SL > 256:
            for r in range(getattr(k, 'topk_rounds', 32)):
                def t_round():
                    k.S.op("vector", lambda e: e.max(out=mx8, in_=acc[:, 0:SL]), reads=[dacc], writes=[dmx])
                    k.S.op("vector", lambda e: e.match_replace(out=acc[:, 0:SL], in_to_replace=mx8, in_values=acc[:, 0:SL], imm_value=-2.0e30), reads=[dacc, dmx], writes=[dacc])
                th_.append(t_round)

            def t_fin():
                TS(k, "vector", madd[:, 0:SL], acc[:, 0:SL], -1.5e30, NEG, ALU.is_gt, ALU.mult, [dacc], [dmadd])
        else:
            def t_fin():
                TS(k, "vector", madd[:, 0:SL], acc[:, 0:SL], -1.0e29, NEG, ALU.is_lt, ALU.mult, [dacc], [dmadd])
        th_.append(t_fin)
        return th_

    NB3 = 3
    dPV = [Dep(), Dep()]

    def bufs(i):
        b3 = i % NB3
        return (Xs[b3], dXs[b3], Ps[b3], dPs[b3], r3(PTs[b3], a=NT), dPTs[b3], sm2s[b3], dsm2s[b3], dgrs[b3], ddgrs[b3], Kh[b3], dKh[b3], Vh[b3], dVh[b3])

    def stageA(i):
        jb, h = divmod(i, NH_)
        SL = (jb + 1) * 128
        nbk = (SL + 511) // 512
        pb = jb % 2
        madd, dmadd = madds[pb], dmadds[pb]
        QTbv = r3(QTbs[pb], a=NH_); dQTb = dQTbs[pb]
        X, dX, P, dP, PTv, dPT, sm2, dsm2, dgr, ddgr, Kb, dKb, Vb, dVb = bufs(i)
        if h == 0:
            DMA(k, "sync", QTbv, QT_d[:, :, jb * 128:(jb + 1) * 128], [d_QT], [dQTb])
        DMA(k, "sync", Kb[:, 0:SL], KT_d[h, :, 0:SL], [dKT], [dKb])
        DMA(k, "sync", Vb[:, 0:SL], V_d[h, :, 0:SL], [dV], [dVb])
        for bk in range(nbk):
            w_ = min(512, SL - bk * 512)
            bank = (bk % 2) + 2 * (i % 2)
            ps, dps = k.ps[bank], k.dps[bank]
            MM(k, ps[:, 0:w_], QTbv[:, h, :], Kb[:, bk * 512:bk * 512 + w_], True, True, [dQTb, dKb], [dps], True)
            STT(k, "vector", X[:, bk * 512:bk * 512 + w_], ps[:, 0:w_], att_scale, madd[:, bk * 512:bk * 512 + w_], ALU.mult, ALU.add, [dps, dmadd], [dX])
        lo = max(0, jb - 1) * 128
        tlo = 0 if jb >= 1 else 128
        TT(k, "vector", X[:, lo:SL], X[:, lo:SL], Tnv[:, h, tlo:256], ALU.add, [dX, dTn], [dX])
        rmax = sm2[:, 0:1]; nmax = sm2[:, 1:2]; rsum = sm2[:, 2:3]
        k.S.op("vector", lambda e: e.tensor_reduce(out=rmax, in_=X[:, 0:SL], axis=AX.X, op=ALU.max), reads=[dX], writes=[dsm2])
        TS(k, "vector", nmax, rmax, -1.0, None, ALU.mult, None, [dsm2], [dsm2])
        ACT(k, P[:, 0:SL], X[:, 0:SL], AF.Exp, [dX, dsm2], [dP, dsm2], bias=nmax, scale=1.0, accum_out=rsum)

    def stageB(i):
        jb, h = divmod(i, NH_)
        X, dX, P, dP, PTv, dPT, sm2, dsm2, dgr, ddgr, Kb, dKb, Vb, dVb = bufs(i)
        rsum = sm2[:, 2:3]; rinv = sm2[:, 3:4]
        k.S.op("vector", lambda e: e.reciprocal(out=rinv, in_=rsum), reads=[dsm2], writes=[dsm2])
        TS(k, "vector", dgr, identb, rinv, None, ALU.mult, None, [dsm2, didb], [ddgr])
        for st_ in range(jb + 1):
            bank = 6 + (st_ // 4) % 2
            ps, dps = k.ps[bank], k.dps[bank]
            last = (st_ % 4 == 3) or (st_ == jb)
            MM(k, ps[:, (st_ % 4) * 128:(st_ % 4 + 1) * 128], P[:, st_ * 128:(st_ + 1) * 128], dgr, True, True, [dP, ddgr], [dps], last)
            if last:
                s0 = (st_ // 4) * 4
                n_ = st_ - s0 + 1
                ACT(k, PTv[:, s0:s0 + n_, :], ps[:, 0:n_ * 128].rearrange("p (a b) -> p a b", a=n_), AF.Copy, [dps], [dPT])
        Vhv = r3(Vb, a=NT)
        pv = k.ps[5][:, (i % 2) * 128:(i % 2 + 1) * 128]
        for st_ in range(jb + 1):
            MM(k, pv, Vhv[:, st_, :], PTv[:, st_, :], st_ == 0, st_ == jb, [dVb, dPT], [dPV[i % 2]], st_ == jb)

    def stageC(i):
        jb, h = divmod(i, NH_)
        pb = jb % 2
        ot, dot = OT[pb], dOTs[pb]
        otv = r3(ot, a=NH_)
        pv = k.ps[5][:, (i % 2) * 128:(i % 2 + 1) * 128]
        CP(k, "scalar", otv[:, h, :], pv, [dPV[i % 2]], [dot])
        if h == NH_ - 1:
            DMA(k, "gpsimd", OT_d[jb], ot, [dot], [d_OT[jb]])

    for t_ in pre_thunks(0):
        t_()
    NHEADS = NT * NH_
    nxt = []
    pos = 0
    per = 0
    for i in range(NHEADS + 2):
        if i < NHEADS:
            jb, h = divmod(i, NH_)
            if h == 0:
                for t_ in nxt[pos:]:
                    t_()
                nxt = pre_thunks(jb + 1) if jb + 1 < NT else []
                per = (len(nxt) + NH_ - 1) // NH_
                pos = 0
            stageA(i)
        if 0 <= i - 1 < NHEADS:
            stageB(i - 1)
        if 0 <= i - 2 < NHEADS:
            stageC(i - 2)
        if i < NHEADS:
            for t_ in nxt[pos:pos + per]:
                t_()
            pos += per
    barrier(k)
    A.reset(m_phase)
    if getattr(k, 'dsa_stop', '') == 'D':
        return
    phase_outproj_ln(k, OT_d, d_OT, W["dsa_w_out"][j], W["ln_mix_g"][li], W["ln_mix_b"][li], h_in, h_out)
import math as _math

S5_KVEC = [0, -1, -2, -3, -4, -5, -6, -7, 7, 6, 5, 4, 3, 2, 1, 0, 0, 1, 2, 3, 4, 5, 6, 7, 1, 2, 3, 4, 5, 6, 7, 8, 8, 16, 32, 64, 128, 256, 512, 1024]
NK = 40


def s5_consts():
    kv = np.tile(np.array(S5_KVEC, np.float32)[None, :], (128, 1))
    s_ = (np.arange(128) // 16)[:, None]
    t_ = (np.arange(128) // 16)[None, :]
    msk = (t_ >= s_).astype(np.float32)
    import ml_dtypes
    sel = np.zeros((128, 64, 128), np.float32)
    for g8 in range(8):
        for s in range(8):
            for p_ in range(16):
                sel[g8 * 16 + p_, g8 * 8 + s, s * 16 + p_] = 1.0
    selT = np.ascontiguousarray(sel.transpose(2, 1, 0))
    return {"c_kv40": kv, "c_s5mask": msk,
            "c_sel": sel.reshape(128, 8192).astype(ml_dtypes.bfloat16),
            "c_selT": selT.reshape(128, 8192).astype(ml_dtypes.bfloat16)}


def bc(ap, axis, shape):
    return ap.unsqueeze(axis).to_broadcast(list(shape))


def phase_s5(k, sj, li, h_in, h_out):
    S = k.S
    A = k.A
    W = k.w
    M_d, W1_d, W2_d, ZT_d = k.s5_M, k.s5_W1, k.s5_W2, k.s5_ZT
    dM = Dep(); dW1 = Dep(); dW2 = Dep()
    d_ZT = [Dep() for _ in range(NT)]
    TWO_PI = 2.0 * _math.pi
    m_phase = A.mark()
    DCOL = A.f32(128); dDCOL = Dep()
    ASr = A.f32(512); ASi = A.f32(512); ASn = A.f32(512); dAS = Dep()
    ASrv = r3(ASr, a=64); ASiv = r3(ASi, a=64); ASnv = r3(ASn, a=64)
    mP = A.mark()
    KV = A.f32(NK); dKV = Dep()
    DMA(k, "sync", KV, k.c["c_kv40"], [], [dKV])
    MASK = A.f32(128); dMASK = Dep()
    DMA(k, "sync", MASK, k.c["c_s5mask"], [], [dMASK])
    PAre = A.f32(64); PAim = A.f32(64); PDT = A.f32(64); dPA = Dep()
    PBre = A.f32(1024); PBim = A.f32(1024); PCre = A.f32(1024); PCim = A.f32(1024); dPB = Dep(); dPC = Dep()
    PBrev = r3(PBre, a=64); PBimv = r3(PBim, a=64); PCrev = r3(PCre, a=64); PCimv = r3(PCim, a=64)
    ld = [A.f32(2048), A.f32(2048)]; dld = [Dep(), Dep()]
    ld2 = A.f32(2048); dld2 = Dep()
    id64 = k.ident[0:64, 0:64]
    DMA(k, "sync", ld[0][:, 0:16], W["s5_d"][sj], [], [dld[0]])
    CP(k, "vector", ld[0][:, 16:144].rearrange("p (t q) -> p t q", t=8), bc(ld[0][:, 0:16], 1, [128, 8, 16]), [dld[0]], [dld[0]])
    TR(k, k.ps[0][:, 0:128], ld[0][:, 16:144], k.ident, [dld[0]], [k.dps[0]], True)
    CP(k, "vector", DCOL, k.ps[0][:, 0:128], [k.dps[0]], [dDCOL])
    DMA(k, "sync", ld[1][0:64, 0:128], W["s5_a_re"][sj].rearrange("(j g2) n -> j (g2 n)", g2=2), [], [dld[1]])
    DMA(k, "sync", ld[1][0:64, 128:256], W["s5_a_im"][sj].rearrange("(j g2) n -> j (g2 n)", g2=2), [], [dld[1]])
    DMA(k, "sync", ld[1][0:64, 256:258], W["s5_log_dt"][sj].rearrange("(j g2) -> j g2", g2=2), [], [dld[1]])
    CP(k, "vector", ld[1][0:64, 384:512].rearrange("p (g n) -> p g n", g=2), bc(ld[1][0:64, 256:258], 2, [64, 2, 64]), [dld[1]], [dld[1]])
    for i_, (c0, dst) in enumerate([(0, PAre), (128, PAim), (384, PDT)]):
        TR(k, k.ps[1][:, i_ * 64:(i_ + 1) * 64], ld[1][0:64, c0:c0 + 128], id64, [dld[1]], [k.dps[1]], True)
        CP(k, "vector", dst, k.ps[1][:, i_ * 64:(i_ + 1) * 64], [k.dps[1]], [dPA])
    cnt_ = 0
    for name, dstv, ddst, is_c in [("s5_b_re", PBrev, dPB, False), ("s5_b_im", PBimv, dPB, False), ("s5_c_re", PCrev, dPC, True), ("s5_c_im", PCimv, dPC, True)]:
        lb = ld[cnt_ % 2]; dlb = dld[cnt_ % 2]
        cnt_ += 1
        if is_c:
            DMA(k, "sync", lb[0:64, :], W[name][sj].rearrange("(j g2) p n -> j (g2 p n)", g2=2), [], [dlb])
            lb2 = ld2[0:64, :]
            CP(k, "vector", lb2.rearrange("j (p g n) -> j p g n", p=16, g=2), lb[0:64, :].rearrange("j (g p n) -> j p g n", g=2, p=16), [dlb, dld2], [dld2])
            lv = lb2.rearrange("j (p gn) -> j p gn", p=16)
        else:
            DMA(k, "sync", lb[0:64, :], W[name][sj].rearrange("(j g2) n p -> j (g2 n p)", g2=2), [], [dlb])
            lv = lb[0:64, :].rearrange("j (gn p) -> j gn p", p=16)
        for q4 in range(2):
            bank = 2 + q4
            ps, dps = k.ps[bank], k.dps[bank]
            for p8 in range(8):
                p_ = q4 * 8 + p8
                src = lv[:, p_, :] if is_c else lv[:, :, p_]
                TR(k, ps[:, p8 * 64:(p8 + 1) * 64], src, id64, [dlb, dld2], [dps], p8 == 7)
            CP(k, "vector", dstv[:, :, q4 * 8:(q4 + 1) * 8].rearrange("p j q -> p q j"), ps.rearrange("p (q j) -> p q j", q=8), [dps], [ddst])
    if getattr(k, 's5_stop', '') == 'P1':
        barrier(k)
        return
    dE = Dep()
    lr = A.f32(64); ldr = A.f32(64); th = A.f32(64); dtt = A.f32(64)
    TS(k, "vector", lr, PAre, -1.0e-4, None, ALU.min, None, [dPA], [dE])
    ACT(k, dtt, PDT, AF.Exp, [dPA], [dE])
    TT(k, "vector", ldr, lr, dtt, ALU.mult, [dE], [dE])
    TT(k, "vector", th, PAim, dtt, ALU.mult, [dE, dPA], [dE])
    NKK = 64 * NK
    shp = [128, 64, NK]
    ARG = A.f32(NKK); PHI = A.f32(NKK); RHO = A.f32(NKK); QF = A.f32(NKK); MSK2 = A.f32(NKK)
    ARE = A.f32(NKK); AIM = A.f32(NKK)
    QI = A.f32(NKK).bitcast(I32)
    v3 = lambda t_: r3(t_, a=64)
    TT(k, "vector", v3(ARG), bc(ldr, 2, shp), bc(KV, 1, shp), ALU.mult, [dE, dKV], [dE])
    ACT(k, RHO, ARG, AF.Exp, [dE], [dE])
    TT(k, "vector", v3(PHI), bc(th, 2, shp), bc(KV, 1, shp), ALU.mult, [dE, dKV], [dE])

    def sin_of(dst, off):
        TS(k, "vector", ARG, PHI, off, None, ALU.add, None, [dE], [dE])
        TS(k, "vector", QF, ARG, 1.0 / TWO_PI, None, ALU.mult, None, [dE], [dE])
        CP(k, "vector", QI, QF, [dE], [dE])
        CP(k, "vector", QF, QI, [dE], [dE])
        STT(k, "vector", ARG, QF, -TWO_PI, ARG, ALU.mult, ALU.add, [dE], [dE])
        TS(k, "vector", MSK2, ARG, _math.pi, -TWO_PI, ALU.is_gt, ALU.mult, [dE], [dE])
        TT(k, "vector", ARG, ARG, MSK2, ALU.add, [dE], [dE])
        TS(k, "vector", MSK2, ARG, -_math.pi, TWO_PI, ALU.is_lt, ALU.mult, [dE], [dE])
        TT(k, "vector", ARG, ARG, MSK2, ALU.add, [dE], [dE])
        ACT(k, dst, ARG, AF.Sin, [dE], [dE])

    sin_of(AIM, 64.0 * _math.pi)
    sin_of(ARE, 64.5 * _math.pi)
    TT(k, "vector", AIM, AIM, RHO, ALU.mult, [dE], [dE])
    TT(k, "vector", ARE, ARE, RHO, ALU.mult, [dE], [dE])
    AREv = v3(ARE); AIMv = v3(AIM)
    CP(k, "vector", ASrv, AREv[:, :, 32:40], [dE], [dAS])
    CP(k, "vector", ASiv, AIMv[:, :, 32:40], [dE], [dAS])
    TS(k, "vector", ASnv, AIMv[:, :, 32:40], -1.0, None, ALU.mult, None, [dE], [dAS])
    er = A.f32(64); ei = A.f32(64); qr = A.f32(64); qi_ = A.f32(64); den = A.f32(64); t1 = A.f32(64); fr = A.f32(64); fi = A.f32(64)
    TS(k, "vector", er, AREv[:, :, 24], -1.0, None, ALU.add, None, [dE], [dE])
    CP(k, "vector", ei, AIMv[:, :, 24], [dE], [dE])
    TT(k, "vector", qr, er, lr, ALU.mult, [dE], [dE])
    TT(k, "vector", t1, ei, PAim, ALU.mult, [dE], [dE])
    TT(k, "vector", qr, qr, t1, ALU.add, [dE], [dE])
    TT(k, "vector", qi_, ei, lr, ALU.mult, [dE], [dE])
    TT(k, "vector", t1, er, PAim, ALU.mult, [dE], [dE])
    TT(k, "vector", qi_, qi_, t1, ALU.subtract, [dE], [dE])
    TT(k, "vector", den, lr, lr, ALU.mult, [dE], [dE])
    TT(k, "vector", t1, PAim, PAim, ALU.mult, [dE], [dE])
    TT(k, "vector", den, den, t1, ALU.add, [dE], [dE])
    k.S.op("vector", lambda e: e.reciprocal(out=den, in_=den), reads=[dE], writes=[dE])
    TT(k, "vector", fr, qr, den, ALU.mult, [dE], [dE])
    TT(k, "vector", fi, qi_, den, ALU.mult, [dE], [dE])
    BBre = A.f32(1024); BBim = A.f32(1024); tb_ = A.f32(1024)
    BBrev = r3(BBre, a=64); BBimv = r3(BBim, a=64); tbv = r3(tb_, a=64)
    s16 = [128, 64, 16]
    TT(k, "vector", BBrev, bc(fr, 2, s16), PBrev, ALU.mult, [dE, dPB], [dE])
    TT(k, "vector", tbv, bc(fi, 2, s16), PBimv, ALU.mult, [dE, dPB], [dE])
    TT(k, "vector", BBre, BBre, tb_, ALU.subtract, [dE], [dE])
    TT(k, "vector", BBimv, bc(fr, 2, s16), PBimv, ALU.mult, [dE, dPB], [dE])
    TT(k, "vector", tbv, bc(fi, 2, s16), PBrev, ALU.mult, [dE, dPB], [dE])
    TT(k, "vector", BBim, BBim, tb_, ALU.add, [dE], [dE])
    if getattr(k, 's5_stop', '') == 'P2':
        barrier(k)
        return
    PB_ = 8
    PM = A.f32(2); dPM = Dep()
    k.S.op("vector", lambda e: e.memset(PM, 0.0), reads=[], writes=[dPM])
    k.S.op("vector", lambda e: e.memset(PM[0:64, 0:1], 1.0), reads=[dPM], writes=[dPM])
    k.S.op("vector", lambda e: e.memset(PM[64:128, 1:2], 1.0), reads=[dPM], writes=[dPM])
    LM = [[A.bf16(1024), A.bf16(1024)], [A.bf16(1024), A.bf16(1024)]]
    T = [A.f32(1024) for _ in range(4)]
    Tv = [t_.rearrange("p (j s q) -> p j s q", j=PB_, s=8) for t_ in T]
    RREb = A.bf16(1024); RIMb = A.bf16(1024)
    L2RE = A.f32(1024); L2IM = A.f32(1024)
    W2o = A.bf16(4096)
    W2ov = W2o.rearrange("p (j r m) -> p j r m", j=PB_, r=4)
    Mout = [A.bf16(512), A.bf16(512)]; dMout = [Dep(), Dep()]
    TAB = [A.bf16(512), A.bf16(512)]; dTAB = [Dep(), Dep()]
    for tb2 in TAB:
        k.S.op("vector", lambda e, tb2=tb2: e.memset(tb2, 0.0), reads=[], writes=[dE])
    dCh = Dep()
    s4 = [128, PB_, 8, 16]
    j8 = lambda t_: r3(t_, a=PB_)

    def products(blk, Bre, Bim, j0):
        a0 = blk * 8
        Ar = AREv[:, j0:j0 + PB_, a0:a0 + 8]; Ai = AIMv[:, j0:j0 + PB_, a0:a0 + 8]
        br = Bre[:, j0:j0 + PB_, :]; bi = Bim[:, j0:j0 + PB_, :]
        TT(k, "vector", Tv[0], bc(Ar, 3, s4), bc(br, 2, s4), ALU.mult, [dE, dPC, dCh], [dCh])
        TT(k, "vector", Tv[1], bc(Ai, 3, s4), bc(bi, 2, s4), ALU.mult, [dE, dPC, dCh], [dCh])
        TT(k, "vector", Tv[2], bc(Ai, 3, s4), bc(br, 2, s4), ALU.mult, [dE, dPC, dCh], [dCh])
        TT(k, "vector", Tv[3], bc(Ar, 3, s4), bc(bi, 2, s4), ALU.mult, [dE, dPC, dCh], [dCh])

    mcnt = 0
    for ch in range(64 // PB_):
        j0 = ch * PB_
        products(0, BBrev, BBimv, j0)
        TT(k, "vector", T[0], T[0], T[1], ALU.subtract, [dCh], [dCh])
        TT(k, "vector", T[2], T[2], T[3], ALU.add, [dCh], [dCh])
        for g2 in range(2):
            TS(k, "vector", LM[g2][0], T[0], PM[:, g2:g2 + 1], None, ALU.mult, None, [dCh, dPM], [dCh])
            TS(k, "vector", LM[g2][1], T[2], PM[:, g2:g2 + 1], None, ALU.mult, None, [dCh, dPM], [dCh])
        products(2, PCrev, PCimv, j0)
        TT(k, "vector", RREb, T[0], T[1], ALU.subtract, [dCh], [dCh])
        STT(k, "vector", RIMb, T[2], -1.0, T[3], ALU.mult, ALU.subtract, [dCh], [dCh])
        for half in range(PB_ // 2):
            bank = mcnt % 2
            mo, dmo = Mout[mcnt % 2], dMout[mcnt % 2]
            mcnt += 1
            ps, dps = k.ps[bank], k.dps[bank]
            for q in range(4):
                jj = half * 2 + q // 2
                g2 = q % 2
                MM(k, ps[:, q * 128:(q + 1) * 128], j8(LM[g2][0])[:, jj, :], j8(RREb)[:, jj, :], True, False, [dCh], [dps], False)
                MM(k, ps[:, q * 128:(q + 1) * 128], j8(LM[g2][1])[:, jj, :], j8(RIMb)[:, jj, :], False, True, [dCh], [dps], q == 3)
            TT(k, "vector", r3(mo, a=4), r3(ps, a=4), bc(MASK, 1, [128, 4, 128]), ALU.mult, [dps, dMASK], [dmo])
            g0 = (j0 + half * 2) * 2
            for q in range(4):
                DMA(k, "gpsimd", M_d[g0 + q], mo[:, q * 128:(q + 1) * 128], [dmo], [dM])
        if getattr(k, 's5_stop', '') == 'P3a':
            continue
        products(1, BBrev, BBimv, j0)
        TT(k, "vector", L2RE, T[0], T[1], ALU.subtract, [dCh], [dCh])
        TT(k, "vector", L2IM, T[2], T[3], ALU.add, [dCh], [dCh])
        for jj in range(PB_):
            bank = 2 + jj % 2
            ps, dps = k.ps[bank], k.dps[bank]
            tab, dtab = TAB[jj % 2], dTAB[jj % 2]
            TR(k, ps[:, 0:128], j8(L2RE)[:, jj, :], k.ident, [dCh], [dps], False)
            TR(k, ps[:, 128:256], j8(L2IM)[:, jj, :], k.ident, [dCh], [dps], True)
            tabv = tab.rearrange("p (r a m) -> p r a m", r=2, a=2)
            psv = ps[:, 0:256].rearrange("p (r m) -> p r m", r=2)
            CP(k, "vector", tabv[:, :, 0, 0:64], psv[:, :, 0:64], [dps], [dtab])
            CP(k, "vector", tabv[:, :, 1, 64:128], psv[:, :, 64:128], [dps], [dtab])
            DMA(k, "gpsimd", W1_d[j0 + jj], tab, [dtab], [dW1])
        if getattr(k, 's5_stop', '') == 'P3b':
            continue
        products(3, PCrev, PCimv, j0)
        TT(k, "vector", T[0], T[0], T[1], ALU.subtract, [dCh], [dCh])
        STT(k, "vector", T[2], T[2], -1.0, T[3], ALU.mult, ALU.subtract, [dCh], [dCh])
        for g2 in range(2):
            TS(k, "vector", W2ov[:, :, 2 * g2, :], j8(T[0]), PM[:, g2:g2 + 1], None, ALU.mult, None, [dCh, dPM], [dCh])
            TS(k, "vector", W2ov[:, :, 2 * g2 + 1, :], j8(T[2]), PM[:, g2:g2 + 1], None, ALU.mult, None, [dCh, dPM], [dCh])
        for jj in range(PB_):
            DMA(k, "gpsimd", W2_d[j0 + jj], W2o[:, jj * 512:(jj + 1) * 512], [dCh], [dW2, dCh])
    barrier(k)
    A.reset(mP)
    if getattr(k, 's5_stop', '') in ('P', 'P3a', 'P3b'):
        return
    R1 = A.bf16(16 * 2048)
    R1v = R1.rearrange("p (b s c) -> p b s c", b=16, s=8)
    dR1 = Dep()
    mR2 = A.mark()
    HT = A.bf16(NT * 2048); HTv = HT.rearrange("p (t c x) -> p t c x", t=NT, c=16); dHT = Dep()
    for t in range(NT):
        DMA(k, "sync", HTv[:, t].rearrange("p c x -> p (c x)"), k.hT[t], [k.d_hT[t]], [dHT])
    stg = Stager(k, 3, 2048)
    wcb = [A.bf16(2048), A.bf16(2048)]; dwcb = [Dep(), Dep()]
    cnt = 0
    for chb in range(16):
        b = chb % 2
        stg.load(W["s5_w_in"][sj].rearrange("(kc p) n -> p kc n", p=128)[:, :, chb * 128:(chb + 1) * 128], r3(wcb[b], a=16), dwcb[b])
        for sl_ in range(4):
            ps, dps = k.ps[cnt % 4], k.dps[cnt % 4]
            cnt += 1
            for kc in range(16):
                MM(k, ps, r3(wcb[b], a=16)[:, kc, :], HTv[:, sl_ * 4:(sl_ + 1) * 4, kc, :], kc == 0, kc == 15, [dwcb[b], dHT], [dps], kc == 15)
            src = ps.rearrange("p (c s) -> p s c", s=8)
            dst = R1v[:, chb, :, sl_ * 64:(sl_ + 1) * 64]
            if cnt % 2 == 0:
                ACT(k, dst, src, AF.Copy, [dps], [dR1])
            else:
                CP(k, "vector", dst, src, [dps], [dR1])
    barrier(k)
    A.reset(mR2)
    if getattr(k, 's5_stop', '') == 'U':
        return
    R2 = A.bf16(128 * 256)
    Xv = r3(R2, a=128)
    dX = Dep()
    SEL = A.bf16(8192); SELv = r3(SEL, a=64); SELT = A.bf16(8192); SELTv = r3(SELT, a=64); dSEL = Dep()
    DMA(k, "sync", SEL, k.c["c_sel"], [], [dSEL])
    DMA(k, "sync", SELT, k.c["c_selT"], [], [dSEL])
    for g0 in range(0, 128, 2):
        bank = (g0 // 2) % 4
        ps, dps = k.ps[bank], k.dps[bank]
        for gi in range(2):
            g = g0 + gi
            for s_ in range(8):
                MM(k, ps[:, gi * 256:(gi + 1) * 256], SELv[:, (g % 8) * 8 + s_, :], R1v[:, g // 8, s_, :], s_ == 0, s_ == 7, [dSEL, dR1], [dps], (gi == 1 and s_ == 7))
        if (g0 // 2) % 2 == 0:
            ACT(k, Xv[:, g0:g0 + 2, :], r3(ps, a=2), AF.Copy, [dps], [dX])
        else:
            CP(k, "vector", Xv[:, g0:g0 + 2, :], r3(ps, a=2), [dps], [dX])
    barrier(k)
    if getattr(k, 's5_stop', '') == 'X':
        return
    mL = A.mark()
    dZF = Dep()
    Mg = [A.bf16(256) for _ in range(4)]; W1t = [A.bf16(512), A.bf16(512)]; W2t = [A.bf16(512) for _ in range(4)]
    dMg = [Dep() for _ in range(4)]; dW1t = [Dep(), Dep()]; dW2t = [Dep() for _ in range(4)]
    REs = [[A.f32(384), A.f32(384)] for _ in range(2)]; IMs = [[A.f32(384), A.f32(384)] for _ in range(2)]
    dSCs = [Dep(), Dep()]
    for sl_ in range(2):
        for t_ in REs[sl_] + IMs[sl_]:
            k.S.op("vector", lambda e, t_=t_: e.memset(t_, 0.0), reads=[], writes=[dSCs[sl_]])
    HREs = [A.bf16(256), A.bf16(256)]; HIMs = [A.bf16(256), A.bf16(256)]; dHs = [Dep(), Dep()]
    ybs = [[A.f32(256), A.f32(256)] for _ in range(2)]; y2bs = [[A.f32(256), A.f32(256)] for _ in range(2)]
    dybs = [[Dep(), Dep()], [Dep(), Dep()]]
    Zall = [A.bf16(8 * 256), A.bf16(8 * 256)]; dZall = [Dep(), Dep()]

    def st_S(j):
        b = j % 2
        b4 = j % 4
        for g2 in range(2):
            DMA(k, "sync", Mg[b4][:, g2 * 128:(g2 + 1) * 128], M_d[2 * j + g2], [dM], [dMg[b4]])
        DMA(k, "sync", W1t[b], W1_d[j], [dW1], [dW1t[b]])
        DMA(k, "sync", W2t[b4], W2_d[j], [dW2], [dW2t[b4]])
        ps, dps = k.ps[b], k.dps[b]
        for ri in range(2):
            MM(k, ps[:, ri * 256:(ri + 1) * 256], W1t[b][:, (2 * ri) * 128:(2 * ri + 1) * 128], Xv[:, 2 * j, :], True, False, [dW1t[b], dX], [dps], False)
            MM(k, ps[:, ri * 256:(ri + 1) * 256], W1t[b][:, (2 * ri + 1) * 128:(2 * ri + 2) * 128], Xv[:, 2 * j + 1, :], False, True, [dW1t[b], dX], [dps], ri == 1)
        ACT(k, REs[b][0][:, 128:384], ps[:, 0:256], AF.Copy, [dps], [dSCs[b]])
        ACT(k, IMs[b][0][:, 128:384], ps[:, 256:512], AF.Copy, [dps], [dSCs[b]])

    def st_scan_step(j, i):
        b = j % 2
        cur = i % 2
        sft = 1 << i
        ra, ia = REs[b][cur], IMs[b][cur]
        rb_, ib_ = REs[b][1 - cur], IMs[b][1 - cur]
        ar = ASrv[:, j, i:i + 1]; ai = ASiv[:, j, i:i + 1]; an = ASnv[:, j, i:i + 1]
        d_ = dSCs[b]
        STT(k, "vector", rb_[:, 128:384], ra[:, 128 - sft:384 - sft], ar, ra[:, 128:384], ALU.mult, ALU.add, [d_, dAS], [d_])
        STT(k, "vector", rb_[:, 128:384], ia[:, 128 - sft:384 - sft], an, rb_[:, 128:384], ALU.mult, ALU.add, [d_, dAS], [d_])
        STT(k, "vector", ib_[:, 128:384], ia[:, 128 - sft:384 - sft], ar, ia[:, 128:384], ALU.mult, ALU.add, [d_, dAS], [d_])
        STT(k, "vector", ib_[:, 128:384], ra[:, 128 - sft:384 - sft], ai, ib_[:, 128:384], ALU.mult, ALU.add, [d_, dAS], [d_])

    def st_cast(j):
        b = j % 2
        ACT(k, HREs[b], REs[b][0][:, 127:383], AF.Copy, [dSCs[b]], [dHs[b]])
        ACT(k, HIMs[b], IMs[b][0][:, 127:383], AF.Copy, [dSCs[b]], [dHs[b]])

    def st_Y(j):
        b = j % 2
        for g2 in range(2):
            g = 2 * j + g2
            py, dpy = k.ps[2 + g2], k.dps[2 + g2]
            b4 = j % 4
            MM(k, py[:, 0:256], r3(Mg[b4], a=2)[:, g2, :], Xv[:, g, :], True, False, [dMg[b4], dX], [dpy], False)
            MM(k, py[:, 0:256], W2t[b4][:, (2 * g2) * 128:(2 * g2 + 1) * 128], HREs[b], False, False, [dW2t[b4], dHs[b]], [dpy], False)
            MM(k, py[:, 0:256], W2t[b4][:, (2 * g2 + 1) * 128:(2 * g2 + 2) * 128], HIMs[b], False, True, [dW2t[b4], dHs[b]], [dpy], True)
            y = ybs[b][g2]; y2 = y2bs[b][g2]; dy_ = dybs[b][g2]
            STT(k, "vector", y, Xv[:, g, :], DCOL[:, g:g + 1], py[:, 0:256], ALU.mult, ALU.add, [dpy, dX, dDCOL], [dy_])
            TT(k, "gpsimd", y2, y, y, ALU.mult, [dy_], [dy_])
            TS(k, "gpsimd", y2, y2, 0.044715, 1.0, ALU.mult, ALU.add, [dy_], [dy_])
            TT(k, "gpsimd", y2, y2, y, ALU.mult, [dy_], [dy_])
            ACT(k, y2, y2, AF.Sigmoid, [dy_], [dy_], scale=1.5957691216057308)
            zb = (g // 8) % 2
            TT(k, "gpsimd", r3(Zall[zb], a=8)[:, g % 8, :], y, y2, ALU.mult, [dy_, dZall[zb]], [dZall[zb]])
        if j % 4 == 3:
            chb = j // 4
            zb = chb % 2
            for sp in range(4):
                bank = 4 + sp % 4
                ps2, dps2 = k.ps[bank], k.dps[bank]
                for si in range(2):
                    s_ = sp * 2 + si
                    for g8 in range(8):
                        MM(k, ps2[:, si * 256:(si + 1) * 256], SELTv[:, g8 * 8 + s_, :], r3(Zall[zb], a=8)[:, g8, :], g8 == 0, g8 == 7, [dSEL, dZall[zb]], [dps2], (si == 1 and g8 == 7))
                if sp % 2 == 0:
                    ACT(k, R1v[:, chb, sp * 2:sp * 2 + 2, :], r3(ps2, a=2), AF.Copy, [dps2], [dZF])
                else:
                    CP(k, "vector", R1v[:, chb, sp * 2:sp * 2 + 2, :], r3(ps2, a=2), [dps2], [dZF])

    for c in range(0, 64, 2):
        st_S(c)
        st_S(c + 1)
        if c >= 2:
            st_Y(c - 2)
            st_Y(c - 1)
        for i in range(8):
            st_scan_step(c, i)
            st_scan_step(c + 1, i)
        st_cast(c)
        st_cast(c + 1)
    st_Y(62)
    st_Y(63)
    barrier(k)
    A.reset(mR2)
    if getattr(k, 's5_stop', '') == 'L':
        return
    Z2N = A.bf16(NT * 2048)
    Z2Nv = Z2N.rearrange("p (t c l s) -> p t c l s", t=NT, c=16, l=16)
    dZ2 = Dep()
    stg = Stager(k, 3, 2048)
    wgb = [A.bf16(2048), A.bf16(2048)]; dwgb = [Dep(), Dep()]
    sg = [A.f32(512), A.f32(512)]; dsg = [Dep(), Dep()]
    cnt = 0
    for nb in range(16):
        b = nb % 2
        stg.load(W["s5_w_glu"][sj].rearrange("(kc p) n -> p kc n", p=128)[:, :, nb * 128:(nb + 1) * 128], r3(wgb[b], a=16), dwgb[b])
        for q in range(4):
            ps, dps = k.ps[cnt % 4], k.dps[cnt % 4]
            sb_ = cnt % 2
            cnt += 1
            zsl = lambda kc: R1v[:, kc, 2 * q:2 * q + 2, :]
            for kc in range(16):
                MM(k, ps, r3(wgb[b], a=16)[:, kc, :], zsl(kc), kc == 0, kc == 15, [dwgb[b], dZF], [dps], kc == 15)
            ACT(k, sg[sb_], ps, AF.Sigmoid, [dps], [dsg[sb_]])
            dst = Z2Nv[:, :, nb, :, 2 * q:2 * q + 2].rearrange("p t l s -> p s t l")
            in0 = sg[sb_].rearrange("p (s t l) -> p s t l", s=2, t=16)
            in1 = R1v[:, nb, 2 * q:2 * q + 2, :].rearrange("p s (t l) -> p s t l", t=16)
            TT(k, "vector", dst, in0, in1, ALU.mult, [dsg[sb_], dZF], [dZ2])
    for t in range(NT):
        DMA(k, "gpsimd", ZT_d[t], Z2N[:, t * 2048:(t + 1) * 2048], [dZ2], [d_ZT[t]])
    barrier(k)
    A.reset(m_phase)
    phase_outproj_ln(k, ZT_d, d_ZT, W["s5_w_out"][sj], W["ln_mix_g"][li], W["ln_mix_b"][li], h_in, h_out)
W_SPECS = [
    ("rel_bias", [32, 16]),
    ("s5_w_in", [2, 2048, 2048]), ("s5_a_re", [2, 128, 64]), ("s5_a_im", [2, 128, 64]), ("s5_log_dt", [2, 128]),
    ("s5_b_re", [2, 128, 64, 16]), ("s5_b_im", [2, 128, 64, 16]), ("s5_c_re", [2, 128, 16, 64]), ("s5_c_im", [2, 128, 16, 64]),
    ("s5_d", [2, 128, 16]), ("s5_w_glu", [2, 2048, 2048]), ("s5_w_out", [2, 2048, 2048]),
    ("dsa_w_in", [2, 2048, 1168]), ("dsa_q_norm", [2, 512]), ("dsa_kv_norm", [2, 512]),
    ("dsa_w_uq", [2, 512, 2048]), ("dsa_w_qidx", [2, 512, 2048]), ("dsa_w_uk", [2, 16, 128, 512]),
    ("dsa_w_uv", [2, 16, 512, 128]), ("dsa_w_out", [2, 2048, 2048]),
    ("moe_w_group", [4, 2048, 4]), ("moe_b_group", [4, 4]), ("moe_w_expert", [4, 2048, 32]), ("moe_b_expert", [4, 32]),
    ("moe_w_gate", [4, 32, 2048, 256]), ("moe_w_up", [4, 32, 2048, 256]), ("moe_w_down", [4, 32, 256, 2048]),
    ("ln_mix_g", [4, 2048]), ("ln_mix_b", [4, 2048]), ("ln_ffn_g", [4, 2048]), ("ln_ffn_b", [4, 2048]),
]


def host_consts():
    c = {}
    c["c_ident"] = np.eye(128, dtype=np.float32)
    c.update(dsa_consts())
    c.update(s5_consts())
    return c


def build_nc(mode="full", used=None, **kw):
    nc = bass.Bass("TRN2", target_bir_lowering=False)
    k = K()
    for a_, b_ in kw.items():
        setattr(k, a_, b_)
    k.nc = nc
    k.x = nc.dram_tensor("x", [L, D], F32, kind="ExternalInput").ap()
    k.w = {}
    for name, shp in W_SPECS:
        if used is not None and name not in used:
            continue
        k.w[name] = nc.dram_tensor(name, getattr(k, 'wshape', {}).get(name, shp), F32, kind="ExternalInput").ap()
    k.c = {}
    for name, arr in host_consts().items():
        k.c[name] = nc.dram_tensor(name, list(arr.shape), F32 if arr.dtype == np.float32 else BF16, kind="ExternalInput").ap()
    k.out = nc.dram_tensor("out", [L, D], F32, kind="ExternalOutput").ap()
    k.hA = nc.dram_tensor("hA", [L, D], F32).ap()
    k.hB = nc.dram_tensor("hB", [L, D], F32).ap()
    k.hT = nc.dram_tensor("hT", [NT, 128, 16 * 128], BF16).ap()
    k.s5_M = nc.dram_tensor("s5_M", [128, 128, 128], BF16).ap()
    k.s5_W1 = nc.dram_tensor("s5_W1", [64, 128, 512], BF16).ap()
    k.s5_W2 = nc.dram_tensor("s5_W2", [64, 128, 512], BF16).ap()
    k.s5_ZT = nc.dram_tensor("s5_ZT", [NT, 128, 16 * 128], BF16).ap()
    k.dsa_QT = nc.dram_tensor("dsa_QT", [128, 16, L], BF16).ap()
    k.dsa_QIT = nc.dram_tensor("dsa_QIT", [128, 16, L], BF16).ap()
    k.dsa_OT = nc.dram_tensor("dsa_OT", [NT, 128, 16 * 128], BF16).ap()
    k.dsa_KT = nc.dram_tensor("dsa_KT", [16, 128, L], BF16).ap()
    k.dsa_V = nc.dram_tensor("dsa_V", [16, 128, L], BF16).ap()
    k.d_h = [Dep() for _ in range(NT)]
    k.d_hT = [Dep() for _ in range(NT)]
    with ExitStack() as st:
        k.S = Sched(nc, st)
        k.A = Arena(nc, st, 53000)
        k.ps = []
        k.dps = []
        for i in range(8):
            k.ps.append(st.enter_context(nc.psum_tensor("ps%d" % i, [128, 512], F32))[:])
            k.dps.append(Dep())
        A = k.A
        k.ident = A.f32(128)
        d_id = Dep()
        k.S.dma("sync", lambda e: e.dma_start(out=k.ident, in_=k.c["c_ident"]), writes=[d_id])
        k.d_hTt = [Dep(), Dep()]
        k.hTt_pos = 0
        barrier(k)
        if mode == "dsa_only":
            phase_prep(k)
            phase_dsa(k, 0, 1, k.x, k.out)
        elif mode == "s5_only":
            phase_prep(k)
            phase_s5(k, 0, 0, k.x, k.out)
        elif mode == "moe_only":
            phase_prep(k)
            phase_moe(k, 0, k.x, k.out, write_hT=False)
        elif mode == "full":
            phase_prep(k)
            h_in = k.x
            bufs = [k.hA, k.hB]
            bi = 0
            for li in range(DEPTH):
                hm = bufs[bi]; bi ^= 1
                if li % 2 == 0:
                    phase_s5(k, li // 2, li, h_in, hm)
                else:
                    phase_dsa(k, li // 2, li, h_in, hm)
                last = (li == DEPTH - 1)
                hf = k.out if last else bufs[bi]
                bi ^= 1
                phase_moe(k, li, hm, hf, write_hT=not last)
                h_in = hf
        k.S.finish(k.d_h)
        k.S.emit()
    return nc


def kernel(**inputs):
    nc = build_nc("full")
    consts = host_consts()
    x = np.ascontiguousarray(inputs["x"], dtype=np.float32)
    shared = {name: np.ascontiguousarray(inputs[name], dtype=np.float32) for name, _ in W_SPECS}
    shared.update(consts)
    in_maps = []
    for c in range(8):
        m = dict(shared)
        m["x"] = x[c]
        in_maps.append(m)
    res = run_bass_kernel_spmd(nc, in_maps, core_ids=list(range(8)))
    return np.stack([np.asarray(r["out"], dtype=np.float32) for r in res.results], axis=0)
```

```python
from concourse.bass_utils import run_bass_kernel_spmd
import numpy as np
import concourse.bass as bass
import concourse.mybir as mybir
from contextlib import ExitStack

F32 = mybir.dt.float32
BF16 = mybir.dt.bfloat16
I32 = mybir.dt.int32
ALU = mybir.AluOpType
AF = mybir.ActivationFunctionType
AX = mybir.AxisListType

ENGINES = ("tensor", "vector", "scalar", "gpsimd", "sync")
DMA_RING = 8


class Dep:
    __slots__ = ("name", "w", "r")

    def __init__(self, name=""):
        self.name = name
        self.w = None
        self.r = {}


class Sched:
    def __init__(self, nc, stack, same_engine_sync=True):
        self.nc = nc
        self.stack = stack
        self.streams = {e: [] for e in ENGINES}
        self.count = {e: 0 for e in ENGINES}
        self.seen = {e: {} for e in ENGINES}
        self.sems = {}
        for e in ENGINES:
            self.sems[e] = stack.enter_context(nc.semaphore("s_" + e))
        self.ring = {}
        self.ring_cnt = {}
        self.ring_pos = {}
        for q in ("sync", "gpsimd", "scalar"):
            self.ring[q] = []
            for i in range(DMA_RING):
                key = "d_%s_%d" % (q, i)
                self.sems[key] = stack.enter_context(nc.semaphore(key))
                self.ring[q].append(key)
            self.ring_cnt[q] = [0] * DMA_RING
            self.ring_pos[q] = 0
        self.same_engine_sync = same_engine_sync
        self.out_deps = []

    def _collect(self, reads, writes):
        need = {}

        def add(kv):
            if kv is None:
                return
            k, v = kv
            if need.get(k, 0) < v:
                need[k] = v
        for d in reads:
            add(d.w)
        for d in writes:
            add(d.w)
            for k, v in d.r.items():
                add((k, v))
        return need

    def _waits(self, eng, need, skip_self):
        ws = []
        seen = self.seen[eng]
        for k, v in need.items():
            if k == eng and skip_self:
                continue
            if seen.get(k, 0) >= v:
                continue
            seen[k] = v
            ws.append((k, v))
        return ws

    def _update(self, reads, writes, ticket):
        k, v = ticket
        for d in writes:
            d.w = ticket
            d.r = {}
        for d in reads:
            if d.r.get(k, 0) < v:
                d.r[k] = v

    def op(self, eng, fn, reads=(), writes=(), inc=True):
        need = self._collect(reads, writes)
        skip_self = (eng == "tensor") or (not self.same_engine_sync)
        ws = self._waits(eng, need, skip_self)
        if inc:
            self.count[eng] += 1
            ticket = (eng, self.count[eng])
        else:
            ticket = (eng, self.count[eng] + 1)
        self.streams[eng].append((ws, fn, (eng, 1) if inc else None))
        self._update(reads, writes, ticket)
        return ticket

    def dma(self, q, fn, reads=(), writes=()):
        need = self._collect(reads, writes)
        pos = self.ring_pos[q]
        self.ring_pos[q] = (pos + 1) % DMA_RING
        key = self.ring[q][pos]
        prev = self.ring_cnt[q][pos]
        if prev > 0:
            if need.get(key, 0) < prev * 16:
                need[key] = prev * 16
        ws = self._waits(q, need, False)
        self.ring_cnt[q][pos] = prev + 1
        ticket = (key, (prev + 1) * 16)
        self.streams[q].append((ws, fn, (key, 16)))
        self._update(reads, writes, ticket)
        return ticket

    def finish(self, deps):
        need = self._collect(deps, deps)
        ws = self._waits("sync", need, False)
        self.streams["sync"].append((ws, None, None))

    def emit(self):
        nc = self.nc
        sems = self.sems
        streams = self.streams

        def run(engh, name):
            for ws, fn, inc in streams[name]:
                for k, v in ws:
                    engh.wait_ge(sems[k], v)
                if fn is not None:
                    ins = fn(engh)
                    if inc is not None:
                        ins.then_inc(sems[inc[0]], inc[1])

        with nc.Block() as block:
            @block.tensor
            def _(e):
                run(e, "tensor")

            @block.vector
            def _(e):
                run(e, "vector")

            @block.scalar
            def _(e):
                run(e, "scalar")

            @block.gpsimd
            def _(e):
                run(e, "gpsimd")

            @block.sync
            def _(e):
                run(e, "sync")
D = 2048
L = 2048
NT = 16
DEPTH = 4
DN_ALPHA = (2 * DEPTH) ** 0.25
LN_EPS = 1e-5
RMS_EPS = 1e-6
NE = 32
FF = 256
NEG = -1.0e30


class Arena:
    def __init__(self, nc, stack, nelem):
        self.t = stack.enter_context(nc.sbuf_tensor("arena", [128, nelem], F32))
        self.n = nelem
        self.off = 0

    def mark(self):
        return self.off

    def reset(self, m):
        self.off = m

    def f32(self, n, shape=None):
        assert self.off + n <= self.n, ("arena overflow", self.off, n, self.n)
        v = self.t[:, self.off:self.off + n]
        self.off += n
        return v

    def bf16(self, n):
        m = (n + 1) // 2
        return self.f32(m).bitcast(BF16)[:, 0:n]


class K:
    pass


def r3(ap, **kw):
    return ap.rearrange("p (a b) -> p a b", **kw)


def barrier(k):
    S = k.S
    cur = {}
    for e in ENGINES:
        if S.count[e] > 0:
            cur[e] = S.count[e]
    for q in S.ring:
        for i, key in enumerate(S.ring[q]):
            if S.ring_cnt[q][i] > 0:
                cur[key] = S.ring_cnt[q][i] * 16
    for e in ENGINES:
        ws = S._waits(e, dict(cur), False)
        if ws:
            S.streams[e].append((ws, None, None))


def ln_load_params(k, g_ap, b_ap, gt, bt, dgb):
    S = k.S
    S.dma("sync", lambda e: e.dma_start(out=gt, in_=g_ap.rearrange("(o d) -> o d", o=1).partition_broadcast(128)), writes=[dgb])
    S.dma("sync", lambda e: e.dma_start(out=bt, in_=b_ap.rearrange("(o d) -> o d", o=1).partition_broadcast(128)), writes=[dgb])


def ln_stats(k, a, da, st, dst):
    S = k.S
    s1, s2, mean, var, rstd, junk = st
    S.op("scalar", lambda e: e.activation(out=junk, in_=a, func=AF.Identity, accum_out=s1), reads=[da], writes=[dst])
    S.op("scalar", lambda e: e.activation(out=junk, in_=a, func=AF.Square, accum_out=s2), reads=[da], writes=[dst])


def ln_norm(k, a, da, gt, bt, dgb, st, dst):
    S = k.S
    s1, s2, mean, var, rstd, junk = st
    S.op("vector", lambda e: e.tensor_scalar(out=mean, in0=s1, scalar1=1.0 / D, scalar2=None, op0=ALU.mult), reads=[dst], writes=[dst])
    S.op("vector", lambda e: e.tensor_tensor(out=var, in0=mean, in1=mean, op=ALU.mult), reads=[dst], writes=[dst])
    S.op("vector", lambda e: e.scalar_tensor_tensor(out=var, in0=s2, scalar=1.0 / D, in1=var, op0=ALU.mult, op1=ALU.subtract), reads=[dst], writes=[dst])
    S.op("vector", lambda e: e.tensor_scalar(out=var, in0=var, scalar1=LN_EPS, scalar2=None, op0=ALU.add), reads=[dst], writes=[dst])
    S.op("scalar", lambda e: e.activation(out=var, in_=var, func=AF.Sqrt), reads=[dst], writes=[dst])
    S.op("vector", lambda e: e.reciprocal(out=rstd, in_=var), reads=[dst], writes=[dst])
    S.op("vector", lambda e: e.tensor_scalar(out=a, in0=a, scalar1=mean, scalar2=rstd, op0=ALU.subtract, op1=ALU.mult), reads=[dst, da], writes=[da])
    S.op("vector", lambda e: e.tensor_tensor(out=a, in0=a, in1=gt, op=ALU.mult), reads=[da, dgb], writes=[da])
    S.op("vector", lambda e: e.tensor_tensor(out=a, in0=a, in1=bt, op=ALU.add), reads=[da, dgb], writes=[da])


def ln_out(k, a, da, tt, h_out, hT_out, write_hT=True):
    k.S.dma("gpsimd", lambda e: e.dma_start(out=h_out[tt * 128:(tt + 1) * 128, :], in_=a), reads=[da], writes=[k.d_h[tt]])
    if write_hT:
        emit_hT(k, a, da, tt, hT_out)


def ln_tile(k, a, da, gt, bt, dgb, tt, h_out, hT_out, st, dst, write_hT=True):
    ln_stats(k, a, da, st, dst)
    ln_norm(k, a, da, gt, bt, dgb, st, dst)
    ln_out(k, a, da, tt, h_out, hT_out, write_hT)


def emit_hT(k, a, da, tt, hT_out):
    S = k.S
    slot = k.hTt_pos
    k.hTt_pos = (slot + 1) % 2
    hTt, dhTt = k.hTt[slot], k.d_hTt[slot]
    for q in range(4):
        bank = 6 + (q % 2)
        ps, dps = k.ps[bank], k.dps[bank]
        for j in range(4):
            kc = q * 4 + j
            S.op("tensor", lambda e, kc=kc, j=j, ps=ps: e.transpose(ps[:, j * 128:(j + 1) * 128], a[:, kc * 128:(kc + 1) * 128], k.ident),
                 reads=[da], writes=[dps], inc=(j == 3))
        S.op("scalar", lambda e, q=q, ps=ps: e.activation(out=hTt[:, q * 512:(q + 1) * 512], in_=ps, func=AF.Copy),
             reads=[dps], writes=[dhTt])
    S.dma("gpsimd", lambda e: e.dma_start(out=hT_out[tt], in_=hTt), reads=[dhTt], writes=[k.d_hT[tt]])


def phase_prep(k):
    S = k.S
    A = k.A
    m = A.mark()
    k.hTt = [A.bf16(2048), A.bf16(2048)]
    xt = [A.f32(D), A.f32(D)]
    dxt = [Dep(), Dep()]
    for tt in range(NT):
        b = tt % 2
        S.dma("sync", lambda e, tt=tt, b=b: e.dma_start(out=xt[b], in_=k.x[tt * 128:(tt + 1) * 128, :]), writes=[dxt[b]])
        emit_hT(k, xt[b], dxt[b], tt, k.hT)
    barrier(k)
    A.reset(m)


def phase_moe(k, li, h_in, h_out, write_hT=True):
    S = k.S
    A = k.A
    m0 = A.mark()
    k.hTt = [A.bf16(2048), A.bf16(2048)]
    NH = 2
    TH = NT // NH
    HT = A.bf16(TH * 16 * 128)
    HTv = HT.rearrange("p (t c x) -> p t c x", t=TH, c=16)
    dHT = Dep()
    yacc = [A.f32(D) for _ in range(TH)]
    dy = [Dep() for _ in range(TH)]
    wg = A.bf16(16 * FF); wu = A.bf16(16 * FF); wd = A.bf16(2 * D)
    wgv = r3(wg, a=16); wuv = r3(wu, a=16); wdv = r3(wd, a=2)
    dwg = [Dep(), Dep()]; dwu = [Dep(), Dep()]; dwd = [Dep(), Dep()]
    NSTG = 3
    stg = [A.f32(2048) for _ in range(NSTG)]
    dstg = [Dep() for _ in range(NSTG)]
    gt = A.f32(D); bt = A.f32(D); dgb = Dep()
    hh2 = [A.bf16(2 * 2 * 512), A.bf16(2 * 2 * 512)]
    hhv2 = [h_.rearrange("p (t f x) -> p t f x", t=2, f=2) for h_ in hh2]
    dhh2 = [[[Dep(), Dep()], [Dep(), Dep()]], [[Dep(), Dep()], [Dep(), Dep()]]]
    wd_b = A.bf16(2 * D)
    wdv2 = [wdv, r3(wd_b, a=2)]
    dwd2 = [dwd, [Dep(), Dep()]]
    sl = [A.f32(512), A.f32(512)]
    dsl = [Dep(), Dep()]
    wr_s = A.f32(16 * 36); wr = A.bf16(16 * 36); dwr = Dep()
    wr_sv = r3(wr_s, a=16); wrv = r3(wr, a=16)
    rb = A.f32(36); drb = Dep()
    gates = A.f32(TH * NE); dgates = [Dep() for _ in range(TH)]
    gv = r3(gates, a=TH)
    RT = A.f32(704); drt = Dep()
    lnst_raw = A.f32(8 * TH + D)
    lnsts = [(lnst_raw[:, 8 * t_ + 0:8 * t_ + 1], lnst_raw[:, 8 * t_ + 1:8 * t_ + 2], lnst_raw[:, 8 * t_ + 2:8 * t_ + 3], lnst_raw[:, 8 * t_ + 3:8 * t_ + 4], lnst_raw[:, 8 * t_ + 4:8 * t_ + 5], lnst_raw[:, 8 * TH:8 * TH + D]) for t_ in range(TH)]
    dlnsts = [Dep() for _ in range(TH)]

    nc = k.nc
    S.dma("sync", lambda e: e.dma_start(out=wr_sv[:, :, 0:4], in_=k.w["moe_w_group"][li].rearrange("(c p) g -> p c g", p=128)), writes=[dwr])
    S.dma("sync", lambda e: e.dma_start(out=wr_sv[:, :, 4:36], in_=k.w["moe_w_expert"][li].rearrange("(c p) g -> p c g", p=128)), writes=[dwr])
    S.op("vector", lambda e: e.tensor_copy(out=wr, in_=wr_s), reads=[dwr], writes=[dwr])
    S.dma("sync", lambda e: e.dma_start(out=rb[:, 0:4], in_=k.w["moe_b_group"][li].rearrange("(o g) -> o g", o=1).partition_broadcast(128)), writes=[drb])
    S.dma("sync", lambda e: e.dma_start(out=rb[:, 4:36], in_=k.w["moe_b_expert"][li].rearrange("(o g) -> o g", o=1).partition_broadcast(128)), writes=[drb])
    ln_load_params(k, k.w["ln_ffn_g"][li], k.w["ln_ffn_b"][li], gt, bt, dgb)

    stg_pos = [0, 0]

    def load_cast(src_ap, dst_ap, ddst, eng):
        i = stg_pos[0]
        stg_pos[0] = (i + 1) % NSTG
        s = stg[i]
        sv = s if len(src_ap.shape) == 2 else r3(s, a=src_ap.shape[1])
        if not getattr(k, 'skip_wdma', False) or stg_pos[1] < 8:
            S.dma("sync", lambda e: e.dma_start(out=sv, in_=src_ap), writes=[dstg[i]])
        stg_pos[1] += 1
        if eng == "scalar":
            S.op(eng, lambda e: e.activation(out=dst_ap, in_=sv, func=AF.Copy), reads=[dstg[i]], writes=[ddst])
        else:
            S.op(eng, lambda e: e.tensor_copy(out=dst_ap, in_=sv), reads=[dstg[i]], writes=[ddst])

    wgate = k.w["moe_w_gate"][li]
    wup = k.w["moe_w_up"][li]
    wdown = k.w["moe_w_down"][li]

    for half in range(NH):
        t0 = half * TH
        for t in range(TH):
            S.dma("sync", lambda e, t=t, t0=t0: e.dma_start(out=HTv[:, t].rearrange("p c x -> p (c x)"), in_=k.hT[t0 + t]),
                  reads=[k.d_hT[t0 + t]], writes=[dHT])
        for t in range(TH):
            S.dma("sync", lambda e, t=t, t0=t0: e.dma_start(out=yacc[t], in_=h_in[(t0 + t) * 128:(t0 + t + 1) * 128, :]),
                  reads=[k.d_h[t0 + t]], writes=[dy[t]])
            S.op("scalar", lambda e, t=t: e.activation(out=yacc[t], in_=yacc[t], func=AF.Copy, scale=DN_ALPHA),
                 reads=[dy[t]], writes=[dy[t]])
        ps, dps = k.ps[6], k.dps[6]
        for t in range(TH):
            for kc in range(16):
                MM(k, ps[:, t * 36:(t + 1) * 36], HTv[:, t, kc, :], wrv[:, kc, :], kc == 0, kc == 15, [dHT, dwr], [dps], (t == TH - 1 and kc == 15))
        lg3 = RT[:, 0:TH * 36].rearrange("p (t x) -> p t x", t=TH)
        o_ = TH * 36
        gmax = RT[:, o_:o_ + TH]; o_ += TH
        gsum = RT[:, o_:o_ + TH]; o_ += TH
        gp = RT[:, o_:o_ + TH]; o_ += TH
        m1 = RT[:, o_:o_ + TH]; o_ += TH
        m2 = RT[:, o_:o_ + TH]; o_ += TH
        den = RT[:, o_:o_ + TH]; o_ += TH
        gexp3 = RT[:, o_:o_ + TH * 4].rearrange("p (t x) -> p t x", t=TH); o_ += TH * 4
        gone3 = RT[:, o_:o_ + TH * 4].rearrange("p (t x) -> p t x", t=TH); o_ += TH * 4
        coef3 = RT[:, o_:o_ + TH * 4].rearrange("p (t x) -> p t x", t=TH); o_ += TH * 4
        elc3 = RT[:, o_:o_ + TH * 8].rearrange("p (t x) -> p t x", t=TH); o_ += TH * 8
        tmp3 = RT[:, o_:o_ + TH * 8].rearrange("p (t x) -> p t x", t=TH); o_ += TH * 8
        ew3 = RT[:, o_:o_ + TH * 8].rearrange("p (t x) -> p t x", t=TH); o_ += TH * 8
        sel3 = RT[:, o_:o_ + TH * 8].rearrange("p (t x) -> p t x", t=TH); o_ += TH * 8
        assert o_ <= 704
        s4_ = [128, TH, 4]; s8_ = [128, TH, 8]
        rd = [drt]; wr_ = [drt]
        TT(k, "vector", lg3, ps[:, 0:TH * 36].rearrange("p (t x) -> p t x", t=TH), rb.unsqueeze(1).to_broadcast([128, TH, 36]), ALU.add, [dps, drb, drt], wr_)
        k.S.op("vector", lambda e: e.tensor_reduce(out=gmax, in_=lg3[:, :, 0:4], axis=AX.X, op=ALU.max), reads=rd, writes=wr_)
        TT(k, "vector", gexp3, lg3[:, :, 0:4], gmax.unsqueeze(2).to_broadcast(s4_), ALU.subtract, rd, wr_)
        ACT(k, gexp3, gexp3, AF.Exp, rd, wr_)
        k.S.op("vector", lambda e: e.tensor_reduce(out=gsum, in_=gexp3, axis=AX.X, op=ALU.add), reads=rd, writes=wr_)
        k.S.op("vector", lambda e: e.reciprocal(out=gp, in_=gsum), reads=rd, writes=wr_)
        TT(k, "vector", gone3, lg3[:, :, 0:4], gmax.unsqueeze(2).to_broadcast(s4_), ALU.is_equal, rd, wr_)
        for g in range(4):
            dst_ = elc3 if g == 0 else tmp3
            TT(k, "vector", dst_, lg3[:, :, 4 + 8 * g:12 + 8 * g], gone3[:, :, g].unsqueeze(2).to_broadcast(s8_), ALU.mult, rd, wr_)
            if g > 0:
                TT(k, "vector", elc3, elc3, tmp3, ALU.add, rd, wr_)
        k.S.op("vector", lambda e: e.tensor_reduce(out=m1, in_=elc3, axis=AX.X, op=ALU.max), reads=rd, writes=wr_)
        TT(k, "vector", tmp3, elc3, m1.unsqueeze(2).to_broadcast(s8_), ALU.is_equal, rd, wr_)
        STT(k, "vector", tmp3, tmp3, NEG, elc3, ALU.mult, ALU.add, rd, wr_)
        k.S.op("vector", lambda e: e.tensor_reduce(out=m2, in_=tmp3, axis=AX.X, op=ALU.max), reads=rd, writes=wr_)
        TT(k, "vector", ew3, elc3, m1.unsqueeze(2).to_broadcast(s8_), ALU.subtract, rd, wr_)
        ACT(k, ew3, ew3, AF.Exp, rd, wr_)
        TT(k, "vector", sel3, elc3, m2.unsqueeze(2).to_broadcast(s8_), ALU.is_ge, rd, wr_)
        TT(k, "vector", ew3, ew3, sel3, ALU.mult, rd, wr_)
        k.S.op("vector", lambda e: e.tensor_reduce(out=den, in_=ew3, axis=AX.X, op=ALU.add), reads=rd, writes=wr_)
        k.S.op("vector", lambda e: e.reciprocal(out=den, in_=den), reads=rd, writes=wr_)
        TT(k, "vector", ew3, ew3, den.unsqueeze(2).to_broadcast(s8_), ALU.mult, rd, wr_)
        TT(k, "vector", coef3, gone3, gp.unsqueeze(2).to_broadcast(s4_), ALU.mult, rd, wr_)
        for g in range(4):
            TT(k, "vector", gv[:, :, g * 8:(g + 1) * 8], ew3, coef3[:, :, g].unsqueeze(2).to_broadcast(s8_), ALU.mult, rd, [drt] + dgates)
        NEX = getattr(k, 'ne_limit', NE)

        def gu_load(ex):
            for hf in range(2):
                load_cast(wgate[ex, hf * 1024:(hf + 1) * 1024, :].rearrange("(c p) f -> p c f", p=128), wgv[:, hf * 8:(hf + 1) * 8, :], dwg[hf], "scalar")
                load_cast(wup[ex, hf * 1024:(hf + 1) * 1024, :].rearrange("(c p) f -> p c f", p=128), wuv[:, hf * 8:(hf + 1) * 8, :], dwu[hf], "scalar")

        def gu_part(ex, tt, fc):
            eb = ex % 2
            pg, dpg = k.ps[fc], k.dps[fc]
            pu, dpu = k.ps[2 + fc], k.dps[2 + fc]
            for kc in range(16):
                MM(k, pg, wgv[:, kc, fc * 128:(fc + 1) * 128], HTv[:, tt * 4:(tt + 1) * 4, kc, :], kc == 0, kc == 15, [dHT, dwg[kc // 8]], [dpg], kc == 15)
            for kc in range(16):
                MM(k, pu, wuv[:, kc, fc * 128:(fc + 1) * 128], HTv[:, tt * 4:(tt + 1) * 4, kc, :], kc == 0, kc == 15, [dHT, dwu[kc // 8]], [dpu], kc == 15)
            ACT(k, sl[fc], pg, AF.Silu, [dpg], [dsl[fc]])
            TT(k, "vector", hhv2[eb][:, tt, fc, :], sl[fc], pu, ALU.mult, [dsl[fc], dpu], [dhh2[eb][tt][fc]])

        def dn_load(ex):
            wb = ex % 2
            for hf in range(2):
                load_cast(wdown[ex, hf * 128:(hf + 1) * 128, :], wdv2[wb][:, hf, :], dwd2[wb][hf], "scalar")

        dn_cnt = [0]

        def dn_part(ex, tt, sub):
            eb = ex % 2
            wb = ex % 2
            t = tt * 4 + sub
            for ds in range(4):
                bank = 4 + (dn_cnt[0] % 2)
                dn_cnt[0] += 1
                po, dpo = k.ps[bank], k.dps[bank]
                for fc in range(2):
                    MM(k, po, hhv2[eb][:, tt, fc, sub * 128:(sub + 1) * 128], wdv2[wb][:, fc, ds * 512:(ds + 1) * 512], fc == 0, fc == 1, [dhh2[eb][tt][fc], dwd2[wb][fc]], [dpo], fc == 1)
                STT(k, "vector", yacc[t][:, ds * 512:(ds + 1) * 512], po, gv[:, t, ex:ex + 1], yacc[t][:, ds * 512:(ds + 1) * 512], ALU.mult, ALU.add, [dpo, dgates[t], dy[t]], [dy[t]])

        gu_load(0)
        dn_load(0)
        for tt in range(2):
            for fc in range(2):
                gu_part(0, tt, fc)
        for ex in range(NEX):
            nx = ex + 1 if ex + 1 < NEX else None
            if nx is not None:
                gu_load(nx)
                dn_load(nx)
            parts = [(tt, sub) for tt in range(2) for sub in range(4)]
            gparts = [(tt, fc) for tt in range(2) for fc in range(2)]
            for p_ in parts[0:4]:
                dn_part(ex, *p_)
            if nx is not None:
                gu_part(nx, *gparts[0])
            for p_ in parts[4:6]:
                dn_part(ex, *p_)
            if nx is not None:
                gu_part(nx, *gparts[1])
            for p_ in parts[6:8]:
                dn_part(ex, *p_)
            if nx is not None:
                gu_part(nx, *gparts[2])
                gu_part(nx, *gparts[3])
        for t in range(TH):
            ln_stats(k, yacc[t], dy[t], lnsts[t], dlnsts[t])
        for t in range(TH):
            ln_norm(k, yacc[t], dy[t], gt, bt, dgb, lnsts[t], dlnsts[t])
            if t >= 1:
                ln_out(k, yacc[t - 1], dy[t - 1], t0 + t - 1, h_out, k.hT, write_hT)
        ln_out(k, yacc[TH - 1], dy[TH - 1], t0 + TH - 1, h_out, k.hT, write_hT)
    barrier(k)
    A.reset(m0)
def MM(k, out, lhsT, rhs, start, stop, reads, writes, inc):
    k.S.op("tensor", lambda e: e.matmul(out, lhsT=lhsT, rhs=rhs, start=start, stop=stop), reads=reads, writes=writes, inc=inc)


def TR(k, out, in_, ident, reads, writes, inc):
    k.S.op("tensor", lambda e: e.transpose(out, in_, ident), reads=reads, writes=writes, inc=inc)


def ACT(k, out, in_, func, reads, writes, bias=None, scale=None, accum_out=None):
    kw = {}
    if bias is not None:
        kw["bias"] = bias
    if scale is not None:
        kw["scale"] = scale
    if accum_out is not None:
        kw["accum_out"] = accum_out
    k.S.op("scalar", lambda e: e.activation(out=out, in_=in_, func=func, **kw), reads=reads, writes=writes)


def TS(k, eng, out, in0, s1, s2, op0, op1, reads, writes):
    if op1 is None:
        k.S.op(eng, lambda e: e.tensor_scalar(out=out, in0=in0, scalar1=s1, scalar2=None, op0=op0), reads=reads, writes=writes)
    else:
        k.S.op(eng, lambda e: e.tensor_scalar(out=out, in0=in0, scalar1=s1, scalar2=s2, op0=op0, op1=op1), reads=reads, writes=writes)


def TT(k, eng, out, in0, in1, op, reads, writes):
    k.S.op(eng, lambda e: e.tensor_tensor(out=out, in0=in0, in1=in1, op=op), reads=reads, writes=writes)


def STT(k, eng, out, in0, scalar, in1, op0, op1, reads, writes):
    k.S.op(eng, lambda e: e.scalar_tensor_tensor(out=out, in0=in0, scalar=scalar, in1=in1, op0=op0, op1=op1), reads=reads, writes=writes)


def CP(k, eng, out, in_, reads, writes):
    if eng == "scalar":
        k.S.op(eng, lambda e: e.activation(out=out, in_=in_, func=AF.Copy), reads=reads, writes=writes)
    else:
        k.S.op(eng, lambda e: e.tensor_copy(out=out, in_=in_), reads=reads, writes=writes)


def DMA(k, q, out, in_, reads, writes, slow=False):
    if slow:
        k.S.dma(q, lambda e: e.dma_start(out=out, in_=in_, allow_slow_non_contiguous=True), reads=reads, writes=writes)
    else:
        k.S.dma(q, lambda e: e.dma_start(out=out, in_=in_), reads=reads, writes=writes)


class Stager:
    def __init__(self, k, nbuf, nelem):
        self.k = k
        self.bufs = [k.A.f32(nelem) for _ in range(nbuf)]
        self.deps = [Dep() for _ in range(nbuf)]
        self.pos = 0
        self.nelem = nelem
        self.engs = ["scalar", "vector"]
        self.epos = 0

    def load(self, src_ap, dst_ap, ddst, eng=None, slow=False):
        i = self.pos
        self.pos = (i + 1) % len(self.bufs)
        n = 1
        for d_ in src_ap.shape[1:]:
            n *= d_
        assert n <= self.nelem, (n, self.nelem)
        s = self.bufs[i][:, 0:n]
        if len(src_ap.shape) == 3:
            s = s.rearrange("p (a b) -> p a b", a=src_ap.shape[1])
        elif len(src_ap.shape) == 4:
            s = s.rearrange("p (a b c) -> p a b c", a=src_ap.shape[1], b=src_ap.shape[2])
        s = s[0:src_ap.shape[0]]
        DMA(self.k, "sync", s, src_ap, [], [self.deps[i]], slow=slow)
        if eng is None:
            eng = self.engs[self.epos]
            self.epos = (self.epos + 1) % len(self.engs)
        CP(self.k, eng, dst_ap, s, [self.deps[i]], [ddst])


def phase_outproj_ln(k, srcT, d_src, w_ap, g_ap, b_ap, h_in, h_out):
    S = k.S
    A = k.A
    m0 = A.mark()
    k.hTt = [A.bf16(2048), A.bf16(2048)]
    wo = A.bf16(16 * D)
    wov = r3(wo, a=16)
    dwo = [Dep() for _ in range(16)]
    stg = Stager(k, 3, 2048)
    gt = A.f32(D); bt = A.f32(D); dgb = Dep()
    src = [A.bf16(2048) for _ in range(3)]
    dsrc = [Dep() for _ in range(3)]
    at = [A.f32(D) for _ in range(3)]
    dat = [Dep() for _ in range(3)]
    lnst_raw = A.f32(24 + D)
    lnsts = [(lnst_raw[:, 8 * t_ + 0:8 * t_ + 1], lnst_raw[:, 8 * t_ + 1:8 * t_ + 2], lnst_raw[:, 8 * t_ + 2:8 * t_ + 3], lnst_raw[:, 8 * t_ + 3:8 * t_ + 4], lnst_raw[:, 8 * t_ + 4:8 * t_ + 5], lnst_raw[:, 24:24 + D]) for t_ in range(3)]
    dlnsts = [Dep() for _ in range(3)]
    ln_load_params(k, g_ap, b_ap, gt, bt, dgb)
    for kc in range(16):
        stg.load(w_ap[kc * 128:(kc + 1) * 128, :], wov[:, kc, :], dwo[kc])
    def mm_stage(tt):
        b = tt % 3
        DMA(k, "sync", src[b], srcT[tt], [d_src[tt]], [dsrc[b]])
        DMA(k, "sync", at[b], h_in[tt * 128:(tt + 1) * 128, :], [k.d_h[tt]], [dat[b]])
        sv = r3(src[b], a=16)
        for ns in range(4):
            ps, dps = k.ps[ns], k.dps[ns]
            for kc in range(16):
                MM(k, ps, sv[:, kc, :], wov[:, kc, ns * 512:(ns + 1) * 512], kc == 0, kc == 15, [dsrc[b], dwo[kc]], [dps], kc == 15)
            STT(k, "vector", at[b][:, ns * 512:(ns + 1) * 512], at[b][:, ns * 512:(ns + 1) * 512], DN_ALPHA, ps, ALU.mult, ALU.add, [dps, dat[b]], [dat[b]])
        ln_stats(k, at[b], dat[b], lnsts[b], dlnsts[b])

    mm_stage(0)
    for tt in range(NT):
        b = tt % 3
        if tt + 1 < NT:
            mm_stage(tt + 1)
        ln_norm(k, at[b], dat[b], gt, bt, dgb, lnsts[b], dlnsts[b])
        ln_out(k, at[b], dat[b], tt, h_out, k.hT, True)
    barrier(k)
    A.reset(m0)


def rel_bucket_np(n):
    n = np.maximum(n, 0)
    nf = np.maximum(n, 1).astype(np.float32)
    large = 16 + (np.log(nf / np.float32(16)) / np.float32(np.log(128 / 16)) * np.float32(16)).astype(np.int32)
    large = np.minimum(large, 31)
    return np.where(n < 16, n, large)


def dsa_consts():
    ql = np.arange(128)[:, None]
    x = np.arange(256)[None, :]
    dist = np.where(x < 128, 128 + ql - x, ql - (x - 128))
    bk = rel_bucket_np(dist)
    oh = np.zeros((128, 32, 256), np.float32)
    for b in range(32):
        oh[:, b, :] = (bk == b)
    caus = np.where(np.arange(128)[None, :] <= np.arange(128)[:, None], 0.0, NEG).astype(np.float32)
    return {"c_ohb": oh.reshape(128, 32 * 256), "c_caus": caus}


def phase_dsa(k, j, li, h_in, h_out):
    S = k.S
    A = k.A
    W = k.w
    NH_ = 16
    att_scale = 128 ** -0.5
    widx_scale = (16 ** -0.5) * (128 ** -0.5)
    QT_d = k.dsa_QT; QIT_d = k.dsa_QIT; OT_d = k.dsa_OT
    d_OT = [Dep() for _ in range(NT)]
    d_QT = Dep(); d_QIT = Dep()
    m_phase = A.mark()
    CQT = A.bf16(4 * L); CQTv = r3(CQT, a=4); dCQT = Dep()
    CKVT = A.bf16(4 * L); CKVTv = r3(CKVT, a=4); dCKVT = Dep()
    KIT = A.bf16(L); dKIT = Dep()
    WI = A.f32(NT * 16); WIv = r3(WI, a=NT); dWI = Dep()
    identb = A.bf16(128); didb = Dep()
    CP(k, "vector", identb, k.ident, [], [didb])
    mA = A.mark()
    HTb = [A.bf16(2048), A.bf16(2048)]; dHTb = [Dep(), Dep()]
    win = A.bf16(16 * 1168); winv = r3(win, a=16); dwin = [Dep() for _ in range(16)]
    stg = Stager(k, 3, 2048)
    qg = A.f32(512); kg = A.f32(512); dqg = Dep()
    DMA(k, "sync", qg, W["dsa_q_norm"][j].rearrange("(o d) -> o d", o=1).partition_broadcast(128), [], [dqg])
    DMA(k, "sync", kg, W["dsa_kv_norm"][j].rearrange("(o d) -> o d", o=1).partition_broadcast(128), [], [dqg])
    for kc in range(16):
        stg.load(W["dsa_w_in"][j, kc * 128:(kc + 1) * 128, :], winv[:, kc, :], dwin[kc])
    pj = [A.f32(1168), A.f32(1168)]; dpj = [Dep(), Dep()]
    sm = A.f32(16); dsm = Dep()
    junk = A.f32(512)
    for t in range(NT):
        b = t % 2
        DMA(k, "sync", HTb[b], k.hT[t], [k.d_hT[t]], [dHTb[b]])
        HTt = r3(HTb[b], a=16)
        for ns, (c0, c1) in enumerate([(0, 512), (512, 1024), (1024, 1168)]):
            ps, dps = k.ps[ns], k.dps[ns]
            for kc in range(16):
                MM(k, ps[:, 0:c1 - c0], HTt[:, kc, :], winv[:, kc, c0:c1], kc == 0, kc == 15, [dHTb[b], dwin[kc]], [dps], kc == 15)
            CP(k, "vector", pj[b][:, c0:c1], ps[:, 0:c1 - c0], [dps], [dpj[b]])
        for qi, (c0, gain) in enumerate([(0, qg), (512, kg)]):
            ss = sm[:, qi * 4:qi * 4 + 1]; rs = sm[:, qi * 4 + 1:qi * 4 + 2]
            ACT(k, junk, pj[b][:, c0:c0 + 512], AF.Square, [dpj[b]], [dsm], accum_out=ss)
            TS(k, "vector", rs, ss, 1.0 / 512, RMS_EPS, ALU.mult, ALU.add, [dsm], [dsm])
            ACT(k, rs, rs, AF.Sqrt, [dsm], [dsm])
            k.S.op("vector", lambda e, rs=rs: e.reciprocal(out=rs, in_=rs), reads=[dsm], writes=[dsm])
            STT(k, "vector", pj[b][:, c0:c0 + 512], pj[b][:, c0:c0 + 512], rs, gain, ALU.mult, ALU.mult, [dsm, dpj[b], dqg], [dpj[b]])
        TS(k, "vector", WIv[:, t, :], pj[b][:, 1152:1168], widx_scale, None, ALU.mult, None, [dpj[b]], [dWI])
        for grp, (c0, dstv, ddst) in enumerate([(0, CQTv, dCQT), (512, CKVTv, dCKVT)]):
            ps, dps = k.ps[4 + grp], k.dps[4 + grp]
            for kc in range(4):
                TR(k, ps[:, kc * 128:(kc + 1) * 128], pj[b][:, c0 + kc * 128:c0 + (kc + 1) * 128], k.ident, [dpj[b]], [dps], kc == 3)
            ACT(k, dstv[:, :, t * 128:(t + 1) * 128], ps.rearrange("p (a b) -> p a b", a=4), AF.Copy, [dps], [ddst])
        ps, dps = k.ps[6], k.dps[6]
        TR(k, ps[:, 0:128], pj[b][:, 1024:1152], k.ident, [dpj[b]], [dps], True)
        ACT(k, KIT[:, t * 128:(t + 1) * 128], ps[:, 0:128], AF.Copy, [dps], [dKIT])
    barrier(k)
    A.reset(mA)
    if getattr(k, 'dsa_stop', '') == 'A':
        return
    mB = A.mark()
    wq = A.bf16(4 * 2048); wqv = r3(wq, a=4); dwq = [Dep() for _ in range(4)]
    stg = Stager(k, 3, 2048)
    ev = [A.bf16(512), A.bf16(512)]; dev_ = [Dep(), Dep()]
    cnt = 0
    for wname, dst_d, ddst in [("dsa_w_uq", QT_d, d_QT), ("dsa_w_qidx", QIT_d, d_QIT)]:
        for kc in range(4):
            stg.load(W[wname][j, kc * 128:(kc + 1) * 128, :], wqv[:, kc, :], dwq[kc])
        for h in range(NH_):
            for sl_ in range(4):
                ps, dps = k.ps[cnt % 4], k.dps[cnt % 4]
                b = cnt % 2
                cnt += 1
                for kc in range(4):
                    MM(k, ps, wqv[:, kc, h * 128:(h + 1) * 128], CQTv[:, kc, sl_ * 512:(sl_ + 1) * 512], kc == 0, kc == 3, [dwq[kc], dCQT], [dps], kc == 3)
                if b == 0:
                    ACT(k, ev[b], ps, AF.Copy, [dps], [dev_[b]])
                else:
                    CP(k, "vector", ev[b], ps, [dps], [dev_[b]])
                DMA(k, "gpsimd", dst_d[:, h, sl_ * 512:(sl_ + 1) * 512], ev[b], [dev_[b]], [ddst])
    barrier(k)
    A.reset(mB)
    if getattr(k, 'dsa_stop', '') == 'B':
        return
    KT_d = k.dsa_KT; V_d = k.dsa_V
    dKT = Dep(); dV = Dep()
    mC = A.mark()
    evc = [A.bf16(512), A.bf16(512)]; devc = [Dep(), Dep()]
    stg = Stager(k, 3, 2048)
    wukT = A.bf16(4 * 2048); wukTv = wukT.rearrange("p (c h d) -> p c h d", c=4, h=NH_); dwukT = Dep()
    wuv = A.bf16(4 * 2048); wuvv = wuv.rearrange("p (c h d) -> p c h d", c=4, h=NH_); dwuv = Dep()
    uk = [A.f32(512), A.f32(512)]; duk = [Dep(), Dep()]
    for h in range(NH_):
        b = h % 2
        DMA(k, "sync", uk[b], W["dsa_w_uk"][j, h], [], [duk[b]])
        ps, dps = k.ps[4 + b], k.dps[4 + b]
        for kc in range(4):
            TR(k, ps[:, kc * 128:(kc + 1) * 128], uk[b][:, kc * 128:(kc + 1) * 128], k.ident, [duk[b]], [dps], kc == 3)
        ACT(k, wukTv[:, :, h, :], ps.rearrange("p (a b) -> p a b", a=4), AF.Copy, [dps], [dwukT])
    for h in range(NH_):
        stg.load(W["dsa_w_uv"][j, h].rearrange("(c p) d -> p c d", p=128), wuvv[:, :, h, :], dwuv)
    cnt = 0
    for h in range(NH_):
        for sl_ in range(4):
            ps, dps = k.ps[cnt % 4], k.dps[cnt % 4]
            cnt += 1
            for kc in range(4):
                MM(k, ps, wukTv[:, kc, h, :], CKVTv[:, kc, sl_ * 512:(sl_ + 1) * 512], kc == 0, kc == 3, [dwukT, dCKVT], [dps], kc == 3)
            b = cnt % 2
            if b == 0:
                ACT(k, evc[b], ps, AF.Copy, [dps], [devc[b]])
            else:
                CP(k, "vector", evc[b], ps, [dps], [devc[b]])
            DMA(k, "gpsimd", KT_d[h, :, sl_ * 512:(sl_ + 1) * 512], evc[b], [devc[b]], [dKT])
    for st_ in range(NT):
        for hg in range(4):
            ps, dps = k.ps[cnt % 4], k.dps[cnt % 4]
            cnt += 1
            for kc in range(4):
                MM(k, ps, CKVTv[:, kc, st_ * 128:(st_ + 1) * 128], wuvv[:, kc, hg * 4:(hg + 1) * 4, :], kc == 0, kc == 3, [dwuv, dCKVT], [dps], kc == 3)
            b = cnt % 2
            if b == 0:
                ACT(k, evc[b], ps, AF.Copy, [dps], [devc[b]])
            else:
                CP(k, "vector", evc[b], ps, [dps], [devc[b]])
            DMA(k, "gpsimd", V_d[hg * 4:(hg + 1) * 4, :, st_ * 128:(st_ + 1) * 128].rearrange("h p d -> p h d"), r3(evc[b], a=4), [devc[b]], [dV])
    barrier(k)
    A.reset(mC)
    if getattr(k, 'dsa_stop', '') == 'C':
        return
    Tn = A.f32(NH_ * 256); Tnv = r3(Tn, a=NH_); dTn = Dep()
    caus = A.f32(128); dcaus = Dep()
    DMA(k, "sync", caus, k.c["c_caus"], [], [dcaus])
    mT = A.mark()
    ohb = A.f32(32 * 256); ohbv = r3(ohb, a=32); dohb = Dep()
    rbB = A.f32(512); drbB = Dep()
    DMA(k, "sync", ohb, k.c["c_ohb"], [], [dohb])
    DMA(k, "sync", rbB, W["rel_bias"].rearrange("(o b) h -> o (b h)", o=1).partition_broadcast(128), [], [drbB])
    for h in range(NH_):
        eng = "vector"
        TS(k, eng, Tnv[:, h, :], ohbv[:, 0, :], rbB[:, h:h + 1], None, ALU.mult, None, [dohb, drbB], [dTn])
        for b_ in range(1, 32):
            STT(k, eng, Tnv[:, h, :], ohbv[:, b_, :], rbB[:, b_ * 16 + h:b_ * 16 + h + 1], Tnv[:, h, :], ALU.mult, ALU.add, [dohb, drbB, dTn], [dTn])
        TS(k, eng, Tnv[:, h, :], Tnv[:, h, :], rbB[:, 31 * 16 + h:31 * 16 + h + 1], None, ALU.subtract, None, [drbB, dTn], [dTn])
    barrier(k)
    A.reset(mT)
    accs = [A.f32(L), A.f32(L)]; daccs = [Dep(), Dep()]
    tmp = [A.f32(512), A.f32(512)]; dtmp = [Dep(), Dep()]
    madds = [A.bf16(L), A.bf16(L)]; dmadds = [Dep(), Dep()]
    Xs = [A.f32(L) for _ in range(3)]; dXs = [Dep() for _ in range(3)]
    Ps = [A.bf16(L) for _ in range(3)]; dPs = [Dep() for _ in range(3)]
    PTs = [A.bf16(NT * 128) for _ in range(3)]; dPTs = [Dep() for _ in range(3)]
    QIbs = [A.bf16(NH_ * 128), A.bf16(NH_ * 128)]; dQIbs = [Dep(), Dep()]
    QTbs = [A.bf16(NH_ * 128), A.bf16(NH_ * 128)]; dQTbs = [Dep(), Dep()]
    OT = [A.bf16(NH_ * 128), A.bf16(NH_ * 128)]; dOTs = [Dep(), Dep()]
    mx8 = A.f32(8); dmx = Dep()
    sm2s = [A.f32(8) for _ in range(3)]; dsm2s = [Dep() for _ in range(3)]
    dgrs = [A.bf16(128) for _ in range(3)]; ddgrs = [Dep() for _ in range(3)]
    Kh = [A.bf16(L) for _ in range(3)]; dKh = [Dep() for _ in range(3)]
    Vh = [A.bf16(L) for _ in range(3)]; dVh = [Dep() for _ in range(3)]
    z1 = A.f32(1); dz1 = Dep()
    k.S.op("vector", lambda e: e.memset(z1, 0.0), reads=[], writes=[dz1])

    def pre_thunks(jb):
        SL = (jb + 1) * 128
        nbk = (SL + 511) // 512
        pb = jb % 2
        acc, dacc = accs[pb], daccs[pb]
        madd, dmadd = madds[pb], dmadds[pb]
        QIbv = r3(QIbs[pb], a=NH_); dQIb = dQIbs[pb]
        th_ = []

        def t_load():
            DMA(k, "sync", QIbv, QIT_d[:, :, jb * 128:(jb + 1) * 128], [d_QIT], [dQIb])
        th_.append(t_load)
        cnt = [0]
        for h in range(NH_):
            def t_head(h=h):
                for bk in range(nbk):
                    w_ = min(512, SL - bk * 512)
                    bank = 4
                    ps, dps = k.ps[bank], k.dps[bank]
                    tb = cnt[0] % 2
                    cnt[0] += 1
                    MM(k, ps[:, 0:w_], QIbv[:, h, :], KIT[:, bk * 512:bk * 512 + w_], True, True, [dQIb, dKIT], [dps], True)
                    ACT(k, tmp[tb][:, 0:w_], ps[:, 0:w_], AF.Relu, [dps], [dtmp[tb]])
                    if h == 0:
                        TS(k, "vector", acc[:, bk * 512:bk * 512 + w_], tmp[tb][:, 0:w_], WIv[:, jb, h:h + 1], None, ALU.mult, None, [dtmp[tb], dWI], [dacc])
                    else:
                        STT(k, "vector", acc[:, bk * 512:bk * 512 + w_], tmp[tb][:, 0:w_], WIv[:, jb, h:h + 1], acc[:, bk * 512:bk * 512 + w_], ALU.mult, ALU.add, [dtmp[tb], dWI, dacc], [dacc])
            th_.append(t_head)

        def t_caus():
            TT(k, "gpsimd", acc[:, jb * 128:SL], acc[:, jb * 128:SL], caus, ALU.add, [dacc, dcaus], [dacc])
        th_.append(t_caus)
        if SL > 256:
            for r in range(getattr(k, 'topk_rounds', 32)):
                def t_round():
                    k.S.op("vector", lambda e: e.max(out=mx8, in_=acc[:, 0:SL]), reads=[dacc], writes=[dmx])
                    k.S.op("vector", lambda e: e.match_replace(out=acc[:, 0:SL], in_to_replace=mx8, in_values=acc[:, 0:SL], imm_value=-2.0e30), reads=[dacc, dmx], writes=[dacc])
                th_.append(t_round)

            def t_fin():
                TS(k, "vector", madd[:, 0:SL], acc[:, 0:SL], -1.5e30, NEG, ALU.is_gt, ALU.mult, [dacc], [dmadd])
        else:
            def t_fin():
                TS(k, "vector", madd[:, 0:SL], acc[:, 0:SL], -1.0e29, NEG, ALU.is_lt, ALU.mult, [dacc], [dmadd])
        th_.append(t_fin)
        return th_

    NB3 = 3
    dPV = [Dep(), Dep()]

    def bufs(i):
        b3 = i % NB3
        return (Xs[b3], dXs[b3], Ps[b3], dPs[b3], r3(PTs[b3], a=NT), dPTs[b3], sm2s[b3], dsm2s[b3], dgrs[b3], ddgrs[b3], Kh[b3], dKh[b3], Vh[b3], dVh[b3])

    def stageA(i):
        jb, h = divmod(i, NH_)
        SL = (jb + 1) * 128
        nbk = (SL + 511) // 512
        pb = jb % 2
        madd, dmadd = madds[pb], dmadds[pb]
        QTbv = r3(QTbs[pb], a=NH_); dQTb = dQTbs[pb]
        X, dX, P, dP, PTv, dPT, sm2, dsm2, dgr, ddgr, Kb, dKb, Vb, dVb = bufs(i)
        if h == 0:
            DMA(k, "sync", QTbv, QT_d[:, :, jb * 128:(jb + 1) * 128], [d_QT], [dQTb])
        DMA(k, "sync", Kb[:, 0:SL], KT_d[h, :, 0:SL], [dKT], [dKb])
        DMA(k, "sync", Vb[:, 0:SL], V_d[h, :, 0:SL], [dV], [dVb])
        for bk in range(nbk):
            w_ = min(512, SL - bk * 512)
            bank = (bk % 2) + 2 * (i % 2)
            ps, dps = k.ps[bank], k.dps[bank]
            MM(k, ps[:, 0:w_], QTbv[:, h, :], Kb[:, bk * 512:bk * 512 + w_], True, False, [dQTb, dKb], [dps], False)
            MM(k, ps[:, 0:w_], identb, madd[:, bk * 512:bk * 512 + w_], False, True, [didb, dmadd], [dps], True)
            ACT(k, X[:, bk * 512:bk * 512 + w_], ps[:, 0:w_], AF.Copy, [dps], [dX], scale=att_scale)
        lo = max(0, jb - 1) * 128
        tlo = 0 if jb >= 1 else 128
        TT(k, "vector", X[:, lo:SL], X[:, lo:SL], Tnv[:, h, tlo:256], ALU.add, [dX, dTn], [dX])
        rmax = sm2[:, 0:1]; nmax = sm2[:, 1:2]; rsum = sm2[:, 2:3]
        k.S.op("vector", lambda e: e.tensor_reduce(out=rmax, in_=X[:, 0:SL], axis=AX.X, op=ALU.max), reads=[dX], writes=[dsm2])
        TS(k, "vector", nmax, rmax, -1.0, None, ALU.mult, None, [dsm2], [dsm2])
        ACT(k, P[:, 0:SL], X[:, 0:SL], AF.Exp, [dX, dsm2], [dP, dsm2], bias=nmax, scale=1.0, accum_out=rsum)

    def stageB(i):
        jb, h = divmod(i, NH_)
        X, dX, P, dP, PTv, dPT, sm2, dsm2, dgr, ddgr, Kb, dKb, Vb, dVb = bufs(i)
        rsum = sm2[:, 2:3]; rinv = sm2[:, 3:4]
        k.S.op("vector", lambda e: e.reciprocal(out=rinv, in_=rsum), reads=[dsm2], writes=[dsm2])
        TS(k, "vector", dgr, identb, rinv, None, ALU.mult, None, [dsm2, didb], [ddgr])
        for st_ in range(jb + 1):
            bank = 6 + (st_ // 4) % 2
            ps, dps = k.ps[bank], k.dps[bank]
            last = (st_ % 4 == 3) or (st_ == jb)
            MM(k, ps[:, (st_ % 4) * 128:(st_ % 4 + 1) * 128], P[:, st_ * 128:(st_ + 1) * 128], dgr, True, True, [dP, ddgr], [dps], last)
            if last:
                s0 = (st_ // 4) * 4
                n_ = st_ - s0 + 1
                ACT(k, PTv[:, s0:s0 + n_, :], ps[:, 0:n_ * 128].rearrange("p (a b) -> p a b", a=n_), AF.Copy, [dps], [dPT])
        Vhv = r3(Vb, a=NT)
        pv = k.ps[5][:, (i % 2) * 128:(i % 2 + 1) * 128]
        for st_ in range(jb + 1):
            MM(k, pv, Vhv[:, st_, :], PTv[:, st_, :], st_ == 0, st_ == jb, [dVb, dPT], [dPV[i % 2]], st_ == jb)

    def stageC(i):
        jb, h = divmod(i, NH_)
        pb = jb % 2
        ot, dot = OT[pb], dOTs[pb]
        otv = r3(ot, a=NH_)
        pv = k.ps[5][:, (i % 2) * 128:(i % 2 + 1) * 128]
        CP(k, "scalar", otv[:, h, :], pv, [dPV[i % 2]], [dot])
        if h == NH_ - 1:
            DMA(k, "gpsimd", OT_d[jb], ot, [dot], [d_OT[jb]])

    for t_ in pre_thunks(0):
        t_()
    NHEADS = NT * NH_
    nxt = []
    pos = 0
    per = 0
    for i in range(NHEADS + 2):
        if i < NHEADS:
            jb, h = divmod(i, NH_)
            if h == 0:
                for t_ in nxt[pos:]:
                    t_()
                nxt = pre_thunks(jb + 1) if jb + 1 < NT else []
                per = (len(nxt) + NH_ - 1) // NH_
                pos = 0
            stageA(i)
        if 0 <= i - 1 < NHEADS:
            stageB(i - 1)
        if 0 <= i - 2 < NHEADS:
            stageC(i - 2)
        if i < NHEADS:
            for t_ in nxt[pos:pos + per]:
                t_()
            pos += per
    barrier(k)
    A.reset(m_phase)
    if getattr(k, 'dsa_stop', '') == 'D':
        return
    phase_outproj_ln(k, OT_d, d_OT, W["dsa_w_out"][j], W["ln_mix_g"][li], W["ln_mix_b"][li], h_in, h_out)
import math as _math

S5_KVEC = [0, -1, -2, -3, -4, -5, -6, -7, 7, 6, 5, 4, 3, 2, 1, 0, 0, 1, 2, 3, 4, 5, 6, 7, 1, 2, 3, 4, 5, 6, 7, 8, 8, 16, 32, 64, 128, 256, 512, 1024]
NK = 40


def s5_consts():
    kv = np.tile(np.array(S5_KVEC, np.float32)[None, :], (128, 1))
    s_ = (np.arange(128) // 16)[:, None]
    t_ = (np.arange(128) // 16)[None, :]
    msk = (t_ >= s_).astype(np.float32)
    import ml_dtypes
    sel = np.zeros((128, 64, 128), np.float32)
    for g8 in range(8):
        for s in range(8):
            for p_ in range(16):
                sel[g8 * 16 + p_, g8 * 8 + s, s * 16 + p_] = 1.0
    selT = np.ascontiguousarray(sel.transpose(2, 1, 0))
    return {"c_kv40": kv, "c_s5mask": msk,
            "c_sel": sel.reshape(128, 8192).astype(ml_dtypes.bfloat16),
            "c_selT": selT.reshape(128, 8192).astype(ml_dtypes.bfloat16)}


def bc(ap, axis, shape):
    return ap.unsqueeze(axis).to_broadcast(list(shape))


def phase_s5(k, sj, li, h_in, h_out):
    S = k.S
    A = k.A
    W = k.w
    M_d, W1_d, W2_d, ZT_d = k.s5_M, k.s5_W1, k.s5_W2, k.s5_ZT
    dM = Dep(); dW1 = Dep(); dW2 = Dep()
    d_ZT = [Dep() for _ in range(NT)]
    TWO_PI = 2.0 * _math.pi
    m_phase = A.mark()
    DCOL = A.f32(128); dDCOL = Dep()
    ASr = A.f32(512); ASi = A.f32(512); ASn = A.f32(512); dAS = Dep()
    ASrv = r3(ASr, a=64); ASiv = r3(ASi, a=64); ASnv = r3(ASn, a=64)
    mP = A.mark()
    KV = A.f32(NK); dKV = Dep()
    DMA(k, "sync", KV, k.c["c_kv40"], [], [dKV])
    MASK = A.f32(128); dMASK = Dep()
    DMA(k, "sync", MASK, k.c["c_s5mask"], [], [dMASK])
    PAre = A.f32(64); PAim = A.f32(64); PDT = A.f32(64); dPA = Dep()
    PBre = A.f32(1024); PBim = A.f32(1024); PCre = A.f32(1024); PCim = A.f32(1024); dPB = Dep(); dPC = Dep()
    PBrev = r3(PBre, a=64); PBimv = r3(PBim, a=64); PCrev = r3(PCre, a=64); PCimv = r3(PCim, a=64)
    ld = [A.f32(2048), A.f32(2048)]; dld = [Dep(), Dep()]
    ld2 = A.f32(2048); dld2 = Dep()
    id64 = k.ident[0:64, 0:64]
    DMA(k, "sync", ld[0][:, 0:16], W["s5_d"][sj], [], [dld[0]])
    CP(k, "vector", ld[0][:, 16:144].rearrange("p (t q) -> p t q", t=8), bc(ld[0][:, 0:16], 1, [128, 8, 16]), [dld[0]], [dld[0]])
    TR(k, k.ps[0][:, 0:128], ld[0][:, 16:144], k.ident, [dld[0]], [k.dps[0]], True)
    CP(k, "vector", DCOL, k.ps[0][:, 0:128], [k.dps[0]], [dDCOL])
    DMA(k, "sync", ld[1][0:64, 0:128], W["s5_a_re"][sj].rearrange("(j g2) n -> j (g2 n)", g2=2), [], [dld[1]])
    DMA(k, "sync", ld[1][0:64, 128:256], W["s5_a_im"][sj].rearrange("(j g2) n -> j (g2 n)", g2=2), [], [dld[1]])
    DMA(k, "sync", ld[1][0:64, 256:258], W["s5_log_dt"][sj].rearrange("(j g2) -> j g2", g2=2), [], [dld[1]])
    CP(k, "vector", ld[1][0:64, 384:512].rearrange("p (g n) -> p g n", g=2), bc(ld[1][0:64, 256:258], 2, [64, 2, 64]), [dld[1]], [dld[1]])
    for i_, (c0, dst) in enumerate([(0, PAre), (128, PAim), (384, PDT)]):
        TR(k, k.ps[1][:, i_ * 64:(i_ + 1) * 64], ld[1][0:64, c0:c0 + 128], id64, [dld[1]], [k.dps[1]], True)
        CP(k, "vector", dst, k.ps[1][:, i_ * 64:(i_ + 1) * 64], [k.dps[1]], [dPA])
    cnt_ = 0
    for name, dstv, ddst, is_c in [("s5_b_re", PBrev, dPB, False), ("s5_b_im", PBimv, dPB, False), ("s5_c_re", PCrev, dPC, True), ("s5_c_im", PCimv, dPC, True)]:
        lb = ld[cnt_ % 2]; dlb = dld[cnt_ % 2]
        cnt_ += 1
        if is_c:
            DMA(k, "sync", lb[0:64, :], W[name][sj].rearrange("(j g2) p n -> j (g2 p n)", g2=2), [], [dlb])
            lb2 = ld2[0:64, :]
            CP(k, "vector", lb2.rearrange("j (p g n) -> j p g n", p=16, g=2), lb[0:64, :].rearrange("j (g p n) -> j p g n", g=2, p=16), [dlb, dld2], [dld2])
            lv = lb2.rearrange("j (p gn) -> j p gn", p=16)
        else:
            DMA(k, "sync", lb[0:64, :], W[name][sj].rearrange("(j g2) n p -> j (g2 n p)", g2=2), [], [dlb])
            lv = lb[0:64, :].rearrange("j (gn p) -> j gn p", p=16)
        for q4 in range(2):
            bank = 2 + q4
            ps, dps = k.ps[bank], k.dps[bank]
            for p8 in range(8):
                p_ = q4 * 8 + p8
                src = lv[:, p_, :] if is_c else lv[:, :, p_]
                TR(k, ps[:, p8 * 64:(p8 + 1) * 64], src, id64, [dlb, dld2], [dps], p8 == 7)
            CP(k, "vector", dstv[:, :, q4 * 8:(q4 + 1) * 8].rearrange("p j q -> p q j"), ps.rearrange("p (q j) -> p q j", q=8), [dps], [ddst])
    if getattr(k, 's5_stop', '') == 'P1':
        barrier(k)
        return
    dE = Dep()
    lr = A.f32(64); ldr = A.f32(64); th = A.f32(64); dtt = A.f32(64)
    TS(k, "vector", lr, PAre, -1.0e-4, None, ALU.min, None, [dPA], [dE])
    ACT(k, dtt, PDT, AF.Exp, [dPA], [dE])
    TT(k, "vector", ldr, lr, dtt, ALU.mult, [dE], [dE])
    TT(k, "vector", th, PAim, dtt, ALU.mult, [dE, dPA], [dE])
    NKK = 64 * NK
    shp = [128, 64, NK]
    ARG = A.f32(NKK); PHI = A.f32(NKK); RHO = A.f32(NKK); QF = A.f32(NKK); MSK2 = A.f32(NKK)
    ARE = A.f32(NKK); AIM = A.f32(NKK)
    QI = A.f32(NKK).bitcast(I32)
    v3 = lambda t_: r3(t_, a=64)
    TT(k, "vector", v3(ARG), bc(ldr, 2, shp), bc(KV, 1, shp), ALU.mult, [dE, dKV], [dE])
    ACT(k, RHO, ARG, AF.Exp, [dE], [dE])
    TT(k, "vector", v3(PHI), bc(th, 2, shp), bc(KV, 1, shp), ALU.mult, [dE, dKV], [dE])

    def sin_of(dst, off):
        TS(k, "vector", ARG, PHI, off, None, ALU.add, None, [dE], [dE])
        TS(k, "vector", QF, ARG, 1.0 / TWO_PI, None, ALU.mult, None, [dE], [dE])
        CP(k, "vector", QI, QF, [dE], [dE])
        CP(k, "vector", QF, QI, [dE], [dE])
        STT(k, "vector", ARG, QF, -TWO_PI, ARG, ALU.mult, ALU.add, [dE], [dE])
        TS(k, "vector", MSK2, ARG, _math.pi, -TWO_PI, ALU.is_gt, ALU.mult, [dE], [dE])
        TT(k, "vector", ARG, ARG, MSK2, ALU.add, [dE], [dE])
        TS(k, "vector", MSK2, ARG, -_math.pi, TWO_PI, ALU.is_lt, ALU.mult, [dE], [dE])
        TT(k, "vector", ARG, ARG, MSK2, ALU.add, [dE], [dE])
        ACT(k, dst, ARG, AF.Sin, [dE], [dE])

    sin_of(AIM, 64.0 * _math.pi)
    sin_of(ARE, 64.5 * _math.pi)
    TT(k, "vector", AIM, AIM, RHO, ALU.mult, [dE], [dE])
    TT(k, "vector", ARE, ARE, RHO, ALU.mult, [dE], [dE])
    AREv = v3(ARE); AIMv = v3(AIM)
    CP(k, "vector", ASrv, AREv[:, :, 32:40], [dE], [dAS])
    CP(k, "vector", ASiv, AIMv[:, :, 32:40], [dE], [dAS])
    TS(k, "vector", ASnv, AIMv[:, :, 32:40], -1.0, None, ALU.mult, None, [dE], [dAS])
    er = A.f32(64); ei = A.f32(64); qr = A.f32(64); qi_ = A.f32(64); den = A.f32(64); t1 = A.f32(64); fr = A.f32(64); fi = A.f32(64)
    TS(k, "vector", er, AREv[:, :, 24], -1.0, None, ALU.add, None, [dE], [dE])
    CP(k, "vector", ei, AIMv[:, :, 24], [dE], [dE])
    TT(k, "vector", qr, er, lr, ALU.mult, [dE], [dE])
    TT(k, "vector", t1, ei, PAim, ALU.mult, [dE], [dE])
    TT(k, "vector", qr, qr, t1, ALU.add, [dE], [dE])
    TT(k, "vector", qi_, ei, lr, ALU.mult, [dE], [dE])
    TT(k, "vector", t1, er, PAim, ALU.mult, [dE], [dE])
    TT(k, "vector", qi_, qi_, t1, ALU.subtract, [dE], [dE])
    TT(k, "vector", den, lr, lr, ALU.mult, [dE], [dE])
    TT(k, "vector", t1, PAim, PAim, ALU.mult, [dE], [dE])
    TT(k, "vector", den, den, t1, ALU.add, [dE], [dE])
    k.S.op("vector", lambda e: e.reciprocal(out=den, in_=den), reads=[dE], writes=[dE])
    TT(k, "vector", fr, qr, den, ALU.mult, [dE], [dE])
    TT(k, "vector", fi, qi_, den, ALU.mult, [dE], [dE])
    BBre = A.f32(1024); BBim = A.f32(1024); tb_ = A.f32(1024)
    BBrev = r3(BBre, a=64); BBimv = r3(BBim, a=64); tbv = r3(tb_, a=64)
    s16 = [128, 64, 16]
    TT(k, "vector", BBrev, bc(fr, 2, s16), PBrev, ALU.mult, [dE, dPB], [dE])
    TT(k, "vector", tbv, bc(fi, 2, s16), PBimv, ALU.mult, [dE, dPB], [dE])
    TT(k, "vector", BBre, BBre, tb_, ALU.subtract, [dE], [dE])
    TT(k, "vector", BBimv, bc(fr, 2, s16), PBimv, ALU.mult, [dE, dPB], [dE])
    TT(k, "vector", tbv, bc(fi, 2, s16), PBrev, ALU.mult, [dE, dPB], [dE])
    TT(k, "vector", BBim, BBim, tb_, ALU.add, [dE], [dE])
    if getattr(k, 's5_stop', '') == 'P2':
        barrier(k)
        return
    PB_ = 8
    PM = A.f32(2); dPM = Dep()
    k.S.op("vector", lambda e: e.memset(PM, 0.0), reads=[], writes=[dPM])
    k.S.op("vector", lambda e: e.memset(PM[0:64, 0:1], 1.0), reads=[dPM], writes=[dPM])
    k.S.op("vector", lambda e: e.memset(PM[64:128, 1:2], 1.0), reads=[dPM], writes=[dPM])
    LM = [[A.bf16(1024), A.bf16(1024)], [A.bf16(1024), A.bf16(1024)]]
    T = [A.f32(1024) for _ in range(4)]
    Tv = [t_.rearrange("p (j s q) -> p j s q", j=PB_, s=8) for t_ in T]
    RREb = A.bf16(1024); RIMb = A.bf16(1024)
    L2RE = A.f32(1024); L2IM = A.f32(1024)
    W2o = A.bf16(4096)
    W2ov = W2o.rearrange("p (j r m) -> p j r m", j=PB_, r=4)
    Mout = [A.bf16(512), A.bf16(512)]; dMout = [Dep(), Dep()]
    TAB = [A.bf16(512), A.bf16(512)]; dTAB = [Dep(), Dep()]
    for tb2 in TAB:
        k.S.op("vector", lambda e, tb2=tb2: e.memset(tb2, 0.0), reads=[], writes=[dE])
    dCh = Dep()
    s4 = [128, PB_, 8, 16]
    j8 = lambda t_: r3(t_, a=PB_)

    def products(blk, Bre, Bim, j0):
        a0 = blk * 8
        Ar = AREv[:, j0:j0 + PB_, a0:a0 + 8]; Ai = AIMv[:, j0:j0 + PB_, a0:a0 + 8]
        br = Bre[:, j0:j0 + PB_, :]; bi = Bim[:, j0:j0 + PB_, :]
        TT(k, "vector", Tv[0], bc(Ar, 3, s4), bc(br, 2, s4), ALU.mult, [dE, dPC, dCh], [dCh])
        TT(k, "vector", Tv[1], bc(Ai, 3, s4), bc(bi, 2, s4), ALU.mult, [dE, dPC, dCh], [dCh])
        TT(k, "vector", Tv[2], bc(Ai, 3, s4), bc(br, 2, s4), ALU.mult, [dE, dPC, dCh], [dCh])
        TT(k, "vector", Tv[3], bc(Ar, 3, s4), bc(bi, 2, s4), ALU.mult, [dE, dPC, dCh], [dCh])

    mcnt = 0
    for ch in range(64 // PB_):
        j0 = ch * PB_
        products(0, BBrev, BBimv, j0)
        TT(k, "vector", T[0], T[0], T[1], ALU.subtract, [dCh], [dCh])
        TT(k, "vector", T[2], T[2], T[3], ALU.add, [dCh], [dCh])
        for g2 in range(2):
            TS(k, "vector", LM[g2][0], T[0], PM[:, g2:g2 + 1], None, ALU.mult, None, [dCh, dPM], [dCh])
            TS(k, "vector", LM[g2][1], T[2], PM[:, g2:g2 + 1], None, ALU.mult, None, [dCh, dPM], [dCh])
        products(2, PCrev, PCimv, j0)
        TT(k, "vector", RREb, T[0], T[1], ALU.subtract, [dCh], [dCh])
        STT(k, "vector", RIMb, T[2], -1.0, T[3], ALU.mult, ALU.subtract, [dCh], [dCh])
        for half in range(PB_ // 2):
            bank = mcnt % 2
            mo, dmo = Mout[mcnt % 2], dMout[mcnt % 2]
            mcnt += 1
            ps, dps = k.ps[bank], k.dps[bank]
            for q in range(4):
                jj = half * 2 + q // 2
                g2 = q % 2
                MM(k, ps[:, q * 128:(q + 1) * 128], j8(LM[g2][0])[:, jj, :], j8(RREb)[:, jj, :], True, False, [dCh], [dps], False)
                MM(k, ps[:, q * 128:(q + 1) * 128], j8(LM[g2][1])[:, jj, :], j8(RIMb)[:, jj, :], False, True, [dCh], [dps], q == 3)
            TT(k, "vector", r3(mo, a=4), r3(ps, a=4), bc(MASK, 1, [128, 4, 128]), ALU.mult, [dps, dMASK], [dmo])
            g0 = (j0 + half * 2) * 2
            for q in range(4):
                DMA(k, "gpsimd", M_d[g0 + q], mo[:, q * 128:(q + 1) * 128], [dmo], [dM])
        if getattr(k, 's5_stop', '') == 'P3a':
            continue
        products(1, BBrev, BBimv, j0)
        TT(k, "vector", L2RE, T[0], T[1], ALU.subtract, [dCh], [dCh])
        TT(k, "vector", L2IM, T[2], T[3], ALU.add, [dCh], [dCh])
        for jj in range(PB_):
            bank = 2 + jj % 2
            ps, dps = k.ps[bank], k.dps[bank]
            tab, dtab = TAB[jj % 2], dTAB[jj % 2]
            TR(k, ps[:, 0:128], j8(L2RE)[:, jj, :], k.ident, [dCh], [dps], False)
            TR(k, ps[:, 128:256], j8(L2IM)[:, jj, :], k.ident, [dCh], [dps], True)
            tabv = tab.rearrange("p (r a m) -> p r a m", r=2, a=2)
            psv = ps[:, 0:256].rearrange("p (r m) -> p r m", r=2)
            CP(k, "vector", tabv[:, :, 0, 0:64], psv[:, :, 0:64], [dps], [dtab])
            CP(k, "vector", tabv[:, :, 1, 64:128], psv[:, :, 64:128], [dps], [dtab])
            DMA(k, "gpsimd", W1_d[j0 + jj], tab, [dtab], [dW1])
        if getattr(k, 's5_stop', '') == 'P3b':
            continue
        products(3, PCrev, PCimv, j0)
        TT(k, "vector", T[0], T[0], T[1], ALU.subtract, [dCh], [dCh])
        STT(k, "vector", T[2], T[2], -1.0, T[3], ALU.mult, ALU.subtract, [dCh], [dCh])
        for g2 in range(2):
            TS(k, "vector", W2ov[:, :, 2 * g2, :], j8(T[0]), PM[:, g2:g2 + 1], None, ALU.mult, None, [dCh, dPM], [dCh])
            TS(k, "vector", W2ov[:, :, 2 * g2 + 1, :], j8(T[2]), PM[:, g2:g2 + 1], None, ALU.mult, None, [dCh, dPM], [dCh])
        for jj in range(PB_):
            DMA(k, "gpsimd", W2_d[j0 + jj], W2o[:, jj * 512:(jj + 1) * 512], [dCh], [dW2, dCh])
    barrier(k)
    A.reset(mP)
    if getattr(k, 's5_stop', '') in ('P', 'P3a', 'P3b'):
        return
    R1 = A.bf16(16 * 2048)
    R1v = R1.rearrange("p (b s c) -> p b s c", b=16, s=8)
    dR1 = Dep()
    mR2 = A.mark()
    HT = A.bf16(NT * 2048); HTv = HT.rearrange("p (t c x) -> p t c x", t=NT, c=16); dHT = Dep()
    for t in range(NT):
        DMA(k, "sync", HTv[:, t].rearrange("p c x -> p (c x)"), k.hT[t], [k.d_hT[t]], [dHT])
    stg = Stager(k, 3, 2048)
    wcb = [A.bf16(2048), A.bf16(2048)]; dwcb = [Dep(), Dep()]
    cnt = 0
    for chb in range(16):
        b = chb % 2
        stg.load(W["s5_w_in"][sj].rearrange("(kc p) n -> p kc n", p=128)[:, :, chb * 128:(chb + 1) * 128], r3(wcb[b], a=16), dwcb[b])
        for sl_ in range(4):
            ps, dps = k.ps[cnt % 4], k.dps[cnt % 4]
            cnt += 1
            for kc in range(16):
                MM(k, ps, r3(wcb[b], a=16)[:, kc, :], HTv[:, sl_ * 4:(sl_ + 1) * 4, kc, :], kc == 0, kc == 15, [dwcb[b], dHT], [dps], kc == 15)
            src = ps.rearrange("p (c s) -> p s c", s=8)
            dst = R1v[:, chb, :, sl_ * 64:(sl_ + 1) * 64]
            if cnt % 2 == 0:
                ACT(k, dst, src, AF.Copy, [dps], [dR1])
            else:
                CP(k, "vector", dst, src, [dps], [dR1])
    barrier(k)
    A.reset(mR2)
    if getattr(k, 's5_stop', '') == 'U':
        return
    R2 = A.bf16(128 * 256)
    Xv = r3(R2, a=128)
    dX = Dep()
    SEL = A.bf16(8192); SELv = r3(SEL, a=64); SELT = A.bf16(8192); SELTv = r3(SELT, a=64); dSEL = Dep()
    DMA(k, "sync", SEL, k.c["c_sel"], [], [dSEL])
    DMA(k, "sync", SELT, k.c["c_selT"], [], [dSEL])
    for g0 in range(0, 128, 2):
        bank = (g0 // 2) % 4
        ps, dps = k.ps[bank], k.dps[bank]
        for gi in range(2):
            g = g0 + gi
            for s_ in range(8):
                MM(k, ps[:, gi * 256:(gi + 1) * 256], SELv[:, (g % 8) * 8 + s_, :], R1v[:, g // 8, s_, :], s_ == 0, s_ == 7, [dSEL, dR1], [dps], (gi == 1 and s_ == 7))
        if (g0 // 2) % 2 == 0:
            ACT(k, Xv[:, g0:g0 + 2, :], r3(ps, a=2), AF.Copy, [dps], [dX])
        else:
            CP(k, "vector", Xv[:, g0:g0 + 2, :], r3(ps, a=2), [dps], [dX])
    barrier(k)
    if getattr(k, 's5_stop', '') == 'X':
        return
    mL = A.mark()
    dZF = Dep()
    Mg = [A.bf16(256) for _ in range(4)]; W1t = [A.bf16(512), A.bf16(512)]; W2t = [A.bf16(512) for _ in range(4)]
    dMg = [Dep() for _ in range(4)]; dW1t = [Dep(), Dep()]; dW2t = [Dep() for _ in range(4)]
    REs = [[A.f32(384), A.f32(384)] for _ in range(2)]; IMs = [[A.f32(384), A.f32(384)] for _ in range(2)]
    dSCs = [Dep(), Dep()]
    for sl_ in range(2):
        for t_ in REs[sl_] + IMs[sl_]:
            k.S.op("vector", lambda e, t_=t_: e.memset(t_, 0.0), reads=[], writes=[dSCs[sl_]])
    HREs = [A.bf16(256), A.bf16(256)]; HIMs = [A.bf16(256), A.bf16(256)]; dHs = [Dep(), Dep()]
    ybs = [[A.f32(256), A.f32(256)] for _ in range(2)]; y2bs = [[A.f32(256), A.f32(256)] for _ in range(2)]
    dybs = [[Dep(), Dep()], [Dep(), Dep()]]
    Zall = [A.bf16(8 * 256), A.bf16(8 * 256)]; dZall = [Dep(), Dep()]

    def st_S(j):
        b = j % 2
        b4 = j % 4
        for g2 in range(2):
            DMA(k, "sync", Mg[b4][:, g2 * 128:(g2 + 1) * 128], M_d[2 * j + g2], [dM], [dMg[b4]])
        DMA(k, "sync", W1t[b], W1_d[j], [dW1], [dW1t[b]])
        DMA(k, "sync", W2t[b4], W2_d[j], [dW2], [dW2t[b4]])
        ps, dps = k.ps[b], k.dps[b]
        for ri in range(2):
            MM(k, ps[:, ri * 256:(ri + 1) * 256], W1t[b][:, (2 * ri) * 128:(2 * ri + 1) * 128], Xv[:, 2 * j, :], True, False, [dW1t[b], dX], [dps], False)
            MM(k, ps[:, ri * 256:(ri + 1) * 256], W1t[b][:, (2 * ri + 1) * 128:(2 * ri + 2) * 128], Xv[:, 2 * j + 1, :], False, True, [dW1t[b], dX], [dps], ri == 1)
        ACT(k, REs[b][0][:, 128:384], ps[:, 0:256], AF.Copy, [dps], [dSCs[b]])
        ACT(k, IMs[b][0][:, 128:384], ps[:, 256:512], AF.Copy, [dps], [dSCs[b]])

    def st_scan_step(j, i):
        b = j % 2
        cur = i % 2
        sft = 1 << i
        ra, ia = REs[b][cur], IMs[b][cur]
        rb_, ib_ = REs[b][1 - cur], IMs[b][1 - cur]
        ar = ASrv[:, j, i:i + 1]; ai = ASiv[:, j, i:i + 1]; an = ASnv[:, j, i:i + 1]
        d_ = dSCs[b]
        STT(k, "vector", rb_[:, 128:384], ra[:, 128 - sft:384 - sft], ar, ra[:, 128:384], ALU.mult, ALU.add, [d_, dAS], [d_])
        STT(k, "vector", rb_[:, 128:384], ia[:, 128 - sft:384 - sft], an, rb_[:, 128:384], ALU.mult, ALU.add, [d_, dAS], [d_])
        STT(k, "vector", ib_[:, 128:384], ia[:, 128 - sft:384 - sft], ar, ia[:, 128:384], ALU.mult, ALU.add, [d_, dAS], [d_])
        STT(k, "vector", ib_[:, 128:384], ra[:, 128 - sft:384 - sft], ai, ib_[:, 128:384], ALU.mult, ALU.add, [d_, dAS], [d_])

    def st_cast(j):
        b = j % 2
        ACT(k, HREs[b], REs[b][0][:, 127:383], AF.Copy, [dSCs[b]], [dHs[b]])
        ACT(k, HIMs[b], IMs[b][0][:, 127:383], AF.Copy, [dSCs[b]], [dHs[b]])

    def st_Y(j):
        b = j % 2
        for g2 in range(2):
            g = 2 * j + g2
            py, dpy = k.ps[2 + g2], k.dps[2 + g2]
            b4 = j % 4
            MM(k, py[:, 0:256], r3(Mg[b4], a=2)[:, g2, :], Xv[:, g, :], True, False, [dMg[b4], dX], [dpy], False)
            MM(k, py[:, 0:256], W2t[b4][:, (2 * g2) * 128:(2 * g2 + 1) * 128], HREs[b], False, False, [dW2t[b4], dHs[b]], [dpy], False)
            MM(k, py[:, 0:256], W2t[b4][:, (2 * g2 + 1) * 128:(2 * g2 + 2) * 128], HIMs[b], False, True, [dW2t[b4], dHs[b]], [dpy], True)
            y = ybs[b][g2]; y2 = y2bs[b][g2]; dy_ = dybs[b][g2]
            STT(k, "vector", y, Xv[:, g, :], DCOL[:, g:g + 1], py[:, 0:256], ALU.mult, ALU.add, [dpy, dX, dDCOL], [dy_])
            TT(k, "gpsimd", y2, y, y, ALU.mult, [dy_], [dy_])
            TS(k, "gpsimd", y2, y2, 0.044715, 1.0, ALU.mult, ALU.add, [dy_], [dy_])
            TT(k, "gpsimd", y2, y2, y, ALU.mult, [dy_], [dy_])
            ACT(k, y2, y2, AF.Sigmoid, [dy_], [dy_], scale=1.5957691216057308)
            zb = (g // 8) % 2
            TT(k, "gpsimd", r3(Zall[zb], a=8)[:, g % 8, :], y, y2, ALU.mult, [dy_, dZall[zb]], [dZall[zb]])
        if j % 4 == 3:
            chb = j // 4
            zb = chb % 2
            for sp in range(4):
                bank = 4 + sp % 4
                ps2, dps2 = k.ps[bank], k.dps[bank]
                for si in range(2):
                    s_ = sp * 2 + si
                    for g8 in range(8):
                        MM(k, ps2[:, si * 256:(si + 1) * 256], SELTv[:, g8 * 8 + s_, :], r3(Zall[zb], a=8)[:, g8, :], g8 == 0, g8 == 7, [dSEL, dZall[zb]], [dps2], (si == 1 and g8 == 7))
                if sp % 2 == 0:
                    ACT(k, R1v[:, chb, sp * 2:sp * 2 + 2, :], r3(ps2, a=2), AF.Copy, [dps2], [dZF])
                else:
                    CP(k, "vector", R1v[:, chb, sp * 2:sp * 2 + 2, :], r3(ps2, a=2), [dps2], [dZF])

    for c in range(0, 64, 2):
        st_S(c)
        st_S(c + 1)
        if c >= 2:
            st_Y(c - 2)
            st_Y(c - 1)
        for i in range(8):
            st_scan_step(c, i)
            st_scan_step(c + 1, i)
        st_cast(c)
        st_cast(c + 1)
    st_Y(62)
    st_Y(63)
    barrier(k)
    A.reset(mR2)
    if getattr(k, 's5_stop', '') == 'L':
        return
    Z2N = A.bf16(NT * 2048)
    Z2Nv = Z2N.rearrange("p (t c l s) -> p t c l s", t=NT, c=16, l=16)
    dZ2 = Dep()
    stg = Stager(k, 3, 2048)
    wgb = [A.bf16(2048), A.bf16(2048)]; dwgb = [Dep(), Dep()]
    sg = [A.f32(512), A.f32(512)]; dsg = [Dep(), Dep()]
    cnt = 0
    for nb in range(16):
        b = nb % 2
        stg.load(W["s5_w_glu"][sj].rearrange("(kc p) n -> p kc n", p=128)[:, :, nb * 128:(nb + 1) * 128], r3(wgb[b], a=16), dwgb[b])
        for q in range(4):
            ps, dps = k.ps[cnt % 4], k.dps[cnt % 4]
            sb_ = cnt % 2
            cnt += 1
            zsl = lambda kc: R1v[:, kc, 2 * q:2 * q + 2, :]
            for kc in range(16):
                MM(k, ps, r3(wgb[b], a=16)[:, kc, :], zsl(kc), kc == 0, kc == 15, [dwgb[b], dZF], [dps], kc == 15)
            ACT(k, sg[sb_], ps, AF.Sigmoid, [dps], [dsg[sb_]])
            dst = Z2Nv[:, :, nb, :, 2 * q:2 * q + 2].rearrange("p t l s -> p s t l")
            in0 = sg[sb_].rearrange("p (s t l) -> p s t l", s=2, t=16)
            in1 = R1v[:, nb, 2 * q:2 * q + 2, :].rearrange("p s (t l) -> p s t l", t=16)
            TT(k, "vector", dst, in0, in1, ALU.mult, [dsg[sb_], dZF], [dZ2])
    for t in range(NT):
        DMA(k, "gpsimd", ZT_d[t], Z2N[:, t * 2048:(t + 1) * 2048], [dZ2], [d_ZT[t]])
    barrier(k)
    A.reset(m_phase)
    phase_outproj_ln(k, ZT_d, d_ZT, W["s5_w_out"][sj], W["ln_mix_g"][li], W["ln_mix_b"][li], h_in, h_out)
W_SPECS = [
    ("rel_bias", [32, 16]),
    ("s5_w_in", [2, 2048, 2048]), ("s5_a_re", [2, 128, 64]), ("s5_a_im", [2, 128, 64]), ("s5_log_dt", [2, 128]),
    ("s5_b_re", [2, 128, 64, 16]), ("s5_b_im", [2, 128, 64, 16]), ("s5_c_re", [2, 128, 16, 64]), ("s5_c_im", [2, 128, 16, 64]),
    ("s5_d", [2, 128, 16]), ("s5_w_glu", [2, 2048, 2048]), ("s5_w_out", [2, 2048, 2048]),
    ("dsa_w_in", [2, 2048, 1168]), ("dsa_q_norm", [2, 512]), ("dsa_kv_norm", [2, 512]),
    ("dsa_w_uq", [2, 512, 2048]), ("dsa_w_qidx", [2, 512, 2048]), ("dsa_w_uk", [2, 16, 128, 512]),
    ("dsa_w_uv", [2, 16, 512, 128]), ("dsa_w_out", [2, 2048, 2048]),
    ("moe_w_group", [4, 2048, 4]), ("moe_b_group", [4, 4]), ("moe_w_expert", [4, 2048, 32]), ("moe_b_expert", [4, 32]),
    ("moe_w_gate", [4, 32, 2048, 256]), ("moe_w_up", [4, 32, 2048, 256]), ("moe_w_down", [4, 32, 256, 2048]),
    ("ln_mix_g", [4, 2048]), ("ln_mix_b", [4, 2048]), ("ln_ffn_g", [4, 2048]), ("ln_ffn_b", [4, 2048]),
]


def host_consts():
    c = {}
    c["c_ident"] = np.eye(128, dtype=np.float32)
    c.update(dsa_consts())
    c.update(s5_consts())
    return c


def build_nc(mode="full", used=None, **kw):
    nc = bass.Bass("TRN2", target_bir_lowering=False)
    k = K()
    for a_, b_ in kw.items():
        setattr(k, a_, b_)
    k.nc = nc
    k.x = nc.dram_tensor("x", [L, D], F32, kind="ExternalInput").ap()
    k.w = {}
    for name, shp in W_SPECS:
        if used is not None and name not in used:
            continue
        k.w[name] = nc.dram_tensor(name, getattr(k, 'wshape', {}).get(name, shp), F32, kind="ExternalInput").ap()
    k.c = {}
    for name, arr in host_consts().items():
        k.c[name] = nc.dram_tensor(name, list(arr.shape), F32 if arr.dtype == np.float32 else BF16, kind="ExternalInput").ap()
    k.out = nc.dram_tensor("out", [L, D], F32, kind="ExternalOutput").ap()
    k.hA = nc.dram_tensor("hA", [L, D], F32).ap()
    k.hB = nc.dram_tensor("hB", [L, D], F32).ap()
    k.hT = nc.dram_tensor("hT", [NT, 128, 16 * 128], BF16).ap()
    k.s5_M = nc.dram_tensor("s5_M", [128, 128, 128], BF16).ap()
    k.s5_W1 = nc.dram_tensor("s5_W1", [64, 128, 512], BF16).ap()
    k.s5_W2 = nc.dram_tensor("s5_W2", [64, 128, 512], BF16).ap()
    k.s5_ZT = nc.dram_tensor("s5_ZT", [NT, 128, 16 * 128], BF16).ap()
    k.dsa_QT = nc.dram_tensor("dsa_QT", [128, 16, L], BF16).ap()
    k.dsa_QIT = nc.dram_tensor("dsa_QIT", [128, 16, L], BF16).ap()
    k.dsa_OT = nc.dram_tensor("dsa_OT", [NT, 128, 16 * 128], BF16).ap()
    k.dsa_KT = nc.dram_tensor("dsa_KT", [16, 128, L], BF16).ap()
    k.dsa_V = nc.dram_tensor("dsa_V", [16, 128, L], BF16).ap()
    k.d_h = [Dep() for _ in range(NT)]
    k.d_hT = [Dep() for _ in range(NT)]
    with ExitStack() as st:
        k.S = Sched(nc, st)
        k.A = Arena(nc, st, 53000)
        k.ps = []
        k.dps = []
        for i in range(8):
            k.ps.append(st.enter_context(nc.psum_tensor("ps%d" % i, [128, 512], F32))[:])
            k.dps.append(Dep())
        A = k.A
        k.ident = A.f32(128)
        d_id = Dep()
        k.S.dma("sync", lambda e: e.dma_start(out=k.ident, in_=k.c["c_ident"]), writes=[d_id])
        k.d_hTt = [Dep(), Dep()]
        k.hTt_pos = 0
        barrier(k)
        if mode == "dsa_only":
            phase_prep(k)
            phase_dsa(k, 0, 1, k.x, k.out)
        elif mode == "s5_only":
            phase_prep(k)
            phase_s5(k, 0, 0, k.x, k.out)
        elif mode == "moe_only":
            phase_prep(k)
            phase_moe(k, 0, k.x, k.out, write_hT=False)
        elif mode == "full":
            phase_prep(k)
            h_in = k.x
            bufs = [k.hA, k.hB]
            bi = 0
            for li in range(DEPTH):
                hm = bufs[bi]; bi ^= 1
                if li % 2 == 0:
                    phase_s5(k, li // 2, li, h_in, hm)
                else:
                    phase_dsa(k, li // 2, li, h_in, hm)
                last = (li == DEPTH - 1)
                hf = k.out if last else bufs[bi]
                bi ^= 1
                phase_moe(k, li, hm, hf, write_hT=not last)
                h_in = hf
        k.S.finish(k.d_h)
        k.S.emit()
    return nc


def kernel(**inputs):
    nc = build_nc("full")
    consts = host_consts()
    x = np.ascontiguousarray(inputs["x"], dtype=np.float32)
    shared = {name: np.ascontiguousarray(inputs[name], dtype=np.float32) for name, _ in W_SPECS}
    shared.update(consts)
    in_maps = []
    for c in range(8):
        m = dict(shared)
        m["x"] = x[c]
        in_maps.append(m)
    res = run_bass_kernel_spmd(nc, in_maps, core_ids=list(range(8)))
    return np.stack([np.asarray(r["out"], dtype=np.float32) for r in res.results], axis=0)
```
